# Optimizing a Trainium2 kernel written in Bass

```python
import jax, jax.numpy as jnp
from jax import lax
import numpy as np

D_MODEL = 1024
BATCH = 16
SEQ = 2048
DEPTH = 1

HEAD_DIM = 64
ROPE_DIM = HEAD_DIM // 4
ROPE_THETA = 500000.0
Q_BLOCK = 128
EPS = 1e-6
NEG_INF = -1e30
FORCED_SCORE = 1e6
NSA_HEADS = 8
NSA_KV_HEADS = 2
NSA_GROUP = NSA_HEADS // NSA_KV_HEADS
CMP_LEN = 32
CMP_STRIDE = 16
CMP_HIDDEN = 128
SLC_BLOCK = 64
SLC_TOPN = 8
WINDOW = 512
DSA_HEADS = 8
KV_LORA = 128
NOPE_DIM = HEAD_DIM - ROPE_DIM
IDX_HEADS = 8
IDX_DIM = 64
IDX_TOPK_MAX = 256
D_FF = ((8 * D_MODEL + 3 * 256 - 1) // (3 * 256)) * 256
IN_SPLITS = (NSA_HEADS * HEAD_DIM,) + (NSA_KV_HEADS * HEAD_DIM,) * 6 + (
    3 * NSA_HEADS, DSA_HEADS * HEAD_DIM, KV_LORA, ROPE_DIM, IDX_HEADS * IDX_DIM, IDX_DIM, IDX_HEADS)
IN_WIDTH = sum(IN_SPLITS)

kernel_name = 'hybrid_nsa_dsa_parallel_heads'


def _rms_norm(x, g):
    xf = x.astype(jnp.float32)
    y = xf * lax.rsqrt(jnp.mean(xf * xf, axis=-1, keepdims=True) + EPS)
    return (y * g.astype(jnp.float32)).astype(x.dtype)


def _rope_tables(seq):
    inv_freq = 1.0 / (ROPE_THETA ** (jnp.arange(0, ROPE_DIM, 2, dtype=jnp.float32) / ROPE_DIM))
    ang = jnp.arange(seq, dtype=jnp.float32)[:, None] * inv_freq[None, :]
    return jnp.cos(ang), jnp.sin(ang)


def _rope(x, cos, sin):
    half = ROPE_DIM // 2
    xr = x[..., :ROPE_DIM].astype(jnp.float32)
    x1, x2 = xr[..., :half], xr[..., half:]
    c, s = cos[:, None, :], sin[:, None, :]
    rot = jnp.concatenate([x1 * c - x2 * s, x2 * c + x1 * s], axis=-1)
    return jnp.concatenate([rot.astype(x.dtype), x[..., ROPE_DIM:]], axis=-1)


def _masked_softmax(s, mask):
    s = jnp.where(mask, s.astype(jnp.float32), NEG_INF)
    m = jnp.max(s, axis=-1, keepdims=True)
    e = jnp.where(mask, jnp.exp(s - m), 0.0)
    return e / jnp.maximum(jnp.sum(e, axis=-1, keepdims=True), 1e-30)


def _q_blocks(a):
    b, s = a.shape[:2]
    return jnp.moveaxis(a.reshape((b, s // Q_BLOCK, Q_BLOCK) + a.shape[2:]), 1, 0)


def _from_blocks(o):
    nb, b, tq = o.shape[:3]
    return jnp.moveaxis(o, 0, 1).reshape((b, nb * tq) + o.shape[3:])


def _compress(k, pe, w1, b1, w2, b2):
    b, s, g, dh = k.shape
    n_cmp = (s - CMP_LEN) // CMP_STRIDE + 1
    idx = np.arange(n_cmp)[:, None] * CMP_STRIDE + np.arange(CMP_LEN)[None, :]
    blocks = k[:, idx] + pe[:, None, :]
    flat = jnp.transpose(blocks, (0, 3, 1, 2, 4)).reshape(b, g, n_cmp, CMP_LEN * dh)
    hid = jax.nn.gelu(flat @ w1 + b1)
    return hid @ w2 + b2


def _nsa_group(q, k_cmp, v_cmp, k_slc, v_slc, k_win, v_win, gates, cos, sin,
               cmp_pe, cmp_w1, cmp_b1, cmp_w2, cmp_b2):
    b, s = q.shape[:2]
    g_, r_, dh = NSA_KV_HEADS, NSA_GROUP, HEAD_DIM
    scale = HEAD_DIM ** -0.5
    tpos = jnp.arange(s)
    qg = _rope(q.reshape(b, s, NSA_HEADS, dh), cos, sin).reshape(b, s, g_, r_, dh)
    kc = _compress(_rope(k_cmp.reshape(b, s, g_, dh), cos, sin), cmp_pe[0], cmp_w1[0], cmp_b1[0], cmp_w2[0], cmp_b2[0])
    vc = _compress(v_cmp.reshape(b, s, g_, dh), cmp_pe[1], cmp_w1[1], cmp_b1[1], cmp_w2[1], cmp_b2[1])
    n_cmp = kc.shape[2]
    cmp_end = jnp.arange(n_cmp) * CMP_STRIDE + CMP_LEN - 1
    mask_c = cmp_end[None, :] <= tpos[:, None]
    p_c = _masked_softmax(jnp.einsum('bsgrd,bgnd->bgrsn', qg, kc) * scale, mask_c)
    o_cmp = jnp.einsum('bgrsn,bgnd->bsgrd', p_c.astype(vc.dtype), vc).reshape(b, s, NSA_HEADS, dh)
    n_slc = s // SLC_BLOCK
    ci = np.arange(n_cmp)[:, None] * CMP_STRIDE
    sj = np.arange(n_slc)[None, :] * SLC_BLOCK
    overlap = ((ci < sj + SLC_BLOCK) & (ci + CMP_LEN > sj)).astype(np.float32)
    imp = jnp.einsum('bgrsn,nj->bgsj', p_c, jnp.asarray(overlap))
    blk_t = (tpos // SLC_BLOCK)[:, None]
    j = jnp.arange(n_slc)[None, :]
    visible = j <= blk_t
    forced = (j == 0) | (j == blk_t) | (j == blk_t - 1)
    imp = jnp.where(visible, jnp.where(forced, FORCED_SCORE, imp), -jnp.inf)
    n_sel = min(SLC_TOPN, n_slc)
    _, sel = lax.top_k(imp, n_sel)
    ks_blocks = jnp.transpose(_rope(k_slc.reshape(b, s, g_, dh), cos, sin).reshape(b, n_slc, SLC_BLOCK, g_, dh), (0, 3, 1, 2, 4))
    vs_blocks = jnp.transpose(v_slc.reshape(b, n_slc, SLC_BLOCK, g_, dh), (0, 3, 1, 2, 4))
    pad = ((0, 0), (WINDOW, 0), (0, 0), (0, 0))
    kw = jnp.pad(_rope(k_win.reshape(b, s, g_, dh), cos, sin), pad)
    vw = jnp.pad(v_win.reshape(b, s, g_, dh), pad)
    gather = jax.vmap(jax.vmap(lambda tab, ix: tab[ix]))

    def block_fn(args):
        q_b, sel_b, t0 = args
        sel_b = jnp.transpose(sel_b, (0, 2, 1, 3))
        tq = t0 + jnp.arange(Q_BLOCK)
        kg = gather(ks_blocks, sel_b).reshape(b, g_, Q_BLOCK, n_sel * SLC_BLOCK, dh)
        vg = gather(vs_blocks, sel_b).reshape(b, g_, Q_BLOCK, n_sel * SLC_BLOCK, dh)
        kpos = (sel_b[..., None] * SLC_BLOCK + jnp.arange(SLC_BLOCK)).reshape(b, g_, Q_BLOCK, n_sel * SLC_BLOCK)
        mask_s = (kpos <= tq[None, None, :, None])[:, :, None]
        p_s = _masked_softmax(jnp.einsum('btgrd,bgtkd->bgrtk', q_b, kg) * scale, mask_s)
        o_s = jnp.einsum('bgrtk,bgtkd->btgrd', p_s.astype(vg.dtype), vg)
        kwb = lax.dynamic_slice_in_dim(kw, t0, Q_BLOCK + WINDOW, axis=1)
        vwb = lax.dynamic_slice_in_dim(vw, t0, Q_BLOCK + WINDOW, axis=1)
        wpos = t0 - WINDOW + jnp.arange(Q_BLOCK + WINDOW)
        mask_w = (wpos[None, :] <= tq[:, None]) & (wpos[None, :] > tq[:, None] - WINDOW) & (wpos[None, :] >= 0)
        p_w = _masked_softmax(jnp.einsum('btgrd,bsgd->bgrts', q_b, kwb) * scale, mask_w)
        o_w = jnp.einsum('bgrts,bsgd->btgrd', p_w.astype(vwb.dtype), vwb)
        return o_s, o_w

    starts = jnp.arange(s // Q_BLOCK, dtype=jnp.int32) * Q_BLOCK
    o_s, o_w = lax.map(block_fn, (_q_blocks(qg), _q_blocks(jnp.transpose(sel, (0, 2, 1, 3))), starts))
    o_s = _from_blocks(o_s).reshape(b, s, NSA_HEADS, dh)
    o_w = _from_blocks(o_w).reshape(b, s, NSA_HEADS, dh)
    gt = jax.nn.sigmoid(gates.astype(jnp.float32)).astype(q.dtype).reshape(b, s, 3, NSA_HEADS, 1)
    o = gt[:, :, 0] * o_cmp + gt[:, :, 1] * o_s + gt[:, :, 2] * o_w
    return o.reshape(b, s, NSA_HEADS * dh)


def _dsa_group(q, c_kv, k_rope, q_idx, k_idx, w_idx, cos, sin, g_kv, w_uk, w_uv):
    b, s = q.shape[:2]
    scale = HEAD_DIM ** -0.5
    q = _rope(q.reshape(b, s, DSA_HEADS, HEAD_DIM), cos, sin)
    q_rope, q_nope = q[..., :ROPE_DIM], q[..., ROPE_DIM:]
    c_kv = _rms_norm(c_kv, g_kv)
    k_rope = _rope(k_rope[:, :, None, :], cos, sin)[:, :, 0]
    q_lat = jnp.einsum('bshe,hre->bshr', q_nope, w_uk)
    q_idx = _rope(q_idx.reshape(b, s, IDX_HEADS, IDX_DIM), cos, sin)
    k_idx = _rope(k_idx[:, :, None, :], cos, sin)[:, :, 0]
    w_idx = w_idx.astype(jnp.float32) * (IDX_HEADS ** -0.5 * IDX_DIM ** -0.5)
    k_sel = min(IDX_TOPK_MAX, s // 4)
    spos = jnp.arange(s)
    gather = jax.vmap(lambda tab, ix: tab[ix])

    def block_fn(args):
        ql_b, qr_b, qi_b, wi_b, t0 = args
        tq = t0 + jnp.arange(Q_BLOCK)
        logits = jnp.einsum('bthd,bsd->bths', qi_b, k_idx).astype(jnp.float32)
        score = jnp.einsum('bths,bth->bts', jax.nn.relu(logits), wi_b)
        score = jnp.where(spos[None, None, :] <= tq[None, :, None], score, -jnp.inf)
        _, idx = lax.top_k(score, k_sel)
        cg = gather(c_kv, idx)
        krg = gather(k_rope, idx)
        sc = (jnp.einsum('bthr,btkr->bhtk', ql_b, cg) + jnp.einsum('bthe,btke->bhtk', qr_b, krg)) * scale
        mask = (idx <= tq[None, :, None])[:, None]
        p = _masked_softmax(sc, mask)
        return jnp.einsum('bhtk,btkr->bthr', p.astype(cg.dtype), cg)

    starts = jnp.arange(s // Q_BLOCK, dtype=jnp.int32) * Q_BLOCK
    o_lat = _from_blocks(lax.map(block_fn, (_q_blocks(q_lat), _q_blocks(q_rope), _q_blocks(q_idx), _q_blocks(w_idx), starts)))
    o = jnp.einsum('bshr,hrd->bshd', o_lat, w_uv)
    return o.reshape(b, s, DSA_HEADS * HEAD_DIM)


def _hybrid_mixer(h, cos, sin, w_in, cmp_pe, cmp_w1, cmp_b1, cmp_w2, cmp_b2, g_kv, w_uk, w_uv, w_out):
    proj = h @ w_in
    split_points = [int(v) for v in np.cumsum(IN_SPLITS)[:-1]]
    (nq, nkc, nvc, nks, nvs, nkw, nvw, ngate, dq, dckv, dkr, iq, ik, iw) = jnp.split(proj, split_points, axis=-1)
    o_a = _nsa_group(nq, nkc, nvc, nks, nvs, nkw, nvw, ngate, cos, sin, cmp_pe, cmp_w1, cmp_b1, cmp_w2, cmp_b2)
    o_b = _dsa_group(dq, dckv, dkr, iq, ik, iw, cos, sin, g_kv, w_uk, w_uv)
    return jnp.concatenate([o_a, o_b], axis=-1) @ w_out


def _swiglu(h, w_gate_up, w_down):
    gate, up = jnp.split(h @ w_gate_up, 2, axis=-1)
    return (jax.nn.silu(gate) * up) @ w_down


def setup_inputs(seed: int = 0) -> dict:
    key = jax.random.key(seed)
    ks = jax.random.split(key, 20)
    L = DEPTH

    def nrm(k, shape, scale):
        return jax.random.normal(k, shape, jnp.float32) * scale

    def gain(k, shape):
        return 1.0 + 0.05 * jax.random.normal(k, shape, jnp.float32)

    return {
        'x': nrm(ks[0], (BATCH, SEQ, D_MODEL), 1.0),
        'c': nrm(ks[1], (BATCH, D_MODEL), 1.0),
        'w_ada': nrm(ks[2], (L, D_MODEL, 6 * D_MODEL), 0.5 * D_MODEL ** -0.5),
        'b_ada': nrm(ks[3], (L, 6 * D_MODEL), 0.01),
        'g_pre_mix': gain(ks[4], (L, D_MODEL)),
        'g_post_mix': gain(ks[5], (L, D_MODEL)),
        'g_pre_ffn': gain(ks[6], (L, D_MODEL)),
        'g_post_ffn': gain(ks[7], (L, D_MODEL)),
        'w_in': nrm(ks[8], (L, D_MODEL, IN_WIDTH), D_MODEL ** -0.5),
        'cmp_pe': nrm(ks[9], (L, 2, CMP_LEN, HEAD_DIM), 0.1),
        'cmp_w1': nrm(ks[10], (L, 2, CMP_LEN * HEAD_DIM, CMP_HIDDEN), (CMP_LEN * HEAD_DIM) ** -0.5),
        'cmp_b1': nrm(ks[11], (L, 2, CMP_HIDDEN), 0.01),
        'cmp_w2': nrm(ks[12], (L, 2, CMP_HIDDEN, HEAD_DIM), CMP_HIDDEN ** -0.5),
        'cmp_b2': nrm(ks[13], (L, 2, HEAD_DIM), 0.01),
        'g_kv_norm': gain(ks[14], (L, KV_LORA)),
        'w_uk': nrm(ks[15], (L, DSA_HEADS, KV_LORA, NOPE_DIM), KV_LORA ** -0.5),
        'w_uv': nrm(ks[16], (L, DSA_HEADS, KV_LORA, HEAD_DIM), KV_LORA ** -0.5),
        'w_out': nrm(ks[17], (L, D_MODEL, D_MODEL), D_MODEL ** -0.5),
        'w_gate_up': nrm(ks[18], (L, D_MODEL, 2 * D_FF), D_MODEL ** -0.5),
        'w_down': nrm(ks[19], (L, D_FF, D_MODEL), D_FF ** -0.5),
    }


def reference(x, c, w_ada, b_ada, g_pre_mix, g_post_mix, g_pre_ffn, g_post_ffn, w_in,
              cmp_pe, cmp_w1, cmp_b1, cmp_w2, cmp_b2, g_kv_norm, w_uk, w_uv, w_out,
              w_gate_up, w_down):
    cos, sin = _rope_tables(x.shape[1])
    c_act = jax.nn.silu(c)
    for l in range(DEPTH):
        mod = (c_act @ w_ada[l] + b_ada[l])[:, None, :]
        sh_m, sc_m, ga_m, sh_f, sc_f, ga_f = jnp.split(mod, 6, axis=-1)
        h = _rms_norm(x, g_pre_mix[l]) * (1.0 + sc_m) + sh_m
        y = _hybrid_mixer(h, cos, sin, w_in[l], cmp_pe[l], cmp_w1[l], cmp_b1[l], cmp_w2[l], cmp_b2[l],
                          g_kv_norm[l], w_uk[l], w_uv[l], w_out[l])
        x = x + ga_m * _rms_norm(y, g_post_mix[l])
        h = _rms_norm(x, g_pre_ffn[l]) * (1.0 + sc_f) + sh_f
        x = x + ga_f * _rms_norm(_swiglu(h, w_gate_up[l], w_down[l]), g_post_ffn[l])
    return x
```

```python
import numpy as np
import concourse.bass as bass
import concourse.mybir as mybir
from concourse.bass_utils import run_bass_kernel_spmd
from contextlib import ExitStack

F32 = mybir.dt.float32
BF16 = mybir.dt.bfloat16
ALU = mybir.AluOpType
AF = mybir.ActivationFunctionType
AX = mybir.AxisListType

S = 2048
D = 1024
NT = 16
DFF = 2816
NEG = -30000.0
IN_SPLITS = (512, 128, 128, 128, 128, 128, 128, 24, 512, 128, 16, 512, 64, 8)
NAMES = ['nq', 'nkc', 'nvc', 'nks', 'nvs', 'nkw', 'nvw', 'ngate', 'dq', 'dckv', 'dkr', 'iq', 'ik', 'iw']
OFF = dict(zip(NAMES, np.cumsum((0,) + IN_SPLITS)[:-1]))
NA_FM = 19 * 128
NA = NA_FM + 280
NB_FM = 18 * 128 + 32
NB = NB_FM + 136
BIS_ITERS = 18


class _E:
    def __init__(self, name, eng, sem):
        self.name, self.eng, self.sem, self.tick, self.waited = name, eng, sem, 0, {}


class _St:
    __slots__ = ("w", "r")

    def __init__(self):
        self.w = None
        self.r = {}


class Buf:
    def __init__(self, ap, key):
        self.ap = ap
        self.key = key

    def __getitem__(self, idx):
        return self.ap[idx]


class Ctx:
    def __init__(self, nc, es, n_dma_sems=8):
        self.nc, self.es = nc, es
        self.E = {}
        for name, eng in (("pe", nc.tensor), ("act", nc.scalar), ("dve", nc.vector),
                          ("pool", nc.gpsimd), ("sp", nc.sync)):
            self.E[name] = _E(name, eng, es.enter_context(nc.semaphore("s_" + name)))
        self.dsems = {q: [[es.enter_context(nc.semaphore("d_%s%d" % (q, i))), 0] for i in range(n_dma_sems)]
                      for q in ("sp", "pool")}
        self.dnext = {"sp": 0, "pool": 0}
        self.st = {}

    def _state(self, b):
        k = b.key if isinstance(b, Buf) else (b if isinstance(b, str) else b.name)
        s = self.st.get(k)
        if s is None:
            s = self.st[k] = _St()
        return s

    def _wait(self, E, dep):
        kind, tk = dep
        if kind == E.name and E.name == "pe":
            return
        if E.waited.get(kind, 0) >= tk:
            return
        sem = self.dsems[kind[0]][kind[1]][0] if isinstance(kind, tuple) else self.E[kind].sem
        E.eng.wait_ge(sem, tk)
        E.waited[kind] = tk

    def _deps(self, E, reads, writes):
        deps = []
        for b in reads:
            s = self._state(b)
            if s.w is not None:
                deps.append(s.w)
        for b in writes:
            s = self._state(b)
            if s.w is not None:
                deps.append(s.w)
            deps.extend(s.r.items())
        for d in deps:
            self._wait(E, d)

    def _mark(self, token, reads, writes):
        for b in reads:
            s = self._state(b)
            if s.r.get(token[0], 0) < token[1]:
                s.r[token[0]] = token[1]
        for b in writes:
            s = self._state(b)
            s.w = token
            s.r = {}

    def op(self, en, fn, *args, reads=(), writes=(), **kw):
        E = self.E[en]
        self._deps(E, reads, writes)
        ins = getattr(E.eng, fn)(*args, **kw)
        E.tick += 1
        ins.then_inc(E.sem, 1)
        self._mark((en, E.tick), reads, writes)
        return ins

    def dma(self, q, out, in_, reads=(), writes=(), **kw):
        E = self.E[q]
        self._deps(E, reads, writes)
        i = self.dnext[q]
        self.dnext[q] = (i + 1) % len(self.dsems[q])
        slot = self.dsems[q][i]
        kind = (q, i)
        if slot[1] > 0:
            self._wait(E, (kind, slot[1]))
        slot[1] += 16
        E.eng.dma_start(out=out, in_=in_, **kw).then_inc(slot[0], 16)
        self._mark((kind, slot[1]), reads, writes)

    def barrier(self):
        toks = [(n, e.tick) for n, e in self.E.items() if e.tick > 0]
        for q in self.dsems:
            for i, slot in enumerate(self.dsems[q]):
                if slot[1] > 0:
                    toks.append(((q, i), slot[1]))
        for n, e in self.E.items():
            for t in toks:
                if t[0] == n:
                    if n != "sp" and e.waited.get(n, 0) < t[1]:
                        e.eng.wait_ge(e.sem, t[1])
                        e.waited[n] = t[1]
                else:
                    self._wait(e, t)
        self.st = {}

    def finish(self):
        E = self.E["sp"]
        for q in self.dsems:
            for i, slot in enumerate(self.dsems[q]):
                if slot[1] > 0:
                    self._wait(E, ((q, i), slot[1]))


class Mem:
    def __init__(self, big):
        self.big = big

    def view(self, key, off, shape, dt, pbase=0):
        esz = 4 if dt == F32 else 2
        nel = int(np.prod(shape[1:]))
        assert off % 4 == 0
        a = off // 2
        n = nel * esz // 2
        ap = self.big[pbase:pbase + shape[0], a:a + n]
        if dt == F32:
            ap = ap.bitcast(F32)
        if len(shape) == 3:
            ap = ap.rearrange("p (a b) -> p a b", a=shape[1])
        elif len(shape) == 4:
            ap = ap.rearrange("p (a b c) -> p a b c", a=shape[1], b=shape[2])
        return Buf(ap, key)


def _swap_head(cols):
    c = np.array(cols).copy()
    c[0:8] = cols[8:16]
    c[8:16] = cols[0:8]
    return c


def _col_index():
    def head(base, h):
        return np.arange(base + 64 * h, base + 64 * h + 64)
    A = []
    for j in range(4):
        a = np.concatenate([head(OFF['nq'], 2 * j), head(OFF['nq'], 2 * j + 1)])
        b = np.concatenate([_swap_head(head(OFF['nq'], 2 * j)), _swap_head(head(OFF['nq'], 2 * j + 1))])
        A += [a, b]
    a = np.concatenate([head(OFF['nkc'], 0), head(OFF['nkc'], 1)])
    b = np.concatenate([_swap_head(head(OFF['nkc'], 0)), _swap_head(head(OFF['nkc'], 1))])
    A += [a, b]
    A += [np.arange(OFF['nvc'], OFF['nvc'] + 128)]
    for nm in ('nks', 'nkw'):
        for g in range(2):
            a = np.concatenate([head(OFF[nm], g), head(OFF[nm], g)])
            b = np.concatenate([_swap_head(head(OFF[nm], g)), _swap_head(head(OFF[nm], g))])
            A += [a, b]
    A += [np.arange(OFF['nvs'], OFF['nvs'] + 128), np.arange(OFF['nvw'], OFF['nvw'] + 128),
          np.arange(OFF['ngate'], OFF['ngate'] + 24)]
    A = np.concatenate(A)
    assert A.size == NA
    B = []
    for nm in ('dq', 'iq'):
        for j in range(4):
            a = np.concatenate([head(OFF[nm], 2 * j), head(OFF[nm], 2 * j + 1)])
            b = np.concatenate([_swap_head(head(OFF[nm], 2 * j)), _swap_head(head(OFF[nm], 2 * j + 1))])
            B += [a, b]
    a = np.concatenate([head(OFF['ik'], 0), head(OFF['ik'], 0)])
    b = np.concatenate([_swap_head(head(OFF['ik'], 0)), _swap_head(head(OFF['ik'], 0))])
    B += [a, b]
    kr = np.arange(OFF['dkr'], OFF['dkr'] + 16)
    B += [kr, _swap_head(kr)]
    B += [np.arange(OFF['dckv'], OFF['dckv'] + 128), np.arange(OFF['iw'], OFF['iw'] + 8)]
    B = np.concatenate(B)
    assert B.size == NB
    return A, B


CF_ID = 0
CF_C = 128
CF_S = CF_C + 2048
CF_V = CF_S + 2048
CF_P2 = CF_V + 84
CF_N = CF_P2 + BIS_ITERS
CB_ID = 0
CB_EX = 128
CB_OV = CB_EX + 2048
CB_PE = CB_OV + 32
CB_SEL = CB_PE + 64
CB_N = CB_SEL + 128


def _host_consts(inp):
    cf = np.zeros((128, CF_N), np.float32)
    cf[:, CF_ID:CF_ID + 128] = np.eye(128, dtype=np.float32)
    inv_freq = 1.0 / (np.float32(500000.0) ** (np.arange(0, 16, 2, dtype=np.float32) / np.float32(16)))
    ang = np.arange(S, dtype=np.float32)[:, None] * inv_freq[None, :].astype(np.float32)
    cos, sin = np.cos(ang).astype(np.float32), np.sin(ang).astype(np.float32)
    for p in range(128):
        j = p % 64
        if j < 16:
            cf[p, CF_C:CF_C + S] = cos[:, j % 8]
            cf[p, CF_S:CF_S + S] = (-1.0 if j < 8 else 1.0) * sin[:, j % 8]
        else:
            cf[p, CF_C:CF_C + S] = 1.0
    v = cf[:, CF_V:CF_V + 84]
    v[:, 0:48] = inp['b_ada'][0].reshape(48, 128).T
    v[:, 48:56] = inp['g_pre_mix'][0].reshape(8, 128).T
    v[:, 56:64] = inp['g_post_mix'][0].reshape(8, 128).T
    v[:, 64:72] = inp['g_pre_ffn'][0].reshape(8, 128).T
    v[:, 72:80] = inp['g_post_ffn'][0].reshape(8, 128).T
    v[:, 80] = inp['cmp_b1'][0, 0]
    v[:, 81] = inp['cmp_b1'][0, 1]
    v[:, 82] = np.concatenate([inp['cmp_b2'][0, 0], inp['cmp_b2'][0, 0]])
    v[:, 83] = inp['g_kv_norm'][0]
    cf[:, CF_P2:CF_P2 + BIS_ITERS] = (0.5 ** np.arange(1, BIS_ITERS + 1, dtype=np.float64)).astype(np.float32)[None, :]
    cb = np.zeros((128, CB_N), np.float32)
    cb[:, CB_ID:CB_ID + 128] = np.eye(128, dtype=np.float32)
    for j in range(32):
        cb[j, CB_EX + 64 * j:CB_EX + 64 * j + 64] = 1.0
    ci = np.arange(127)[:, None] * 16
    sj = np.arange(32)[None, :] * 64
    cb[0:127, CB_OV:CB_OV + 32] = ((ci < sj + 64) & (ci + 32 > sj)).astype(np.float32)
    cb[0:64, CB_PE:CB_PE + 32] = inp['cmp_pe'][0, 0].T
    cb[0:64, CB_PE + 32:CB_PE + 64] = inp['cmp_pe'][0, 1].T
    for i in range(16):
        cb[i, CB_SEL + i] = 1.0
        cb[i, CB_SEL + 64 + i] = 1.0
    t = (np.arange(16)[None, :, None] * 128 + np.arange(128)[:, None, None])
    blk = t // 64
    j = np.arange(32)[None, None, :]
    visible = j <= blk
    forced = (j == 0) | (j == blk) | (j == blk - 1)
    mm = (visible & ~forced).astype(np.float32)
    ba = np.where(visible, np.where(forced, 1e6, 0.0), -1e30).astype(np.float32)
    imt = np.concatenate([mm.reshape(128, 512), ba.reshape(128, 512)], axis=1)
    wuk = np.zeros((128, 4, 128), np.float32)
    for h in range(8):
        wuk[:, h // 2, (h % 2) * 64 + 16:(h % 2) * 64 + 64] = inp['w_uk'][0, h]
    wuv = np.transpose(inp['w_uv'][0], (1, 0, 2)).reshape(128, 512)
    w2 = np.concatenate([inp['cmp_w2'][0, 0], inp['cmp_w2'][0, 0], inp['cmp_w2'][0, 1]], axis=1)
    wsm = np.concatenate([wuk.reshape(128, 512), wuv, w2], axis=1).astype(np.float32)
    return cf, cb, imt, wsm


def build(n_seq=2, stage=99, dbg_cols=0):
    nc = bass.Bass("TRN2", target_bir_lowering=False)
    dt_ = nc.dram_tensor
    x_d = dt_("x", [2, S, D], F32, kind="ExternalInput").ap()
    cT_d = dt_("cT", [128, 8, 2], F32, kind="ExternalInput").ap()
    wada_d = dt_("w_ada", [D, 6 * D], F32, kind="ExternalInput").ap()
    cf_d = dt_("cf", [128, CF_N], F32, kind="ExternalInput").ap()
    cb_d = dt_("cb", [128, CB_N], F32, kind="ExternalInput").ap()
    imt_d = dt_("imt", [128, 1024], F32, kind="ExternalInput").ap()
    wsm_d = dt_("wsm", [128, 1216], F32, kind="ExternalInput").ap()
    winA_d = dt_("w_inA", [D, NA], F32, kind="ExternalInput").ap()
    winB_d = dt_("w_inB", [D, NB], F32, kind="ExternalInput").ap()
    w1_d = dt_("cmp_w1", [2, 2048, 128], F32, kind="ExternalInput").ap()
    b2v_d = dt_("b2v", [64], F32, kind="ExternalInput").ap()
    wout_d = dt_("w_out", [D, D], F32, kind="ExternalInput").ap()
    wgu_d = dt_("w_gate_up", [D, 2 * DFF], F32, kind="ExternalInput").ap()
    wdn_d = dt_("w_down", [DFF, D], F32, kind="ExternalInput").ap()
    out_d = dt_("out", [2, S, D], F32, kind="ExternalOutput").ap()
    dbg_d = dt_("dbg", [128, dbg_cols], F32, kind="ExternalOutput").ap() if dbg_cols else None

    with ExitStack() as es:
        cx = Ctx(nc, es)
        TOT = 206 * 1024
        big = es.enter_context(nc.sbuf_tensor("big", [128, TOT // 2], BF16))
        mem = Mem(big)
        PS = [Buf(es.enter_context(nc.psum_tensor("ps%d" % i, [128, 512], F32))[:], "ps%d" % i) for i in range(8)]

        def psb(i):
            return PS[i].ap.bitcast(BF16)

        KB = 1024
        o = 0
        CFB = mem.view("cf", o, [128, CF_N], F32); o += CF_N * 4
        CBB = mem.view("cb", o, [128, CB_N], BF16); o += CB_N * 2
        MSK = mem.view("msk", o, [128, 8, 512], BF16); o += 8 * 512 * 2
        CMN = mem.view("cmn", o, [128, 2048], BF16); o += 2048 * 2
        ONESF = mem.view("onesf", o, [128, 128], F32); o += 512
        MODC = mem.view("modc", o, [128, 48, 2], F32); o += 384
        DERV = mem.view("derv", o, [128, 6, 8, 2], F32); o += 384
        CACT = mem.view("cact", o, [128, 8, 2], F32); o += 64
        SMALL = mem.view("small", o, [128, 64], F32); o += 256
        G1 = mem.view("G1", o, [128, 1024], F32); o += 4096
        G2 = mem.view("G2", o, [128, 1024], F32); o += 4096
        assert o <= 44 * KB, o
        R_OT = 44 * KB
        R_HT = 76 * KB
        R_WIN = 108 * KB
        R_PH = 152 * KB
        OT = mem.view("OT", R_OT, [128, 8, 2048], BF16)
        HT = mem.view("HT", R_HT, [128, 8, 2048], BF16)

        ident_f = CFB[:, CF_ID:CF_ID + 128]
        ident_b = CBB[:, CB_ID:CB_ID + 128]
        ropeC = CFB[:, CF_C:CF_C + S]
        ropeS = CFB[:, CF_S:CF_S + S]
        vec = lambda c0, c1: CFB[:, CF_V + c0:CF_V + c1]

        def dbg_dump(ap, col0, ncols, rd):
            if dbg_d is not None:
                cx.dma("pool", dbg_d[0:ap.shape[0], col0:col0 + ncols], ap, reads=[rd])

        cx.dma("sp", CFB[:], cf_d, writes=[CFB])
        cx.dma("pool", CBB[:], cb_d, writes=[CBB])
        cx.dma("sp", CACT[:], cT_d, writes=[CACT])
        cx.op("pool", "memset", MSK[:], 0.0, writes=[MSK])
        for k in range(4):
            cx.op("pool", "affine_select", out=MSK[:, k, :], in_=MSK[:, k, :], pattern=[[1, 512]],
                  compare_op=ALU.is_ge, fill=NEG, base=-128 * k, channel_multiplier=-1, reads=[MSK], writes=[MSK])
        for k in range(1, 5):
            cx.op("pool", "affine_select", out=MSK[:, 3 + k, :], in_=MSK[:, 3 + k, :], pattern=[[-1, 512]],
                  compare_op=ALU.is_gt, fill=NEG, base=512 - 128 * k, channel_multiplier=1, reads=[MSK], writes=[MSK])
        cx.op("pool", "memset", CMN[:], 0.0, writes=[CMN])
        cx.op("pool", "affine_select", out=CMN[:], in_=CMN[:], pattern=[[1, 2048]],
              compare_op=ALU.is_ge, fill=NEG, base=-31, channel_multiplier=-16, reads=[CMN], writes=[CMN])
        cx.op("pool", "memset", ONESF[:], 1.0, writes=[ONESF])
        cx.op("act", "activation", out=CACT[:], in_=CACT[:], func=AF.Silu, reads=[CACT], writes=[CACT])
        WA = [mem.view("wa0", R_HT, [128, 8, 1024], F32), mem.view("wa1", R_WIN, [128, 8, 1024], F32)]
        wada_v = wada_d.rearrange("(kc p) n -> p kc n", p=128)
        for v in range(6):
            wb = WA[v % 2]
            for kc in range(8):
                cx.dma("sp", wb[:, kc, :], wada_v[:, kc, 1024 * v:1024 * v + 1024], writes=[wb])
            for fc in range(8):
                col = (v * 8 + fc) * 2
                for kc in range(8):
                    cx.op("pe", "matmul", PS[0][:, col:col + 2], wb[:, kc, 128 * fc:128 * fc + 128], CACT[:, kc, :],
                          start=(kc == 0), stop=(kc == 7), reads=[wb, CACT], writes=[PS[0]])
        cx.op("dve", "tensor_tensor", out=MODC[:], in0=PS[0][:, 0:96].rearrange("p (a b) -> p a b", b=2),
              in1=vec(0, 48).unsqueeze(2).to_broadcast([128, 48, 2]), op=ALU.add, reads=[PS[0], CFB], writes=[MODC])
        def gb(c0):
            return vec(c0, c0 + 8).unsqueeze(2).to_broadcast([128, 8, 2])
        cx.op("dve", "scalar_tensor_tensor", out=DERV[:, 0], in0=MODC[:, 8:16, :], scalar=1.0, in1=gb(48),
              op0=ALU.add, op1=ALU.mult, reads=[MODC, CFB], writes=[DERV])
        cx.op("dve", "tensor_copy", out=DERV[:, 1], in_=MODC[:, 0:8, :], reads=[MODC], writes=[DERV])
        cx.op("dve", "scalar_tensor_tensor", out=DERV[:, 2], in0=MODC[:, 32:40, :], scalar=1.0, in1=gb(64),
              op0=ALU.add, op1=ALU.mult, reads=[MODC, CFB], writes=[DERV])
        cx.op("dve", "tensor_copy", out=DERV[:, 3], in_=MODC[:, 24:32, :], reads=[MODC], writes=[DERV])
        cx.op("dve", "tensor_tensor", out=DERV[:, 4], in0=MODC[:, 16:24, :], in1=gb(56), op=ALU.mult,
              reads=[MODC, CFB], writes=[DERV])
        cx.op("dve", "tensor_tensor", out=DERV[:, 5], in0=MODC[:, 40:48, :], in1=gb(72), op=ALU.mult,
              reads=[MODC, CFB], writes=[DERV])
        cx.barrier()

        for seq in range(n_seq):
            DG = mem.view("dg", R_WIN, [128, 128], F32)
            for gi, GT_ in ((4, G1), (5, G2)):
                for fc in range(8):
                    cx.op("dve", "tensor_scalar", out=DG[:], in0=ident_f, scalar1=DERV[:, gi, fc, seq:seq + 1],
                          scalar2=None, op0=ALU.mult, reads=[CFB, DERV], writes=[DG])
                    pb = PS[fc // 4]
                    cx.op("pe", "matmul", pb[:, 128 * (fc % 4):128 * (fc % 4) + 128], ONESF[:], DG[:],
                          start=True, stop=True, reads=[ONESF, DG], writes=[pb])
                    if fc % 4 == 3:
                        cx.op("act", "copy", out=GT_[:, 512 * (fc // 4):512 * (fc // 4) + 512], in_=pb[:],
                              reads=[pb], writes=[GT_])
            cx.barrier()
            XIN = [mem.view("xin%d" % i, R_WIN + 4 * KB * i, [128, 1024], F32) for i in range(2)]
            XS = [mem.view("xs%d" % i, R_WIN + 8 * KB + 4 * KB * i, [128, 1024], F32) for i in range(2)]
            JUNK = mem.view("junk", R_WIN + 16 * KB, [128, 1024], F32)
            SS = [mem.view("ss%d" % i, R_WIN + 20 * KB + 64 * i, [128, 4], F32) for i in range(2)]
            for i in range(NT):
                xi, xs, ss = XIN[i % 2], XS[i % 2], SS[i % 2]
                cx.dma("sp", xi[:], x_d[seq, 128 * i:128 * i + 128, :], writes=[xi])
                cx.op("act", "activation", out=JUNK[:], in_=xi[:], func=AF.Square, accum_out=ss[:, 0:1],
                      reads=[xi], writes=[JUNK, ss])
                cx.op("act", "activation", out=ss[:, 1:2], in_=ss[:, 0:1], func=AF.Sqrt, scale=1.0 / D, bias=1e-6,
                      reads=[ss], writes=[ss])
                cx.op("dve", "reciprocal", out=ss[:, 2:3], in_=ss[:, 1:2], reads=[ss], writes=[ss])
                cx.op("dve", "tensor_scalar", out=xs[:], in0=xi[:], scalar1=ss[:, 2:3], scalar2=None, op0=ALU.mult,
                      reads=[xi, ss], writes=[xs])
                for fc in range(8):
                    pb = PS[2 * (i % 2) + fc // 4]
                    cx.op("pe", "transpose", pb[:, 128 * (fc % 4):128 * (fc % 4) + 128],
                          xs[:, 128 * fc:128 * fc + 128], ident_f, reads=[xs, CFB], writes=[pb])
                for fc in range(8):
                    pb = PS[2 * (i % 2) + fc // 4]
                    cx.op("act", "activation", out=HT[:, fc, 128 * i:128 * i + 128],
                          in_=pb[:, 128 * (fc % 4):128 * (fc % 4) + 128], func=AF.Identity,
                          scale=DERV[:, 0, fc, seq:seq + 1], bias=DERV[:, 1, fc, seq:seq + 1],
                          reads=[pb, DERV], writes=[HT])
            cx.barrier()

            WIN = mem.view("win", R_WIN, [128, 8, NA], BF16)
            cx.dma("pool", WIN[:], winA_d.rearrange("(kc p) n -> p kc n", p=128), writes=[WIN])
            o = R_PH
            QN = mem.view("QN", o, [128, 4, 2048], BF16); o += 16 * KB
            KS = mem.view("KS", o, [128, 2, 2048], BF16); o += 8 * KB
            KW = mem.view("KW", o, [128, 2, 2048], BF16); o += 8 * KB
            KCR = mem.view("KCR", o, [128, 2048], BF16); o += 4 * KB
            VCR = mem.view("VCR", o, [128, 2048], BF16); o += 4 * KB
            VT = mem.view("VT", o, [128, 16, 4, 65], BF16); o += 8320
            GT = mem.view("GT", o, [128, 16, 24], F32); o += 1536
            o_T1 = o
            T1 = mem.view("T1", o, [128, 512], F32); o += 2048
            T2 = mem.view("T2", o, [128, 512], F32); o += 2048
            assert o <= TOT, o
            ucnt = [0]

            def proj_unit(WINb, ca, cb_, M, tc, dst_ap, dstbuf):
                u = ucnt[0]; ucnt[0] += 1
                pa, pb = PS[2 * (u % 2)], PS[2 * (u % 2) + 1]
                for kc in range(8):
                    cx.op("pe", "matmul", pa[0:M, :], WINb[:, kc, ca:ca + M], HT[:, kc, 512 * tc:512 * tc + 512],
                          start=(kc == 0), stop=(kc == 7), reads=[WINb, HT], writes=[pa])
                if cb_ is None:
                    cx.op("act", "copy", out=dst_ap, in_=pa[0:M, :], reads=[pa], writes=[dstbuf])
                    return
                for kc in range(8):
                    cx.op("pe", "matmul", pb[0:M, :], WINb[:, kc, cb_:cb_ + M], HT[:, kc, 512 * tc:512 * tc + 512],
                          start=(kc == 0), stop=(kc == 7), reads=[WINb, HT], writes=[pb])
                cx.op("dve", "tensor_tensor", out=T1[0:M, :], in0=pa[0:M, :], in1=ropeC[0:M, 512 * tc:512 * tc + 512],
                      op=ALU.mult, reads=[pa, CFB], writes=[T1])
                cx.op("dve", "tensor_tensor", out=T2[0:M, :], in0=pb[0:M, :], in1=ropeS[0:M, 512 * tc:512 * tc + 512],
                      op=ALU.mult, reads=[pb, CFB], writes=[T2])
                cx.op("pool", "tensor_tensor", out=dst_ap, in0=T1[0:M, :], in1=T2[0:M, :], op=ALU.add,
                      reads=[T1, T2], writes=[dstbuf])

            cx.op("pool", "memset", VT[:, :, :, 64:65], 1.0, writes=[VT])
            for tc in range(4):
                sl = slice(512 * tc, 512 * tc + 512)
                for j in range(4):
                    proj_unit(WIN, 256 * j, 256 * j + 128, 128, tc, QN[:, j, sl], QN)
                proj_unit(WIN, 1024, 1152, 128, tc, KCR[:, sl], KCR)
                proj_unit(WIN, 1280, None, 128, tc, VCR[:, sl], VCR)
                for g in range(2):
                    proj_unit(WIN, 1408 + 256 * g, 1536 + 256 * g, 128, tc, KS[:, g, sl], KS)
                    proj_unit(WIN, 1920 + 256 * g, 2048 + 256 * g, 128, tc, KW[:, g, sl], KW)
            for i in range(NT):
                pb = PS[4 + i % 2]
                for kc in range(8):
                    cx.op("pe", "matmul", pb[:, 0:280], HT[:, kc, 128 * i:128 * i + 128], WIN[:, kc, NA_FM:NA],
                          start=(kc == 0), stop=(kc == 7), reads=[HT, WIN], writes=[pb])
                cx.op("act", "copy", out=VT[:, i, :, 0:64], in_=pb[:, 0:256].rearrange("p (a b) -> p a b", a=4),
                      reads=[pb], writes=[VT])
                cx.op("act", "activation", out=GT[:, i, :], in_=pb[:, 256:280], func=AF.Sigmoid, reads=[pb], writes=[GT])
            cx.barrier()

            o = R_WIN
            W1 = mem.view("W1", o, [128, 2, 32, 128], BF16); o += 16 * KB
            WSM = mem.view("WSM", o, [128, 1216], BF16); o += 2432
            IMT = mem.view("IMT", o, [128, 2, 16, 32], F32); o += 4096
            PT = []
            for i in range(3):
                PT.append(mem.view("PT%d" % i, o, [128, 512], BF16)); o += 1024
            XG = mem.view("XG", o, [128, 128], F32); o += 512
            UU = mem.view("UU", o, [128, 128], F32); o += 512
            HIDT = mem.view("HIDT", o, [128, 128], BF16); o += 256
            KCT = mem.view("KCT", o, [128, 2, 128], BF16); o += 512
            VCX = mem.view("VCX", o, [128, 2, 98], BF16); o += 392
            B2V = mem.view("B2V", o, [128, 64], F32); o += 256
            BIAS1 = mem.view("BIAS1", o, [128, 2], F32); o += 8
            OA = mem.view("OA", o, [128, 4, 512], F32); o += 8192
            OAB = mem.view("OAB", o_T1, [128, 4, 512], BF16)
            IMPS = []
            for g in range(2):
                IMPS.append(mem.view("IMP%d" % g, o, [128, 4, 32], F32)); o += 512
            TMPI = mem.view("TMPI", o, [128, 4, 32], F32); o += 512
            IMPM = mem.view("IMPM", o, [128, 4, 32], F32); o += 512
            TOP8 = mem.view("TOP8", o, [128, 4, 8], F32); o += 128
            NSELB = mem.view("NSELB", o, [128, 4, 32], BF16); o += 256
            NSELT = mem.view("NSELT", o, [128, 2, 512], BF16); o += 2048
            TMPO = mem.view("TMPO", o, [128, 4, 64], F32); o += 1024
            assert o <= R_PH, o
            RINV = Buf(SMALL[:, 0:4], "RINV")
            COEF = Buf(SMALL[:, 4:8], "COEF")

            for kv in range(2):
                src = w1_d[kv].rearrange("(l d) j -> d l j", d=64)
                cx.dma("pool", W1[0:64, kv], src, writes=[W1])
                cx.dma("pool", W1[64:128, kv], src, writes=[W1])
            cx.dma("pool", WSM[:], wsm_d, writes=[WSM])
            cx.dma("sp", IMT[:].rearrange("p a b c -> p (a b c)"), imt_d, writes=[IMT])
            cx.dma("sp", B2V[:], b2v_d.partition_broadcast(128), writes=[B2V])
            cx.op("pool", "memset", HIDT[:], 0.0, writes=[HIDT])
            cx.op("pool", "memset", VCX[:, :, 64:65], 1.0, writes=[VCX])
            for g in range(2):
                cx.op("pool", "tensor_copy", out=VCX[:, g, 65:97], in_=CBB[:, CB_OV:CB_OV + 32], reads=[CBB], writes=[VCX])
            for kv in range(2):
                for l in range(32):
                    cx.op("pe", "matmul", PS[6][:, kv:kv + 1], W1[0:64, kv, l, :], CBB[0:64, CB_PE + 32 * kv + l:CB_PE + 32 * kv + l + 1],
                          start=(l == 0), stop=(l == 31), reads=[W1, CBB], writes=[PS[6]])
            cx.op("dve", "tensor_tensor", out=BIAS1[:], in0=PS[6][:, 0:2], in1=vec(80, 82), op=ALU.add,
                  reads=[PS[6], CFB], writes=[BIAS1])
            for kv in range(2):
                for g in range(2):
                    srcb = KCR if kv == 0 else VCR
                    hps = PS[4 + g]
                    for l in range(32):
                        cx.op("pe", "matmul", hps[:, 0:127], W1[64 * g:64 * g + 64, kv, l, :],
                              srcb[64 * g:64 * g + 64, l:l + 2017:16], start=(l == 0), stop=(l == 31),
                              reads=[W1, srcb], writes=[hps])
                    cx.op("act", "activation", out=XG[:, 0:127], in_=hps[:, 0:127], func=AF.Identity,
                          bias=BIAS1[:, kv:kv + 1], reads=[hps, BIAS1], writes=[XG])
                    cx.op("dve", "tensor_tensor", out=UU[:, 0:127], in0=XG[:, 0:127], in1=XG[:, 0:127], op=ALU.mult,
                          reads=[XG], writes=[UU])
                    cx.op("dve", "tensor_scalar", out=UU[:, 0:127], in0=UU[:, 0:127], scalar1=0.044715, scalar2=1.0,
                          op0=ALU.mult, op1=ALU.add, reads=[UU], writes=[UU])
                    cx.op("dve", "tensor_tensor", out=UU[:, 0:127], in0=UU[:, 0:127], in1=XG[:, 0:127], op=ALU.mult,
                          reads=[UU, XG], writes=[UU])
                    cx.op("act", "activation", out=UU[:, 0:127], in_=UU[:, 0:127], func=AF.Sigmoid, scale=1.5957691216057308,
                          reads=[UU], writes=[UU])
                    cx.op("dve", "tensor_tensor", out=HIDT[:, 0:127], in0=XG[:, 0:127], in1=UU[:, 0:127], op=ALU.mult,
                          reads=[XG, UU], writes=[HIDT])
                    if kv == 0:
                        cx.op("pe", "matmul", PS[6][:, 0:128], WSM[:, 1024:1152], HIDT[:], start=True, stop=True,
                              reads=[WSM, HIDT], writes=[PS[6]])
                        cx.op("act", "activation", out=KCT[:, g, :], in_=PS[6][:, 0:128], func=AF.Identity,
                              bias=vec(82, 83), reads=[PS[6], CFB], writes=[KCT])
                    else:
                        cx.op("pe", "matmul", PS[6][:, 0:64], HIDT[:], WSM[:, 1152:1216], start=True, stop=True,
                              reads=[WSM, HIDT], writes=[PS[6]])
                        cx.op("dve", "tensor_tensor", out=VCX[:, g, 0:64], in0=PS[6][:, 0:64], in1=B2V[:], op=ALU.add,
                              reads=[PS[6], B2V], writes=[VCX])

            pipe = {"pend": None, "u": 0, "job": 0}

            def unit(kT, qT, extras, V, acc, ncols, first, rk, rq, rv, after=None):
                u = pipe["u"]; pipe["u"] += 1
                sbk = PS[u % 2]
                pt = PT[u % 3]
                cx.op("pe", "matmul", sbk[:], kT, qT, start=True, stop=(len(extras) == 0), reads=[rk, rq], writes=[sbk])
                for n_, (l_, r_, c0, c1, rd) in enumerate(extras):
                    cx.op("pe", "matmul", sbk[:, c0:c1], l_, r_, start=False, stop=(n_ == len(extras) - 1),
                          reads=rd, writes=[sbk])
                cx.op("act", "activation", out=pt[:], in_=sbk[:], func=AF.Exp, scale=0.125, reads=[sbk], writes=[pt])

                def pv():
                    for j in range(4):
                        cx.op("pe", "matmul", acc[:, ncols * j:ncols * j + ncols], pt[:, 128 * j:128 * j + 128], V,
                              start=(first and j == 0), stop=True, reads=[pt, rv], writes=[acc])
                    if after is not None:
                        after()
                prev = pipe["pend"]
                pipe["pend"] = pv
                if prev is not None:
                    prev()

            def flush():
                if pipe["pend"] is not None:
                    pipe["pend"]()
                    pipe["pend"] = None

            def next_acc():
                a = PS[2 + pipe["job"] % 2]
                pipe["job"] += 1
                return a

            def nsa_final(acc, ncols, c, h, br, first_branch):
                accv = acc[:, 0:4 * ncols].rearrange("p (j n) -> p j n", j=4)

                def f():
                    cx.op("dve", "tensor_scalar", out=RINV[:], in0=accv[:, :, 64], scalar1=1e-30, scalar2=None,
                          op0=ALU.max, reads=[acc], writes=[RINV])
                    cx.op("dve", "reciprocal", out=RINV[:], in_=RINV[:], reads=[RINV], writes=[RINV])
                    if br == 0:
                        fi = (h % 4 == 0)
                        IMP = IMPS[h // 4]
                        dst = IMP if fi else TMPI
                        cx.op("dve", "tensor_tensor", out=dst[:], in0=accv[:, :, 65:97],
                              in1=RINV[:].unsqueeze(2).to_broadcast([128, 4, 32]), op=ALU.mult,
                              reads=[acc, RINV], writes=[dst])
                        if not fi:
                            cx.op("pool", "tensor_tensor", out=IMP[:], in0=IMP[:], in1=TMPI[:], op=ALU.add,
                                  reads=[IMP, TMPI], writes=[IMP])
                    cx.op("dve", "tensor_tensor", out=COEF[:], in0=RINV[:], in1=GT[:, 4 * c:4 * c + 4, 8 * br + h],
                          op=ALU.mult, reads=[RINV, GT], writes=[COEF])
                    cb3 = COEF[:].unsqueeze(2).to_broadcast([128, 4, 64])
                    if first_branch:
                        cx.op("dve", "tensor_tensor", out=OA[:, :, 64 * h:64 * h + 64], in0=accv[:, :, 0:64], in1=cb3,
                              op=ALU.mult, reads=[acc, COEF], writes=[OA])
                    else:
                        cx.op("dve", "tensor_tensor", out=TMPO[:], in0=accv[:, :, 0:64], in1=cb3, op=ALU.mult,
                              reads=[acc, COEF], writes=[TMPO])
                        cx.op("pool", "tensor_tensor", out=OA[:, :, 64 * h:64 * h + 64], in0=OA[:, :, 64 * h:64 * h + 64],
                              in1=TMPO[:], op=ALU.add, reads=[OA, TMPO], writes=[OA])
                return f

            def to_OT(SRC, c, fc0):
                for fc in range(4):
                    for j in range(4):
                        col = ((fc % 2) * 4 + j) * 128
                        cx.op("pe", "transpose", psb(6 + fc // 2)[:, col:col + 128], SRC[:, j, 128 * fc:128 * fc + 128],
                              ident_b, reads=[SRC, CBB], writes=[PS[6 + fc // 2]])
                for fc in range(4):
                    cx.op("act", "copy", out=OT[:, fc0 + fc, 512 * c:512 * c + 512],
                          in_=psb(6 + fc // 2)[:, (fc % 2) * 512:(fc % 2) * 512 + 512], reads=[PS[6 + fc // 2]], writes=[OT])

            for c in range(4):
                qs = slice(512 * c, 512 * c + 512)
                for h in range(8):
                    g, b_ = h // 4, 64 * (h % 2)
                    acc = next_acc()
                    unit(KCT[b_:b_ + 64, g, :], QN[b_:b_ + 64, h // 2, qs],
                         [(ident_b, CMN[:, qs], 0, 512, [CBB, CMN])], VCX[:, g, 0:97], acc, 97, True,
                         KCT, QN, VCX, after=nsa_final(acc, 97, c, h, 0, True))
                for h in range(8):
                    g, b_ = h // 4, 64 * (h % 2)
                    acc = next_acc()
                    tiles = list(range(max(0, 4 * c - 4), 4 * c + 4))
                    for n_, i in enumerate(tiles):
                        mk = MSK[:, 3 + (4 * c - i), :] if i < 4 * c else MSK[:, i - 4 * c, :]
                        unit(KW[b_:b_ + 64, g, 128 * i:128 * i + 128], QN[b_:b_ + 64, h // 2, qs],
                             [(ident_b, mk, 0, 512, [CBB, MSK])], VT[:, i, 2 + g, :], acc, 65, n_ == 0, KW, QN, VT,
                             after=(nsa_final(acc, 65, c, h, 2, False) if n_ == len(tiles) - 1 else None))
                flush()
                for g in range(2):
                    IMPg = IMPS[g]
                    cx.op("dve", "tensor_tensor", out=IMPM[:], in0=IMPg[:], in1=IMT[:, 0, 4 * c:4 * c + 4, :], op=ALU.mult,
                          reads=[IMPg, IMT], writes=[IMPM])
                    cx.op("dve", "tensor_tensor", out=IMPM[:], in0=IMPM[:], in1=IMT[:, 1, 4 * c:4 * c + 4, :], op=ALU.add,
                          reads=[IMPM, IMT], writes=[IMPM])
                    for j in range(4):
                        cx.op("dve", "max", out=TOP8[:, j, :], in_=IMPM[:, j, :], reads=[IMPM], writes=[TOP8])
                    for j in range(4):
                        cx.op("dve", "tensor_scalar", out=NSELB[:, j, :], in0=IMPM[:, j, :], scalar1=TOP8[:, j, 7:8],
                              scalar2=NEG, op0=ALU.is_lt, op1=ALU.mult, reads=[IMPM, TOP8], writes=[NSELB])
                    for j in range(4):
                        cx.op("pe", "transpose", psb(6)[0:32, 128 * j:128 * j + 128], NSELB[:, j, :], ident_b,
                              reads=[NSELB, CBB], writes=[PS[6]])
                    cx.op("act", "copy", out=NSELT[0:32, g, :], in_=psb(6)[0:32, 0:512], reads=[PS[6]], writes=[NSELT])
                for h in range(8):
                    g, b_ = h // 4, 64 * (h % 2)
                    acc = next_acc()
                    tiles = list(range(0, 4 * c + 4))
                    for n_, i in enumerate(tiles):
                        ex = [(CBB[0:32, CB_EX + 128 * i:CB_EX + 128 * i + 128], NSELT[0:32, g, :], 0, 512, [CBB, NSELT])]
                        if i >= 4 * c:
                            ex.append((ident_b, MSK[:, i - 4 * c, :], 0, 512, [CBB, MSK]))
                        unit(KS[b_:b_ + 64, g, 128 * i:128 * i + 128], QN[b_:b_ + 64, h // 2, qs], ex,
                             VT[:, i, g, :], acc, 65, n_ == 0, KS, QN, VT,
                             after=(nsa_final(acc, 65, c, h, 1, False) if n_ == len(tiles) - 1 else None))
                flush()
                cx.op("act", "copy", out=OAB[:], in_=OA[:], reads=[OA], writes=[OAB])
                to_OT(OAB, c, 0)
            cx.barrier()

            WINB = mem.view("winb", R_WIN, [128, 8, NB], BF16)
            cx.dma("pool", WINB[:], winB_d.rearrange("(kc p) n -> p kc n", p=128), writes=[WINB])
            o = R_PH
            QD = mem.view("QD", o, [128, 4, 2048], BF16); o += 16 * KB
            QI = mem.view("QI", o, [128, 4, 2048], BF16); o += 16 * KB
            KI = mem.view("KI", o, [128, 2048], BF16); o += 4 * KB
            CKT = mem.view("CKT", o, [128, 2048], BF16); o += 4 * KB
            KRT = mem.view("KRT", o, [128, 2048], BF16); o += 4 * KB
            WI = mem.view("WI", o, [128, 16, 8], F32); o += 512
            T1 = mem.view("T1", o, [128, 512], F32); o_JB = o; o += 2048
            T2 = mem.view("T2", o, [128, 512], F32); o += 2048
            CKN = []
            for i in range(2):
                CKN.append(mem.view("CKN%d" % i, o, [128, 128], F32)); o += 512
            o_RB = o
            assert o + 4096 <= TOT, o
            SSD = Buf(SMALL[:, 40:44], "SSD")
            for tc in range(4):
                sl = slice(512 * tc, 512 * tc + 512)
                for j in range(4):
                    proj_unit(WINB, 256 * j, 256 * j + 128, 128, tc, QD[:, j, sl], QD)
                for j in range(4):
                    proj_unit(WINB, 1024 + 256 * j, 1024 + 256 * j + 128, 128, tc, QI[:, j, sl], QI)
                proj_unit(WINB, 2048, 2176, 128, tc, KI[:, sl], KI)
                proj_unit(WINB, 2304, 2320, 16, tc, KRT[0:16, sl], KRT)
            for i in range(NT):
                pb = PS[4 + i % 2]
                ck = CKN[i % 2]
                for kc in range(8):
                    cx.op("pe", "matmul", pb[:, 0:136], HT[:, kc, 128 * i:128 * i + 128], WINB[:, kc, NB_FM:NB],
                          start=(kc == 0), stop=(kc == 7), reads=[HT, WINB], writes=[pb])
                cx.op("act", "activation", out=ck[:], in_=pb[:, 0:128], func=AF.Square, accum_out=SSD[:, 0:1],
                      reads=[pb], writes=[ck, SSD])
                cx.op("act", "activation", out=SSD[:, 1:2], in_=SSD[:, 0:1], func=AF.Sqrt, scale=1.0 / 128, bias=1e-6,
                      reads=[SSD], writes=[SSD])
                cx.op("dve", "reciprocal", out=SSD[:, 2:3], in_=SSD[:, 1:2], reads=[SSD], writes=[SSD])
                cx.op("dve", "tensor_scalar", out=ck[:], in0=pb[:, 0:128], scalar1=SSD[:, 2:3], scalar2=None, op0=ALU.mult,
                      reads=[pb, SSD], writes=[ck])
                cx.op("act", "mul", out=WI[:, i, :], in_=pb[:, 128:136], mul=float(8 ** -0.5 * 64 ** -0.5), reads=[pb], writes=[WI])
                cx.op("pe", "transpose", PS[6][:, 128 * (i % 4):128 * (i % 4) + 128], ck[:], ident_f, reads=[ck, CFB], writes=[PS[6]])
                if i % 4 == 3:
                    cx.op("act", "activation", out=CKT[:, 512 * (i // 4):512 * (i // 4) + 512], in_=PS[6][:], func=AF.Identity,
                          scale=vec(83, 84), reads=[PS[6], CFB], writes=[CKT])
            cx.barrier()

            o = R_WIN
            KHT = mem.view("KHT", o, [128, 4, 2048], BF16); o += 16 * KB
            VH = mem.view("VH", o, [128, 16, 8, 65], BF16); o += 16640
            WSM = mem.view("WSM", o, [128, 1216], BF16); o += 2432
            PT = []
            for i in range(3):
                PT.append(mem.view("PT%d" % i, o, [128, 512], BF16)); o += 1024
            ODB = mem.view("ODB", o, [128, 4, 512], BF16); o += 4096
            assert o <= R_PH, o
            NM = [mem.view("NM%d" % j, R_HT + 4 * KB * j, [128, 2048], BF16) for j in range(4)]
            IB = [mem.view("IB%d" % j, R_HT + 16 * KB + 8 * KB * j, [128, 2048], F32) for j in range(2)]
            JB = mem.view("JB", o_JB, [128, 2048], BF16)
            RB = [mem.view("RB%d" % j, o_RB + 2048 * j, [128, 512], F32) for j in range(2)]
            LO = Buf(SMALL[:, 8:9], "LO"); MID = Buf(SMALL[:, 9:10], "MID"); CNT = Buf(SMALL[:, 10:11], "CNT")
            TMPS = Buf(SMALL[:, 11:12], "TMPS"); MX8 = Buf(SMALL[:, 12:20], "MX8"); WK = Buf(SMALL[:, 20:38], "WK")
            W0 = Buf(SMALL[:, 38:39], "W0"); MN = Buf(SMALL[:, 39:40], "MN")
            cx.dma("pool", WSM[:], wsm_d, writes=[WSM])
            cx.op("pool", "memset", VH[:, :, :, 64:65], 1.0, writes=[VH])
            n_ = 0
            for j in range(4):
                for tc in range(4):
                    pb = PS[4 + n_ % 2]; n_ += 1
                    cx.op("pe", "matmul", pb[:], WSM[:, 128 * j:128 * j + 128], CKT[:, 512 * tc:512 * tc + 512],
                          start=True, stop=False, reads=[WSM, CKT], writes=[pb])
                    cx.op("pe", "matmul", pb[:], CBB[0:16, CB_SEL:CB_SEL + 128], KRT[0:16, 512 * tc:512 * tc + 512],
                          start=False, stop=True, reads=[CBB, KRT], writes=[pb])
                    cx.op("act", "copy", out=KHT[:, j, 512 * tc:512 * tc + 512], in_=pb[:], reads=[pb], writes=[KHT])
            for i in range(NT):
                pb = PS[4 + n_ % 2]; n_ += 1
                cx.op("pe", "matmul", pb[:], CKT[:, 128 * i:128 * i + 128], WSM[:, 512:1024], start=True, stop=True,
                      reads=[CKT, WSM], writes=[pb])
                cx.op("act", "copy", out=VH[:, i, :, 0:64], in_=pb[:].rearrange("p (h d) -> p h d", h=8), reads=[pb], writes=[VH])

            pipe["pend"] = None
            for c in range(4):
                qs = slice(512 * c, 512 * c + 512)
                Wc = 512 * (c + 1)
                for j in range(4):
                    T = 4 * c + j
                    Wv = 128 * (T + 1)
                    IBt = IB[T % 2]
                    for sc in range(c + 1):
                        N = min(512, Wv - 512 * sc)
                        for h in range(8):
                            b_ = 64 * (h % 2)
                            L = PS[4 + n_ % 2]; n_ += 1
                            cx.op("pe", "matmul", L[:, 0:N], QI[b_:b_ + 64, h // 2, 128 * T:128 * T + 128],
                                  KI[b_:b_ + 64, 512 * sc:512 * sc + N], start=True, stop=True, reads=[QI, KI], writes=[L])
                            if h == 0:
                                cx.op("dve", "tensor_scalar", out=IBt[:, 512 * sc:512 * sc + N], in0=L[:, 0:N], scalar1=0.0,
                                      scalar2=WI[:, T, h:h + 1], op0=ALU.max, op1=ALU.mult, reads=[L, WI], writes=[IBt])
                            else:
                                rb = RB[h % 2]
                                cx.op("dve", "tensor_scalar", out=rb[:, 0:N], in0=L[:, 0:N], scalar1=0.0,
                                      scalar2=WI[:, T, h:h + 1], op0=ALU.max, op1=ALU.mult, reads=[L, WI], writes=[rb])
                                cx.op("pool", "tensor_tensor", out=IBt[:, 512 * sc:512 * sc + N], in0=IBt[:, 512 * sc:512 * sc + N],
                                      in1=rb[:, 0:N], op=ALU.add, reads=[IBt, rb], writes=[IBt])
                    if T >= 2:
                        cx.op("dve", "max", out=MX8[:], in_=IBt[:, 0:Wv], reads=[IBt], writes=[MX8])
                        cx.op("dve", "tensor_reduce", out=MN[:], in_=IBt[:, 0:Wv], axis=AX.X, op=ALU.min, reads=[IBt], writes=[MN])
                    cx.op("pool", "affine_select", out=IBt[:, 128 * T:128 * T + 128], in_=IBt[:, 128 * T:128 * T + 128],
                          pattern=[[-1, 128]], compare_op=ALU.is_ge, fill=-3.0e38, base=0, channel_multiplier=1,
                          reads=[IBt], writes=[IBt])
                    if Wv < Wc:
                        cx.op("pool", "memset", IBt[:, Wv:Wc], -3.0e38, writes=[IBt])
                    if T >= 2:
                        cx.op("dve", "tensor_copy", out=LO[:], in_=MN[:], reads=[MN], writes=[LO])
                        cx.op("dve", "tensor_tensor", out=W0[:], in0=MX8[:, 0:1], in1=MN[:], op=ALU.subtract,
                              reads=[MX8, MN], writes=[W0])
                        cx.op("dve", "tensor_scalar", out=WK[:], in0=CFB[:, CF_P2:CF_P2 + BIS_ITERS], scalar1=W0[:],
                              scalar2=None, op0=ALU.mult, reads=[CFB, W0], writes=[WK])
                        for k in range(BIS_ITERS):
                            cx.op("dve", "tensor_tensor", out=MID[:], in0=LO[:], in1=WK[:, k:k + 1], op=ALU.add,
                                  reads=[LO, WK], writes=[MID])
                            cx.op("dve", "tensor_scalar", out=JB[:, 0:Wv], in0=IBt[:, 0:Wv], scalar1=MID[:], scalar2=0.0,
                                  op0=ALU.is_ge, op1=ALU.add, accum_out=CNT[:], reads=[IBt, MID], writes=[JB, CNT])
                            cx.op("dve", "tensor_scalar", out=TMPS[:], in0=CNT[:], scalar1=255.5, scalar2=WK[:, k:k + 1],
                                  op0=ALU.is_ge, op1=ALU.mult, reads=[CNT, WK], writes=[TMPS])
                            cx.op("dve", "tensor_tensor", out=LO[:], in0=LO[:], in1=TMPS[:], op=ALU.add,
                                  reads=[LO, TMPS], writes=[LO])
                    else:
                        cx.op("dve", "memset", LO[:], -1.0e30, writes=[LO])
                    cx.op("dve", "tensor_scalar", out=NM[j][:, 0:Wc], in0=IBt[:, 0:Wc], scalar1=LO[:], scalar2=NEG,
                          op0=ALU.is_lt, op1=ALU.mult, reads=[IBt, LO], writes=[NM[j]])
                for h in range(8):
                    b_ = 64 * (h % 2)
                    acc = next_acc()
                    accv = acc[:, 0:260].rearrange("p (j n) -> p j n", j=4)

                    def fin(acc=acc, accv=accv, h=h):
                        cx.op("dve", "reciprocal", out=RINV[:], in_=accv[:, :, 64], reads=[acc], writes=[RINV])
                        cx.op("dve", "tensor_tensor", out=ODB[:, :, 64 * h:64 * h + 64], in0=accv[:, :, 0:64],
                              in1=RINV[:].unsqueeze(2).to_broadcast([128, 4, 64]), op=ALU.mult,
                              reads=[acc, RINV], writes=[ODB])
                    tiles = list(range(0, 4 * c + 4))
                    for q_, i in enumerate(tiles):
                        ex = [(NM[j][:, 128 * i:128 * i + 128], ident_b, 128 * j, 128 * j + 128, [NM[j], CBB]) for j in range(4)]
                        unit(KHT[b_:b_ + 64, h // 2, 128 * i:128 * i + 128], QD[b_:b_ + 64, h // 2, qs], ex,
                             VH[:, i, h, :], acc, 65, q_ == 0, KHT, QD, VH, after=(fin if q_ == len(tiles) - 1 else None))
                flush()
                to_OT(ODB, c, 4)
            cx.barrier()

            o = R_HT
            X1 = mem.view("X1", o, [128, 4, 1024], F32); o += 16 * KB
            H2T = mem.view("H2T", o, [128, 8, 512], BF16); o += 8 * KB
            ACTT = mem.view("ACTT", o, [128, 22, 512], BF16)
            WOUT = mem.view("WOUT", o, [128, 8, 1024], BF16); o += 22 * KB
            WG = []
            for i in range(3):
                WG.append(mem.view("WG%d" % i, o, [128, 8, 256], BF16)); o += 4 * KB
            WDN = mem.view("WDN", o, [128, 22, 1024], BF16); o += 44 * KB
            XIN = []
            for i in range(2):
                XIN.append(mem.view("xin%d" % i, o, [128, 1024], F32)); o += 4 * KB
            JUNK = mem.view("junk", o, [128, 1024], F32); o += 4 * KB
            XS = mem.view("xs", o, [128, 1024], F32); o += 4 * KB
            TMPY = mem.view("tmpy", o, [128, 1024], F32); o += 4 * KB
            SIL = []
            for i in range(2):
                SIL.append(mem.view("sil%d" % i, o, [128, 512], F32)); o += 2 * KB
            assert o <= TOT, o
            ST = Buf(SMALL[:, 44:52], "ST")
            cx.dma("pool", WDN[:], wdn_d.rearrange("(c p) n -> p c n", p=128), writes=[WDN])
            wout_v = wout_d.rearrange("(kc p) n -> p kc n", p=128)
            wgu_v = wgu_d.rearrange("(kc p) n -> p kc n", p=128)

            for gi in range(4):
                cx.dma("pool", WOUT[:], wout_v, writes=[WOUT, ACTT])
                for j in range(4):
                    T = 4 * gi + j
                    xi = XIN[j % 2]
                    cx.dma("sp", xi[:], x_d[seq, 128 * T:128 * T + 128, :], writes=[xi])
                    yb = [PS[2 * (j % 2)], PS[2 * (j % 2) + 1]]
                    for n in range(2):
                        for kc in range(8):
                            cx.op("pe", "matmul", yb[n][:], OT[:, kc, 128 * T:128 * T + 128], WOUT[:, kc, 512 * n:512 * n + 512],
                                  start=(kc == 0), stop=(kc == 7), reads=[OT, WOUT], writes=[yb[n]])
                    for n in range(2):
                        cx.op("act", "activation", out=JUNK[:, 512 * n:512 * n + 512], in_=yb[n][:], func=AF.Square,
                              accum_out=ST[:, n:n + 1], reads=[yb[n]], writes=[JUNK, ST])
                    cx.op("dve", "tensor_tensor", out=ST[:, 2:3], in0=ST[:, 0:1], in1=ST[:, 1:2], op=ALU.add, reads=[ST], writes=[ST])
                    cx.op("act", "activation", out=ST[:, 3:4], in_=ST[:, 2:3], func=AF.Sqrt, scale=1.0 / D, bias=1e-6, reads=[ST], writes=[ST])
                    cx.op("dve", "reciprocal", out=ST[:, 4:5], in_=ST[:, 3:4], reads=[ST], writes=[ST])
                    for n in range(2):
                        cx.op("dve", "scalar_tensor_tensor", out=TMPY[:, 512 * n:512 * n + 512], in0=yb[n][:], scalar=ST[:, 4:5],
                              in1=G1[:, 512 * n:512 * n + 512], op0=ALU.mult, op1=ALU.mult, reads=[yb[n], ST, G1], writes=[TMPY])
                    cx.op("pool", "tensor_tensor", out=X1[:, j, :], in0=TMPY[:], in1=xi[:], op=ALU.add, reads=[TMPY, xi], writes=[X1])
                    cx.op("act", "activation", out=JUNK[:], in_=X1[:, j, :], func=AF.Square, accum_out=ST[:, 0:1],
                          reads=[X1], writes=[JUNK, ST])
                    cx.op("act", "activation", out=ST[:, 3:4], in_=ST[:, 0:1], func=AF.Sqrt, scale=1.0 / D, bias=1e-6, reads=[ST], writes=[ST])
                    cx.op("dve", "reciprocal", out=ST[:, 4:5], in_=ST[:, 3:4], reads=[ST], writes=[ST])
                    cx.op("dve", "tensor_scalar", out=XS[:], in0=X1[:, j, :], scalar1=ST[:, 4:5], scalar2=None, op0=ALU.mult,
                          reads=[X1, ST], writes=[XS])
                    for fc in range(8):
                        pb = PS[4 + fc // 4]
                        cx.op("pe", "transpose", pb[:, 128 * (fc % 4):128 * (fc % 4) + 128], XS[:, 128 * fc:128 * fc + 128],
                              ident_f, reads=[XS, CFB], writes=[pb])
                    for fc in range(8):
                        pb = PS[4 + fc // 4]
                        cx.op("act", "activation", out=H2T[:, fc, 128 * j:128 * j + 128],
                              in_=pb[:, 128 * (fc % 4):128 * (fc % 4) + 128], func=AF.Identity,
                              scale=DERV[:, 2, fc, seq:seq + 1], bias=DERV[:, 3, fc, seq:seq + 1],
                              reads=[pb, DERV], writes=[H2T])
                for ch in range(22):
                    wg = WG[ch % 3]
                    cx.dma("pool", wg[:, :, 0:128], wgu_v[:, :, 128 * ch:128 * ch + 128], writes=[wg])
                    cx.dma("pool", wg[:, :, 128:256], wgu_v[:, :, DFF + 128 * ch:DFF + 128 * ch + 128], writes=[wg])
                    pg, pu = PS[2 * (ch % 2)], PS[2 * (ch % 2) + 1]
                    for kc in range(8):
                        cx.op("pe", "matmul", pg[:], wg[:, kc, 0:128], H2T[:, kc, :], start=(kc == 0), stop=(kc == 7),
                              reads=[wg, H2T], writes=[pg])
                    for kc in range(8):
                        cx.op("pe", "matmul", pu[:], wg[:, kc, 128:256], H2T[:, kc, :], start=(kc == 0), stop=(kc == 7),
                              reads=[wg, H2T], writes=[pu])
                    sl_ = SIL[ch % 2]
                    cx.op("act", "activation", out=sl_[:], in_=pg[:], func=AF.Silu, reads=[pg], writes=[sl_])
                    cx.op("dve", "tensor_tensor", out=ACTT[:, ch, :], in0=sl_[:], in1=pu[:], op=ALU.mult,
                          reads=[sl_, pu], writes=[ACTT, WOUT])
                for j in range(4):
                    T = 4 * gi + j
                    zb = [PS[4 + 2 * (j % 2)], PS[5 + 2 * (j % 2)]]
                    for n in range(2):
                        for ch in range(22):
                            cx.op("pe", "matmul", zb[n][:], ACTT[:, ch, 128 * j:128 * j + 128], WDN[:, ch, 512 * n:512 * n + 512],
                                  start=(ch == 0), stop=(ch == 21), reads=[ACTT, WDN], writes=[zb[n]])
                    for n in range(2):
                        cx.op("act", "activation", out=JUNK[:, 512 * n:512 * n + 512], in_=zb[n][:], func=AF.Square,
                              accum_out=ST[:, n:n + 1], reads=[zb[n]], writes=[JUNK, ST])
                    cx.op("dve", "tensor_tensor", out=ST[:, 2:3], in0=ST[:, 0:1], in1=ST[:, 1:2], op=ALU.add, reads=[ST], writes=[ST])
                    cx.op("act", "activation", out=ST[:, 3:4], in_=ST[:, 2:3], func=AF.Sqrt, scale=1.0 / D, bias=1e-6, reads=[ST], writes=[ST])
                    cx.op("dve", "reciprocal", out=ST[:, 4:5], in_=ST[:, 3:4], reads=[ST], writes=[ST])
                    for n in range(2):
                        cx.op("dve", "scalar_tensor_tensor", out=TMPY[:, 512 * n:512 * n + 512], in0=zb[n][:], scalar=ST[:, 4:5],
                              in1=G2[:, 512 * n:512 * n + 512], op0=ALU.mult, op1=ALU.mult, reads=[zb[n], ST, G2], writes=[TMPY])
                    cx.op("pool", "tensor_tensor", out=XS[:], in0=TMPY[:], in1=X1[:, j, :], op=ALU.add, reads=[TMPY, X1], writes=[XS])
                    cx.dma("sp", out_d[seq, 128 * T:128 * T + 128, :], XS[:], reads=[XS])
            cx.barrier()

        cx.barrier()
        cx.finish()
    return nc


def _prep(inputs):
    inp = {k: np.asarray(v) for k, v in inputs.items()}
    cf, cb, imt, wsm = _host_consts(inp)
    A, B = _col_index()
    shared = {
        "w_ada": np.ascontiguousarray(inp['w_ada'][0]),
        "cf": cf, "cb": cb, "imt": imt, "wsm": wsm,
        "w_inA": np.ascontiguousarray(inp['w_in'][0][:, A]),
        "w_inB": np.ascontiguousarray(inp['w_in'][0][:, B]),
        "cmp_w1": np.ascontiguousarray(inp['cmp_w1'][0]),
        "b2v": np.ascontiguousarray(inp['cmp_b2'][0, 1]),
        "w_out": np.ascontiguousarray(inp['w_out'][0]),
        "w_gate_up": np.ascontiguousarray(inp['w_gate_up'][0]),
        "w_down": np.ascontiguousarray(inp['w_down'][0]),
    }
    maps = []
    for c in range(8):
        m = dict(shared)
        m["x"] = np.ascontiguousarray(inp['x'][2 * c:2 * c + 2])
        m["cT"] = np.ascontiguousarray(inp['c'][2 * c:2 * c + 2].T.reshape(8, 128, 2).transpose(1, 0, 2))
        maps.append(m)
    return maps


def kernel(**inputs):
    maps = _prep(inputs)
    nc = build()
    res = run_bass_kernel_spmd(nc, maps, core_ids=list(range(8)))
    return np.concatenate([r["out"] for r in res.results], axis=0).astype(np.float32)
```

```python
import numpy as np
import concourse.bass as bass
import concourse.mybir as mybir
from concourse.bass_utils import run_bass_kernel_spmd
from contextlib import ExitStack

F32 = mybir.dt.float32
BF16 = mybir.dt.bfloat16
ALU = mybir.AluOpType
AF = mybir.ActivationFunctionType
AX = mybir.AxisListType

S = 2048
D = 1024
NT = 16
DFF = 2816
NEG = -30000.0
IN_SPLITS = (512, 128, 128, 128, 128, 128, 128, 24, 512, 128, 16, 512, 64, 8)
NAMES = ['nq', 'nkc', 'nvc', 'nks', 'nvs', 'nkw', 'nvw', 'ngate', 'dq', 'dckv', 'dkr', 'iq', 'ik', 'iw']
OFF = dict(zip(NAMES, np.cumsum((0,) + IN_SPLITS)[:-1]))
NA_FM = 19 * 128
NA = NA_FM + 280
NB_FM = 18 * 128 + 32
NB = NB_FM + 136
BIS_ITERS = 18


class _E:
    def __init__(self, name, eng, sem):
        self.name, self.eng, self.sem, self.tick, self.waited = name, eng, sem, 0, {}


class _St:
    __slots__ = ("w", "r")

    def __init__(self):
        self.w = None
        self.r = {}


class Buf:
    def __init__(self, ap, key):
        self.ap = ap
        self.key = key

    def __getitem__(self, idx):
        return self.ap[idx]


class Ctx:
    def __init__(self, nc, es, n_dma_sems=8):
        self.nc, self.es = nc, es
        self.E = {}
        for name, eng in (("pe", nc.tensor), ("act", nc.scalar), ("dve", nc.vector),
                          ("pool", nc.gpsimd), ("sp", nc.sync)):
            self.E[name] = _E(name, eng, es.enter_context(nc.semaphore("s_" + name)))
        self.dsems = {q: [[es.enter_context(nc.semaphore("d_%s%d" % (q, i))), 0] for i in range(n_dma_sems)]
                      for q in ("sp", "pool")}
        self.dnext = {"sp": 0, "pool": 0}
        self.st = {}

    def _state(self, b):
        k = b.key if isinstance(b, Buf) else (b if isinstance(b, str) else b.name)
        s = self.st.get(k)
        if s is None:
            s = self.st[k] = _St()
        return s

    def _wait(self, E, dep):
        kind, tk = dep
        if kind == E.name and E.name == "pe":
            return
        if E.waited.get(kind, 0) >= tk:
            return
        sem = self.dsems[kind[0]][kind[1]][0] if isinstance(kind, tuple) else self.E[kind].sem
        E.eng.wait_ge(sem, tk)
        E.waited[kind] = tk

    def _deps(self, E, reads, writes):
        deps = []
        for b in reads:
            s = self._state(b)
            if s.w is not None:
                deps.append(s.w)
        for b in writes:
            s = self._state(b)
            if s.w is not None:
                deps.append(s.w)
            deps.extend(s.r.items())
        for d in deps:
            self._wait(E, d)

    def _mark(self, token, reads, writes):
        for b in reads:
            s = self._state(b)
            if s.r.get(token[0], 0) < token[1]:
                s.r[token[0]] = token[1]
        for b in writes:
            s = self._state(b)
            s.w = token
            s.r = {}

    def op(self, en, fn, *args, reads=(), writes=(), **kw):
        E = self.E[en]
        self._deps(E, reads, writes)
        ins = getattr(E.eng, fn)(*args, **kw)
        E.tick += 1
        ins.then_inc(E.sem, 1)
        self._mark((en, E.tick), reads, writes)
        return ins

    def dma(self, q, out, in_, reads=(), writes=(), **kw):
        E = self.E[q]
        self._deps(E, reads, writes)
        i = self.dnext[q]
        self.dnext[q] = (i + 1) % len(self.dsems[q])
        slot = self.dsems[q][i]
        kind = (q, i)
        if slot[1] > 0:
            self._wait(E, (kind, slot[1]))
        slot[1] += 16
        E.eng.dma_start(out=out, in_=in_, **kw).then_inc(slot[0], 16)
        self._mark((kind, slot[1]), reads, writes)

    def barrier(self):
        toks = [(n, e.tick) for n, e in self.E.items() if e.tick > 0]
        for q in self.dsems:
            for i, slot in enumerate(self.dsems[q]):
                if slot[1] > 0:
                    toks.append(((q, i), slot[1]))
        for n, e in self.E.items():
            for t in toks:
                if t[0] == n:
                    if n != "sp" and e.waited.get(n, 0) < t[1]:
                        e.eng.wait_ge(e.sem, t[1])
                        e.waited[n] = t[1]
                else:
                    self._wait(e, t)
        self.st = {}

    def finish(self):
        E = self.E["sp"]
        for q in self.dsems:
            for i, slot in enumerate(self.dsems[q]):
                if slot[1] > 0:
                    self._wait(E, ((q, i), slot[1]))


class Mem:
    def __init__(self, big):
        self.big = big

    def view(self, key, off, shape, dt, pbase=0):
        esz = 4 if dt == F32 else 2
        nel = int(np.prod(shape[1:]))
        assert off % 4 == 0
        a = off // 2
        n = nel * esz // 2
        ap = self.big[pbase:pbase + shape[0], a:a + n]
        if dt == F32:
            ap = ap.bitcast(F32)
        if len(shape) == 3:
            ap = ap.rearrange("p (a b) -> p a b", a=shape[1])
        elif len(shape) == 4:
            ap = ap.rearrange("p (a b c) -> p a b c", a=shape[1], b=shape[2])
        return Buf(ap, key)


def _swap_head(cols):
    c = np.array(cols).copy()
    c[0:8] = cols[8:16]
    c[8:16] = cols[0:8]
    return c


def _col_index():
    def head(base, h):
        return np.arange(base + 64 * h, base + 64 * h + 64)
    A = []
    for j in range(4):
        a = np.concatenate([head(OFF['nq'], 2 * j), head(OFF['nq'], 2 * j + 1)])
        b = np.concatenate([_swap_head(head(OFF['nq'], 2 * j)), _swap_head(head(OFF['nq'], 2 * j + 1))])
        A += [a, b]
    a = np.concatenate([head(OFF['nkc'], 0), head(OFF['nkc'], 1)])
    b = np.concatenate([_swap_head(head(OFF['nkc'], 0)), _swap_head(head(OFF['nkc'], 1))])
    A += [a, b]
    A += [np.arange(OFF['nvc'], OFF['nvc'] + 128)]
    for nm in ('nks', 'nkw'):
        for g in range(2):
            a = np.concatenate([head(OFF[nm], g), head(OFF[nm], g)])
            b = np.concatenate([_swap_head(head(OFF[nm], g)), _swap_head(head(OFF[nm], g))])
            A += [a, b]
    A += [np.arange(OFF['nvs'], OFF['nvs'] + 128), np.arange(OFF['nvw'], OFF['nvw'] + 128),
          np.arange(OFF['ngate'], OFF['ngate'] + 24)]
    A = np.concatenate(A)
    assert A.size == NA
    B = []
    for nm in ('dq', 'iq'):
        for j in range(4):
            a = np.concatenate([head(OFF[nm], 2 * j), head(OFF[nm], 2 * j + 1)])
            b = np.concatenate([_swap_head(head(OFF[nm], 2 * j)), _swap_head(head(OFF[nm], 2 * j + 1))])
            B += [a, b]
    a = np.concatenate([head(OFF['ik'], 0), head(OFF['ik'], 0)])
    b = np.concatenate([_swap_head(head(OFF['ik'], 0)), _swap_head(head(OFF['ik'], 0))])
    B += [a, b]
    kr = np.arange(OFF['dkr'], OFF['dkr'] + 16)
    B += [kr, _swap_head(kr)]
    B += [np.arange(OFF['dckv'], OFF['dckv'] + 128), np.arange(OFF['iw'], OFF['iw'] + 8)]
    B = np.concatenate(B)
    assert B.size == NB
    return A, B


CF_ID = 0
CF_C = 128
CF_S = CF_C + 2048
CF_V = CF_S + 2048
CF_P2 = CF_V + 84
CF_N = CF_P2 + BIS_ITERS
CB_ID = 0
CB_EX = 128
CB_OV = CB_EX + 2048
CB_PE = CB_OV + 32
CB_SEL = CB_PE + 64
CB_N = CB_SEL + 128


def _host_consts(inp):
    cf = np.zeros((128, CF_N), np.float32)
    cf[:, CF_ID:CF_ID + 128] = np.eye(128, dtype=np.float32)
    inv_freq = 1.0 / (np.float32(500000.0) ** (np.arange(0, 16, 2, dtype=np.float32) / np.float32(16)))
    ang = np.arange(S, dtype=np.float32)[:, None] * inv_freq[None, :].astype(np.float32)
    cos, sin = np.cos(ang).astype(np.float32), np.sin(ang).astype(np.float32)
    for p in range(128):
        j = p % 64
        if j < 16:
            cf[p, CF_C:CF_C + S] = cos[:, j % 8]
            cf[p, CF_S:CF_S + S] = (-1.0 if j < 8 else 1.0) * sin[:, j % 8]
        else:
            cf[p, CF_C:CF_C + S] = 1.0
    v = cf[:, CF_V:CF_V + 84]
    v[:, 0:48] = inp['b_ada'][0].reshape(48, 128).T
    v[:, 48:56] = inp['g_pre_mix'][0].reshape(8, 128).T
    v[:, 56:64] = inp['g_post_mix'][0].reshape(8, 128).T
    v[:, 64:72] = inp['g_pre_ffn'][0].reshape(8, 128).T
    v[:, 72:80] = inp['g_post_ffn'][0].reshape(8, 128).T
    v[:, 80] = inp['cmp_b1'][0, 0]
    v[:, 81] = inp['cmp_b1'][0, 1]
    v[:, 82] = np.concatenate([inp['cmp_b2'][0, 0], inp['cmp_b2'][0, 0]])
    v[:, 83] = inp['g_kv_norm'][0]
    cf[:, CF_P2:CF_P2 + BIS_ITERS] = (0.5 ** np.arange(1, BIS_ITERS + 1, dtype=np.float64)).astype(np.float32)[None, :]
    cb = np.zeros((128, CB_N), np.float32)
    cb[:, CB_ID:CB_ID + 128] = np.eye(128, dtype=np.float32)
    for j in range(32):
        cb[j, CB_EX + 64 * j:CB_EX + 64 * j + 64] = 1.0
    ci = np.arange(127)[:, None] * 16
    sj = np.arange(32)[None, :] * 64
    cb[0:127, CB_OV:CB_OV + 32] = ((ci < sj + 64) & (ci + 32 > sj)).astype(np.float32)
    cb[0:64, CB_PE:CB_PE + 32] = inp['cmp_pe'][0, 0].T
    cb[0:64, CB_PE + 32:CB_PE + 64] = inp['cmp_pe'][0, 1].T
    for i in range(16):
        cb[i, CB_SEL + i] = 1.0
        cb[i, CB_SEL + 64 + i] = 1.0
    t = (np.arange(16)[None, :, None] * 128 + np.arange(128)[:, None, None])
    blk = t // 64
    j = np.arange(32)[None, None, :]
    visible = j <= blk
    forced = (j == 0) | (j == blk) | (j == blk - 1)
    mm = (visible & ~forced).astype(np.float32)
    ba = np.where(visible, np.where(forced, 1e6, 0.0), -1e30).astype(np.float32)
    imt = np.concatenate([mm.reshape(128, 512), ba.reshape(128, 512)], axis=1)
    wuk = np.zeros((128, 4, 128), np.float32)
    for h in range(8):
        wuk[:, h // 2, (h % 2) * 64 + 16:(h % 2) * 64 + 64] = inp['w_uk'][0, h]
    wuv = np.transpose(inp['w_uv'][0], (1, 0, 2)).reshape(128, 512)
    w2 = np.concatenate([inp['cmp_w2'][0, 0], inp['cmp_w2'][0, 0], inp['cmp_w2'][0, 1]], axis=1)
    wsm = np.concatenate([wuk.reshape(128, 512), wuv, w2], axis=1).astype(np.float32)
    return cf, cb, imt, wsm


def build(n_seq=2, stage=99, dbg_cols=0):
    nc = bass.Bass("TRN2", target_bir_lowering=False)
    dt_ = nc.dram_tensor
    x_d = dt_("x", [2, S, D], F32, kind="ExternalInput").ap()
    cT_d = dt_("cT", [128, 8, 2], F32, kind="ExternalInput").ap()
    wada_d = dt_("w_ada", [D, 6 * D], F32, kind="ExternalInput").ap()
    cf_d = dt_("cf", [128, CF_N], F32, kind="ExternalInput").ap()
    cb_d = dt_("cb", [128, CB_N], F32, kind="ExternalInput").ap()
    imt_d = dt_("imt", [128, 1024], F32, kind="ExternalInput").ap()
    wsm_d = dt_("wsm", [128, 1216], F32, kind="ExternalInput").ap()
    winA_d = dt_("w_inA", [D, NA], F32, kind="ExternalInput").ap()
    winB_d = dt_("w_inB", [D, NB], F32, kind="ExternalInput").ap()
    w1_d = dt_("cmp_w1", [2, 2048, 128], F32, kind="ExternalInput").ap()
    b2v_d = dt_("b2v", [64], F32, kind="ExternalInput").ap()
    wout_d = dt_("w_out", [D, D], F32, kind="ExternalInput").ap()
    wgu_d = dt_("w_gate_up", [22, 128, 2048], F32, kind="ExternalInput").ap()
    wdn_d = dt_("w_down", [DFF, D], F32, kind="ExternalInput").ap()
    out_d = dt_("out", [2, S, D], F32, kind="ExternalOutput").ap()
    dbg_d = dt_("dbg", [128, dbg_cols], F32, kind="ExternalOutput").ap() if dbg_cols else None

    with ExitStack() as es:
        cx = Ctx(nc, es)
        TOT = 206 * 1024
        big = es.enter_context(nc.sbuf_tensor("big", [128, TOT // 2], BF16))
        mem = Mem(big)
        PS = [Buf(es.enter_context(nc.psum_tensor("ps%d" % i, [128, 512], F32))[:], "ps%d" % i) for i in range(8)]

        def psb(i):
            return PS[i].ap.bitcast(BF16)

        KB = 1024
        o = 0
        CFB = mem.view("cf", o, [128, CF_N], F32); o += CF_N * 4
        CBB = mem.view("cb", o, [128, CB_N], BF16); o += CB_N * 2
        MSK = mem.view("msk", o, [128, 8, 512], BF16); o += 8 * 512 * 2
        CMN = mem.view("cmn", o, [128, 2048], BF16); o += 2048 * 2
        ONESF = mem.view("onesf", o, [128, 128], F32); o += 512
        MODC = mem.view("modc", o, [128, 48, 2], F32); o += 384
        DERV = mem.view("derv", o, [128, 6, 8, 2], F32); o += 384
        CACT = mem.view("cact", o, [128, 8, 2], F32); o += 64
        SMALL = mem.view("small", o, [128, 64], F32); o += 256
        G1 = mem.view("G1", o, [128, 1024], F32); o += 4096
        G2 = mem.view("G2", o, [128, 1024], F32); o += 4096
        assert o <= 44 * KB, o
        R_OT = 44 * KB
        R_HT = 76 * KB
        R_WIN = 108 * KB
        R_PH = 152 * KB
        OT = mem.view("OT", R_OT, [128, 8, 2048], BF16)
        HT = mem.view("HT", R_HT, [128, 8, 2048], BF16)

        ident_f = CFB[:, CF_ID:CF_ID + 128]
        ident_b = CBB[:, CB_ID:CB_ID + 128]
        ropeC = CFB[:, CF_C:CF_C + S]
        ropeS = CFB[:, CF_S:CF_S + S]
        vec = lambda c0, c1: CFB[:, CF_V + c0:CF_V + c1]

        def dbg_dump(ap, col0, ncols, rd):
            if dbg_d is not None:
                cx.dma("pool", dbg_d[0:ap.shape[0], col0:col0 + ncols], ap, reads=[rd])

        cx.dma("sp", CFB[:], cf_d, writes=[CFB])
        cx.dma("pool", CBB[:], cb_d, writes=[CBB])
        cx.dma("sp", CACT[:], cT_d, writes=[CACT])
        cx.op("pool", "memset", MSK[:], 0.0, writes=[MSK])
        for k in range(4):
            cx.op("pool", "affine_select", out=MSK[:, k, :], in_=MSK[:, k, :], pattern=[[1, 512]],
                  compare_op=ALU.is_ge, fill=NEG, base=-128 * k, channel_multiplier=-1, reads=[MSK], writes=[MSK])
        for k in range(1, 5):
            cx.op("pool", "affine_select", out=MSK[:, 3 + k, :], in_=MSK[:, 3 + k, :], pattern=[[-1, 512]],
                  compare_op=ALU.is_gt, fill=NEG, base=512 - 128 * k, channel_multiplier=1, reads=[MSK], writes=[MSK])
        cx.op("pool", "memset", CMN[:], 0.0, writes=[CMN])
        cx.op("pool", "affine_select", out=CMN[:], in_=CMN[:], pattern=[[1, 2048]],
              compare_op=ALU.is_ge, fill=NEG, base=-31, channel_multiplier=-16, reads=[CMN], writes=[CMN])
        cx.op("pool", "memset", ONESF[:], 1.0, writes=[ONESF])
        cx.op("act", "activation", out=CACT[:], in_=CACT[:], func=AF.Silu, reads=[CACT], writes=[CACT])
        WA = [mem.view("wa0", R_HT, [128, 8, 1024], F32), mem.view("wa1", R_WIN, [128, 8, 1024], F32)]
        wada_v = wada_d.rearrange("(kc p) n -> p kc n", p=128)
        for v in range(6):
            wb = WA[v % 2]
            for kc in range(8):
                cx.dma("sp", wb[:, kc, :], wada_v[:, kc, 1024 * v:1024 * v + 1024], writes=[wb])
            for fc in range(8):
                col = (v * 8 + fc) * 2
                for kc in range(8):
                    cx.op("pe", "matmul", PS[0][:, col:col + 2], wb[:, kc, 128 * fc:128 * fc + 128], CACT[:, kc, :],
                          start=(kc == 0), stop=(kc == 7), reads=[wb, CACT], writes=[PS[0]])
        cx.op("dve", "tensor_tensor", out=MODC[:], in0=PS[0][:, 0:96].rearrange("p (a b) -> p a b", b=2),
              in1=vec(0, 48).unsqueeze(2).to_broadcast([128, 48, 2]), op=ALU.add, reads=[PS[0], CFB], writes=[MODC])
        def gb(c0):
            return vec(c0, c0 + 8).unsqueeze(2).to_broadcast([128, 8, 2])
        cx.op("dve", "scalar_tensor_tensor", out=DERV[:, 0], in0=MODC[:, 8:16, :], scalar=1.0, in1=gb(48),
              op0=ALU.add, op1=ALU.mult, reads=[MODC, CFB], writes=[DERV])
        cx.op("dve", "tensor_copy", out=DERV[:, 1], in_=MODC[:, 0:8, :], reads=[MODC], writes=[DERV])
        cx.op("dve", "scalar_tensor_tensor", out=DERV[:, 2], in0=MODC[:, 32:40, :], scalar=1.0, in1=gb(64),
              op0=ALU.add, op1=ALU.mult, reads=[MODC, CFB], writes=[DERV])
        cx.op("dve", "tensor_copy", out=DERV[:, 3], in_=MODC[:, 24:32, :], reads=[MODC], writes=[DERV])
        cx.op("dve", "tensor_tensor", out=DERV[:, 4], in0=MODC[:, 16:24, :], in1=gb(56), op=ALU.mult,
              reads=[MODC, CFB], writes=[DERV])
        cx.op("dve", "tensor_tensor", out=DERV[:, 5], in0=MODC[:, 40:48, :], in1=gb(72), op=ALU.mult,
              reads=[MODC, CFB], writes=[DERV])
        cx.barrier()

        for seq in range(n_seq):
            if seq > 0:
                cx.dma("sp", CFB[:, CF_C:CF_C + 2 * S], cf_d[:, CF_C:CF_C + 2 * S], writes=[CFB])
            WIN = mem.view("win", R_WIN, [128, 8, NA], BF16)
            cx.dma("pool", WIN[:], winA_d.rearrange("(kc p) n -> p kc n", p=128), writes=[WIN])
            DG = mem.view("dg", R_PH, [128, 128], F32)
            for gi, GT_ in ((4, G1), (5, G2)):
                for fc in range(8):
                    cx.op("dve", "tensor_scalar", out=DG[:], in0=ident_f, scalar1=DERV[:, gi, fc, seq:seq + 1],
                          scalar2=None, op0=ALU.mult, reads=[CFB, DERV], writes=[DG])
                    pb = PS[fc // 4]
                    cx.op("pe", "matmul", pb[:, 128 * (fc % 4):128 * (fc % 4) + 128], ONESF[:], DG[:],
                          start=True, stop=True, reads=[ONESF, DG], writes=[pb])
                    if fc % 4 == 3:
                        cx.op("act", "copy", out=GT_[:, 512 * (fc // 4):512 * (fc // 4) + 512], in_=pb[:],
                              reads=[pb], writes=[GT_])
            cx.barrier()
            XIN = [mem.view("xin%d" % i, R_PH + 4 * KB * i, [128, 1024], F32) for i in range(2)]
            XS = [mem.view("xs%d" % i, R_PH + 8 * KB + 4 * KB * i, [128, 1024], F32) for i in range(2)]
            JUNK = mem.view("junk", R_PH + 16 * KB, [128, 1024], F32)
            SS = [mem.view("ss%d" % i, R_PH + 20 * KB + 64 * i, [128, 4], F32) for i in range(2)]
            for i in range(NT):
                xi, xs, ss = XIN[i % 2], XS[i % 2], SS[i % 2]
                cx.dma("sp", xi[:], x_d[seq, 128 * i:128 * i + 128, :], writes=[xi])
                cx.op("act", "activation", out=JUNK[:], in_=xi[:], func=AF.Square, accum_out=ss[:, 0:1],
                      reads=[xi], writes=[JUNK, ss])
                cx.op("act", "activation", out=ss[:, 1:2], in_=ss[:, 0:1], func=AF.Sqrt, scale=1.0 / D, bias=1e-6,
                      reads=[ss], writes=[ss])
                cx.op("dve", "reciprocal", out=ss[:, 2:3], in_=ss[:, 1:2], reads=[ss], writes=[ss])
                cx.op("dve", "tensor_scalar", out=xs[:], in0=xi[:], scalar1=ss[:, 2:3], scalar2=None, op0=ALU.mult,
                      reads=[xi, ss], writes=[xs])
                for fc in range(8):
                    pb = PS[2 * (i % 2) + fc // 4]
                    cx.op("pe", "transpose", pb[:, 128 * (fc % 4):128 * (fc % 4) + 128],
                          xs[:, 128 * fc:128 * fc + 128], ident_f, reads=[xs, CFB], writes=[pb])
                for fc in range(8):
                    pb = PS[2 * (i % 2) + fc // 4]
                    cx.op("act", "activation", out=HT[:, fc, 128 * i:128 * i + 128],
                          in_=pb[:, 128 * (fc % 4):128 * (fc % 4) + 128], func=AF.Identity,
                          scale=DERV[:, 0, fc, seq:seq + 1], bias=DERV[:, 1, fc, seq:seq + 1],
                          reads=[pb, DERV], writes=[HT])
            cx.barrier()

            o = R_PH
            QN = mem.view("QN", o, [128, 4, 2048], BF16); o += 16 * KB
            KS = mem.view("KS", o, [128, 2, 2048], BF16); o += 8 * KB
            KW = mem.view("KW", o, [128, 2, 2048], BF16); o += 8 * KB
            KCR = mem.view("KCR", o, [128, 2048], BF16); o += 4 * KB
            VCR = mem.view("VCR", o, [128, 2048], BF16); o += 4 * KB
            VT = mem.view("VT", o, [128, 16, 4, 65], BF16); o += 8320
            GT = mem.view("GT", o, [128, 16, 24], F32); o += 1536
            o_T1 = o
            T1 = mem.view("T1", o, [128, 512], F32); o += 2048
            T2 = mem.view("T2", o, [128, 512], F32); o += 2048
            assert o <= TOT, o
            ucnt = [0]

            def proj_unit(WINb, ca, cb_, M, tc, dst_ap, dstbuf):
                u = ucnt[0]; ucnt[0] += 1
                pa, pb = PS[2 * (u % 2)], PS[2 * (u % 2) + 1]
                for kc in range(8):
                    cx.op("pe", "matmul", pa[0:M, :], WINb[:, kc, ca:ca + M], HT[:, kc, 512 * tc:512 * tc + 512],
                          start=(kc == 0), stop=(kc == 7), reads=[WINb, HT], writes=[pa])
                if cb_ is None:
                    cx.op("act", "copy", out=dst_ap, in_=pa[0:M, :], reads=[pa], writes=[dstbuf])
                    return
                for kc in range(8):
                    cx.op("pe", "matmul", pb[0:M, :], WINb[:, kc, cb_:cb_ + M], HT[:, kc, 512 * tc:512 * tc + 512],
                          start=(kc == 0), stop=(kc == 7), reads=[WINb, HT], writes=[pb])
                cx.op("dve", "tensor_tensor", out=T1[0:M, :], in0=pa[0:M, :], in1=ropeC[0:M, 512 * tc:512 * tc + 512],
                      op=ALU.mult, reads=[pa, CFB], writes=[T1])
                cx.op("dve", "tensor_tensor", out=T2[0:M, :], in0=pb[0:M, :], in1=ropeS[0:M, 512 * tc:512 * tc + 512],
                      op=ALU.mult, reads=[pb, CFB], writes=[T2])
                cx.op("pool", "tensor_tensor", out=dst_ap, in0=T1[0:M, :], in1=T2[0:M, :], op=ALU.add,
                      reads=[T1, T2], writes=[dstbuf])

            cx.op("pool", "memset", VT[:, :, :, 64:65], 1.0, writes=[VT])
            for tc in range(4):
                sl = slice(512 * tc, 512 * tc + 512)
                for j in range(4):
                    proj_unit(WIN, 256 * j, 256 * j + 128, 128, tc, QN[:, j, sl], QN)
                proj_unit(WIN, 1024, 1152, 128, tc, KCR[:, sl], KCR)
                proj_unit(WIN, 1280, None, 128, tc, VCR[:, sl], VCR)
                for g in range(2):
                    proj_unit(WIN, 1408 + 256 * g, 1536 + 256 * g, 128, tc, KS[:, g, sl], KS)
                    proj_unit(WIN, 1920 + 256 * g, 2048 + 256 * g, 128, tc, KW[:, g, sl], KW)
            for i in range(NT):
                pb = PS[4 + i % 2]
                for kc in range(8):
                    cx.op("pe", "matmul", pb[:, 0:280], HT[:, kc, 128 * i:128 * i + 128], WIN[:, kc, NA_FM:NA],
                          start=(kc == 0), stop=(kc == 7), reads=[HT, WIN], writes=[pb])
                cx.op("act", "copy", out=VT[:, i, :, 0:64], in_=pb[:, 0:256].rearrange("p (a b) -> p a b", a=4),
                      reads=[pb], writes=[VT])
                cx.op("act", "activation", out=GT[:, i, :], in_=pb[:, 256:280], func=AF.Sigmoid, reads=[pb], writes=[GT])
            cx.barrier()

            o = R_WIN
            W1 = mem.view("W1", o, [128, 2, 32, 128], BF16); o += 16 * KB
            WSM = mem.view("WSM", o, [128, 1216], BF16); o += 2432
            IMT = mem.view("IMT", o, [128, 2, 16, 32], F32); o += 4096
            PT = []
            for i in range(4):
                PT.append(mem.view("PT%d" % i, o, [128, 512], BF16)); o += 1024
            XG = mem.view("XG", o, [128, 128], F32); o += 512
            UU = mem.view("UU", o, [128, 128], F32); o += 512
            HIDT = mem.view("HIDT", o, [128, 128], BF16); o += 256
            KCT = mem.view("KCT", o, [128, 2, 128], BF16); o += 512
            VCX = mem.view("VCX", o, [128, 2, 98], BF16); o += 392
            B2V = mem.view("B2V", o, [128, 64], F32); o += 256
            BIAS1 = mem.view("BIAS1", o, [128, 2], F32); o += 8
            OA = mem.view("OA", o, [128, 4, 512], F32); o += 8192
            OAB = mem.view("OAB", o_T1, [128, 4, 512], BF16)
            IMPS = []
            for g in range(2):
                IMPS.append(mem.view("IMP%d" % g, o, [128, 4, 32], F32)); o += 512
            TMPI = mem.view("TMPI", o, [128, 4, 32], F32); o += 512
            IMPM = mem.view("IMPM", o, [128, 4, 32], F32); o += 512
            TOP8 = mem.view("TOP8", o, [128, 4, 8], F32); o += 128
            NSELB = mem.view("NSELB", o, [128, 4, 32], BF16); o += 256
            NSELT = mem.view("NSELT", o, [128, 2, 512], BF16); o += 2048
            TMPO = mem.view("TMPO", o, [128, 4, 64], F32); o += 1024
            assert o <= R_PH, o
            RINV = Buf(SMALL[:, 0:4], "RINV")
            COEF = Buf(SMALL[:, 4:8], "COEF")

            for kv in range(2):
                src = w1_d[kv].rearrange("(l d) j -> d l j", d=64)
                cx.dma("pool", W1[0:64, kv], src, writes=[W1])
                cx.dma("pool", W1[64:128, kv], src, writes=[W1])
            cx.dma("pool", WSM[:], wsm_d, writes=[WSM])
            cx.dma("sp", IMT[:].rearrange("p a b c -> p (a b c)"), imt_d, writes=[IMT])
            cx.dma("sp", B2V[:], b2v_d.partition_broadcast(128), writes=[B2V])
            cx.op("pool", "memset", HIDT[:], 0.0, writes=[HIDT])
            cx.op("pool", "memset", VCX[:, :, 64:65], 1.0, writes=[VCX])
            for g in range(2):
                cx.op("pool", "tensor_copy", out=VCX[:, g, 65:97], in_=CBB[:, CB_OV:CB_OV + 32], reads=[CBB], writes=[VCX])
            for kv in range(2):
                for l in range(32):
                    cx.op("pe", "matmul", PS[6][:, kv:kv + 1], W1[0:64, kv, l, :], CBB[0:64, CB_PE + 32 * kv + l:CB_PE + 32 * kv + l + 1],
                          start=(l == 0), stop=(l == 31), reads=[W1, CBB], writes=[PS[6]])
            cx.op("dve", "tensor_tensor", out=BIAS1[:], in0=PS[6][:, 0:2], in1=vec(80, 82), op=ALU.add,
                  reads=[PS[6], CFB], writes=[BIAS1])
            for kv in range(2):
                for g in range(2):
                    srcb = KCR if kv == 0 else VCR
                    hps = PS[4 + g]
                    for l in range(32):
                        cx.op("pe", "matmul", hps[:, 0:127], W1[64 * g:64 * g + 64, kv, l, :],
                              srcb[64 * g:64 * g + 64, l:l + 2017:16], start=(l == 0), stop=(l == 31),
                              reads=[W1, srcb], writes=[hps])
                    cx.op("act", "activation", out=XG[:, 0:127], in_=hps[:, 0:127], func=AF.Identity,
                          bias=BIAS1[:, kv:kv + 1], reads=[hps, BIAS1], writes=[XG])
                    cx.op("dve", "tensor_tensor", out=UU[:, 0:127], in0=XG[:, 0:127], in1=XG[:, 0:127], op=ALU.mult,
                          reads=[XG], writes=[UU])
                    cx.op("dve", "tensor_scalar", out=UU[:, 0:127], in0=UU[:, 0:127], scalar1=0.044715, scalar2=1.0,
                          op0=ALU.mult, op1=ALU.add, reads=[UU], writes=[UU])
                    cx.op("dve", "tensor_tensor", out=UU[:, 0:127], in0=UU[:, 0:127], in1=XG[:, 0:127], op=ALU.mult,
                          reads=[UU, XG], writes=[UU])
                    cx.op("act", "activation", out=UU[:, 0:127], in_=UU[:, 0:127], func=AF.Sigmoid, scale=1.5957691216057308,
                          reads=[UU], writes=[UU])
                    cx.op("dve", "tensor_tensor", out=HIDT[:, 0:127], in0=XG[:, 0:127], in1=UU[:, 0:127], op=ALU.mult,
                          reads=[XG, UU], writes=[HIDT])
                    if kv == 0:
                        cx.op("pe", "matmul", PS[6][:, 0:128], WSM[:, 1024:1152], HIDT[:], start=True, stop=True,
                              reads=[WSM, HIDT], writes=[PS[6]])
                        cx.op("act", "activation", out=KCT[:, g, :], in_=PS[6][:, 0:128], func=AF.Identity,
                              bias=vec(82, 83), reads=[PS[6], CFB], writes=[KCT])
                    else:
                        cx.op("pe", "matmul", PS[6][:, 0:64], HIDT[:], WSM[:, 1152:1216], start=True, stop=True,
                              reads=[WSM, HIDT], writes=[PS[6]])
                        cx.op("dve", "tensor_tensor", out=VCX[:, g, 0:64], in0=PS[6][:, 0:64], in1=B2V[:], op=ALU.add,
                              reads=[PS[6], B2V], writes=[VCX])

            pipe = {"pend": [], "u": 0, "job": 0}
            SCB = [PS[0], PS[1], PS[4], PS[5]]

            def unit(kT, qT, extras, V, acc, ncols, first, rk, rq, rv, after=None):
                u = pipe["u"]; pipe["u"] += 1
                sbk = SCB[u % 4]
                pt = PT[u % 4]
                cx.op("pe", "matmul", sbk[:], kT, qT, start=True, stop=(len(extras) == 0), reads=[rk, rq], writes=[sbk])
                for n_, (l_, r_, c0, c1, rd) in enumerate(extras):
                    cx.op("pe", "matmul", sbk[:, c0:c1], l_, r_, start=False, stop=(n_ == len(extras) - 1),
                          reads=rd, writes=[sbk])
                cx.op("act", "activation", out=pt[:], in_=sbk[:], func=AF.Exp, scale=0.125, reads=[sbk], writes=[pt])

                def pv():
                    for j in range(4):
                        cx.op("pe", "matmul", acc[:, ncols * j:ncols * j + ncols], pt[:, 128 * j:128 * j + 128], V,
                              start=(first and j == 0), stop=True, reads=[pt, rv], writes=[acc])
                    if after is not None:
                        after()
                pipe["pend"].append(pv)
                if len(pipe["pend"]) > 2:
                    pipe["pend"].pop(0)()

            def flush():
                while pipe["pend"]:
                    pipe["pend"].pop(0)()

            def next_acc():
                a = PS[2 + pipe["job"] % 2]
                pipe["job"] += 1
                return a

            def nsa_final(acc, ncols, c, h, br, first_branch):
                accv = acc[:, 0:4 * ncols].rearrange("p (j n) -> p j n", j=4)

                def f():
                    cx.op("dve", "tensor_scalar", out=RINV[:], in0=accv[:, :, 64], scalar1=1e-30, scalar2=None,
                          op0=ALU.max, reads=[acc], writes=[RINV])
                    cx.op("dve", "reciprocal", out=RINV[:], in_=RINV[:], reads=[RINV], writes=[RINV])
                    if br == 0:
                        fi = (h % 4 == 0)
                        IMP = IMPS[h // 4]
                        dst = IMP if fi else TMPI
                        cx.op("dve", "tensor_tensor", out=dst[:], in0=accv[:, :, 65:97],
                              in1=RINV[:].unsqueeze(2).to_broadcast([128, 4, 32]), op=ALU.mult,
                              reads=[acc, RINV], writes=[dst])
                        if not fi:
                            cx.op("pool", "tensor_tensor", out=IMP[:], in0=IMP[:], in1=TMPI[:], op=ALU.add,
                                  reads=[IMP, TMPI], writes=[IMP])
                    cx.op("dve", "tensor_tensor", out=COEF[:], in0=RINV[:], in1=GT[:, 4 * c:4 * c + 4, 8 * br + h],
                          op=ALU.mult, reads=[RINV, GT], writes=[COEF])
                    cb3 = COEF[:].unsqueeze(2).to_broadcast([128, 4, 64])
                    if first_branch:
                        cx.op("dve", "tensor_tensor", out=OA[:, :, 64 * h:64 * h + 64], in0=accv[:, :, 0:64], in1=cb3,
                              op=ALU.mult, reads=[acc, COEF], writes=[OA])
                    else:
                        cx.op("dve", "tensor_tensor", out=TMPO[:], in0=accv[:, :, 0:64], in1=cb3, op=ALU.mult,
                              reads=[acc, COEF], writes=[TMPO])
                        cx.op("pool", "tensor_tensor", out=OA[:, :, 64 * h:64 * h + 64], in0=OA[:, :, 64 * h:64 * h + 64],
                              in1=TMPO[:], op=ALU.add, reads=[OA, TMPO], writes=[OA])
                return f

            def to_OT(SRC, c, fc0):
                for fc in range(4):
                    for j in range(4):
                        col = ((fc % 2) * 4 + j) * 128
                        cx.op("pe", "transpose", psb(6 + fc // 2)[:, col:col + 128], SRC[:, j, 128 * fc:128 * fc + 128],
                              ident_b, reads=[SRC, CBB], writes=[PS[6 + fc // 2]])
                for fc in range(4):
                    cx.op("act", "copy", out=OT[:, fc0 + fc, 512 * c:512 * c + 512],
                          in_=psb(6 + fc // 2)[:, (fc % 2) * 512:(fc % 2) * 512 + 512], reads=[PS[6 + fc // 2]], writes=[OT])

            for c in range(4):
                qs = slice(512 * c, 512 * c + 512)
                for h in range(8):
                    g, b_ = h // 4, 64 * (h % 2)
                    acc = next_acc()
                    unit(KCT[b_:b_ + 64, g, :], QN[b_:b_ + 64, h // 2, qs],
                         [(ident_b, CMN[:, qs], 0, 512, [CBB, CMN])], VCX[:, g, 0:97], acc, 97, True,
                         KCT, QN, VCX, after=nsa_final(acc, 97, c, h, 0, True))
                for h in range(8):
                    g, b_ = h // 4, 64 * (h % 2)
                    acc = next_acc()
                    tiles = list(range(max(0, 4 * c - 4), 4 * c + 4))
                    for n_, i in enumerate(tiles):
                        mk = MSK[:, 3 + (4 * c - i), :] if i < 4 * c else MSK[:, i - 4 * c, :]
                        unit(KW[b_:b_ + 64, g, 128 * i:128 * i + 128], QN[b_:b_ + 64, h // 2, qs],
                             [(ident_b, mk, 0, 512, [CBB, MSK])], VT[:, i, 2 + g, :], acc, 65, n_ == 0, KW, QN, VT,
                             after=(nsa_final(acc, 65, c, h, 2, False) if n_ == len(tiles) - 1 else None))
                flush()
                for g in range(2):
                    IMPg = IMPS[g]
                    cx.op("dve", "tensor_tensor", out=IMPM[:], in0=IMPg[:], in1=IMT[:, 0, 4 * c:4 * c + 4, :], op=ALU.mult,
                          reads=[IMPg, IMT], writes=[IMPM])
                    cx.op("dve", "tensor_tensor", out=IMPM[:], in0=IMPM[:], in1=IMT[:, 1, 4 * c:4 * c + 4, :], op=ALU.add,
                          reads=[IMPM, IMT], writes=[IMPM])
                    for j in range(4):
                        cx.op("dve", "max", out=TOP8[:, j, :], in_=IMPM[:, j, :], reads=[IMPM], writes=[TOP8])
                    for j in range(4):
                        cx.op("dve", "tensor_scalar", out=NSELB[:, j, :], in0=IMPM[:, j, :], scalar1=TOP8[:, j, 7:8],
                              scalar2=NEG, op0=ALU.is_lt, op1=ALU.mult, reads=[IMPM, TOP8], writes=[NSELB])
                    for j in range(4):
                        cx.op("pe", "transpose", psb(6)[0:32, 128 * j:128 * j + 128], NSELB[:, j, :], ident_b,
                              reads=[NSELB, CBB], writes=[PS[6]])
                    cx.op("act", "copy", out=NSELT[0:32, g, :], in_=psb(6)[0:32, 0:512], reads=[PS[6]], writes=[NSELT])
                for h in range(8):
                    g, b_ = h // 4, 64 * (h % 2)
                    acc = next_acc()
                    tiles = list(range(0, 4 * c + 4))
                    for n_, i in enumerate(tiles):
                        ex = [(CBB[0:32, CB_EX + 128 * i:CB_EX + 128 * i + 128], NSELT[0:32, g, :], 0, 512, [CBB, NSELT])]
                        if i >= 4 * c:
                            ex.append((ident_b, MSK[:, i - 4 * c, :], 0, 512, [CBB, MSK]))
                        unit(KS[b_:b_ + 64, g, 128 * i:128 * i + 128], QN[b_:b_ + 64, h // 2, qs], ex,
                             VT[:, i, g, :], acc, 65, n_ == 0, KS, QN, VT,
                             after=(nsa_final(acc, 65, c, h, 1, False) if n_ == len(tiles) - 1 else None))
                flush()
                cx.op("act", "copy", out=OAB[:], in_=OA[:], reads=[OA], writes=[OAB])
                to_OT(OAB, c, 0)
            cx.barrier()

            WINB = mem.view("winb", R_WIN, [128, 8, NB], BF16)
            cx.dma("pool", WINB[:], winB_d.rearrange("(kc p) n -> p kc n", p=128), writes=[WINB])
            o = R_PH
            QD = mem.view("QD", o, [128, 4, 2048], BF16); o += 16 * KB
            QI = mem.view("QI", o, [128, 4, 2048], BF16); o += 16 * KB
            KI = mem.view("KI", o, [128, 2048], BF16); o += 4 * KB
            CKT = mem.view("CKT", o, [128, 2048], BF16); o += 4 * KB
            KRT = mem.view("KRT", o, [128, 2048], BF16); o += 4 * KB
            WI = mem.view("WI", o, [128, 16, 8], F32); o += 512
            T1 = mem.view("T1", o, [128, 512], F32); o_JB = o; o += 2048
            T2 = mem.view("T2", o, [128, 512], F32); o += 2048
            CKN = []
            for i in range(2):
                CKN.append(mem.view("CKN%d" % i, o, [128, 128], F32)); o += 512
            o_RB = o
            assert o + 4096 <= TOT, o
            SSD = Buf(SMALL[:, 40:44], "SSD")
            for tc in range(4):
                sl = slice(512 * tc, 512 * tc + 512)
                for j in range(4):
                    proj_unit(WINB, 256 * j, 256 * j + 128, 128, tc, QD[:, j, sl], QD)
                for j in range(4):
                    proj_unit(WINB, 1024 + 256 * j, 1024 + 256 * j + 128, 128, tc, QI[:, j, sl], QI)
                proj_unit(WINB, 2048, 2176, 128, tc, KI[:, sl], KI)
                proj_unit(WINB, 2304, 2320, 16, tc, KRT[0:16, sl], KRT)
            for i in range(NT):
                pb = PS[4 + i % 2]
                ck = CKN[i % 2]
                for kc in range(8):
                    cx.op("pe", "matmul", pb[:, 0:136], HT[:, kc, 128 * i:128 * i + 128], WINB[:, kc, NB_FM:NB],
                          start=(kc == 0), stop=(kc == 7), reads=[HT, WINB], writes=[pb])
                cx.op("act", "activation", out=ck[:], in_=pb[:, 0:128], func=AF.Square, accum_out=SSD[:, 0:1],
                      reads=[pb], writes=[ck, SSD])
                cx.op("act", "activation", out=SSD[:, 1:2], in_=SSD[:, 0:1], func=AF.Sqrt, scale=1.0 / 128, bias=1e-6,
                      reads=[SSD], writes=[SSD])
                cx.op("dve", "reciprocal", out=SSD[:, 2:3], in_=SSD[:, 1:2], reads=[SSD], writes=[SSD])
                cx.op("dve", "tensor_scalar", out=ck[:], in0=pb[:, 0:128], scalar1=SSD[:, 2:3], scalar2=None, op0=ALU.mult,
                      reads=[pb, SSD], writes=[ck])
                cx.op("act", "mul", out=WI[:, i, :], in_=pb[:, 128:136], mul=float(8 ** -0.5 * 64 ** -0.5), reads=[pb], writes=[WI])
                cx.op("pe", "transpose", PS[6][:, 128 * (i % 4):128 * (i % 4) + 128], ck[:], ident_f, reads=[ck, CFB], writes=[PS[6]])
                if i % 4 == 3:
                    cx.op("act", "activation", out=CKT[:, 512 * (i // 4):512 * (i // 4) + 512], in_=PS[6][:], func=AF.Identity,
                          scale=vec(83, 84), reads=[PS[6], CFB], writes=[CKT])
            cx.barrier()

            o = R_WIN
            KHT = mem.view("KHT", o, [128, 4, 2048], BF16); o += 16 * KB
            VH = mem.view("VH", o, [128, 16, 8, 65], BF16); o += 16640
            WSM = mem.view("WSM", o, [128, 1216], BF16); o += 2432
            PT = []
            for i in range(4):
                PT.append(mem.view("PT%d" % i, o, [128, 512], BF16)); o += 1024
            ODB = mem.view("ODB", o, [128, 4, 512], BF16); o += 4096
            assert o <= R_PH, o
            NMS = [[mem.view("NMA%d" % j, R_HT + 3 * KB * j, [128, 1536], BF16) for j in range(4)],
                   [mem.view("NMB%d" % j, CF_C * 4 + 4 * KB * j, [128, 2048], BF16) for j in range(4)]]
            IB = [mem.view("IB%d" % j, R_HT + 12 * KB + 8 * KB * j, [128, 2048], F32) for j in range(2)]
            JB = mem.view("JB", o_JB, [128, 2048], BF16)
            RB = [mem.view("RB%d" % j, o_RB + 2048 * j, [128, 512], F32) for j in range(2)]
            LO = Buf(SMALL[:, 8:9], "LO"); MID = Buf(SMALL[:, 9:10], "MID"); CNT = Buf(SMALL[:, 10:11], "CNT")
            TMPS = Buf(SMALL[:, 11:12], "TMPS"); MX8 = Buf(SMALL[:, 12:20], "MX8"); WK = Buf(SMALL[:, 20:38], "WK")
            W0 = Buf(SMALL[:, 38:39], "W0"); MN = Buf(SMALL[:, 39:40], "MN")
            cx.dma("pool", WSM[:], wsm_d, writes=[WSM])
            cx.op("pool", "memset", VH[:, :, :, 64:65], 1.0, writes=[VH])
            n_ = 0
            for j in range(4):
                for tc in range(4):
                    pb = PS[4 + n_ % 2]; n_ += 1
                    cx.op("pe", "matmul", pb[:], WSM[:, 128 * j:128 * j + 128], CKT[:, 512 * tc:512 * tc + 512],
                          start=True, stop=False, reads=[WSM, CKT], writes=[pb])
                    cx.op("pe", "matmul", pb[:], CBB[0:16, CB_SEL:CB_SEL + 128], KRT[0:16, 512 * tc:512 * tc + 512],
                          start=False, stop=True, reads=[CBB, KRT], writes=[pb])
                    cx.op("act", "copy", out=KHT[:, j, 512 * tc:512 * tc + 512], in_=pb[:], reads=[pb], writes=[KHT])
            for i in range(NT):
                pb = PS[4 + n_ % 2]; n_ += 1
                cx.op("pe", "matmul", pb[:], CKT[:, 128 * i:128 * i + 128], WSM[:, 512:1024], start=True, stop=True,
                      reads=[CKT, WSM], writes=[pb])
                cx.op("act", "copy", out=VH[:, i, :, 0:64], in_=pb[:].rearrange("p (h d) -> p h d", h=8), reads=[pb], writes=[VH])

            pipe["pend"] = []
            def idx_scores(c, j):
                T = 4 * c + j
                Wc = 512 * (c + 1)
                Wv = 128 * (T + 1)
                IBt = IB[T % 2]
                for sc in range(c + 1):
                    N = min(512, Wv - 512 * sc)
                    for h in range(8):
                        b_ = 64 * (h % 2)
                        L = PS[6 + lcnt[0] % 2]; lcnt[0] += 1
                        cx.op("pe", "matmul", L[:, 0:N], QI[b_:b_ + 64, h // 2, 128 * T:128 * T + 128],
                              KI[b_:b_ + 64, 512 * sc:512 * sc + N], start=True, stop=True, reads=[QI, KI], writes=[L])
                        if h == 0:
                            cx.op("dve", "tensor_scalar", out=IBt[:, 512 * sc:512 * sc + N], in0=L[:, 0:N], scalar1=0.0,
                                  scalar2=WI[:, T, h:h + 1], op0=ALU.max, op1=ALU.mult, reads=[L, WI], writes=[IBt])
                        else:
                            rb = RB[h % 2]
                            cx.op("dve", "tensor_scalar", out=rb[:, 0:N], in0=L[:, 0:N], scalar1=0.0,
                                  scalar2=WI[:, T, h:h + 1], op0=ALU.max, op1=ALU.mult, reads=[L, WI], writes=[rb])
                            cx.op("pool", "tensor_tensor", out=IBt[:, 512 * sc:512 * sc + N], in0=IBt[:, 512 * sc:512 * sc + N],
                                  in1=rb[:, 0:N], op=ALU.add, reads=[IBt, rb], writes=[IBt])
                if T >= 2:
                    cx.op("dve", "max", out=MX8[:], in_=IBt[:, 0:Wv], reads=[IBt], writes=[MX8])
                    cx.op("dve", "tensor_reduce", out=MN[:], in_=IBt[:, 0:Wv], axis=AX.X, op=ALU.min, reads=[IBt], writes=[MN])
                cx.op("pool", "affine_select", out=IBt[:, 128 * T:128 * T + 128], in_=IBt[:, 128 * T:128 * T + 128],
                      pattern=[[-1, 128]], compare_op=ALU.is_ge, fill=-3.0e38, base=0, channel_multiplier=1,
                      reads=[IBt], writes=[IBt])
                if Wv < Wc:
                    cx.op("pool", "memset", IBt[:, Wv:Wc], -3.0e38, writes=[IBt])

            def idx_bisect(c, j):
                T = 4 * c + j
                Wc = 512 * (c + 1)
                Wv = 128 * (T + 1)
                IBt = IB[T % 2]
                nm = NMS[c % 2][j]
                if T >= 2:
                    cx.op("dve", "tensor_copy", out=LO[:], in_=MN[:], reads=[MN], writes=[LO])
                    cx.op("dve", "tensor_tensor", out=W0[:], in0=MX8[:, 0:1], in1=MN[:], op=ALU.subtract,
                          reads=[MX8, MN], writes=[W0])
                    cx.op("dve", "tensor_scalar", out=WK[:], in0=CFB[:, CF_P2:CF_P2 + BIS_ITERS], scalar1=W0[:],
                          scalar2=None, op0=ALU.mult, reads=[CFB, W0], writes=[WK])
                    for k in range(BIS_ITERS):
                        cx.op("dve", "tensor_tensor", out=MID[:], in0=LO[:], in1=WK[:, k:k + 1], op=ALU.add,
                              reads=[LO, WK], writes=[MID])
                        cx.op("dve", "tensor_scalar", out=JB[:, 0:Wv], in0=IBt[:, 0:Wv], scalar1=MID[:], scalar2=0.0,
                              op0=ALU.is_ge, op1=ALU.add, accum_out=CNT[:], reads=[IBt, MID], writes=[JB, CNT])
                        cx.op("dve", "tensor_scalar", out=TMPS[:], in0=CNT[:], scalar1=255.5, scalar2=WK[:, k:k + 1],
                              op0=ALU.is_ge, op1=ALU.mult, reads=[CNT, WK], writes=[TMPS])
                        cx.op("dve", "tensor_tensor", out=LO[:], in0=LO[:], in1=TMPS[:], op=ALU.add,
                              reads=[LO, TMPS], writes=[LO])
                else:
                    cx.op("dve", "memset", LO[:], -1.0e30, writes=[LO])
                cx.op("dve", "tensor_scalar", out=nm[:, 0:Wc], in0=IBt[:, 0:Wc], scalar1=LO[:], scalar2=NEG,
                      op0=ALU.is_lt, op1=ALU.mult, reads=[IBt, LO], writes=[nm])

            def idx_slices(c):
                out_ = []
                for j in range(4):
                    out_.append(lambda c=c, j=j: idx_scores(c, j))
                    out_.append(lambda c=c, j=j: idx_bisect(c, j))
                return out_

            lcnt = [0]
            for f_ in idx_slices(0):
                f_()
            for c in range(4):
                qs = slice(512 * c, 512 * c + 512)
                NMc = NMS[c % 2]
                sl_next = idx_slices(c + 1) if c < 3 else []
                for h in range(8):
                    if sl_next:
                        sl_next[h]()
                    b_ = 64 * (h % 2)
                    acc = next_acc()
                    accv = acc[:, 0:260].rearrange("p (j n) -> p j n", j=4)

                    def fin(acc=acc, accv=accv, h=h):
                        cx.op("dve", "reciprocal", out=RINV[:], in_=accv[:, :, 64], reads=[acc], writes=[RINV])
                        cx.op("dve", "tensor_tensor", out=ODB[:, :, 64 * h:64 * h + 64], in0=accv[:, :, 0:64],
                              in1=RINV[:].unsqueeze(2).to_broadcast([128, 4, 64]), op=ALU.mult,
                              reads=[acc, RINV], writes=[ODB])
                    tiles = list(range(0, 4 * c + 4))
                    for q_, i in enumerate(tiles):
                        ex = [(NMc[j][:, 128 * i:128 * i + 128], ident_b, 128 * j, 128 * j + 128, [NMc[j], CBB]) for j in range(4)]
                        unit(KHT[b_:b_ + 64, h // 2, 128 * i:128 * i + 128], QD[b_:b_ + 64, h // 2, qs], ex,
                             VH[:, i, h, :], acc, 65, q_ == 0, KHT, QD, VH, after=(fin if q_ == len(tiles) - 1 else None))
                flush()
                to_OT(ODB, c, 4)
            cx.barrier()

            o = R_HT
            X1 = mem.view("X1", o, [128, 4, 1024], F32); o += 16 * KB
            H2T = mem.view("H2T", o, [128, 8, 512], BF16); o += 8 * KB
            ACTT = mem.view("ACTT", o, [128, 22, 512], BF16)
            WOUT = mem.view("WOUT", o, [128, 8, 1024], BF16); o += 22 * KB
            WG = []
            for i in range(3):
                WG.append(mem.view("WG%d" % i, o, [128, 8, 256], BF16)); o += 4 * KB
            WDN = mem.view("WDN", o, [128, 22, 1024], BF16); o += 44 * KB
            XIN = []
            for i in range(2):
                XIN.append(mem.view("xin%d" % i, o, [128, 1024], F32)); o += 4 * KB
            JUNK = mem.view("junk", o, [128, 1024], F32); o += 4 * KB
            XS = mem.view("xs", o, [128, 1024], F32); o += 4 * KB
            TMPY = mem.view("tmpy", o, [128, 1024], F32); o += 4 * KB
            SIL = []
            for i in range(2):
                SIL.append(mem.view("sil%d" % i, o, [128, 512], F32)); o += 2 * KB
            assert o <= TOT, o
            ST = Buf(SMALL[:, 44:52], "ST")
            cx.dma("pool", WDN[:], wdn_d.rearrange("(c p) n -> p c n", p=128), writes=[WDN])
            wout_v = wout_d.rearrange("(kc p) n -> p kc n", p=128)

            for gi in range(4):
                cx.dma("pool", WOUT[:], wout_v, writes=[WOUT, ACTT])
                for j in range(4):
                    T = 4 * gi + j
                    xi = XIN[j % 2]
                    cx.dma("sp", xi[:], x_d[seq, 128 * T:128 * T + 128, :], writes=[xi])
                    yb = [PS[2 * (j % 2)], PS[2 * (j % 2) + 1]]
                    for n in range(2):
                        for kc in range(8):
                            cx.op("pe", "matmul", yb[n][:], OT[:, kc, 128 * T:128 * T + 128], WOUT[:, kc, 512 * n:512 * n + 512],
                                  start=(kc == 0), stop=(kc == 7), reads=[OT, WOUT], writes=[yb[n]])
                    for n in range(2):
                        cx.op("act", "activation", out=JUNK[:, 512 * n:512 * n + 512], in_=yb[n][:], func=AF.Square,
                              accum_out=ST[:, n:n + 1], reads=[yb[n]], writes=[JUNK, ST])
                    cx.op("dve", "tensor_tensor", out=ST[:, 2:3], in0=ST[:, 0:1], in1=ST[:, 1:2], op=ALU.add, reads=[ST], writes=[ST])
                    cx.op("act", "activation", out=ST[:, 3:4], in_=ST[:, 2:3], func=AF.Sqrt, scale=1.0 / D, bias=1e-6, reads=[ST], writes=[ST])
                    cx.op("dve", "reciprocal", out=ST[:, 4:5], in_=ST[:, 3:4], reads=[ST], writes=[ST])
                    for n in range(2):
                        cx.op("dve", "scalar_tensor_tensor", out=TMPY[:, 512 * n:512 * n + 512], in0=yb[n][:], scalar=ST[:, 4:5],
                              in1=G1[:, 512 * n:512 * n + 512], op0=ALU.mult, op1=ALU.mult, reads=[yb[n], ST, G1], writes=[TMPY])
                    cx.op("pool", "tensor_tensor", out=X1[:, j, :], in0=TMPY[:], in1=xi[:], op=ALU.add, reads=[TMPY, xi], writes=[X1])
                    cx.op("act", "activation", out=JUNK[:], in_=X1[:, j, :], func=AF.Square, accum_out=ST[:, 0:1],
                          reads=[X1], writes=[JUNK, ST])
                    cx.op("act", "activation", out=ST[:, 3:4], in_=ST[:, 0:1], func=AF.Sqrt, scale=1.0 / D, bias=1e-6, reads=[ST], writes=[ST])
                    cx.op("dve", "reciprocal", out=ST[:, 4:5], in_=ST[:, 3:4], reads=[ST], writes=[ST])
                    cx.op("dve", "tensor_scalar", out=XS[:], in0=X1[:, j, :], scalar1=ST[:, 4:5], scalar2=None, op0=ALU.mult,
                          reads=[X1, ST], writes=[XS])
                    for fc in range(8):
                        pb = PS[4 + fc // 4]
                        cx.op("pe", "transpose", pb[:, 128 * (fc % 4):128 * (fc % 4) + 128], XS[:, 128 * fc:128 * fc + 128],
                              ident_f, reads=[XS, CFB], writes=[pb])
                    for fc in range(8):
                        pb = PS[4 + fc // 4]
                        cx.op("act", "activation", out=H2T[:, fc, 128 * j:128 * j + 128],
                              in_=pb[:, 128 * (fc % 4):128 * (fc % 4) + 128], func=AF.Identity,
                              scale=DERV[:, 2, fc, seq:seq + 1], bias=DERV[:, 3, fc, seq:seq + 1],
                              reads=[pb, DERV], writes=[H2T])
                for ch in range(22):
                    wg = WG[ch % 3]
                    cx.dma("pool", wg[:].rearrange("p a b -> p (a b)"), wgu_d[ch], writes=[wg])
                    pg, pu = PS[2 * (ch % 2)], PS[2 * (ch % 2) + 1]
                    for kc in range(8):
                        cx.op("pe", "matmul", pg[:], wg[:, kc, 0:128], H2T[:, kc, :], start=(kc == 0), stop=(kc == 7),
                              reads=[wg, H2T], writes=[pg])
                    for kc in range(8):
                        cx.op("pe", "matmul", pu[:], wg[:, kc, 128:256], H2T[:, kc, :], start=(kc == 0), stop=(kc == 7),
                              reads=[wg, H2T], writes=[pu])
                    sl_ = SIL[ch % 2]
                    cx.op("act", "activation", out=sl_[:], in_=pg[:], func=AF.Silu, reads=[pg], writes=[sl_])
                    cx.op("dve", "tensor_tensor", out=ACTT[:, ch, :], in0=sl_[:], in1=pu[:], op=ALU.mult,
                          reads=[sl_, pu], writes=[ACTT, WOUT])
                for j in range(4):
                    T = 4 * gi + j
                    zb = [PS[4 + 2 * (j % 2)], PS[5 + 2 * (j % 2)]]
                    for n in range(2):
                        for ch in range(22):
                            cx.op("pe", "matmul", zb[n][:], ACTT[:, ch, 128 * j:128 * j + 128], WDN[:, ch, 512 * n:512 * n + 512],
                                  start=(ch == 0), stop=(ch == 21), reads=[ACTT, WDN], writes=[zb[n]])
                    for n in range(2):
                        cx.op("act", "activation", out=JUNK[:, 512 * n:512 * n + 512], in_=zb[n][:], func=AF.Square,
                              accum_out=ST[:, n:n + 1], reads=[zb[n]], writes=[JUNK, ST])
                    cx.op("dve", "tensor_tensor", out=ST[:, 2:3], in0=ST[:, 0:1], in1=ST[:, 1:2], op=ALU.add, reads=[ST], writes=[ST])
                    cx.op("act", "activation", out=ST[:, 3:4], in_=ST[:, 2:3], func=AF.Sqrt, scale=1.0 / D, bias=1e-6, reads=[ST], writes=[ST])
                    cx.op("dve", "reciprocal", out=ST[:, 4:5], in_=ST[:, 3:4], reads=[ST], writes=[ST])
                    for n in range(2):
                        cx.op("dve", "scalar_tensor_tensor", out=TMPY[:, 512 * n:512 * n + 512], in0=zb[n][:], scalar=ST[:, 4:5],
                              in1=G2[:, 512 * n:512 * n + 512], op0=ALU.mult, op1=ALU.mult, reads=[zb[n], ST, G2], writes=[TMPY])
                    cx.op("pool", "tensor_tensor", out=XS[:], in0=TMPY[:], in1=X1[:, j, :], op=ALU.add, reads=[TMPY, X1], writes=[XS])
                    cx.dma("sp", out_d[seq, 128 * T:128 * T + 128, :], XS[:], reads=[XS])
            cx.barrier()

        cx.barrier()
        cx.finish()
    return nc


def _prep(inputs):
    inp = {k: np.asarray(v) for k, v in inputs.items()}
    cf, cb, imt, wsm = _host_consts(inp)
    A, B = _col_index()
    shared = {
        "w_ada": np.ascontiguousarray(inp['w_ada'][0]),
        "cf": cf, "cb": cb, "imt": imt, "wsm": wsm,
        "w_inA": np.ascontiguousarray(inp['w_in'][0][:, A]),
        "w_inB": np.ascontiguousarray(inp['w_in'][0][:, B]),
        "cmp_w1": np.ascontiguousarray(inp['cmp_w1'][0]),
        "b2v": np.ascontiguousarray(inp['cmp_b2'][0, 1]),
        "w_out": np.ascontiguousarray(inp['w_out'][0]),
        "w_gate_up": np.ascontiguousarray(
            inp['w_gate_up'][0].reshape(8, 128, 2, 22, 128).transpose(3, 1, 0, 2, 4).reshape(22, 128, 2048)),
        "w_down": np.ascontiguousarray(inp['w_down'][0]),
    }
    maps = []
    for c in range(8):
        m = dict(shared)
        m["x"] = np.ascontiguousarray(inp['x'][2 * c:2 * c + 2])
        m["cT"] = np.ascontiguousarray(inp['c'][2 * c:2 * c + 2].T.reshape(8, 128, 2).transpose(1, 0, 2))
        maps.append(m)
    return maps


def kernel(**inputs):
    maps = _prep(inputs)
    nc = build()
    res = run_bass_kernel_spmd(nc, maps, core_ids=list(range(8)))
    return np.concatenate([r["out"] for r in res.results], axis=0).astype(np.float32)
```

```python
import numpy as np
import concourse.bass as bass
import concourse.mybir as mybir
from concourse.bass_utils import run_bass_kernel_spmd
from contextlib import ExitStack

F32 = mybir.dt.float32
BF16 = mybir.dt.bfloat16
ALU = mybir.AluOpType
AF = mybir.ActivationFunctionType
AX = mybir.AxisListType

S = 2048
D = 1024
NT = 16
DFF = 2816
NEG = -30000.0
IN_SPLITS = (512, 128, 128, 128, 128, 128, 128, 24, 512, 128, 16, 512, 64, 8)
NAMES = ['nq', 'nkc', 'nvc', 'nks', 'nvs', 'nkw', 'nvw', 'ngate', 'dq', 'dckv', 'dkr', 'iq', 'ik', 'iw']
OFF = dict(zip(NAMES, np.cumsum((0,) + IN_SPLITS)[:-1]))
NA_FM = 19 * 128
NA = NA_FM + 280
NB_FM = 18 * 128 + 32
NB = NB_FM + 136
BIS_ITERS = 18
import os as _os
_DBG = _os.environ.get('KDBG', '')


class _E:
    def __init__(self, name, eng, sem):
        self.name, self.eng, self.sem, self.tick, self.waited = name, eng, sem, 0, {}


class _St:
    __slots__ = ("w", "r")

    def __init__(self):
        self.w = None
        self.r = {}


class Buf:
    def __init__(self, ap, key):
        self.ap = ap
        self.key = key

    def __getitem__(self, idx):
        return self.ap[idx]


class Ctx:
    def __init__(self, nc, es, n_dma_sems=8):
        self.nc, self.es = nc, es
        self.E = {}
        for name, eng in (("pe", nc.tensor), ("act", nc.scalar), ("dve", nc.vector),
                          ("pool", nc.gpsimd), ("sp", nc.sync)):
            self.E[name] = _E(name, eng, es.enter_context(nc.semaphore("s_" + name)))
        self.dsems = {q: [[es.enter_context(nc.semaphore("d_%s%d" % (q, i))), 0] for i in range(n_dma_sems)]
                      for q in ("sp", "pool")}
        self.dnext = {"sp": 0, "pool": 0}
        self.st = {}

    def _state(self, b):
        k = b.key if isinstance(b, Buf) else (b if isinstance(b, str) else b.name)
        s = self.st.get(k)
        if s is None:
            s = self.st[k] = _St()
        return s

    def _wait(self, E, dep):
        kind, tk = dep
        if kind == E.name and E.name == "pe":
            return
        if E.waited.get(kind, 0) >= tk:
            return
        sem = self.dsems[kind[0]][kind[1]][0] if isinstance(kind, tuple) else self.E[kind].sem
        E.eng.wait_ge(sem, tk)
        E.waited[kind] = tk

    def _deps(self, E, reads, writes):
        deps = []
        for b in reads:
            s = self._state(b)
            if s.w is not None:
                deps.append(s.w)
        for b in writes:
            s = self._state(b)
            if s.w is not None:
                deps.append(s.w)
            deps.extend(s.r.items())
        for d in deps:
            self._wait(E, d)

    def _mark(self, token, reads, writes):
        for b in reads:
            s = self._state(b)
            if s.r.get(token[0], 0) < token[1]:
                s.r[token[0]] = token[1]
        for b in writes:
            s = self._state(b)
            s.w = token
            s.r = {}

    def op(self, en, fn, *args, reads=(), writes=(), **kw):
        E = self.E[en]
        self._deps(E, reads, writes)
        ins = getattr(E.eng, fn)(*args, **kw)
        E.tick += 1
        ins.then_inc(E.sem, 1)
        self._mark((en, E.tick), reads, writes)
        return ins

    def dma(self, q, out, in_, reads=(), writes=(), **kw):
        E = self.E[q]
        self._deps(E, reads, writes)
        i = self.dnext[q]
        self.dnext[q] = (i + 1) % len(self.dsems[q])
        slot = self.dsems[q][i]
        kind = (q, i)
        if slot[1] > 0:
            self._wait(E, (kind, slot[1]))
        slot[1] += 16
        E.eng.dma_start(out=out, in_=in_, **kw).then_inc(slot[0], 16)
        self._mark((kind, slot[1]), reads, writes)

    def barrier(self):
        toks = [(n, e.tick) for n, e in self.E.items() if e.tick > 0]
        for q in self.dsems:
            for i, slot in enumerate(self.dsems[q]):
                if slot[1] > 0:
                    toks.append(((q, i), slot[1]))
        for n, e in self.E.items():
            for t in toks:
                if t[0] == n:
                    if n != "sp" and e.waited.get(n, 0) < t[1]:
                        e.eng.wait_ge(e.sem, t[1])
                        e.waited[n] = t[1]
                else:
                    self._wait(e, t)
        self.st = {}

    def finish(self):
        E = self.E["sp"]
        for q in self.dsems:
            for i, slot in enumerate(self.dsems[q]):
                if slot[1] > 0:
                    self._wait(E, ((q, i), slot[1]))


class Mem:
    def __init__(self, big):
        self.big = big

    def view(self, key, off, shape, dt, pbase=0):
        esz = 4 if dt == F32 else 2
        nel = int(np.prod(shape[1:]))
        assert off % 4 == 0
        a = off // 2
        n = nel * esz // 2
        ap = self.big[pbase:pbase + shape[0], a:a + n]
        if dt == F32:
            ap = ap.bitcast(F32)
        if len(shape) == 3:
            ap = ap.rearrange("p (a b) -> p a b", a=shape[1])
        elif len(shape) == 4:
            ap = ap.rearrange("p (a b c) -> p a b c", a=shape[1], b=shape[2])
        return Buf(ap, key)


def _swap_head(cols):
    c = np.array(cols).copy()
    c[0:8] = cols[8:16]
    c[8:16] = cols[0:8]
    return c


def _col_index():
    def head(base, h):
        return np.arange(base + 64 * h, base + 64 * h + 64)
    A = []
    for j in range(4):
        a = np.concatenate([head(OFF['nq'], 2 * j), head(OFF['nq'], 2 * j + 1)])
        b = np.concatenate([_swap_head(head(OFF['nq'], 2 * j)), _swap_head(head(OFF['nq'], 2 * j + 1))])
        A += [a, b]
    a = np.concatenate([head(OFF['nkc'], 0), head(OFF['nkc'], 1)])
    b = np.concatenate([_swap_head(head(OFF['nkc'], 0)), _swap_head(head(OFF['nkc'], 1))])
    A += [a, b]
    A += [np.arange(OFF['nvc'], OFF['nvc'] + 128)]
    for nm in ('nks', 'nkw'):
        for g in range(2):
            a = np.concatenate([head(OFF[nm], g), head(OFF[nm], g)])
            b = np.concatenate([_swap_head(head(OFF[nm], g)), _swap_head(head(OFF[nm], g))])
            A += [a, b]
    A += [np.arange(OFF['nvs'], OFF['nvs'] + 128), np.arange(OFF['nvw'], OFF['nvw'] + 128),
          np.arange(OFF['ngate'], OFF['ngate'] + 24)]
    A = np.concatenate(A)
    assert A.size == NA
    B = []
    for nm in ('dq', 'iq'):
        for j in range(4):
            a = np.concatenate([head(OFF[nm], 2 * j), head(OFF[nm], 2 * j + 1)])
            b = np.concatenate([_swap_head(head(OFF[nm], 2 * j)), _swap_head(head(OFF[nm], 2 * j + 1))])
            B += [a, b]
    a = np.concatenate([head(OFF['ik'], 0), head(OFF['ik'], 0)])
    b = np.concatenate([_swap_head(head(OFF['ik'], 0)), _swap_head(head(OFF['ik'], 0))])
    B += [a, b]
    kr = np.arange(OFF['dkr'], OFF['dkr'] + 16)
    B += [kr, _swap_head(kr)]
    B += [np.arange(OFF['dckv'], OFF['dckv'] + 128), np.arange(OFF['iw'], OFF['iw'] + 8)]
    B = np.concatenate(B)
    assert B.size == NB
    return A, B


CF_ID = 0
CF_C = 128
CF_S = CF_C + 2048
CF_V = CF_S + 2048
CF_P2 = CF_V + 84
CF_N = CF_P2 + BIS_ITERS
CB_ID = 0
CB_EX = 128
CB_OV = CB_EX + 2048
CB_PE = CB_OV + 32
CB_SEL = CB_PE + 64
CB_N = CB_SEL + 128


def _host_consts(inp):
    cf = np.zeros((128, CF_N), np.float32)
    cf[:, CF_ID:CF_ID + 128] = np.eye(128, dtype=np.float32)
    inv_freq = 1.0 / (np.float32(500000.0) ** (np.arange(0, 16, 2, dtype=np.float32) / np.float32(16)))
    ang = np.arange(S, dtype=np.float32)[:, None] * inv_freq[None, :].astype(np.float32)
    cos, sin = np.cos(ang).astype(np.float32), np.sin(ang).astype(np.float32)
    for p in range(128):
        j = p % 64
        if j < 16:
            cf[p, CF_C:CF_C + S] = cos[:, j % 8]
            cf[p, CF_S:CF_S + S] = (-1.0 if j < 8 else 1.0) * sin[:, j % 8]
        else:
            cf[p, CF_C:CF_C + S] = 1.0
    v = cf[:, CF_V:CF_V + 84]
    v[:, 0:48] = inp['b_ada'][0].reshape(48, 128).T
    v[:, 48:56] = inp['g_pre_mix'][0].reshape(8, 128).T
    v[:, 56:64] = inp['g_post_mix'][0].reshape(8, 128).T
    v[:, 64:72] = inp['g_pre_ffn'][0].reshape(8, 128).T
    v[:, 72:80] = inp['g_post_ffn'][0].reshape(8, 128).T
    v[:, 80] = inp['cmp_b1'][0, 0]
    v[:, 81] = inp['cmp_b1'][0, 1]
    v[:, 82] = np.concatenate([inp['cmp_b2'][0, 0], inp['cmp_b2'][0, 0]])
    v[:, 83] = inp['g_kv_norm'][0]
    cf[:, CF_P2:CF_P2 + BIS_ITERS] = (0.5 ** np.arange(1, BIS_ITERS + 1, dtype=np.float64)).astype(np.float32)[None, :]
    cb = np.zeros((128, CB_N), np.float32)
    cb[:, CB_ID:CB_ID + 128] = np.eye(128, dtype=np.float32)
    for j in range(32):
        cb[j, CB_EX + 64 * j:CB_EX + 64 * j + 64] = 1.0
    ci = np.arange(127)[:, None] * 16
    sj = np.arange(32)[None, :] * 64
    cb[0:127, CB_OV:CB_OV + 32] = ((ci < sj + 64) & (ci + 32 > sj)).astype(np.float32)
    cb[0:64, CB_PE:CB_PE + 32] = inp['cmp_pe'][0, 0].T
    cb[0:64, CB_PE + 32:CB_PE + 64] = inp['cmp_pe'][0, 1].T
    for i in range(16):
        cb[i, CB_SEL + i] = 1.0
        cb[i, CB_SEL + 64 + i] = 1.0
    t = (np.arange(16)[None, :, None] * 128 + np.arange(128)[:, None, None])
    blk = t // 64
    j = np.arange(32)[None, None, :]
    visible = j <= blk
    forced = (j == 0) | (j == blk) | (j == blk - 1)
    mm = (visible & ~forced).astype(np.float32)
    ba = np.where(visible, np.where(forced, 1e6, 0.0), -1e30).astype(np.float32)
    imt = np.concatenate([mm.reshape(128, 512), ba.reshape(128, 512)], axis=1)
    wuk = np.zeros((128, 4, 128), np.float32)
    for h in range(8):
        wuk[:, h // 2, (h % 2) * 64 + 16:(h % 2) * 64 + 64] = inp['w_uk'][0, h]
    wuv = np.transpose(inp['w_uv'][0], (1, 0, 2)).reshape(128, 512)
    w2 = np.concatenate([inp['cmp_w2'][0, 0], inp['cmp_w2'][0, 0], inp['cmp_w2'][0, 1]], axis=1)
    wsm = np.concatenate([wuk.reshape(128, 512), wuv, w2], axis=1).astype(np.float32)
    return cf, cb, imt, wsm


def build(n_seq=2, stage=99, dbg_cols=0):
    nc = bass.Bass("TRN2", target_bir_lowering=False)
    dt_ = nc.dram_tensor
    x_d = dt_("x", [2, S, D], F32, kind="ExternalInput").ap()
    cT_d = dt_("cT", [128, 8, 2], F32, kind="ExternalInput").ap()
    wada_d = dt_("w_ada", [D, 6 * D], F32, kind="ExternalInput").ap()
    cf_d = dt_("cf", [128, CF_N], F32, kind="ExternalInput").ap()
    cb_d = dt_("cb", [128, CB_N], F32, kind="ExternalInput").ap()
    imt_d = dt_("imt", [128, 1024], F32, kind="ExternalInput").ap()
    wsm_d = dt_("wsm", [128, 1216], F32, kind="ExternalInput").ap()
    winA_d = dt_("w_inA", [D, NA], F32, kind="ExternalInput").ap()
    winB_d = dt_("w_inB", [D, NB], F32, kind="ExternalInput").ap()
    w1_d = dt_("cmp_w1", [2, 2048, 128], F32, kind="ExternalInput").ap()
    b2v_d = dt_("b2v", [64], F32, kind="ExternalInput").ap()
    wout_d = dt_("w_out", [D, D], F32, kind="ExternalInput").ap()
    wgu_d = dt_("w_gate_up", [22, 128, 2048], F32, kind="ExternalInput").ap()
    wdn_d = dt_("w_down", [DFF, D], F32, kind="ExternalInput").ap()
    out_d = dt_("out", [2, S, D], F32, kind="ExternalOutput").ap()
    dbg_d = dt_("dbg", [128, dbg_cols], F32, kind="ExternalOutput").ap() if dbg_cols else None

    with ExitStack() as es:
        cx = Ctx(nc, es)
        TOT = 206 * 1024
        big = es.enter_context(nc.sbuf_tensor("big", [128, TOT // 2], BF16))
        mem = Mem(big)
        PS = [Buf(es.enter_context(nc.psum_tensor("ps%d" % i, [128, 512], F32))[:], "ps%d" % i) for i in range(8)]

        def psb(i):
            return PS[i].ap.bitcast(BF16)

        KB = 1024
        o = 0
        CFB = mem.view("cf", o, [128, CF_N], F32); o += CF_N * 4
        CBB = mem.view("cb", o, [128, CB_N], BF16); o += CB_N * 2
        MSK = mem.view("msk", o, [128, 8, 512], BF16); o += 8 * 512 * 2
        CMN = mem.view("cmn", o, [128, 2048], BF16); o += 2048 * 2
        ONESF = mem.view("onesf", o, [128, 128], F32); o += 512
        MODC = mem.view("modc", o, [128, 48, 2], F32); o += 384
        DERV = mem.view("derv", o, [128, 6, 8, 2], F32); o += 384
        CACT = mem.view("cact", o, [128, 8, 2], F32); o += 64
        SMALL = mem.view("small", o, [128, 64], F32); o += 256
        G1 = mem.view("G1", o, [128, 1024], F32); o += 4096
        G2 = mem.view("G2", o, [128, 1024], F32); o += 4096
        assert o <= 44 * KB, o
        R_OT = 44 * KB
        R_HT = 76 * KB
        R_WIN = 108 * KB
        R_PH = 152 * KB
        OT = mem.view("OT", R_OT, [128, 8, 2048], BF16)
        HT = mem.view("HT", R_HT, [128, 8, 2048], BF16)

        ident_f = CFB[:, CF_ID:CF_ID + 128]
        ident_b = CBB[:, CB_ID:CB_ID + 128]
        ropeC = CFB[:, CF_C:CF_C + S]
        ropeS = CFB[:, CF_S:CF_S + S]
        vec = lambda c0, c1: CFB[:, CF_V + c0:CF_V + c1]

        def dbg_dump(ap, col0, ncols, rd):
            if dbg_d is not None:
                cx.dma("pool", dbg_d[0:ap.shape[0], col0:col0 + ncols], ap, reads=[rd])

        cx.dma("sp", CFB[:], cf_d, writes=[CFB])
        cx.dma("pool", CBB[:], cb_d, writes=[CBB])
        cx.dma("sp", CACT[:], cT_d, writes=[CACT])
        cx.op("pool", "memset", MSK[:], 0.0, writes=[MSK])
        for k in range(4):
            cx.op("pool", "affine_select", out=MSK[:, k, :], in_=MSK[:, k, :], pattern=[[-1, 512]],
                  compare_op=ALU.is_gt, fill=1.0, base=128 * k, channel_multiplier=1, reads=[MSK], writes=[MSK])
        for k in range(1, 5):
            cx.op("pool", "affine_select", out=MSK[:, 3 + k, :], in_=MSK[:, 3 + k, :], pattern=[[1, 512]],
                  compare_op=ALU.is_ge, fill=1.0, base=-512 + 128 * k, channel_multiplier=-1, reads=[MSK], writes=[MSK])
        cx.op("pool", "memset", CMN[:], 0.0, writes=[CMN])
        cx.op("pool", "affine_select", out=CMN[:], in_=CMN[:], pattern=[[-1, 2048]],
              compare_op=ALU.is_gt, fill=1.0, base=31, channel_multiplier=16, reads=[CMN], writes=[CMN])
        cx.op("pool", "memset", ONESF[:], 1.0, writes=[ONESF])
        cx.op("act", "activation", out=CACT[:], in_=CACT[:], func=AF.Silu, reads=[CACT], writes=[CACT])
        WA = [mem.view("wa0", R_HT, [128, 8, 1024], F32), mem.view("wa1", R_WIN, [128, 8, 1024], F32)]
        wada_v = wada_d.rearrange("(kc p) n -> p kc n", p=128)
        for v in range(6):
            wb = WA[v % 2]
            for kc in range(8):
                cx.dma("sp", wb[:, kc, :], wada_v[:, kc, 1024 * v:1024 * v + 1024], writes=[wb])
            for fc in range(8):
                col = (v * 8 + fc) * 2
                for kc in range(8):
                    cx.op("pe", "matmul", PS[0][:, col:col + 2], wb[:, kc, 128 * fc:128 * fc + 128], CACT[:, kc, :],
                          start=(kc == 0), stop=(kc == 7), reads=[wb, CACT], writes=[PS[0]])
        cx.op("dve", "tensor_tensor", out=MODC[:], in0=PS[0][:, 0:96].rearrange("p (a b) -> p a b", b=2),
              in1=vec(0, 48).unsqueeze(2).to_broadcast([128, 48, 2]), op=ALU.add, reads=[PS[0], CFB], writes=[MODC])
        def gb(c0):
            return vec(c0, c0 + 8).unsqueeze(2).to_broadcast([128, 8, 2])
        cx.op("dve", "scalar_tensor_tensor", out=DERV[:, 0], in0=MODC[:, 8:16, :], scalar=1.0, in1=gb(48),
              op0=ALU.add, op1=ALU.mult, reads=[MODC, CFB], writes=[DERV])
        cx.op("dve", "tensor_copy", out=DERV[:, 1], in_=MODC[:, 0:8, :], reads=[MODC], writes=[DERV])
        cx.op("dve", "scalar_tensor_tensor", out=DERV[:, 2], in0=MODC[:, 32:40, :], scalar=1.0, in1=gb(64),
              op0=ALU.add, op1=ALU.mult, reads=[MODC, CFB], writes=[DERV])
        cx.op("dve", "tensor_copy", out=DERV[:, 3], in_=MODC[:, 24:32, :], reads=[MODC], writes=[DERV])
        cx.op("dve", "tensor_tensor", out=DERV[:, 4], in0=MODC[:, 16:24, :], in1=gb(56), op=ALU.mult,
              reads=[MODC, CFB], writes=[DERV])
        cx.op("dve", "tensor_tensor", out=DERV[:, 5], in0=MODC[:, 40:48, :], in1=gb(72), op=ALU.mult,
              reads=[MODC, CFB], writes=[DERV])
        cx.barrier()

        for seq in range(n_seq):
            if seq > 0:
                cx.dma("sp", CFB[:, CF_C:CF_C + 2 * S], cf_d[:, CF_C:CF_C + 2 * S], writes=[CFB])
            WIN = mem.view("win", R_WIN, [128, 8, NA], BF16)
            cx.dma("pool", WIN[:], winA_d.rearrange("(kc p) n -> p kc n", p=128), writes=[WIN])
            DG = mem.view("dg", R_PH, [128, 128], F32)
            for gi, GT_ in ((4, G1), (5, G2)):
                for fc in range(8):
                    cx.op("dve", "tensor_scalar", out=DG[:], in0=ident_f, scalar1=DERV[:, gi, fc, seq:seq + 1],
                          scalar2=None, op0=ALU.mult, reads=[CFB, DERV], writes=[DG])
                    pb = PS[fc // 4]
                    cx.op("pe", "matmul", pb[:, 128 * (fc % 4):128 * (fc % 4) + 128], ONESF[:], DG[:],
                          start=True, stop=True, reads=[ONESF, DG], writes=[pb])
                    if fc % 4 == 3:
                        cx.op("act", "copy", out=GT_[:, 512 * (fc // 4):512 * (fc // 4) + 512], in_=pb[:],
                              reads=[pb], writes=[GT_])
            cx.barrier()
            XIN = [mem.view("xin%d" % i, R_PH + 4 * KB * i, [128, 1024], F32) for i in range(2)]
            XS = [mem.view("xs%d" % i, R_PH + 8 * KB + 4 * KB * i, [128, 1024], F32) for i in range(2)]
            JUNK = mem.view("junk", R_PH + 16 * KB, [128, 1024], F32)
            SS = [mem.view("ss%d" % i, R_PH + 20 * KB + 64 * i, [128, 4], F32) for i in range(2)]
            for i in range(NT):
                xi, xs, ss = XIN[i % 2], XS[i % 2], SS[i % 2]
                cx.dma("sp", xi[:], x_d[seq, 128 * i:128 * i + 128, :], writes=[xi])
                cx.op("act", "activation", out=JUNK[:], in_=xi[:], func=AF.Square, accum_out=ss[:, 0:1],
                      reads=[xi], writes=[JUNK, ss])
                cx.op("act", "activation", out=ss[:, 1:2], in_=ss[:, 0:1], func=AF.Sqrt, scale=1.0 / D, bias=1e-6,
                      reads=[ss], writes=[ss])
                cx.op("dve", "reciprocal", out=ss[:, 2:3], in_=ss[:, 1:2], reads=[ss], writes=[ss])
                cx.op("dve", "tensor_scalar", out=xs[:], in0=xi[:], scalar1=ss[:, 2:3], scalar2=None, op0=ALU.mult,
                      reads=[xi, ss], writes=[xs])
                for fc in range(8):
                    pb = PS[2 * (i % 2) + fc // 4]
                    cx.op("pe", "transpose", pb[:, 128 * (fc % 4):128 * (fc % 4) + 128],
                          xs[:, 128 * fc:128 * fc + 128], ident_f, reads=[xs, CFB], writes=[pb])
                for fc in range(8):
                    pb = PS[2 * (i % 2) + fc // 4]
                    cx.op("act", "activation", out=HT[:, fc, 128 * i:128 * i + 128],
                          in_=pb[:, 128 * (fc % 4):128 * (fc % 4) + 128], func=AF.Identity,
                          scale=DERV[:, 0, fc, seq:seq + 1], bias=DERV[:, 1, fc, seq:seq + 1],
                          reads=[pb, DERV], writes=[HT])
            cx.barrier()

            o = R_PH
            QN = mem.view("QN", o, [128, 4, 2048], BF16); o += 16 * KB
            KS = mem.view("KS", o, [128, 2, 2048], BF16); o += 8 * KB
            KW = mem.view("KW", o, [128, 2, 2048], BF16); o += 8 * KB
            KCR = mem.view("KCR", o, [128, 2048], BF16); o += 4 * KB
            VCR = mem.view("VCR", o, [128, 2048], BF16); o += 4 * KB
            VT = mem.view("VT", o, [128, 16, 4, 65], BF16); o += 8320
            GT = mem.view("GT", o, [128, 16, 24], F32); o += 1536
            o_T1 = o
            T1 = mem.view("T1", o, [128, 512], F32); o += 2048
            T2 = mem.view("T2", o, [128, 512], F32); o += 2048
            assert o <= TOT, o
            ucnt = [0]

            def proj_unit(WINb, ca, cb_, M, tc, dst_ap, dstbuf):
                u = ucnt[0]; ucnt[0] += 1
                pa, pb = PS[2 * (u % 2)], PS[2 * (u % 2) + 1]
                for kc in range(8):
                    cx.op("pe", "matmul", pa[0:M, :], WINb[:, kc, ca:ca + M], HT[:, kc, 512 * tc:512 * tc + 512],
                          start=(kc == 0), stop=(kc == 7), reads=[WINb, HT], writes=[pa])
                if cb_ is None:
                    cx.op("act", "copy", out=dst_ap, in_=pa[0:M, :], reads=[pa], writes=[dstbuf])
                    return
                for kc in range(8):
                    cx.op("pe", "matmul", pb[0:M, :], WINb[:, kc, cb_:cb_ + M], HT[:, kc, 512 * tc:512 * tc + 512],
                          start=(kc == 0), stop=(kc == 7), reads=[WINb, HT], writes=[pb])
                cx.op("dve", "tensor_tensor", out=T1[0:M, :], in0=pa[0:M, :], in1=ropeC[0:M, 512 * tc:512 * tc + 512],
                      op=ALU.mult, reads=[pa, CFB], writes=[T1])
                cx.op("dve", "tensor_tensor", out=T2[0:M, :], in0=pb[0:M, :], in1=ropeS[0:M, 512 * tc:512 * tc + 512],
                      op=ALU.mult, reads=[pb, CFB], writes=[T2])
                cx.op("pool", "tensor_tensor", out=dst_ap, in0=T1[0:M, :], in1=T2[0:M, :], op=ALU.add,
                      reads=[T1, T2], writes=[dstbuf])

            cx.op("pool", "memset", VT[:, :, :, 64:65], 1.0, writes=[VT])
            for tc in range(4):
                sl = slice(512 * tc, 512 * tc + 512)
                for j in range(4):
                    proj_unit(WIN, 256 * j, 256 * j + 128, 128, tc, QN[:, j, sl], QN)
                proj_unit(WIN, 1024, 1152, 128, tc, KCR[:, sl], KCR)
                proj_unit(WIN, 1280, None, 128, tc, VCR[:, sl], VCR)
                for g in range(2):
                    proj_unit(WIN, 1408 + 256 * g, 1536 + 256 * g, 128, tc, KS[:, g, sl], KS)
                    proj_unit(WIN, 1920 + 256 * g, 2048 + 256 * g, 128, tc, KW[:, g, sl], KW)
            for i in range(NT):
                pb = PS[4 + i % 2]
                for kc in range(8):
                    cx.op("pe", "matmul", pb[:, 0:280], HT[:, kc, 128 * i:128 * i + 128], WIN[:, kc, NA_FM:NA],
                          start=(kc == 0), stop=(kc == 7), reads=[HT, WIN], writes=[pb])
                cx.op("act", "copy", out=VT[:, i, :, 0:64], in_=pb[:, 0:256].rearrange("p (a b) -> p a b", a=4),
                      reads=[pb], writes=[VT])
                cx.op("act", "activation", out=GT[:, i, :], in_=pb[:, 256:280], func=AF.Sigmoid, reads=[pb], writes=[GT])
            cx.barrier()

            o = R_WIN
            W1 = mem.view("W1", o, [128, 2, 32, 128], BF16); o += 16 * KB
            WSM = mem.view("WSM", o, [128, 1216], BF16); o += 2432
            IMT = mem.view("IMT", o, [128, 2, 16, 32], F32); o += 4096
            PT = []
            for i in range(4):
                PT.append(mem.view("PT%d" % i, o, [128, 512], BF16)); o += 1024
            XG = mem.view("XG", o, [128, 128], F32); o += 512
            UU = mem.view("UU", o, [128, 128], F32); o += 512
            HIDT = mem.view("HIDT", o, [128, 128], BF16); o += 256
            KCT = mem.view("KCT", o, [128, 2, 128], BF16); o += 512
            VCX = mem.view("VCX", o, [128, 2, 98], BF16); o += 392
            B2V = mem.view("B2V", o, [128, 64], F32); o += 256
            BIAS1 = mem.view("BIAS1", o, [128, 2], F32); o += 8
            OA = mem.view("OA", o, [128, 4, 512], F32); o += 8192
            OAB = mem.view("OAB", o_T1, [128, 4, 512], BF16)
            IMPS = []
            for g in range(2):
                IMPS.append(mem.view("IMP%d" % g, o, [128, 4, 32], F32)); o += 512
            TMPI = mem.view("TMPI", o, [128, 4, 32], F32); o += 512
            IMPM = mem.view("IMPM", o, [128, 4, 32], F32); o += 512
            TOP8 = mem.view("TOP8", o, [128, 4, 8], F32); o += 128
            NSELB = mem.view("NSELB", o, [128, 4, 32], BF16); o += 256
            NSELT = mem.view("NSELT", o, [128, 2, 512], BF16); o += 2048
            TMPO = mem.view("TMPO", o, [128, 4, 64], F32); o += 1024
            assert o <= R_PH, o
            RINV = Buf(SMALL[:, 0:4], "RINV")
            COEF = Buf(SMALL[:, 4:8], "COEF")

            for kv in range(2):
                src = w1_d[kv].rearrange("(l d) j -> d l j", d=64)
                cx.dma("pool", W1[0:64, kv], src, writes=[W1])
                cx.dma("pool", W1[64:128, kv], src, writes=[W1])
            cx.dma("pool", WSM[:], wsm_d, writes=[WSM])
            cx.dma("sp", IMT[:].rearrange("p a b c -> p (a b c)"), imt_d, writes=[IMT])
            cx.dma("sp", B2V[:], b2v_d.partition_broadcast(128), writes=[B2V])
            cx.op("pool", "memset", HIDT[:], 0.0, writes=[HIDT])
            cx.op("pool", "memset", NSELT[:], 0.0, writes=[NSELT])
            cx.op("pool", "memset", VCX[:, :, 64:65], 1.0, writes=[VCX])
            for g in range(2):
                cx.op("pool", "tensor_copy", out=VCX[:, g, 65:97], in_=CBB[:, CB_OV:CB_OV + 32], reads=[CBB], writes=[VCX])
            for kv in range(2):
                for l in range(32):
                    cx.op("pe", "matmul", PS[6][:, kv:kv + 1], W1[0:64, kv, l, :], CBB[0:64, CB_PE + 32 * kv + l:CB_PE + 32 * kv + l + 1],
                          start=(l == 0), stop=(l == 31), reads=[W1, CBB], writes=[PS[6]])
            cx.op("dve", "tensor_tensor", out=BIAS1[:], in0=PS[6][:, 0:2], in1=vec(80, 82), op=ALU.add,
                  reads=[PS[6], CFB], writes=[BIAS1])
            for kv in range(2):
                for g in range(2):
                    srcb = KCR if kv == 0 else VCR
                    hps = PS[4 + g]
                    for l in range(32):
                        cx.op("pe", "matmul", hps[:, 0:127], W1[64 * g:64 * g + 64, kv, l, :],
                              srcb[64 * g:64 * g + 64, l:l + 2017:16], start=(l == 0), stop=(l == 31),
                              reads=[W1, srcb], writes=[hps])
                    cx.op("act", "activation", out=XG[:, 0:127], in_=hps[:, 0:127], func=AF.Identity,
                          bias=BIAS1[:, kv:kv + 1], reads=[hps, BIAS1], writes=[XG])
                    cx.op("dve", "tensor_tensor", out=UU[:, 0:127], in0=XG[:, 0:127], in1=XG[:, 0:127], op=ALU.mult,
                          reads=[XG], writes=[UU])
                    cx.op("dve", "tensor_scalar", out=UU[:, 0:127], in0=UU[:, 0:127], scalar1=0.044715, scalar2=1.0,
                          op0=ALU.mult, op1=ALU.add, reads=[UU], writes=[UU])
                    cx.op("dve", "tensor_tensor", out=UU[:, 0:127], in0=UU[:, 0:127], in1=XG[:, 0:127], op=ALU.mult,
                          reads=[UU, XG], writes=[UU])
                    cx.op("act", "activation", out=UU[:, 0:127], in_=UU[:, 0:127], func=AF.Sigmoid, scale=1.5957691216057308,
                          reads=[UU], writes=[UU])
                    cx.op("dve", "tensor_tensor", out=HIDT[:, 0:127], in0=XG[:, 0:127], in1=UU[:, 0:127], op=ALU.mult,
                          reads=[XG, UU], writes=[HIDT])
                    if kv == 0:
                        cx.op("pe", "matmul", PS[6][:, 0:128], WSM[:, 1024:1152], HIDT[:], start=True, stop=True,
                              reads=[WSM, HIDT], writes=[PS[6]])
                        cx.op("act", "activation", out=KCT[:, g, :], in_=PS[6][:, 0:128], func=AF.Identity,
                              bias=vec(82, 83), reads=[PS[6], CFB], writes=[KCT])
                    else:
                        cx.op("pe", "matmul", PS[6][:, 0:64], HIDT[:], WSM[:, 1152:1216], start=True, stop=True,
                              reads=[WSM, HIDT], writes=[PS[6]])
                        cx.op("dve", "tensor_tensor", out=VCX[:, g, 0:64], in0=PS[6][:, 0:64], in1=B2V[:], op=ALU.add,
                              reads=[PS[6], B2V], writes=[VCX])

            pipe = {"pend": [], "u": 0, "job": 0}
            SCB = [PS[0], PS[1], PS[4], PS[5]]
            grp = []

            def unit(kT, qT, extras, V, acc, ncols, first, rk, rq, rv, after=None, mmask=None, ex128=(), last=False):
                u = pipe["u"]; pipe["u"] += 1
                grp.append(dict(u=u, kT=kT, qT=qT, ex=extras, ex128=ex128, V=V, acc=acc, ncols=ncols, first=first,
                                rk=rk, rq=rq, rv=rv, after=after, mmask=mmask, last=last))
                if len(grp) == (1 if 'G1' in _DBG else 2):
                    emit_group()

            def emit_group():
                if not grp:
                    return
                for d in grp:
                    sbk = SCB[d["u"] % 4]
                    nex = len(d["ex"]) + len(d["ex128"])
                    cx.op("pe", "matmul", sbk[:], d["kT"], d["qT"], start=True, stop=(nex == 0),
                          reads=[d["rk"], d["rq"]], writes=[sbk])
                    for n_, (l_, r_, c0, c1, rd) in enumerate(d["ex"]):
                        cx.op("pe", "matmul", sbk[:, c0:c1], l_, r_, start=False, stop=(n_ == nex - 1), reads=rd, writes=[sbk])
                for d in grp:
                    sbk = SCB[d["u"] % 4]
                    nex = len(d["ex"]) + len(d["ex128"])
                    for n_, (l_, r_, c0, c1, rd) in enumerate(d["ex128"]):
                        cx.op("pe", "matmul", sbk[:, c0:c1], l_, r_, start=False, stop=(len(d["ex"]) + n_ == nex - 1),
                              reads=rd, writes=[sbk])
                for pvf in pipe["pend"]:
                    pvf()
                pipe["pend"] = []
                for d in grp:
                    sbk = SCB[d["u"] % 4]
                    pt = PT[d["u"] % 4]
                    cx.op("act", "activation", out=pt[:], in_=sbk[:], func=AF.Exp, scale=0.125, reads=[sbk], writes=[pt])
                    if d["mmask"] is not None and 'NM' not in _DBG:
                        cx.op("dve", "tensor_tensor", out=pt[:], in0=pt[:], in1=d["mmask"][0], op=ALU.mult,
                              reads=[pt, d["mmask"][1]], writes=[pt])

                    def pv(d=d, pt=pt):
                        for j in range(4):
                            cx.op("pe", "matmul", d["acc"][:, d["ncols"] * j:d["ncols"] * j + d["ncols"]],
                                  pt[:, 128 * j:128 * j + 128], d["V"], start=(d["first"] and j == 0),
                                  stop=(True if 'ST' in _DBG else (d["last"] and j == 3)), reads=[pt, d["rv"]], writes=[d["acc"]])
                        if d["after"] is not None:
                            d["after"]()
                    pipe["pend"].append(pv)
                grp.clear()

            def flush():
                emit_group()
                for pvf in pipe["pend"]:
                    pvf()
                pipe["pend"] = []

            def next_acc():
                a = PS[2 + pipe["job"] % 2]
                pipe["job"] += 1
                return a

            def nsa_final(acc, ncols, c, h, br, first_branch):
                accv = acc[:, 0:4 * ncols].rearrange("p (j n) -> p j n", j=4)

                def f():
                    cx.op("dve", "tensor_scalar", out=RINV[:], in0=accv[:, :, 64], scalar1=1e-30, scalar2=None,
                          op0=ALU.max, reads=[acc], writes=[RINV])
                    cx.op("dve", "reciprocal", out=RINV[:], in_=RINV[:], reads=[RINV], writes=[RINV])
                    if br == 0:
                        fi = (h % 4 == 0)
                        IMP = IMPS[h // 4]
                        dst = IMP if fi else TMPI
                        cx.op("dve", "tensor_tensor", out=dst[:], in0=accv[:, :, 65:97],
                              in1=RINV[:].unsqueeze(2).to_broadcast([128, 4, 32]), op=ALU.mult,
                              reads=[acc, RINV], writes=[dst])
                        if not fi:
                            cx.op("pool", "tensor_tensor", out=IMP[:], in0=IMP[:], in1=TMPI[:], op=ALU.add,
                                  reads=[IMP, TMPI], writes=[IMP])
                    cx.op("dve", "tensor_tensor", out=COEF[:], in0=RINV[:], in1=GT[:, 4 * c:4 * c + 4, 8 * br + h],
                          op=ALU.mult, reads=[RINV, GT], writes=[COEF])
                    cb3 = COEF[:].unsqueeze(2).to_broadcast([128, 4, 64])
                    if first_branch:
                        cx.op("dve", "tensor_tensor", out=OA[:, :, 64 * h:64 * h + 64], in0=accv[:, :, 0:64], in1=cb3,
                              op=ALU.mult, reads=[acc, COEF], writes=[OA])
                    else:
                        cx.op("dve", "tensor_tensor", out=TMPO[:], in0=accv[:, :, 0:64], in1=cb3, op=ALU.mult,
                              reads=[acc, COEF], writes=[TMPO])
                        cx.op("pool", "tensor_tensor", out=OA[:, :, 64 * h:64 * h + 64], in0=OA[:, :, 64 * h:64 * h + 64],
                              in1=TMPO[:], op=ALU.add, reads=[OA, TMPO], writes=[OA])
                return f

            def to_OT(SRC, c, fc0):
                for fc in range(4):
                    for j in range(4):
                        col = ((fc % 2) * 4 + j) * 128
                        cx.op("pe", "transpose", psb(6 + fc // 2)[:, col:col + 128], SRC[:, j, 128 * fc:128 * fc + 128],
                              ident_b, reads=[SRC, CBB], writes=[PS[6 + fc // 2]])
                for fc in range(4):
                    cx.op("act", "copy", out=OT[:, fc0 + fc, 512 * c:512 * c + 512],
                          in_=psb(6 + fc // 2)[:, (fc % 2) * 512:(fc % 2) * 512 + 512], reads=[PS[6 + fc // 2]], writes=[OT])

            for c in range(4):
                qs = slice(512 * c, 512 * c + 512)
                for h in range(8):
                    g, b_ = h // 4, 64 * (h % 2)
                    acc = next_acc()
                    unit(KCT[b_:b_ + 64, g, :], QN[b_:b_ + 64, h // 2, qs],
                         [], VCX[:, g, 0:97], acc, 97, True,
                         KCT, QN, VCX, after=nsa_final(acc, 97, c, h, 0, True), mmask=(CMN[:, qs], CMN), last=True)
                for h in range(8):
                    g, b_ = h // 4, 64 * (h % 2)
                    acc = next_acc()
                    tiles = list(range(max(0, 4 * c - 4), 4 * c + 4))
                    for n_, i in enumerate(tiles):
                        mk = MSK[:, 3 + (4 * c - i), :] if i < 4 * c else MSK[:, i - 4 * c, :]
                        unit(KW[b_:b_ + 64, g, 128 * i:128 * i + 128], QN[b_:b_ + 64, h // 2, qs],
                             [], VT[:, i, 2 + g, :], acc, 65, n_ == 0, KW, QN, VT,
                             after=(nsa_final(acc, 65, c, h, 2, False) if n_ == len(tiles) - 1 else None), mmask=(mk, MSK),
                             last=(n_ == len(tiles) - 1))
                flush()
                for g in range(2):
                    IMPg = IMPS[g]
                    cx.op("dve", "tensor_tensor", out=IMPM[:], in0=IMPg[:], in1=IMT[:, 0, 4 * c:4 * c + 4, :], op=ALU.mult,
                          reads=[IMPg, IMT], writes=[IMPM])
                    cx.op("dve", "tensor_tensor", out=IMPM[:], in0=IMPM[:], in1=IMT[:, 1, 4 * c:4 * c + 4, :], op=ALU.add,
                          reads=[IMPM, IMT], writes=[IMPM])
                    for j in range(4):
                        cx.op("dve", "max", out=TOP8[:, j, :], in_=IMPM[:, j, :], reads=[IMPM], writes=[TOP8])
                    for j in range(4):
                        cx.op("dve", "tensor_scalar", out=NSELB[:, j, :], in0=IMPM[:, j, :], scalar1=TOP8[:, j, 7:8],
                              scalar2=NEG, op0=ALU.is_lt, op1=ALU.mult, reads=[IMPM, TOP8], writes=[NSELB])
                    for j in range(4):
                        cx.op("pe", "transpose", psb(6)[0:32, 128 * j:128 * j + 128], NSELB[:, j, :], ident_b,
                              reads=[NSELB, CBB], writes=[PS[6]])
                    cx.op("act", "copy", out=NSELT[0:32, g, :], in_=psb(6)[0:32, 0:512], reads=[PS[6]], writes=[NSELT])
                for h in range(8):
                    g, b_ = h // 4, 64 * (h % 2)
                    acc = next_acc()
                    tiles = list(range(0, 4 * c + 4))
                    for n_, i in enumerate(tiles):
                        KX = 32
                        ex = [(CBB[0:KX, CB_EX + 128 * i:CB_EX + 128 * i + 128], NSELT[0:KX, g, :], 0, 512, [CBB, NSELT])]
                        unit(KS[b_:b_ + 64, g, 128 * i:128 * i + 128], QN[b_:b_ + 64, h // 2, qs], ex,
                             VT[:, i, g, :], acc, 65, n_ == 0, KS, QN, VT,
                             after=(nsa_final(acc, 65, c, h, 1, False) if n_ == len(tiles) - 1 else None),
                             mmask=((MSK[:, i - 4 * c, :], MSK) if i >= 4 * c else None), last=(n_ == len(tiles) - 1))
                flush()
                cx.op("act", "copy", out=OAB[:], in_=OA[:], reads=[OA], writes=[OAB])
                to_OT(OAB, c, 0)
            cx.barrier()

            WINB = mem.view("winb", R_WIN, [128, 8, NB], BF16)
            cx.dma("pool", WINB[:], winB_d.rearrange("(kc p) n -> p kc n", p=128), writes=[WINB])
            o = R_PH
            QD = mem.view("QD", o, [128, 4, 2048], BF16); o += 16 * KB
            QI = mem.view("QI", o, [128, 4, 2048], BF16); o += 16 * KB
            KI = mem.view("KI", o, [128, 2048], BF16); o += 4 * KB
            CKT = mem.view("CKT", o, [128, 2048], BF16); o += 4 * KB
            KRT = mem.view("KRT", o, [128, 2048], BF16); o += 4 * KB
            WI = mem.view("WI", o, [128, 16, 8], F32); o += 512
            T1 = mem.view("T1", o, [128, 512], F32); o_JB = o; o += 2048
            T2 = mem.view("T2", o, [128, 512], F32); o += 2048
            CKN = []
            for i in range(2):
                CKN.append(mem.view("CKN%d" % i, o, [128, 128], F32)); o += 512
            o_RB = o
            assert o + 4096 <= TOT, o
            SSD = Buf(SMALL[:, 40:44], "SSD")
            for tc in range(4):
                sl = slice(512 * tc, 512 * tc + 512)
                for j in range(4):
                    proj_unit(WINB, 256 * j, 256 * j + 128, 128, tc, QD[:, j, sl], QD)
                for j in range(4):
                    proj_unit(WINB, 1024 + 256 * j, 1024 + 256 * j + 128, 128, tc, QI[:, j, sl], QI)
                proj_unit(WINB, 2048, 2176, 128, tc, KI[:, sl], KI)
                proj_unit(WINB, 2304, 2320, 16, tc, KRT[0:16, sl], KRT)
            for i in range(NT):
                pb = PS[4 + i % 2]
                ck = CKN[i % 2]
                for kc in range(8):
                    cx.op("pe", "matmul", pb[:, 0:136], HT[:, kc, 128 * i:128 * i + 128], WINB[:, kc, NB_FM:NB],
                          start=(kc == 0), stop=(kc == 7), reads=[HT, WINB], writes=[pb])
                cx.op("act", "activation", out=ck[:], in_=pb[:, 0:128], func=AF.Square, accum_out=SSD[:, 0:1],
                      reads=[pb], writes=[ck, SSD])
                cx.op("act", "activation", out=SSD[:, 1:2], in_=SSD[:, 0:1], func=AF.Sqrt, scale=1.0 / 128, bias=1e-6,
                      reads=[SSD], writes=[SSD])
                cx.op("dve", "reciprocal", out=SSD[:, 2:3], in_=SSD[:, 1:2], reads=[SSD], writes=[SSD])
                cx.op("dve", "tensor_scalar", out=ck[:], in0=pb[:, 0:128], scalar1=SSD[:, 2:3], scalar2=None, op0=ALU.mult,
                      reads=[pb, SSD], writes=[ck])
                cx.op("act", "mul", out=WI[:, i, :], in_=pb[:, 128:136], mul=float(8 ** -0.5 * 64 ** -0.5), reads=[pb], writes=[WI])
                cx.op("pe", "transpose", PS[6][:, 128 * (i % 4):128 * (i % 4) + 128], ck[:], ident_f, reads=[ck, CFB], writes=[PS[6]])
                if i % 4 == 3:
                    cx.op("act", "activation", out=CKT[:, 512 * (i // 4):512 * (i // 4) + 512], in_=PS[6][:], func=AF.Identity,
                          scale=vec(83, 84), reads=[PS[6], CFB], writes=[CKT])
            cx.barrier()

            o = R_WIN
            KHT = mem.view("KHT", o, [128, 4, 2048], BF16); o += 16 * KB
            VH = mem.view("VH", o, [128, 16, 8, 65], BF16); o += 16640
            WSM = mem.view("WSM", o, [128, 1216], BF16); o += 2432
            PT = []
            for i in range(4):
                PT.append(mem.view("PT%d" % i, o, [128, 512], BF16)); o += 1024
            ODB = mem.view("ODB", o, [128, 4, 512], BF16); o += 4096
            assert o <= R_PH, o
            NMS = [[mem.view("NMA%d" % j, R_HT + 3 * KB * j, [128, 1536], BF16) for j in range(4)],
                   [mem.view("NMB%d" % j, CF_C * 4 + 4 * KB * j, [128, 2048], BF16) for j in range(4)]]
            IB = [mem.view("IB%d" % j, R_HT + 12 * KB + 8 * KB * j, [128, 2048], F32) for j in range(2)]
            JB = mem.view("JB", o_JB, [128, 2048], BF16)
            RB = [mem.view("RB%d" % j, o_RB + 2048 * j, [128, 512], F32) for j in range(2)]
            LO = Buf(SMALL[:, 8:9], "LO"); MID = Buf(SMALL[:, 9:10], "MID"); CNT = Buf(SMALL[:, 10:11], "CNT")
            TMPS = Buf(SMALL[:, 11:12], "TMPS"); MX8 = Buf(SMALL[:, 12:20], "MX8"); WK = Buf(SMALL[:, 20:38], "WK")
            W0 = Buf(SMALL[:, 38:39], "W0"); MN = Buf(SMALL[:, 39:40], "MN")
            cx.dma("pool", WSM[:], wsm_d, writes=[WSM])
            cx.op("pool", "memset", VH[:, :, :, 64:65], 1.0, writes=[VH])
            n_ = 0
            for j in range(4):
                for tc in range(4):
                    pb = PS[4 + n_ % 2]; n_ += 1
                    cx.op("pe", "matmul", pb[:], WSM[:, 128 * j:128 * j + 128], CKT[:, 512 * tc:512 * tc + 512],
                          start=True, stop=False, reads=[WSM, CKT], writes=[pb])
                    cx.op("pe", "matmul", pb[:], CBB[0:16, CB_SEL:CB_SEL + 128], KRT[0:16, 512 * tc:512 * tc + 512],
                          start=False, stop=True, reads=[CBB, KRT], writes=[pb])
                    cx.op("act", "copy", out=KHT[:, j, 512 * tc:512 * tc + 512], in_=pb[:], reads=[pb], writes=[KHT])
            for i in range(NT):
                pb = PS[4 + n_ % 2]; n_ += 1
                cx.op("pe", "matmul", pb[:], CKT[:, 128 * i:128 * i + 128], WSM[:, 512:1024], start=True, stop=True,
                      reads=[CKT, WSM], writes=[pb])
                cx.op("act", "copy", out=VH[:, i, :, 0:64], in_=pb[:].rearrange("p (h d) -> p h d", h=8), reads=[pb], writes=[VH])

            pipe["pend"] = []
            grp.clear()
            def idx_scores(c, j):
                T = 4 * c + j
                Wc = 512 * (c + 1)
                Wv = 128 * (T + 1)
                IBt = IB[T % 2]
                for sc in range(c + 1):
                    N = min(512, Wv - 512 * sc)
                    for h in range(8):
                        b_ = 64 * (h % 2)
                        L = PS[6 + lcnt[0] % 2]; lcnt[0] += 1
                        cx.op("pe", "matmul", L[:, 0:N], QI[b_:b_ + 64, h // 2, 128 * T:128 * T + 128],
                              KI[b_:b_ + 64, 512 * sc:512 * sc + N], start=True, stop=True, reads=[QI, KI], writes=[L])
                        if h == 0:
                            cx.op("dve", "tensor_scalar", out=IBt[:, 512 * sc:512 * sc + N], in0=L[:, 0:N], scalar1=0.0,
                                  scalar2=WI[:, T, h:h + 1], op0=ALU.max, op1=ALU.mult, reads=[L, WI], writes=[IBt])
                        else:
                            rb = RB[h % 2]
                            cx.op("dve", "tensor_scalar", out=rb[:, 0:N], in0=L[:, 0:N], scalar1=0.0,
                                  scalar2=WI[:, T, h:h + 1], op0=ALU.max, op1=ALU.mult, reads=[L, WI], writes=[rb])
                            cx.op("pool", "tensor_tensor", out=IBt[:, 512 * sc:512 * sc + N], in0=IBt[:, 512 * sc:512 * sc + N],
                                  in1=rb[:, 0:N], op=ALU.add, reads=[IBt, rb], writes=[IBt])
                if T >= 2:
                    cx.op("dve", "max", out=MX8[:], in_=IBt[:, 0:Wv], reads=[IBt], writes=[MX8])
                    cx.op("dve", "tensor_reduce", out=MN[:], in_=IBt[:, 0:Wv], axis=AX.X, op=ALU.min, reads=[IBt], writes=[MN])
                cx.op("pool", "affine_select", out=IBt[:, 128 * T:128 * T + 128], in_=IBt[:, 128 * T:128 * T + 128],
                      pattern=[[-1, 128]], compare_op=ALU.is_ge, fill=-3.0e38, base=0, channel_multiplier=1,
                      reads=[IBt], writes=[IBt])
                if Wv < Wc:
                    cx.op("pool", "memset", IBt[:, Wv:Wc], -3.0e38, writes=[IBt])

            def idx_bisect(c, j):
                T = 4 * c + j
                Wc = 512 * (c + 1)
                Wv = 128 * (T + 1)
                IBt = IB[T % 2]
                nm = NMS[c % 2][j]
                if T >= 2:
                    cx.op("dve", "tensor_copy", out=LO[:], in_=MN[:], reads=[MN], writes=[LO])
                    cx.op("dve", "tensor_tensor", out=W0[:], in0=MX8[:, 0:1], in1=MN[:], op=ALU.subtract,
                          reads=[MX8, MN], writes=[W0])
                    cx.op("dve", "tensor_scalar", out=WK[:], in0=CFB[:, CF_P2:CF_P2 + BIS_ITERS], scalar1=W0[:],
                          scalar2=None, op0=ALU.mult, reads=[CFB, W0], writes=[WK])
                    for k in range(BIS_ITERS):
                        cx.op("dve", "tensor_tensor", out=MID[:], in0=LO[:], in1=WK[:, k:k + 1], op=ALU.add,
                              reads=[LO, WK], writes=[MID])
                        cx.op("dve", "tensor_scalar", out=JB[:, 0:Wv], in0=IBt[:, 0:Wv], scalar1=MID[:], scalar2=0.0,
                              op0=ALU.is_ge, op1=ALU.add, accum_out=CNT[:], reads=[IBt, MID], writes=[JB, CNT])
                        cx.op("dve", "tensor_scalar", out=TMPS[:], in0=CNT[:], scalar1=255.5, scalar2=WK[:, k:k + 1],
                              op0=ALU.is_ge, op1=ALU.mult, reads=[CNT, WK], writes=[TMPS])
                        cx.op("dve", "tensor_tensor", out=LO[:], in0=LO[:], in1=TMPS[:], op=ALU.add,
                              reads=[LO, TMPS], writes=[LO])
                else:
                    cx.op("dve", "memset", LO[:], -1.0e30, writes=[LO])
                cx.op("dve", "tensor_scalar", out=nm[:, 0:Wc], in0=IBt[:, 0:Wc], scalar1=LO[:], scalar2=NEG,
                      op0=ALU.is_lt, op1=ALU.mult, reads=[IBt, LO], writes=[nm])

            def idx_slices(c):
                out_ = []
                for j in range(4):
                    out_.append(lambda c=c, j=j: idx_scores(c, j))
                    out_.append(lambda c=c, j=j: idx_bisect(c, j))
                return out_

            lcnt = [0]
            for f_ in idx_slices(0):
                f_()
            for c in range(4):
                qs = slice(512 * c, 512 * c + 512)
                NMc = NMS[c % 2]
                sl_next = idx_slices(c + 1) if c < 3 else []
                for h in range(8):
                    if sl_next:
                        sl_next[h]()
                    b_ = 64 * (h % 2)
                    acc = next_acc()
                    accv = acc[:, 0:260].rearrange("p (j n) -> p j n", j=4)

                    def fin(acc=acc, accv=accv, h=h):
                        cx.op("dve", "reciprocal", out=RINV[:], in_=accv[:, :, 64], reads=[acc], writes=[RINV])
                        cx.op("dve", "tensor_tensor", out=ODB[:, :, 64 * h:64 * h + 64], in0=accv[:, :, 0:64],
                              in1=RINV[:].unsqueeze(2).to_broadcast([128, 4, 64]), op=ALU.mult,
                              reads=[acc, RINV], writes=[ODB])
                    tiles = list(range(0, 4 * c + 4))
                    for q_, i in enumerate(tiles):
                        ex = [(NMc[j][:, 128 * i:128 * i + 128], ident_b, 128 * j, 128 * j + 128, [NMc[j], CBB]) for j in range(4)]
                        unit(KHT[b_:b_ + 64, h // 2, 128 * i:128 * i + 128], QD[b_:b_ + 64, h // 2, qs], [],
                             VH[:, i, h, :], acc, 65, q_ == 0, KHT, QD, VH, after=(fin if q_ == len(tiles) - 1 else None),
                             ex128=ex, last=(q_ == len(tiles) - 1))
                flush()
                to_OT(ODB, c, 4)
            cx.barrier()

            o = R_HT
            X1 = mem.view("X1", o, [128, 4, 1024], F32); o += 16 * KB
            H2T = mem.view("H2T", o, [128, 8, 512], BF16); o += 8 * KB
            ACTT = mem.view("ACTT", o, [128, 22, 512], BF16)
            WOUT = mem.view("WOUT", o, [128, 8, 1024], BF16); o += 22 * KB
            WG = []
            for i in range(3):
                WG.append(mem.view("WG%d" % i, o, [128, 8, 256], BF16)); o += 4 * KB
            WDN = mem.view("WDN", o, [128, 22, 1024], BF16); o += 44 * KB
            XIN = []
            for i in range(2):
                XIN.append(mem.view("xin%d" % i, o, [128, 1024], F32)); o += 4 * KB
            JUNK = mem.view("junk", o, [128, 1024], F32); o += 4 * KB
            XS = mem.view("xs", o, [128, 1024], F32); o += 4 * KB
            TMPY = mem.view("tmpy", o, [128, 1024], F32); o += 4 * KB
            SIL = []
            for i in range(2):
                SIL.append(mem.view("sil%d" % i, o, [128, 512], F32)); o += 2 * KB
            assert o <= TOT, o
            ST = Buf(SMALL[:, 44:52], "ST")
            cx.dma("pool", WDN[:], wdn_d.rearrange("(c p) n -> p c n", p=128), writes=[WDN])
            wout_v = wout_d.rearrange("(kc p) n -> p kc n", p=128)

            for gi in range(4):
                cx.dma("pool", WOUT[:], wout_v, writes=[WOUT, ACTT])
                for j in range(4):
                    T = 4 * gi + j
                    xi = XIN[j % 2]
                    cx.dma("sp", xi[:], x_d[seq, 128 * T:128 * T + 128, :], writes=[xi])
                    yb = [PS[2 * (j % 2)], PS[2 * (j % 2) + 1]]
                    for n in range(2):
                        for kc in range(8):
                            cx.op("pe", "matmul", yb[n][:], OT[:, kc, 128 * T:128 * T + 128], WOUT[:, kc, 512 * n:512 * n + 512],
                                  start=(kc == 0), stop=(kc == 7), reads=[OT, WOUT], writes=[yb[n]])
                    for n in range(2):
                        cx.op("act", "activation", out=JUNK[:, 512 * n:512 * n + 512], in_=yb[n][:], func=AF.Square,
                              accum_out=ST[:, n:n + 1], reads=[yb[n]], writes=[JUNK, ST])
                    cx.op("dve", "tensor_tensor", out=ST[:, 2:3], in0=ST[:, 0:1], in1=ST[:, 1:2], op=ALU.add, reads=[ST], writes=[ST])
                    cx.op("act", "activation", out=ST[:, 3:4], in_=ST[:, 2:3], func=AF.Sqrt, scale=1.0 / D, bias=1e-6, reads=[ST], writes=[ST])
                    cx.op("dve", "reciprocal", out=ST[:, 4:5], in_=ST[:, 3:4], reads=[ST], writes=[ST])
                    for n in range(2):
                        cx.op("dve", "scalar_tensor_tensor", out=TMPY[:, 512 * n:512 * n + 512], in0=yb[n][:], scalar=ST[:, 4:5],
                              in1=G1[:, 512 * n:512 * n + 512], op0=ALU.mult, op1=ALU.mult, reads=[yb[n], ST, G1], writes=[TMPY])
                    cx.op("pool", "tensor_tensor", out=X1[:, j, :], in0=TMPY[:], in1=xi[:], op=ALU.add, reads=[TMPY, xi], writes=[X1])
                    cx.op("act", "activation", out=JUNK[:], in_=X1[:, j, :], func=AF.Square, accum_out=ST[:, 0:1],
                          reads=[X1], writes=[JUNK, ST])
                    cx.op("act", "activation", out=ST[:, 3:4], in_=ST[:, 0:1], func=AF.Sqrt, scale=1.0 / D, bias=1e-6, reads=[ST], writes=[ST])
                    cx.op("dve", "reciprocal", out=ST[:, 4:5], in_=ST[:, 3:4], reads=[ST], writes=[ST])
                    cx.op("dve", "tensor_scalar", out=XS[:], in0=X1[:, j, :], scalar1=ST[:, 4:5], scalar2=None, op0=ALU.mult,
                          reads=[X1, ST], writes=[XS])
                    for fc in range(8):
                        pb = PS[4 + fc // 4]
                        cx.op("pe", "transpose", pb[:, 128 * (fc % 4):128 * (fc % 4) + 128], XS[:, 128 * fc:128 * fc + 128],
                              ident_f, reads=[XS, CFB], writes=[pb])
                    for fc in range(8):
                        pb = PS[4 + fc // 4]
                        cx.op("act", "activation", out=H2T[:, fc, 128 * j:128 * j + 128],
                              in_=pb[:, 128 * (fc % 4):128 * (fc % 4) + 128], func=AF.Identity,
                              scale=DERV[:, 2, fc, seq:seq + 1], bias=DERV[:, 3, fc, seq:seq + 1],
                              reads=[pb, DERV], writes=[H2T])
                for ch in range(22):
                    wg = WG[ch % 3]
                    cx.dma("pool", wg[:].rearrange("p a b -> p (a b)"), wgu_d[ch], writes=[wg])
                    pg, pu = PS[2 * (ch % 2)], PS[2 * (ch % 2) + 1]
                    for kc in range(8):
                        cx.op("pe", "matmul", pg[:], wg[:, kc, 0:128], H2T[:, kc, :], start=(kc == 0), stop=(kc == 7),
                              reads=[wg, H2T], writes=[pg])
                    for kc in range(8):
                        cx.op("pe", "matmul", pu[:], wg[:, kc, 128:256], H2T[:, kc, :], start=(kc == 0), stop=(kc == 7),
                              reads=[wg, H2T], writes=[pu])
                    sl_ = SIL[ch % 2]
                    cx.op("act", "activation", out=sl_[:], in_=pg[:], func=AF.Silu, reads=[pg], writes=[sl_])
                    cx.op("dve", "tensor_tensor", out=ACTT[:, ch, :], in0=sl_[:], in1=pu[:], op=ALU.mult,
                          reads=[sl_, pu], writes=[ACTT, WOUT])
                for j in range(4):
                    T = 4 * gi + j
                    zb = [PS[4 + 2 * (j % 2)], PS[5 + 2 * (j % 2)]]
                    for n in range(2):
                        for ch in range(22):
                            cx.op("pe", "matmul", zb[n][:], ACTT[:, ch, 128 * j:128 * j + 128], WDN[:, ch, 512 * n:512 * n + 512],
                                  start=(ch == 0), stop=(ch == 21), reads=[ACTT, WDN], writes=[zb[n]])
                    for n in range(2):
                        cx.op("act", "activation", out=JUNK[:, 512 * n:512 * n + 512], in_=zb[n][:], func=AF.Square,
                              accum_out=ST[:, n:n + 1], reads=[zb[n]], writes=[JUNK, ST])
                    cx.op("dve", "tensor_tensor", out=ST[:, 2:3], in0=ST[:, 0:1], in1=ST[:, 1:2], op=ALU.add, reads=[ST], writes=[ST])
                    cx.op("act", "activation", out=ST[:, 3:4], in_=ST[:, 2:3], func=AF.Sqrt, scale=1.0 / D, bias=1e-6, reads=[ST], writes=[ST])
                    cx.op("dve", "reciprocal", out=ST[:, 4:5], in_=ST[:, 3:4], reads=[ST], writes=[ST])
                    for n in range(2):
                        cx.op("dve", "scalar_tensor_tensor", out=TMPY[:, 512 * n:512 * n + 512], in0=zb[n][:], scalar=ST[:, 4:5],
                              in1=G2[:, 512 * n:512 * n + 512], op0=ALU.mult, op1=ALU.mult, reads=[zb[n], ST, G2], writes=[TMPY])
                    cx.op("pool", "tensor_tensor", out=XS[:], in0=TMPY[:], in1=X1[:, j, :], op=ALU.add, reads=[TMPY, X1], writes=[XS])
                    cx.dma("sp", out_d[seq, 128 * T:128 * T + 128, :], XS[:], reads=[XS])
            cx.barrier()

        cx.barrier()
        cx.finish()
    return nc


def _prep(inputs):
    inp = {k: np.asarray(v) for k, v in inputs.items()}
    cf, cb, imt, wsm = _host_consts(inp)
    A, B = _col_index()
    shared = {
        "w_ada": np.ascontiguousarray(inp['w_ada'][0]),
        "cf": cf, "cb": cb, "imt": imt, "wsm": wsm,
        "w_inA": np.ascontiguousarray(inp['w_in'][0][:, A]),
        "w_inB": np.ascontiguousarray(inp['w_in'][0][:, B]),
        "cmp_w1": np.ascontiguousarray(inp['cmp_w1'][0]),
        "b2v": np.ascontiguousarray(inp['cmp_b2'][0, 1]),
        "w_out": np.ascontiguousarray(inp['w_out'][0]),
        "w_gate_up": np.ascontiguousarray(
            inp['w_gate_up'][0].reshape(8, 128, 2, 22, 128).transpose(3, 1, 0, 2, 4).reshape(22, 128, 2048)),
        "w_down": np.ascontiguousarray(inp['w_down'][0]),
    }
    maps = []
    for c in range(8):
        m = dict(shared)
        m["x"] = np.ascontiguousarray(inp['x'][2 * c:2 * c + 2])
        m["cT"] = np.ascontiguousarray(inp['c'][2 * c:2 * c + 2].T.reshape(8, 128, 2).transpose(1, 0, 2))
        maps.append(m)
    return maps


def kernel(**inputs):
    maps = _prep(inputs)
    nc = build()
    res = run_bass_kernel_spmd(nc, maps, core_ids=list(range(8)))
    return np.concatenate([r["out"] for r in res.results], axis=0).astype(np.float32)
```

```python
import numpy as np
import concourse.bass as bass
import concourse.mybir as mybir
from concourse.bass_utils import run_bass_kernel_spmd
from contextlib import ExitStack

F32 = mybir.dt.float32
BF16 = mybir.dt.bfloat16
ALU = mybir.AluOpType
AF = mybir.ActivationFunctionType
AX = mybir.AxisListType

S = 2048
D = 1024
NT = 16
DFF = 2816
NEG = -30000.0
IN_SPLITS = (512, 128, 128, 128, 128, 128, 128, 24, 512, 128, 16, 512, 64, 8)
NAMES = ['nq', 'nkc', 'nvc', 'nks', 'nvs', 'nkw', 'nvw', 'ngate', 'dq', 'dckv', 'dkr', 'iq', 'ik', 'iw']
OFF = dict(zip(NAMES, np.cumsum((0,) + IN_SPLITS)[:-1]))
NA_FM = 19 * 128
NA = NA_FM + 280
NB_FM = 18 * 128 + 32
NB = NB_FM + 136
BIS_ITERS = 15
import os as _os
_DBG = _os.environ.get('KDBG', '')


class _E:
    def __init__(self, name, eng, sem):
        self.name, self.eng, self.sem, self.tick, self.waited = name, eng, sem, 0, {}


class _St:
    __slots__ = ("w", "r")

    def __init__(self):
        self.w = None
        self.r = {}


class Buf:
    def __init__(self, ap, key):
        self.ap = ap
        self.key = key

    def __getitem__(self, idx):
        return self.ap[idx]


class Ctx:
    def __init__(self, nc, es, n_dma_sems=8):
        self.nc, self.es = nc, es
        self.E = {}
        for name, eng in (("pe", nc.tensor), ("act", nc.scalar), ("dve", nc.vector),
                          ("pool", nc.gpsimd), ("sp", nc.sync)):
            self.E[name] = _E(name, eng, es.enter_context(nc.semaphore("s_" + name)))
        self.dsems = {q: [[es.enter_context(nc.semaphore("d_%s%d" % (q, i))), 0] for i in range(n_dma_sems)]
                      for q in ("sp", "pool")}
        self.dnext = {"sp": 0, "pool": 0}
        self.st = {}

    def _state(self, b):
        k = b.key if isinstance(b, Buf) else (b if isinstance(b, str) else b.name)
        s = self.st.get(k)
        if s is None:
            s = self.st[k] = _St()
        return s

    def _wait(self, E, dep):
        kind, tk = dep
        if kind == E.name and E.name == "pe":
            return
        if E.waited.get(kind, 0) >= tk:
            return
        sem = self.dsems[kind[0]][kind[1]][0] if isinstance(kind, tuple) else self.E[kind].sem
        E.eng.wait_ge(sem, tk)
        E.waited[kind] = tk

    def _deps(self, E, reads, writes):
        deps = []
        for b in reads:
            s = self._state(b)
            if s.w is not None:
                deps.append(s.w)
        for b in writes:
            s = self._state(b)
            if s.w is not None:
                deps.append(s.w)
            deps.extend(s.r.items())
        for d in deps:
            self._wait(E, d)

    def _mark(self, token, reads, writes):
        for b in reads:
            s = self._state(b)
            if s.r.get(token[0], 0) < token[1]:
                s.r[token[0]] = token[1]
        for b in writes:
            s = self._state(b)
            s.w = token
            s.r = {}

    def op(self, en, fn, *args, reads=(), writes=(), **kw):
        E = self.E[en]
        self._deps(E, reads, writes)
        ins = getattr(E.eng, fn)(*args, **kw)
        E.tick += 1
        ins.then_inc(E.sem, 1)
        self._mark((en, E.tick), reads, writes)
        return ins

    def dma(self, q, out, in_, reads=(), writes=(), **kw):
        E = self.E[q]
        self._deps(E, reads, writes)
        i = self.dnext[q]
        self.dnext[q] = (i + 1) % len(self.dsems[q])
        slot = self.dsems[q][i]
        kind = (q, i)
        if slot[1] > 0:
            self._wait(E, (kind, slot[1]))
        slot[1] += 16
        E.eng.dma_start(out=out, in_=in_, **kw).then_inc(slot[0], 16)
        self._mark((kind, slot[1]), reads, writes)

    def barrier(self):
        toks = [(n, e.tick) for n, e in self.E.items() if e.tick > 0]
        for q in self.dsems:
            for i, slot in enumerate(self.dsems[q]):
                if slot[1] > 0:
                    toks.append(((q, i), slot[1]))
        for n, e in self.E.items():
            for t in toks:
                if t[0] == n:
                    if n != "sp" and e.waited.get(n, 0) < t[1]:
                        e.eng.wait_ge(e.sem, t[1])
                        e.waited[n] = t[1]
                else:
                    self._wait(e, t)
        self.st = {}

    def finish(self):
        E = self.E["sp"]
        for q in self.dsems:
            for i, slot in enumerate(self.dsems[q]):
                if slot[1] > 0:
                    self._wait(E, ((q, i), slot[1]))


class Mem:
    def __init__(self, big):
        self.big = big

    def view(self, key, off, shape, dt, pbase=0):
        esz = 4 if dt == F32 else 2
        nel = int(np.prod(shape[1:]))
        assert off % 4 == 0
        a = off // 2
        n = nel * esz // 2
        ap = self.big[pbase:pbase + shape[0], a:a + n]
        if dt == F32:
            ap = ap.bitcast(F32)
        if len(shape) == 3:
            ap = ap.rearrange("p (a b) -> p a b", a=shape[1])
        elif len(shape) == 4:
            ap = ap.rearrange("p (a b c) -> p a b c", a=shape[1], b=shape[2])
        return Buf(ap, key)


def _swap_head(cols):
    c = np.array(cols).copy()
    c[0:8] = cols[8:16]
    c[8:16] = cols[0:8]
    return c


def _col_index():
    def head(base, h):
        return np.arange(base + 64 * h, base + 64 * h + 64)
    A = []
    for j in range(4):
        a = np.concatenate([head(OFF['nq'], 2 * j), head(OFF['nq'], 2 * j + 1)])
        b = np.concatenate([_swap_head(head(OFF['nq'], 2 * j)), _swap_head(head(OFF['nq'], 2 * j + 1))])
        A += [a, b]
    a = np.concatenate([head(OFF['nkc'], 0), head(OFF['nkc'], 1)])
    b = np.concatenate([_swap_head(head(OFF['nkc'], 0)), _swap_head(head(OFF['nkc'], 1))])
    A += [a, b]
    A += [np.arange(OFF['nvc'], OFF['nvc'] + 128)]
    for nm in ('nks', 'nkw'):
        for g in range(2):
            a = np.concatenate([head(OFF[nm], g), head(OFF[nm], g)])
            b = np.concatenate([_swap_head(head(OFF[nm], g)), _swap_head(head(OFF[nm], g))])
            A += [a, b]
    A += [np.arange(OFF['nvs'], OFF['nvs'] + 128), np.arange(OFF['nvw'], OFF['nvw'] + 128),
          np.arange(OFF['ngate'], OFF['ngate'] + 24)]
    A = np.concatenate(A)
    assert A.size == NA
    B = []
    for nm in ('dq', 'iq'):
        for j in range(4):
            a = np.concatenate([head(OFF[nm], 2 * j), head(OFF[nm], 2 * j + 1)])
            b = np.concatenate([_swap_head(head(OFF[nm], 2 * j)), _swap_head(head(OFF[nm], 2 * j + 1))])
            B += [a, b]
    a = np.concatenate([head(OFF['ik'], 0), head(OFF['ik'], 0)])
    b = np.concatenate([_swap_head(head(OFF['ik'], 0)), _swap_head(head(OFF['ik'], 0))])
    B += [a, b]
    kr = np.arange(OFF['dkr'], OFF['dkr'] + 16)
    B += [kr, _swap_head(kr)]
    B += [np.arange(OFF['dckv'], OFF['dckv'] + 128), np.arange(OFF['iw'], OFF['iw'] + 8)]
    B = np.concatenate(B)
    assert B.size == NB
    return A, B


CF_ID = 0
CF_C = 128
CF_S = CF_C + 2048
CF_V = CF_S + 2048
CF_P2 = CF_V + 84
CF_N = CF_P2 + BIS_ITERS
CB_ID = 0
CB_EX = 128
CB_OV = CB_EX + 2048
CB_PE = CB_OV + 32
CB_SEL = CB_PE + 64
CB_N = CB_SEL + 128


def _host_consts(inp):
    cf = np.zeros((128, CF_N), np.float32)
    cf[:, CF_ID:CF_ID + 128] = np.eye(128, dtype=np.float32)
    inv_freq = 1.0 / (np.float32(500000.0) ** (np.arange(0, 16, 2, dtype=np.float32) / np.float32(16)))
    ang = np.arange(S, dtype=np.float32)[:, None] * inv_freq[None, :].astype(np.float32)
    cos, sin = np.cos(ang).astype(np.float32), np.sin(ang).astype(np.float32)
    for p in range(128):
        j = p % 64
        if j < 16:
            cf[p, CF_C:CF_C + S] = cos[:, j % 8]
            cf[p, CF_S:CF_S + S] = (-1.0 if j < 8 else 1.0) * sin[:, j % 8]
        else:
            cf[p, CF_C:CF_C + S] = 1.0
    v = cf[:, CF_V:CF_V + 84]
    v[:, 0:48] = inp['b_ada'][0].reshape(48, 128).T
    v[:, 48:56] = inp['g_pre_mix'][0].reshape(8, 128).T
    v[:, 56:64] = inp['g_post_mix'][0].reshape(8, 128).T
    v[:, 64:72] = inp['g_pre_ffn'][0].reshape(8, 128).T
    v[:, 72:80] = inp['g_post_ffn'][0].reshape(8, 128).T
    v[:, 80] = inp['cmp_b1'][0, 0]
    v[:, 81] = inp['cmp_b1'][0, 1]
    v[:, 82] = np.concatenate([inp['cmp_b2'][0, 0], inp['cmp_b2'][0, 0]])
    v[:, 83] = inp['g_kv_norm'][0]
    cf[:, CF_P2:CF_P2 + BIS_ITERS] = (0.5 ** np.arange(1, BIS_ITERS + 1, dtype=np.float64)).astype(np.float32)[None, :]
    cb = np.zeros((128, CB_N), np.float32)
    cb[:, CB_ID:CB_ID + 128] = np.eye(128, dtype=np.float32)
    for j in range(32):
        cb[j, CB_EX + 64 * j:CB_EX + 64 * j + 64] = 1.0
    ci = np.arange(127)[:, None] * 16
    sj = np.arange(32)[None, :] * 64
    cb[0:127, CB_OV:CB_OV + 32] = ((ci < sj + 64) & (ci + 32 > sj)).astype(np.float32)
    cb[0:64, CB_PE:CB_PE + 32] = inp['cmp_pe'][0, 0].T
    cb[0:64, CB_PE + 32:CB_PE + 64] = inp['cmp_pe'][0, 1].T
    for i in range(16):
        cb[i, CB_SEL + i] = 1.0
        cb[i, CB_SEL + 64 + i] = 1.0
    t = (np.arange(16)[None, :, None] * 128 + np.arange(128)[:, None, None])
    blk = t // 64
    j = np.arange(32)[None, None, :]
    visible = j <= blk
    forced = (j == 0) | (j == blk) | (j == blk - 1)
    mm = (visible & ~forced).astype(np.float32)
    ba = np.where(visible, np.where(forced, 1e6, 0.0), -1e30).astype(np.float32)
    imt = np.concatenate([mm.reshape(128, 512), ba.reshape(128, 512)], axis=1)
    wuk = np.zeros((128, 4, 128), np.float32)
    for h in range(8):
        wuk[:, h // 2, (h % 2) * 64 + 16:(h % 2) * 64 + 64] = inp['w_uk'][0, h]
    wuv = np.transpose(inp['w_uv'][0], (1, 0, 2)).reshape(128, 512)
    w2 = np.concatenate([inp['cmp_w2'][0, 0], inp['cmp_w2'][0, 0], inp['cmp_w2'][0, 1]], axis=1)
    wsm = np.concatenate([wuk.reshape(128, 512), wuv, w2], axis=1).astype(np.float32)
    return cf, cb, imt, wsm


def build(n_seq=2, stage=99, dbg_cols=0):
    nc = bass.Bass("TRN2", target_bir_lowering=False)
    dt_ = nc.dram_tensor
    x_d = dt_("x", [2, S, D], F32, kind="ExternalInput").ap()
    cT_d = dt_("cT", [128, 8, 2], F32, kind="ExternalInput").ap()
    wada_d = dt_("w_ada", [D, 6 * D], F32, kind="ExternalInput").ap()
    cf_d = dt_("cf", [128, CF_N], F32, kind="ExternalInput").ap()
    cb_d = dt_("cb", [128, CB_N], F32, kind="ExternalInput").ap()
    imt_d = dt_("imt", [128, 1024], F32, kind="ExternalInput").ap()
    wsm_d = dt_("wsm", [128, 1216], F32, kind="ExternalInput").ap()
    winA_d = dt_("w_inA", [D, NA], F32, kind="ExternalInput").ap()
    winB_d = dt_("w_inB", [D, NB], F32, kind="ExternalInput").ap()
    w1_d = dt_("cmp_w1", [2, 2048, 128], F32, kind="ExternalInput").ap()
    b2v_d = dt_("b2v", [64], F32, kind="ExternalInput").ap()
    wout_d = dt_("w_out", [D, D], F32, kind="ExternalInput").ap()
    wgu_d = dt_("w_gate_up", [22, 128, 2048], F32, kind="ExternalInput").ap()
    wdn_d = dt_("w_down", [DFF, D], F32, kind="ExternalInput").ap()
    out_d = dt_("out", [2, S, D], F32, kind="ExternalOutput").ap()
    dbg_d = dt_("dbg", [128, dbg_cols], F32, kind="ExternalOutput").ap() if dbg_cols else None

    with ExitStack() as es:
        cx = Ctx(nc, es)
        TOT = 206 * 1024
        big = es.enter_context(nc.sbuf_tensor("big", [128, TOT // 2], BF16))
        mem = Mem(big)
        PS = [Buf(es.enter_context(nc.psum_tensor("ps%d" % i, [128, 512], F32))[:], "ps%d" % i) for i in range(8)]

        def psb(i):
            return PS[i].ap.bitcast(BF16)

        KB = 1024
        o = 0
        CFB = mem.view("cf", o, [128, CF_N], F32); o += CF_N * 4
        CBB = mem.view("cb", o, [128, CB_N], BF16); o += CB_N * 2
        MSK = mem.view("msk", o, [128, 8, 512], BF16); o += 8 * 512 * 2
        CMN = mem.view("cmn", o, [128, 2048], BF16); o += 2048 * 2
        ONESF = mem.view("onesf", o, [128, 128], F32); o += 512
        MODC = mem.view("modc", o, [128, 48, 2], F32); o += 384
        DERV = mem.view("derv", o, [128, 6, 8, 2], F32); o += 384
        CACT = mem.view("cact", o, [128, 8, 2], F32); o += 64
        SMALL = mem.view("small", o, [128, 64], F32); o += 256
        G1 = mem.view("G1", o, [128, 1024], F32); o += 4096
        G2 = mem.view("G2", o, [128, 1024], F32); o += 4096
        assert o <= 44 * KB, o
        R_OT = 44 * KB
        R_HT = 76 * KB
        R_WIN = 108 * KB
        R_PH = 152 * KB
        OT = mem.view("OT", R_OT, [128, 8, 2048], BF16)
        HT = mem.view("HT", R_HT, [128, 8, 2048], BF16)

        ident_f = CFB[:, CF_ID:CF_ID + 128]
        ident_b = CBB[:, CB_ID:CB_ID + 128]
        ropeC = CFB[:, CF_C:CF_C + S]
        ropeS = CFB[:, CF_S:CF_S + S]
        vec = lambda c0, c1: CFB[:, CF_V + c0:CF_V + c1]

        def dbg_dump(ap, col0, ncols, rd):
            if dbg_d is not None:
                cx.dma("pool", dbg_d[0:ap.shape[0], col0:col0 + ncols], ap, reads=[rd])

        cx.dma("sp", CFB[:], cf_d, writes=[CFB])
        cx.dma("pool", CBB[:], cb_d, writes=[CBB])
        cx.dma("sp", CACT[:], cT_d, writes=[CACT])
        cx.op("pool", "memset", MSK[:], 0.0, writes=[MSK])
        for k in range(4):
            cx.op("pool", "affine_select", out=MSK[:, k, :], in_=MSK[:, k, :], pattern=[[-1, 512]],
                  compare_op=ALU.is_gt, fill=1.0, base=128 * k, channel_multiplier=1, reads=[MSK], writes=[MSK])
        for k in range(1, 5):
            cx.op("pool", "affine_select", out=MSK[:, 3 + k, :], in_=MSK[:, 3 + k, :], pattern=[[1, 512]],
                  compare_op=ALU.is_ge, fill=1.0, base=-512 + 128 * k, channel_multiplier=-1, reads=[MSK], writes=[MSK])
        cx.op("pool", "memset", CMN[:], 0.0, writes=[CMN])
        cx.op("pool", "affine_select", out=CMN[:], in_=CMN[:], pattern=[[-1, 2048]],
              compare_op=ALU.is_gt, fill=1.0, base=31, channel_multiplier=16, reads=[CMN], writes=[CMN])
        cx.op("pool", "memset", ONESF[:], 1.0, writes=[ONESF])
        cx.op("act", "activation", out=CACT[:], in_=CACT[:], func=AF.Silu, reads=[CACT], writes=[CACT])
        WA = [mem.view("wa0", R_HT, [128, 8, 1024], F32), mem.view("wa1", R_WIN, [128, 8, 1024], F32)]
        wada_v = wada_d.rearrange("(kc p) n -> p kc n", p=128)
        for v in range(6):
            wb = WA[v % 2]
            for kc in range(8):
                cx.dma("sp", wb[:, kc, :], wada_v[:, kc, 1024 * v:1024 * v + 1024], writes=[wb])
            for fc in range(8):
                col = (v * 8 + fc) * 2
                for kc in range(8):
                    cx.op("pe", "matmul", PS[0][:, col:col + 2], wb[:, kc, 128 * fc:128 * fc + 128], CACT[:, kc, :],
                          start=(kc == 0), stop=(kc == 7), reads=[wb, CACT], writes=[PS[0]])
        cx.op("dve", "tensor_tensor", out=MODC[:], in0=PS[0][:, 0:96].rearrange("p (a b) -> p a b", b=2),
              in1=vec(0, 48).unsqueeze(2).to_broadcast([128, 48, 2]), op=ALU.add, reads=[PS[0], CFB], writes=[MODC])
        def gb(c0):
            return vec(c0, c0 + 8).unsqueeze(2).to_broadcast([128, 8, 2])
        cx.op("dve", "scalar_tensor_tensor", out=DERV[:, 0], in0=MODC[:, 8:16, :], scalar=1.0, in1=gb(48),
              op0=ALU.add, op1=ALU.mult, reads=[MODC, CFB], writes=[DERV])
        cx.op("dve", "tensor_copy", out=DERV[:, 1], in_=MODC[:, 0:8, :], reads=[MODC], writes=[DERV])
        cx.op("dve", "scalar_tensor_tensor", out=DERV[:, 2], in0=MODC[:, 32:40, :], scalar=1.0, in1=gb(64),
              op0=ALU.add, op1=ALU.mult, reads=[MODC, CFB], writes=[DERV])
        cx.op("dve", "tensor_copy", out=DERV[:, 3], in_=MODC[:, 24:32, :], reads=[MODC], writes=[DERV])
        cx.op("dve", "tensor_tensor", out=DERV[:, 4], in0=MODC[:, 16:24, :], in1=gb(56), op=ALU.mult,
              reads=[MODC, CFB], writes=[DERV])
        cx.op("dve", "tensor_tensor", out=DERV[:, 5], in0=MODC[:, 40:48, :], in1=gb(72), op=ALU.mult,
              reads=[MODC, CFB], writes=[DERV])
        cx.barrier()

        for seq in range(n_seq):
            if seq > 0:
                cx.dma("sp", CFB[:, CF_C:CF_C + 2 * S], cf_d[:, CF_C:CF_C + 2 * S], writes=[CFB])
            WIN = mem.view("win", R_WIN, [128, 8, NA], BF16)
            cx.dma("pool", WIN[:], winA_d.rearrange("(kc p) n -> p kc n", p=128), writes=[WIN])
            DG = mem.view("dg", R_PH, [128, 128], F32)
            for gi, GT_ in ((4, G1), (5, G2)):
                for fc in range(8):
                    cx.op("dve", "tensor_scalar", out=DG[:], in0=ident_f, scalar1=DERV[:, gi, fc, seq:seq + 1],
                          scalar2=None, op0=ALU.mult, reads=[CFB, DERV], writes=[DG])
                    pb = PS[fc // 4]
                    cx.op("pe", "matmul", pb[:, 128 * (fc % 4):128 * (fc % 4) + 128], ONESF[:], DG[:],
                          start=True, stop=True, reads=[ONESF, DG], writes=[pb])
                    if fc % 4 == 3:
                        cx.op("act", "copy", out=GT_[:, 512 * (fc // 4):512 * (fc // 4) + 512], in_=pb[:],
                              reads=[pb], writes=[GT_])
            cx.barrier()
            XIN = [mem.view("xin%d" % i, R_PH + 4 * KB * i, [128, 1024], F32) for i in range(2)]
            XS = [mem.view("xs%d" % i, R_PH + 8 * KB + 4 * KB * i, [128, 1024], F32) for i in range(2)]
            JUNK = mem.view("junk", R_PH + 16 * KB, [128, 1024], F32)
            SS = [mem.view("ss%d" % i, R_PH + 20 * KB + 64 * i, [128, 4], F32) for i in range(2)]
            for i in range(NT):
                xi, xs, ss = XIN[i % 2], XS[i % 2], SS[i % 2]
                cx.dma("sp", xi[:], x_d[seq, 128 * i:128 * i + 128, :], writes=[xi])
                cx.op("act", "activation", out=JUNK[:], in_=xi[:], func=AF.Square, accum_out=ss[:, 0:1],
                      reads=[xi], writes=[JUNK, ss])
                cx.op("act", "activation", out=ss[:, 1:2], in_=ss[:, 0:1], func=AF.Sqrt, scale=1.0 / D, bias=1e-6,
                      reads=[ss], writes=[ss])
                cx.op("dve", "reciprocal", out=ss[:, 2:3], in_=ss[:, 1:2], reads=[ss], writes=[ss])
                cx.op("dve", "tensor_scalar", out=xs[:], in0=xi[:], scalar1=ss[:, 2:3], scalar2=None, op0=ALU.mult,
                      reads=[xi, ss], writes=[xs])
                for fc in range(8):
                    pb = PS[2 * (i % 2) + fc // 4]
                    cx.op("pe", "transpose", pb[:, 128 * (fc % 4):128 * (fc % 4) + 128],
                          xs[:, 128 * fc:128 * fc + 128], ident_f, reads=[xs, CFB], writes=[pb])
                for fc in range(8):
                    pb = PS[2 * (i % 2) + fc // 4]
                    cx.op("act", "activation", out=HT[:, fc, 128 * i:128 * i + 128],
                          in_=pb[:, 128 * (fc % 4):128 * (fc % 4) + 128], func=AF.Identity,
                          scale=DERV[:, 0, fc, seq:seq + 1], bias=DERV[:, 1, fc, seq:seq + 1],
                          reads=[pb, DERV], writes=[HT])
            cx.barrier()

            o = R_PH
            QN = mem.view("QN", o, [128, 4, 2048], BF16); o += 16 * KB
            KS = mem.view("KS", o, [128, 2, 2048], BF16); o += 8 * KB
            KW = mem.view("KW", o, [128, 2, 2048], BF16); o += 8 * KB
            KCR = mem.view("KCR", o, [128, 2048], BF16); o += 4 * KB
            VCR = mem.view("VCR", o, [128, 2048], BF16); o += 4 * KB
            VT = mem.view("VT", o, [128, 16, 4, 65], BF16); o += 8320
            GT = mem.view("GT", o, [128, 16, 24], F32); o += 1536
            o_T1 = o
            T1 = mem.view("T1", o, [128, 512], F32); o += 2048
            T2 = mem.view("T2", o, [128, 512], F32); o += 2048
            assert o <= TOT, o
            ucnt = [0]

            def proj_unit(WINb, ca, cb_, M, tc, dst_ap, dstbuf):
                u = ucnt[0]; ucnt[0] += 1
                pa, pb = PS[2 * (u % 2)], PS[2 * (u % 2) + 1]
                for kc in range(8):
                    cx.op("pe", "matmul", pa[0:M, :], WINb[:, kc, ca:ca + M], HT[:, kc, 512 * tc:512 * tc + 512],
                          start=(kc == 0), stop=(kc == 7), reads=[WINb, HT], writes=[pa])
                if cb_ is None:
                    cx.op("act", "copy", out=dst_ap, in_=pa[0:M, :], reads=[pa], writes=[dstbuf])
                    return
                for kc in range(8):
                    cx.op("pe", "matmul", pb[0:M, :], WINb[:, kc, cb_:cb_ + M], HT[:, kc, 512 * tc:512 * tc + 512],
                          start=(kc == 0), stop=(kc == 7), reads=[WINb, HT], writes=[pb])
                cx.op("dve", "tensor_tensor", out=T1[0:M, :], in0=pa[0:M, :], in1=ropeC[0:M, 512 * tc:512 * tc + 512],
                      op=ALU.mult, reads=[pa, CFB], writes=[T1])
                cx.op("dve", "tensor_tensor", out=T2[0:M, :], in0=pb[0:M, :], in1=ropeS[0:M, 512 * tc:512 * tc + 512],
                      op=ALU.mult, reads=[pb, CFB], writes=[T2])
                cx.op("pool", "tensor_tensor", out=dst_ap, in0=T1[0:M, :], in1=T2[0:M, :], op=ALU.add,
                      reads=[T1, T2], writes=[dstbuf])

            cx.op("pool", "memset", VT[:, :, :, 64:65], 1.0, writes=[VT])
            for tc in range(4):
                sl = slice(512 * tc, 512 * tc + 512)
                for j in range(4):
                    proj_unit(WIN, 256 * j, 256 * j + 128, 128, tc, QN[:, j, sl], QN)
                proj_unit(WIN, 1024, 1152, 128, tc, KCR[:, sl], KCR)
                proj_unit(WIN, 1280, None, 128, tc, VCR[:, sl], VCR)
                for g in range(2):
                    proj_unit(WIN, 1408 + 256 * g, 1536 + 256 * g, 128, tc, KS[:, g, sl], KS)
                    proj_unit(WIN, 1920 + 256 * g, 2048 + 256 * g, 128, tc, KW[:, g, sl], KW)
            for i in range(NT):
                pb = PS[4 + i % 2]
                for kc in range(8):
                    cx.op("pe", "matmul", pb[:, 0:280], HT[:, kc, 128 * i:128 * i + 128], WIN[:, kc, NA_FM:NA],
                          start=(kc == 0), stop=(kc == 7), reads=[HT, WIN], writes=[pb])
                cx.op("act", "copy", out=VT[:, i, :, 0:64], in_=pb[:, 0:256].rearrange("p (a b) -> p a b", a=4),
                      reads=[pb], writes=[VT])
                cx.op("act", "activation", out=GT[:, i, :], in_=pb[:, 256:280], func=AF.Sigmoid, reads=[pb], writes=[GT])
            cx.barrier()

            o = R_WIN
            W1 = mem.view("W1", o, [128, 2, 32, 128], BF16); o += 16 * KB
            WSM = mem.view("WSM", o, [128, 1216], BF16); o += 2432
            IMT = mem.view("IMT", o, [128, 2, 16, 32], F32); o += 4096
            PT = []
            for i in range(4):
                PT.append(mem.view("PT%d" % i, o, [128, 512], BF16)); o += 1024
            XG = mem.view("XG", o, [128, 128], F32); o += 512
            UU = mem.view("UU", o, [128, 128], F32); o += 512
            HIDT = mem.view("HIDT", o, [128, 128], BF16); o += 256
            KCT = mem.view("KCT", o, [128, 2, 128], BF16); o += 512
            VCX = mem.view("VCX", o, [128, 2, 98], BF16); o += 392
            B2V = mem.view("B2V", o, [128, 64], F32); o += 256
            BIAS1 = mem.view("BIAS1", o, [128, 2], F32); o += 8
            OA = mem.view("OA", o, [128, 4, 512], F32); o += 8192
            OAB = mem.view("OAB", o_T1, [128, 4, 512], BF16)
            IMPS = []
            for g in range(2):
                IMPS.append(mem.view("IMP%d" % g, o, [128, 4, 32], F32)); o += 512
            TMPI = mem.view("TMPI", o, [128, 4, 32], F32); o += 512
            IMPM = mem.view("IMPM", o, [128, 4, 32], F32); o += 512
            TOP8 = mem.view("TOP8", o, [128, 4, 8], F32); o += 128
            NSELB = mem.view("NSELB", o, [128, 4, 32], BF16); o += 256
            NSELT = mem.view("NSELT", o, [128, 2, 512], BF16); o += 2048
            TMPO = mem.view("TMPO", o, [128, 4, 64], F32); o += 1024
            assert o <= R_PH, o
            RINV = Buf(SMALL[:, 0:4], "RINV")
            COEF = Buf(SMALL[:, 4:8], "COEF")

            for kv in range(2):
                src = w1_d[kv].rearrange("(l d) j -> d l j", d=64)
                cx.dma("pool", W1[0:64, kv], src, writes=[W1])
                cx.dma("pool", W1[64:128, kv], src, writes=[W1])
            cx.dma("pool", WSM[:], wsm_d, writes=[WSM])
            cx.dma("sp", IMT[:].rearrange("p a b c -> p (a b c)"), imt_d, writes=[IMT])
            cx.dma("sp", B2V[:], b2v_d.partition_broadcast(128), writes=[B2V])
            cx.op("pool", "memset", HIDT[:], 0.0, writes=[HIDT])
            cx.op("pool", "memset", NSELT[:], 0.0, writes=[NSELT])
            cx.op("pool", "memset", VCX[:, :, 64:65], 1.0, writes=[VCX])
            for g in range(2):
                cx.op("pool", "tensor_copy", out=VCX[:, g, 65:97], in_=CBB[:, CB_OV:CB_OV + 32], reads=[CBB], writes=[VCX])
            for kv in range(2):
                for l in range(32):
                    cx.op("pe", "matmul", PS[6][:, kv:kv + 1], W1[0:64, kv, l, :], CBB[0:64, CB_PE + 32 * kv + l:CB_PE + 32 * kv + l + 1],
                          start=(l == 0), stop=(l == 31), reads=[W1, CBB], writes=[PS[6]])
            cx.op("dve", "tensor_tensor", out=BIAS1[:], in0=PS[6][:, 0:2], in1=vec(80, 82), op=ALU.add,
                  reads=[PS[6], CFB], writes=[BIAS1])
            for kv in range(2):
                for g in range(2):
                    srcb = KCR if kv == 0 else VCR
                    hps = PS[4 + g]
                    for l in range(32):
                        cx.op("pe", "matmul", hps[:, 0:127], W1[64 * g:64 * g + 64, kv, l, :],
                              srcb[64 * g:64 * g + 64, l:l + 2017:16], start=(l == 0), stop=(l == 31),
                              reads=[W1, srcb], writes=[hps])
                    cx.op("act", "activation", out=XG[:, 0:127], in_=hps[:, 0:127], func=AF.Identity,
                          bias=BIAS1[:, kv:kv + 1], reads=[hps, BIAS1], writes=[XG])
                    cx.op("dve", "tensor_tensor", out=UU[:, 0:127], in0=XG[:, 0:127], in1=XG[:, 0:127], op=ALU.mult,
                          reads=[XG], writes=[UU])
                    cx.op("dve", "tensor_scalar", out=UU[:, 0:127], in0=UU[:, 0:127], scalar1=0.044715, scalar2=1.0,
                          op0=ALU.mult, op1=ALU.add, reads=[UU], writes=[UU])
                    cx.op("dve", "tensor_tensor", out=UU[:, 0:127], in0=UU[:, 0:127], in1=XG[:, 0:127], op=ALU.mult,
                          reads=[UU, XG], writes=[UU])
                    cx.op("act", "activation", out=UU[:, 0:127], in_=UU[:, 0:127], func=AF.Sigmoid, scale=1.5957691216057308,
                          reads=[UU], writes=[UU])
                    cx.op("dve", "tensor_tensor", out=HIDT[:, 0:127], in0=XG[:, 0:127], in1=UU[:, 0:127], op=ALU.mult,
                          reads=[XG, UU], writes=[HIDT])
                    if kv == 0:
                        cx.op("pe", "matmul", PS[6][:, 0:128], WSM[:, 1024:1152], HIDT[:], start=True, stop=True,
                              reads=[WSM, HIDT], writes=[PS[6]])
                        cx.op("act", "activation", out=KCT[:, g, :], in_=PS[6][:, 0:128], func=AF.Identity,
                              bias=vec(82, 83), reads=[PS[6], CFB], writes=[KCT])
                    else:
                        cx.op("pe", "matmul", PS[6][:, 0:64], HIDT[:], WSM[:, 1152:1216], start=True, stop=True,
                              reads=[WSM, HIDT], writes=[PS[6]])
                        cx.op("dve", "tensor_tensor", out=VCX[:, g, 0:64], in0=PS[6][:, 0:64], in1=B2V[:], op=ALU.add,
                              reads=[PS[6], B2V], writes=[VCX])

            pipe = {"pend": [], "u": 0, "job": 0}
            SCB = [PS[0], PS[1], PS[4], PS[5]]
            grp = []

            def unit(kT, qT, extras, V, acc, ncols, first, rk, rq, rv, after=None, mmask=None, ex128=(), last=False):
                u = pipe["u"]; pipe["u"] += 1
                grp.append(dict(u=u, kT=kT, qT=qT, ex=extras, ex128=ex128, V=V, acc=acc, ncols=ncols, first=first,
                                rk=rk, rq=rq, rv=rv, after=after, mmask=mmask, last=last))
                if len(grp) == (1 if 'G1' in _DBG else 2):
                    emit_group()

            def emit_group():
                if not grp:
                    return
                for d in grp:
                    sbk = SCB[d["u"] % 4]
                    nex = len(d["ex"]) + len(d["ex128"])
                    cx.op("pe", "matmul", sbk[:], d["kT"], d["qT"], start=True, stop=(nex == 0),
                          reads=[d["rk"], d["rq"]], writes=[sbk])
                    for n_, (l_, r_, c0, c1, rd) in enumerate(d["ex"]):
                        cx.op("pe", "matmul", sbk[:, c0:c1], l_, r_, start=False, stop=(n_ == nex - 1), reads=rd, writes=[sbk])
                for d in grp:
                    sbk = SCB[d["u"] % 4]
                    nex = len(d["ex"]) + len(d["ex128"])
                    for n_, (l_, r_, c0, c1, rd) in enumerate(d["ex128"]):
                        cx.op("pe", "matmul", sbk[:, c0:c1], l_, r_, start=False, stop=(len(d["ex"]) + n_ == nex - 1),
                              reads=rd, writes=[sbk])
                for pvf in pipe["pend"]:
                    pvf()
                pipe["pend"] = []
                for d in grp:
                    sbk = SCB[d["u"] % 4]
                    pt = PT[d["u"] % 4]
                    cx.op("act", "activation", out=pt[:], in_=sbk[:], func=AF.Exp, scale=0.125, reads=[sbk], writes=[pt])
                    if d["mmask"] is not None and 'NM' not in _DBG:
                        cx.op("dve", "tensor_tensor", out=pt[:], in0=pt[:], in1=d["mmask"][0], op=ALU.mult,
                              reads=[pt, d["mmask"][1]], writes=[pt])

                    def pv(d=d, pt=pt):
                        for j in range(4):
                            cx.op("pe", "matmul", d["acc"][:, d["ncols"] * j:d["ncols"] * j + d["ncols"]],
                                  pt[:, 128 * j:128 * j + 128], d["V"], start=(d["first"] and j == 0),
                                  stop=(True if 'ST' in _DBG else (d["last"] and j == 3)), reads=[pt, d["rv"]], writes=[d["acc"]])
                        if d["after"] is not None:
                            d["after"]()
                    pipe["pend"].append(pv)
                grp.clear()

            def flush():
                emit_group()
                for pvf in pipe["pend"]:
                    pvf()
                pipe["pend"] = []

            def next_acc():
                a = PS[2 + pipe["job"] % 2]
                pipe["job"] += 1
                return a

            def nsa_final(acc, ncols, c, h, br, first_branch):
                accv = acc[:, 0:4 * ncols].rearrange("p (j n) -> p j n", j=4)

                def f():
                    cx.op("dve", "tensor_scalar", out=RINV[:], in0=accv[:, :, 64], scalar1=1e-30, scalar2=None,
                          op0=ALU.max, reads=[acc], writes=[RINV])
                    cx.op("dve", "reciprocal", out=RINV[:], in_=RINV[:], reads=[RINV], writes=[RINV])
                    if br == 0:
                        fi = (h % 4 == 0)
                        IMP = IMPS[h // 4]
                        dst = IMP if fi else TMPI
                        cx.op("dve", "tensor_tensor", out=dst[:], in0=accv[:, :, 65:97],
                              in1=RINV[:].unsqueeze(2).to_broadcast([128, 4, 32]), op=ALU.mult,
                              reads=[acc, RINV], writes=[dst])
                        if not fi:
                            cx.op("pool", "tensor_tensor", out=IMP[:], in0=IMP[:], in1=TMPI[:], op=ALU.add,
                                  reads=[IMP, TMPI], writes=[IMP])
                    cx.op("dve", "tensor_tensor", out=COEF[:], in0=RINV[:], in1=GT[:, 4 * c:4 * c + 4, 8 * br + h],
                          op=ALU.mult, reads=[RINV, GT], writes=[COEF])
                    cb3 = COEF[:].unsqueeze(2).to_broadcast([128, 4, 64])
                    if first_branch:
                        cx.op("dve", "tensor_tensor", out=OA[:, :, 64 * h:64 * h + 64], in0=accv[:, :, 0:64], in1=cb3,
                              op=ALU.mult, reads=[acc, COEF], writes=[OA])
                    else:
                        cx.op("dve", "tensor_tensor", out=TMPO[:], in0=accv[:, :, 0:64], in1=cb3, op=ALU.mult,
                              reads=[acc, COEF], writes=[TMPO])
                        cx.op("pool", "tensor_tensor", out=OA[:, :, 64 * h:64 * h + 64], in0=OA[:, :, 64 * h:64 * h + 64],
                              in1=TMPO[:], op=ALU.add, reads=[OA, TMPO], writes=[OA])
                return f

            def to_OT(SRC, c, fc0):
                for fc in range(4):
                    for j in range(4):
                        col = ((fc % 2) * 4 + j) * 128
                        cx.op("pe", "transpose", psb(6 + fc // 2)[:, col:col + 128], SRC[:, j, 128 * fc:128 * fc + 128],
                              ident_b, reads=[SRC, CBB], writes=[PS[6 + fc // 2]])
                for fc in range(4):
                    cx.op("act", "copy", out=OT[:, fc0 + fc, 512 * c:512 * c + 512],
                          in_=psb(6 + fc // 2)[:, (fc % 2) * 512:(fc % 2) * 512 + 512], reads=[PS[6 + fc // 2]], writes=[OT])

            for c in range(4):
                qs = slice(512 * c, 512 * c + 512)
                for h in range(8):
                    g, b_ = h // 4, 64 * (h % 2)
                    acc = next_acc()
                    unit(KCT[b_:b_ + 64, g, :], QN[b_:b_ + 64, h // 2, qs],
                         [], VCX[:, g, 0:97], acc, 97, True,
                         KCT, QN, VCX, after=nsa_final(acc, 97, c, h, 0, True), mmask=(CMN[:, qs], CMN), last=True)
                for h in range(8):
                    g, b_ = h // 4, 64 * (h % 2)
                    acc = next_acc()
                    tiles = list(range(max(0, 4 * c - 4), 4 * c + 4))
                    for n_, i in enumerate(tiles):
                        mk = MSK[:, 3 + (4 * c - i), :] if i < 4 * c else MSK[:, i - 4 * c, :]
                        unit(KW[b_:b_ + 64, g, 128 * i:128 * i + 128], QN[b_:b_ + 64, h // 2, qs],
                             [], VT[:, i, 2 + g, :], acc, 65, n_ == 0, KW, QN, VT,
                             after=(nsa_final(acc, 65, c, h, 2, False) if n_ == len(tiles) - 1 else None), mmask=(mk, MSK),
                             last=(n_ == len(tiles) - 1))
                flush()
                for g in range(2):
                    IMPg = IMPS[g]
                    cx.op("dve", "tensor_tensor", out=IMPM[:], in0=IMPg[:], in1=IMT[:, 0, 4 * c:4 * c + 4, :], op=ALU.mult,
                          reads=[IMPg, IMT], writes=[IMPM])
                    cx.op("dve", "tensor_tensor", out=IMPM[:], in0=IMPM[:], in1=IMT[:, 1, 4 * c:4 * c + 4, :], op=ALU.add,
                          reads=[IMPM, IMT], writes=[IMPM])
                    for j in range(4):
                        cx.op("dve", "max", out=TOP8[:, j, :], in_=IMPM[:, j, :], reads=[IMPM], writes=[TOP8])
                    for j in range(4):
                        cx.op("dve", "tensor_scalar", out=NSELB[:, j, :], in0=IMPM[:, j, :], scalar1=TOP8[:, j, 7:8],
                              scalar2=NEG, op0=ALU.is_lt, op1=ALU.mult, reads=[IMPM, TOP8], writes=[NSELB])
                    for j in range(4):
                        cx.op("pe", "transpose", psb(6)[0:32, 128 * j:128 * j + 128], NSELB[:, j, :], ident_b,
                              reads=[NSELB, CBB], writes=[PS[6]])
                    cx.op("act", "copy", out=NSELT[0:32, g, :], in_=psb(6)[0:32, 0:512], reads=[PS[6]], writes=[NSELT])
                for h in range(8):
                    g, b_ = h // 4, 64 * (h % 2)
                    acc = next_acc()
                    tiles = list(range(0, 4 * c + 4))
                    for n_, i in enumerate(tiles):
                        KX = 32
                        ex = [(CBB[0:KX, CB_EX + 128 * i:CB_EX + 128 * i + 128], NSELT[0:KX, g, :], 0, 512, [CBB, NSELT])]
                        unit(KS[b_:b_ + 64, g, 128 * i:128 * i + 128], QN[b_:b_ + 64, h // 2, qs], ex,
                             VT[:, i, g, :], acc, 65, n_ == 0, KS, QN, VT,
                             after=(nsa_final(acc, 65, c, h, 1, False) if n_ == len(tiles) - 1 else None),
                             mmask=((MSK[:, i - 4 * c, :], MSK) if i >= 4 * c else None), last=(n_ == len(tiles) - 1))
                flush()
                cx.op("act", "copy", out=OAB[:], in_=OA[:], reads=[OA], writes=[OAB])
                to_OT(OAB, c, 0)
            cx.barrier()

            WINB = mem.view("winb", R_WIN, [128, 8, NB], BF16)
            cx.dma("pool", WINB[:], winB_d.rearrange("(kc p) n -> p kc n", p=128), writes=[WINB])
            o = R_PH
            QD = mem.view("QD", o, [128, 4, 2048], BF16); o += 16 * KB
            QI = mem.view("QI", o, [128, 4, 2048], BF16); o += 16 * KB
            KI = mem.view("KI", o, [128, 2048], BF16); o += 4 * KB
            CKT = mem.view("CKT", o, [128, 2048], BF16); o += 4 * KB
            KRT = mem.view("KRT", o, [128, 2048], BF16); o += 4 * KB
            WI = mem.view("WI", o, [128, 16, 8], F32); o += 512
            T1 = mem.view("T1", o, [128, 512], F32); o_JB = o; o += 2048
            T2 = mem.view("T2", o, [128, 512], F32); o += 2048
            CKN = []
            for i in range(2):
                CKN.append(mem.view("CKN%d" % i, o, [128, 128], F32)); o += 512
            o_RB = o
            assert o + 4096 + 128 <= TOT, o
            SSD = Buf(SMALL[:, 56:60], "SSD")
            for tc in range(4):
                sl = slice(512 * tc, 512 * tc + 512)
                for j in range(4):
                    proj_unit(WINB, 256 * j, 256 * j + 128, 128, tc, QD[:, j, sl], QD)
                for j in range(4):
                    proj_unit(WINB, 1024 + 256 * j, 1024 + 256 * j + 128, 128, tc, QI[:, j, sl], QI)
                proj_unit(WINB, 2048, 2176, 128, tc, KI[:, sl], KI)
                proj_unit(WINB, 2304, 2320, 16, tc, KRT[0:16, sl], KRT)
            for i in range(NT):
                pb = PS[4 + i % 2]
                ck = CKN[i % 2]
                for kc in range(8):
                    cx.op("pe", "matmul", pb[:, 0:136], HT[:, kc, 128 * i:128 * i + 128], WINB[:, kc, NB_FM:NB],
                          start=(kc == 0), stop=(kc == 7), reads=[HT, WINB], writes=[pb])
                cx.op("act", "activation", out=ck[:], in_=pb[:, 0:128], func=AF.Square, accum_out=SSD[:, 0:1],
                      reads=[pb], writes=[ck, SSD])
                cx.op("act", "activation", out=SSD[:, 1:2], in_=SSD[:, 0:1], func=AF.Sqrt, scale=1.0 / 128, bias=1e-6,
                      reads=[SSD], writes=[SSD])
                cx.op("dve", "reciprocal", out=SSD[:, 2:3], in_=SSD[:, 1:2], reads=[SSD], writes=[SSD])
                cx.op("dve", "tensor_scalar", out=ck[:], in0=pb[:, 0:128], scalar1=SSD[:, 2:3], scalar2=None, op0=ALU.mult,
                      reads=[pb, SSD], writes=[ck])
                cx.op("act", "mul", out=WI[:, i, :], in_=pb[:, 128:136], mul=float(8 ** -0.5 * 64 ** -0.5), reads=[pb], writes=[WI])
                cx.op("pe", "transpose", PS[6][:, 128 * (i % 4):128 * (i % 4) + 128], ck[:], ident_f, reads=[ck, CFB], writes=[PS[6]])
                if i % 4 == 3:
                    cx.op("act", "activation", out=CKT[:, 512 * (i // 4):512 * (i // 4) + 512], in_=PS[6][:], func=AF.Identity,
                          scale=vec(83, 84), reads=[PS[6], CFB], writes=[CKT])
            cx.barrier()

            o = R_WIN
            KHT = mem.view("KHT", o, [128, 4, 2048], BF16); o += 16 * KB
            VH = mem.view("VH", o, [128, 16, 8, 65], BF16); o += 16640
            WSM = mem.view("WSM", o, [128, 1216], BF16); o += 2432
            PT = []
            for i in range(4):
                PT.append(mem.view("PT%d" % i, o, [128, 512], BF16)); o += 1024
            ODB = mem.view("ODB", o, [128, 4, 512], BF16); o += 4096
            assert o <= R_PH, o
            NMS = [[mem.view("NMA%d" % j, R_HT + 3 * KB * j, [128, 1536], BF16) for j in range(4)],
                   [mem.view("NMB%d" % j, CF_C * 4 + 4 * KB * j, [128, 2048], BF16) for j in range(4)]]
            IB = [mem.view("IB%d" % j, R_HT + 12 * KB + 8 * KB * j, [128, 2048], F32) for j in range(2)]
            RB = [mem.view("RB%d" % j, o_RB + 2048 * j, [128, 512], F32) for j in range(2)]
            BS = []
            for n2, c0 in enumerate((8, 40)):
                BS.append(dict(LO=Buf(SMALL[:, c0:c0 + 1], "LO%d" % n2), MID=Buf(SMALL[:, c0 + 1:c0 + 2], "MID%d" % n2),
                               CNT=Buf(SMALL[:, c0 + 2:c0 + 3], "CNT%d" % n2), TMPS=Buf(SMALL[:, c0 + 3:c0 + 4], "TMPS%d" % n2),
                               W0=Buf(SMALL[:, c0 + 4:c0 + 5], "W0%d" % n2), MN=Buf(SMALL[:, c0 + 5:c0 + 6], "MN%d" % n2),
                               MX8=Buf(SMALL[:, c0 + 6:c0 + 14], "MX8%d" % n2),
                               WK=mem.view("WK%d" % n2, o_RB + 4096 + 64 * n2, [128, 16], F32),
                               JB=Buf(mem.view("JBx%d" % n2, o_JB + 2048 * n2, [128, 1024], BF16).ap.bitcast(mybir.dt.uint8), "JB%d" % n2)))
            cx.dma("pool", WSM[:], wsm_d, writes=[WSM])
            cx.op("pool", "memset", VH[:, :, :, 64:65], 1.0, writes=[VH])
            n_ = 0
            for j in range(4):
                for tc in range(4):
                    pb = PS[4 + n_ % 2]; n_ += 1
                    cx.op("pe", "matmul", pb[:], WSM[:, 128 * j:128 * j + 128], CKT[:, 512 * tc:512 * tc + 512],
                          start=True, stop=False, reads=[WSM, CKT], writes=[pb])
                    cx.op("pe", "matmul", pb[:], CBB[0:16, CB_SEL:CB_SEL + 128], KRT[0:16, 512 * tc:512 * tc + 512],
                          start=False, stop=True, reads=[CBB, KRT], writes=[pb])
                    cx.op("act", "copy", out=KHT[:, j, 512 * tc:512 * tc + 512], in_=pb[:], reads=[pb], writes=[KHT])
            for i in range(NT):
                pb = PS[4 + n_ % 2]; n_ += 1
                cx.op("pe", "matmul", pb[:], CKT[:, 128 * i:128 * i + 128], WSM[:, 512:1024], start=True, stop=True,
                      reads=[CKT, WSM], writes=[pb])
                cx.op("act", "copy", out=VH[:, i, :, 0:64], in_=pb[:].rearrange("p (h d) -> p h d", h=8), reads=[pb], writes=[VH])

            pipe["pend"] = []
            grp.clear()
            def idx_scores(c, j):
                T = 4 * c + j
                Wc = 512 * (c + 1)
                Wv = 128 * (T + 1)
                IBt = IB[T % 2]
                for sc in range(c + 1):
                    N = min(512, Wv - 512 * sc)
                    for h in range(8):
                        b_ = 64 * (h % 2)
                        L = PS[6 + lcnt[0] % 2]; lcnt[0] += 1
                        cx.op("pe", "matmul", L[:, 0:N], QI[b_:b_ + 64, h // 2, 128 * T:128 * T + 128],
                              KI[b_:b_ + 64, 512 * sc:512 * sc + N], start=True, stop=True, reads=[QI, KI], writes=[L])
                        if h == 0:
                            cx.op("dve", "tensor_scalar", out=IBt[:, 512 * sc:512 * sc + N], in0=L[:, 0:N], scalar1=0.0,
                                  scalar2=WI[:, T, h:h + 1], op0=ALU.max, op1=ALU.mult, reads=[L, WI], writes=[IBt])
                        else:
                            rb = RB[h % 2]
                            cx.op("dve", "tensor_scalar", out=rb[:, 0:N], in0=L[:, 0:N], scalar1=0.0,
                                  scalar2=WI[:, T, h:h + 1], op0=ALU.max, op1=ALU.mult, reads=[L, WI], writes=[rb])
                            cx.op("pool", "tensor_tensor", out=IBt[:, 512 * sc:512 * sc + N], in0=IBt[:, 512 * sc:512 * sc + N],
                                  in1=rb[:, 0:N], op=ALU.add, reads=[IBt, rb], writes=[IBt])
                MX8, MN = BS[j % 2]["MX8"], BS[j % 2]["MN"]
                if T >= 2:
                    cx.op("dve", "max", out=MX8[:], in_=IBt[:, 0:Wv], reads=[IBt], writes=[MX8])
                    cx.op("dve", "tensor_reduce", out=MN[:], in_=IBt[:, 0:Wv], axis=AX.X, op=ALU.min, reads=[IBt], writes=[MN])
                cx.op("pool", "affine_select", out=IBt[:, 128 * T:128 * T + 128], in_=IBt[:, 128 * T:128 * T + 128],
                      pattern=[[-1, 128]], compare_op=ALU.is_ge, fill=-3.0e38, base=0, channel_multiplier=1,
                      reads=[IBt], writes=[IBt])
                if Wv < Wc:
                    cx.op("pool", "memset", IBt[:, Wv:Wc], -3.0e38, writes=[IBt])

            def idx_bisect_pair(c, js):
                Wc = 512 * (c + 1)
                chains = []
                for j in js:
                    T = 4 * c + j
                    chains.append((j, T, 128 * (T + 1), IB[T % 2], BS[j % 2]))
                act_ = [ch for ch in chains if ch[1] >= 2]
                for (j, T, Wv, IBt, B) in chains:
                    if T >= 2:
                        cx.op("dve", "tensor_copy", out=B["LO"][:], in_=B["MN"][:], reads=[B["MN"]], writes=[B["LO"]])
                        cx.op("dve", "tensor_tensor", out=B["W0"][:], in0=B["MX8"][:, 0:1], in1=B["MN"][:], op=ALU.subtract,
                              reads=[B["MX8"], B["MN"]], writes=[B["W0"]])
                        cx.op("dve", "tensor_scalar", out=B["WK"][:, 0:BIS_ITERS], in0=CFB[:, CF_P2:CF_P2 + BIS_ITERS],
                              scalar1=B["W0"][:], scalar2=None, op0=ALU.mult, reads=[CFB, B["W0"]], writes=[B["WK"]])
                    else:
                        cx.op("dve", "memset", B["LO"][:], -1.0e30, writes=[B["LO"]])
                for k in range(BIS_ITERS):
                    for (j, T, Wv, IBt, B) in act_:
                        cx.op("dve", "tensor_tensor", out=B["MID"][:], in0=B["LO"][:], in1=B["WK"][:, k:k + 1], op=ALU.add,
                              reads=[B["LO"], B["WK"]], writes=[B["MID"]])
                    for (j, T, Wv, IBt, B) in act_:
                        cx.op("dve", "tensor_scalar", out=B["JB"][:, 0:Wv], in0=IBt[:, 0:Wv], scalar1=B["MID"][:], scalar2=0.0,
                              op0=ALU.is_ge, op1=ALU.add, accum_out=B["CNT"][:], reads=[IBt, B["MID"]], writes=[B["JB"], B["CNT"]])
                    for (j, T, Wv, IBt, B) in act_:
                        cx.op("dve", "tensor_scalar", out=B["TMPS"][:], in0=B["CNT"][:], scalar1=255.5, scalar2=B["WK"][:, k:k + 1],
                              op0=ALU.is_ge, op1=ALU.mult, reads=[B["CNT"], B["WK"]], writes=[B["TMPS"]])
                    for (j, T, Wv, IBt, B) in act_:
                        cx.op("dve", "tensor_tensor", out=B["LO"][:], in0=B["LO"][:], in1=B["TMPS"][:], op=ALU.add,
                              reads=[B["LO"], B["TMPS"]], writes=[B["LO"]])
                for (j, T, Wv, IBt, B) in chains:
                    nm = NMS[c % 2][j]
                    cx.op("dve", "tensor_scalar", out=nm[:, 0:Wc], in0=IBt[:, 0:Wc], scalar1=B["LO"][:], scalar2=NEG,
                          op0=ALU.is_lt, op1=ALU.mult, reads=[IBt, B["LO"]], writes=[nm])

            def idx_slices(c):
                return [lambda: idx_scores(c, 0), lambda: idx_scores(c, 1), lambda: idx_bisect_pair(c, (0, 1)), lambda: None,
                        lambda: idx_scores(c, 2), lambda: idx_scores(c, 3), lambda: idx_bisect_pair(c, (2, 3)), lambda: None]

            lcnt = [0]
            for f_ in idx_slices(0):
                f_()
            for c in range(4):
                qs = slice(512 * c, 512 * c + 512)
                NMc = NMS[c % 2]
                sl_next = idx_slices(c + 1) if c < 3 else []
                for h in range(8):
                    if sl_next:
                        sl_next[h]()
                    b_ = 64 * (h % 2)
                    acc = next_acc()
                    accv = acc[:, 0:260].rearrange("p (j n) -> p j n", j=4)

                    def fin(acc=acc, accv=accv, h=h):
                        cx.op("dve", "reciprocal", out=RINV[:], in_=accv[:, :, 64], reads=[acc], writes=[RINV])
                        cx.op("dve", "tensor_tensor", out=ODB[:, :, 64 * h:64 * h + 64], in0=accv[:, :, 0:64],
                              in1=RINV[:].unsqueeze(2).to_broadcast([128, 4, 64]), op=ALU.mult,
                              reads=[acc, RINV], writes=[ODB])
                    tiles = list(range(0, 4 * c + 4))
                    for q_, i in enumerate(tiles):
                        ex = [(NMc[j][:, 128 * i:128 * i + 128], ident_b, 128 * j, 128 * j + 128, [NMc[j], CBB]) for j in range(4)]
                        unit(KHT[b_:b_ + 64, h // 2, 128 * i:128 * i + 128], QD[b_:b_ + 64, h // 2, qs], [],
                             VH[:, i, h, :], acc, 65, q_ == 0, KHT, QD, VH, after=(fin if q_ == len(tiles) - 1 else None),
                             ex128=ex, last=(q_ == len(tiles) - 1))
                flush()
                to_OT(ODB, c, 4)
            cx.barrier()

            o = R_HT
            X1 = mem.view("X1", o, [128, 4, 1024], F32); o += 16 * KB
            H2T = mem.view("H2T", o, [128, 8, 512], BF16); o += 8 * KB
            ACTT = mem.view("ACTT", o, [128, 22, 512], BF16)
            WOUT = mem.view("WOUT", o, [128, 8, 1024], BF16); o += 22 * KB
            WG = []
            for i in range(3):
                WG.append(mem.view("WG%d" % i, o, [128, 8, 256], BF16)); o += 4 * KB
            WDN = mem.view("WDN", o, [128, 22, 1024], BF16); o += 44 * KB
            XIN = []
            for i in range(2):
                XIN.append(mem.view("xin%d" % i, o, [128, 1024], F32)); o += 4 * KB
            JUNK = mem.view("junk", o, [128, 1024], F32); o += 4 * KB
            XS = mem.view("xs", o, [128, 1024], F32); o += 4 * KB
            TMPY = mem.view("tmpy", o, [128, 1024], F32); o += 4 * KB
            SIL = []
            for i in range(2):
                SIL.append(mem.view("sil%d" % i, o, [128, 512], F32)); o += 2 * KB
            assert o <= TOT, o
            ST = Buf(SMALL[:, 44:52], "ST")
            cx.dma("pool", WDN[:], wdn_d.rearrange("(c p) n -> p c n", p=128), writes=[WDN])
            wout_v = wout_d.rearrange("(kc p) n -> p kc n", p=128)

            for gi in range(4):
                cx.dma("pool", WOUT[:], wout_v, writes=[WOUT, ACTT])
                for j in range(4):
                    T = 4 * gi + j
                    xi = XIN[j % 2]
                    cx.dma("sp", xi[:], x_d[seq, 128 * T:128 * T + 128, :], writes=[xi])
                    yb = [PS[2 * (j % 2)], PS[2 * (j % 2) + 1]]
                    for n in range(2):
                        for kc in range(8):
                            cx.op("pe", "matmul", yb[n][:], OT[:, kc, 128 * T:128 * T + 128], WOUT[:, kc, 512 * n:512 * n + 512],
                                  start=(kc == 0), stop=(kc == 7), reads=[OT, WOUT], writes=[yb[n]])
                    for n in range(2):
                        cx.op("act", "activation", out=JUNK[:, 512 * n:512 * n + 512], in_=yb[n][:], func=AF.Square,
                              accum_out=ST[:, n:n + 1], reads=[yb[n]], writes=[JUNK, ST])
                    cx.op("dve", "tensor_tensor", out=ST[:, 2:3], in0=ST[:, 0:1], in1=ST[:, 1:2], op=ALU.add, reads=[ST], writes=[ST])
                    cx.op("act", "activation", out=ST[:, 3:4], in_=ST[:, 2:3], func=AF.Sqrt, scale=1.0 / D, bias=1e-6, reads=[ST], writes=[ST])
                    cx.op("dve", "reciprocal", out=ST[:, 4:5], in_=ST[:, 3:4], reads=[ST], writes=[ST])
                    for n in range(2):
                        cx.op("dve", "scalar_tensor_tensor", out=TMPY[:, 512 * n:512 * n + 512], in0=yb[n][:], scalar=ST[:, 4:5],
                              in1=G1[:, 512 * n:512 * n + 512], op0=ALU.mult, op1=ALU.mult, reads=[yb[n], ST, G1], writes=[TMPY])
                    cx.op("pool", "tensor_tensor", out=X1[:, j, :], in0=TMPY[:], in1=xi[:], op=ALU.add, reads=[TMPY, xi], writes=[X1])
                    cx.op("act", "activation", out=JUNK[:], in_=X1[:, j, :], func=AF.Square, accum_out=ST[:, 0:1],
                          reads=[X1], writes=[JUNK, ST])
                    cx.op("act", "activation", out=ST[:, 3:4], in_=ST[:, 0:1], func=AF.Sqrt, scale=1.0 / D, bias=1e-6, reads=[ST], writes=[ST])
                    cx.op("dve", "reciprocal", out=ST[:, 4:5], in_=ST[:, 3:4], reads=[ST], writes=[ST])
                    cx.op("dve", "tensor_scalar", out=XS[:], in0=X1[:, j, :], scalar1=ST[:, 4:5], scalar2=None, op0=ALU.mult,
                          reads=[X1, ST], writes=[XS])
                    for fc in range(8):
                        pb = PS[4 + fc // 4]
                        cx.op("pe", "transpose", pb[:, 128 * (fc % 4):128 * (fc % 4) + 128], XS[:, 128 * fc:128 * fc + 128],
                              ident_f, reads=[XS, CFB], writes=[pb])
                    for fc in range(8):
                        pb = PS[4 + fc // 4]
                        cx.op("act", "activation", out=H2T[:, fc, 128 * j:128 * j + 128],
                              in_=pb[:, 128 * (fc % 4):128 * (fc % 4) + 128], func=AF.Identity,
                              scale=DERV[:, 2, fc, seq:seq + 1], bias=DERV[:, 3, fc, seq:seq + 1],
                              reads=[pb, DERV], writes=[H2T])
                for ch in range(22):
                    wg = WG[ch % 3]
                    cx.dma("pool", wg[:].rearrange("p a b -> p (a b)"), wgu_d[ch], writes=[wg])
                    pg, pu = PS[2 * (ch % 2)], PS[2 * (ch % 2) + 1]
                    for kc in range(8):
                        cx.op("pe", "matmul", pg[:], wg[:, kc, 0:128], H2T[:, kc, :], start=(kc == 0), stop=(kc == 7),
                              reads=[wg, H2T], writes=[pg])
                    for kc in range(8):
                        cx.op("pe", "matmul", pu[:], wg[:, kc, 128:256], H2T[:, kc, :], start=(kc == 0), stop=(kc == 7),
                              reads=[wg, H2T], writes=[pu])
                    sl_ = SIL[ch % 2]
                    cx.op("act", "activation", out=sl_[:], in_=pg[:], func=AF.Silu, reads=[pg], writes=[sl_])
                    cx.op("dve", "tensor_tensor", out=ACTT[:, ch, :], in0=sl_[:], in1=pu[:], op=ALU.mult,
                          reads=[sl_, pu], writes=[ACTT, WOUT])
                for j in range(4):
                    T = 4 * gi + j
                    zb = [PS[4 + 2 * (j % 2)], PS[5 + 2 * (j % 2)]]
                    for n in range(2):
                        for ch in range(22):
                            cx.op("pe", "matmul", zb[n][:], ACTT[:, ch, 128 * j:128 * j + 128], WDN[:, ch, 512 * n:512 * n + 512],
                                  start=(ch == 0), stop=(ch == 21), reads=[ACTT, WDN], writes=[zb[n]])
                    for n in range(2):
                        cx.op("act", "activation", out=JUNK[:, 512 * n:512 * n + 512], in_=zb[n][:], func=AF.Square,
                              accum_out=ST[:, n:n + 1], reads=[zb[n]], writes=[JUNK, ST])
                    cx.op("dve", "tensor_tensor", out=ST[:, 2:3], in0=ST[:, 0:1], in1=ST[:, 1:2], op=ALU.add, reads=[ST], writes=[ST])
                    cx.op("act", "activation", out=ST[:, 3:4], in_=ST[:, 2:3], func=AF.Sqrt, scale=1.0 / D, bias=1e-6, reads=[ST], writes=[ST])
                    cx.op("dve", "reciprocal", out=ST[:, 4:5], in_=ST[:, 3:4], reads=[ST], writes=[ST])
                    for n in range(2):
                        cx.op("dve", "scalar_tensor_tensor", out=TMPY[:, 512 * n:512 * n + 512], in0=zb[n][:], scalar=ST[:, 4:5],
                              in1=G2[:, 512 * n:512 * n + 512], op0=ALU.mult, op1=ALU.mult, reads=[zb[n], ST, G2], writes=[TMPY])
                    cx.op("pool", "tensor_tensor", out=XS[:], in0=TMPY[:], in1=X1[:, j, :], op=ALU.add, reads=[TMPY, X1], writes=[XS])
                    cx.dma("sp", out_d[seq, 128 * T:128 * T + 128, :], XS[:], reads=[XS])
            cx.barrier()

        cx.barrier()
        cx.finish()
    return nc


def _prep(inputs):
    inp = {k: np.asarray(v) for k, v in inputs.items()}
    cf, cb, imt, wsm = _host_consts(inp)
    A, B = _col_index()
    shared = {
        "w_ada": np.ascontiguousarray(inp['w_ada'][0]),
        "cf": cf, "cb": cb, "imt": imt, "wsm": wsm,
        "w_inA": np.ascontiguousarray(inp['w_in'][0][:, A]),
        "w_inB": np.ascontiguousarray(inp['w_in'][0][:, B]),
        "cmp_w1": np.ascontiguousarray(inp['cmp_w1'][0]),
        "b2v": np.ascontiguousarray(inp['cmp_b2'][0, 1]),
        "w_out": np.ascontiguousarray(inp['w_out'][0]),
        "w_gate_up": np.ascontiguousarray(
            inp['w_gate_up'][0].reshape(8, 128, 2, 22, 128).transpose(3, 1, 0, 2, 4).reshape(22, 128, 2048)),
        "w_down": np.ascontiguousarray(inp['w_down'][0]),
    }
    maps = []
    for c in range(8):
        m = dict(shared)
        m["x"] = np.ascontiguousarray(inp['x'][2 * c:2 * c + 2])
        m["cT"] = np.ascontiguousarray(inp['c'][2 * c:2 * c + 2].T.reshape(8, 128, 2).transpose(1, 0, 2))
        maps.append(m)
    return maps


def kernel(**inputs):
    maps = _prep(inputs)
    nc = build()
    res = run_bass_kernel_spmd(nc, maps, core_ids=list(range(8)))
    return np.concatenate([r["out"] for r in res.results], axis=0).astype(np.float32)
```

```python
import numpy as np
import concourse.bass as bass
import concourse.mybir as mybir
from concourse.bass_utils import run_bass_kernel_spmd
from contextlib import ExitStack

F32 = mybir.dt.float32
BF16 = mybir.dt.bfloat16
ALU = mybir.AluOpType
AF = mybir.ActivationFunctionType
AX = mybir.AxisListType

S = 2048
D = 1024
NT = 16
DFF = 2816
NEG = -30000.0
IN_SPLITS = (512, 128, 128, 128, 128, 128, 128, 24, 512, 128, 16, 512, 64, 8)
NAMES = ['nq', 'nkc', 'nvc', 'nks', 'nvs', 'nkw', 'nvw', 'ngate', 'dq', 'dckv', 'dkr', 'iq', 'ik', 'iw']
OFF = dict(zip(NAMES, np.cumsum((0,) + IN_SPLITS)[:-1]))
NA_FM = 19 * 128
NA = NA_FM + 280
NB_FM = 18 * 128 + 32
NB = NB_FM + 136
BIS_ITERS = 15
import os as _os
_DBG = _os.environ.get('KDBG', '')


class _E:
    def __init__(self, name, eng, sem):
        self.name, self.eng, self.sem, self.tick, self.waited = name, eng, sem, 0, {}


class _St:
    __slots__ = ("w", "r")

    def __init__(self):
        self.w = None
        self.r = {}


class Buf:
    def __init__(self, ap, key):
        self.ap = ap
        self.key = key

    def __getitem__(self, idx):
        return self.ap[idx]


class Ctx:
    def __init__(self, nc, es, n_dma_sems=8):
        self.nc, self.es = nc, es
        self.E = {}
        for name, eng in (("pe", nc.tensor), ("act", nc.scalar), ("dve", nc.vector),
                          ("pool", nc.gpsimd), ("sp", nc.sync)):
            self.E[name] = _E(name, eng, es.enter_context(nc.semaphore("s_" + name)))
        self.dsems = {q: [[es.enter_context(nc.semaphore("d_%s%d" % (q, i))), 0] for i in range(n_dma_sems)]
                      for q in ("sp", "pool")}
        self.dnext = {"sp": 0, "pool": 0}
        self.st = {}

    def _state(self, b):
        k = b.key if isinstance(b, Buf) else (b if isinstance(b, str) else b.name)
        s = self.st.get(k)
        if s is None:
            s = self.st[k] = _St()
        return s

    def _wait(self, E, dep):
        kind, tk = dep
        if kind == E.name and E.name == "pe":
            return
        if E.waited.get(kind, 0) >= tk:
            return
        sem = self.dsems[kind[0]][kind[1]][0] if isinstance(kind, tuple) else self.E[kind].sem
        E.eng.wait_ge(sem, tk)
        E.waited[kind] = tk

    def _deps(self, E, reads, writes):
        deps = []
        for b in reads:
            s = self._state(b)
            if s.w is not None:
                deps.append(s.w)
        for b in writes:
            s = self._state(b)
            if s.w is not None:
                deps.append(s.w)
            deps.extend(s.r.items())
        for d in deps:
            self._wait(E, d)

    def _mark(self, token, reads, writes):
        for b in reads:
            s = self._state(b)
            if s.r.get(token[0], 0) < token[1]:
                s.r[token[0]] = token[1]
        for b in writes:
            s = self._state(b)
            s.w = token
            s.r = {}

    def op(self, en, fn, *args, reads=(), writes=(), **kw):
        E = self.E[en]
        self._deps(E, reads, writes)
        ins = getattr(E.eng, fn)(*args, **kw)
        E.tick += 1
        ins.then_inc(E.sem, 1)
        self._mark((en, E.tick), reads, writes)
        return ins

    def dma(self, q, out, in_, reads=(), writes=(), **kw):
        E = self.E[q]
        self._deps(E, reads, writes)
        i = self.dnext[q]
        self.dnext[q] = (i + 1) % len(self.dsems[q])
        slot = self.dsems[q][i]
        kind = (q, i)
        if slot[1] > 0:
            self._wait(E, (kind, slot[1]))
        slot[1] += 16
        E.eng.dma_start(out=out, in_=in_, **kw).then_inc(slot[0], 16)
        self._mark((kind, slot[1]), reads, writes)

    def barrier(self):
        toks = [(n, e.tick) for n, e in self.E.items() if e.tick > 0]
        for q in self.dsems:
            for i, slot in enumerate(self.dsems[q]):
                if slot[1] > 0:
                    toks.append(((q, i), slot[1]))
        for n, e in self.E.items():
            for t in toks:
                if t[0] == n:
                    if n != "sp" and e.waited.get(n, 0) < t[1]:
                        e.eng.wait_ge(e.sem, t[1])
                        e.waited[n] = t[1]
                else:
                    self._wait(e, t)
        self.st = {}

    def finish(self):
        E = self.E["sp"]
        for q in self.dsems:
            for i, slot in enumerate(self.dsems[q]):
                if slot[1] > 0:
                    self._wait(E, ((q, i), slot[1]))


class Mem:
    def __init__(self, big):
        self.big = big

    def view(self, key, off, shape, dt, pbase=0):
        esz = 4 if dt == F32 else 2
        nel = int(np.prod(shape[1:]))
        assert off % 4 == 0
        a = off // 2
        n = nel * esz // 2
        ap = self.big[pbase:pbase + shape[0], a:a + n]
        if dt == F32:
            ap = ap.bitcast(F32)
        if len(shape) == 3:
            ap = ap.rearrange("p (a b) -> p a b", a=shape[1])
        elif len(shape) == 4:
            ap = ap.rearrange("p (a b c) -> p a b c", a=shape[1], b=shape[2])
        return Buf(ap, key)


def _swap_head(cols):
    c = np.array(cols).copy()
    c[0:8] = cols[8:16]
    c[8:16] = cols[0:8]
    return c


def _col_index():
    def head(base, h):
        return np.arange(base + 64 * h, base + 64 * h + 64)
    A = []
    for j in range(4):
        a = np.concatenate([head(OFF['nq'], 2 * j), head(OFF['nq'], 2 * j + 1)])
        b = np.concatenate([_swap_head(head(OFF['nq'], 2 * j)), _swap_head(head(OFF['nq'], 2 * j + 1))])
        A += [a, b]
    a = np.concatenate([head(OFF['nkc'], 0), head(OFF['nkc'], 1)])
    b = np.concatenate([_swap_head(head(OFF['nkc'], 0)), _swap_head(head(OFF['nkc'], 1))])
    A += [a, b]
    A += [np.arange(OFF['nvc'], OFF['nvc'] + 128)]
    for nm in ('nks', 'nkw'):
        for g in range(2):
            a = np.concatenate([head(OFF[nm], g), head(OFF[nm], g)])
            b = np.concatenate([_swap_head(head(OFF[nm], g)), _swap_head(head(OFF[nm], g))])
            A += [a, b]
    A += [np.arange(OFF['nvs'], OFF['nvs'] + 128), np.arange(OFF['nvw'], OFF['nvw'] + 128),
          np.arange(OFF['ngate'], OFF['ngate'] + 24)]
    A = np.concatenate(A)
    assert A.size == NA
    B = []
    for nm in ('dq', 'iq'):
        for j in range(4):
            a = np.concatenate([head(OFF[nm], 2 * j), head(OFF[nm], 2 * j + 1)])
            b = np.concatenate([_swap_head(head(OFF[nm], 2 * j)), _swap_head(head(OFF[nm], 2 * j + 1))])
            B += [a, b]
    a = np.concatenate([head(OFF['ik'], 0), head(OFF['ik'], 0)])
    b = np.concatenate([_swap_head(head(OFF['ik'], 0)), _swap_head(head(OFF['ik'], 0))])
    B += [a, b]
    kr = np.arange(OFF['dkr'], OFF['dkr'] + 16)
    B += [kr, _swap_head(kr)]
    B += [np.arange(OFF['dckv'], OFF['dckv'] + 128), np.arange(OFF['iw'], OFF['iw'] + 8)]
    B = np.concatenate(B)
    assert B.size == NB
    return A, B


CF_ID = 0
CF_C = 128
CF_S = CF_C + 2048
CF_V = CF_S + 2048
CF_P2 = CF_V + 84
CF_N = CF_P2 + BIS_ITERS
CB_ID = 0
CB_EX = 128
CB_OV = CB_EX + 2048
CB_PE = CB_OV + 32
CB_SEL = CB_PE + 64
CB_N = CB_SEL + 128


def _host_consts(inp):
    cf = np.zeros((128, CF_N), np.float32)
    cf[:, CF_ID:CF_ID + 128] = np.eye(128, dtype=np.float32)
    inv_freq = 1.0 / (np.float32(500000.0) ** (np.arange(0, 16, 2, dtype=np.float32) / np.float32(16)))
    ang = np.arange(S, dtype=np.float32)[:, None] * inv_freq[None, :].astype(np.float32)
    cos, sin = np.cos(ang).astype(np.float32), np.sin(ang).astype(np.float32)
    for p in range(128):
        j = p % 64
        if j < 16:
            cf[p, CF_C:CF_C + S] = cos[:, j % 8]
            cf[p, CF_S:CF_S + S] = (-1.0 if j < 8 else 1.0) * sin[:, j % 8]
        else:
            cf[p, CF_C:CF_C + S] = 1.0
    v = cf[:, CF_V:CF_V + 84]
    v[:, 0:48] = inp['b_ada'][0].reshape(48, 128).T
    v[:, 48:56] = inp['g_pre_mix'][0].reshape(8, 128).T
    v[:, 56:64] = inp['g_post_mix'][0].reshape(8, 128).T
    v[:, 64:72] = inp['g_pre_ffn'][0].reshape(8, 128).T
    v[:, 72:80] = inp['g_post_ffn'][0].reshape(8, 128).T
    v[:, 80] = inp['cmp_b1'][0, 0]
    v[:, 81] = inp['cmp_b1'][0, 1]
    v[:, 82] = np.concatenate([inp['cmp_b2'][0, 0], inp['cmp_b2'][0, 0]])
    v[:, 83] = inp['g_kv_norm'][0]
    cf[:, CF_P2:CF_P2 + BIS_ITERS] = (0.5 ** np.arange(1, BIS_ITERS + 1, dtype=np.float64)).astype(np.float32)[None, :]
    cb = np.zeros((128, CB_N), np.float32)
    cb[:, CB_ID:CB_ID + 128] = np.eye(128, dtype=np.float32)
    for j in range(32):
        cb[j, CB_EX + 64 * j:CB_EX + 64 * j + 64] = 1.0
    ci = np.arange(127)[:, None] * 16
    sj = np.arange(32)[None, :] * 64
    cb[0:127, CB_OV:CB_OV + 32] = ((ci < sj + 64) & (ci + 32 > sj)).astype(np.float32)
    cb[0:64, CB_PE:CB_PE + 32] = inp['cmp_pe'][0, 0].T
    cb[0:64, CB_PE + 32:CB_PE + 64] = inp['cmp_pe'][0, 1].T
    for i in range(16):
        cb[i, CB_SEL + i] = 1.0
        cb[i, CB_SEL + 64 + i] = 1.0
    t = (np.arange(16)[None, :, None] * 128 + np.arange(128)[:, None, None])
    blk = t // 64
    j = np.arange(32)[None, None, :]
    visible = j <= blk
    forced = (j == 0) | (j == blk) | (j == blk - 1)
    mm = (visible & ~forced).astype(np.float32)
    ba = np.where(visible, np.where(forced, 1e6, 0.0), -1e30).astype(np.float32)
    imt = np.concatenate([mm.reshape(128, 512), ba.reshape(128, 512)], axis=1)
    wuk = np.zeros((128, 4, 128), np.float32)
    for h in range(8):
        wuk[:, h // 2, (h % 2) * 64 + 16:(h % 2) * 64 + 64] = inp['w_uk'][0, h]
    wuv = np.transpose(inp['w_uv'][0], (1, 0, 2)).reshape(128, 512)
    w2 = np.concatenate([inp['cmp_w2'][0, 0], inp['cmp_w2'][0, 0], inp['cmp_w2'][0, 1]], axis=1)
    wsm = np.concatenate([wuk.reshape(128, 512), wuv, w2], axis=1).astype(np.float32)
    return cf, cb, imt, wsm


def build(n_seq=2, stage=99, dbg_cols=0):
    nc = bass.Bass("TRN2", target_bir_lowering=False)
    dt_ = nc.dram_tensor
    x_d = dt_("x", [2, S, D], F32, kind="ExternalInput").ap()
    cT_d = dt_("cT", [128, 8, 2], F32, kind="ExternalInput").ap()
    wada_d = dt_("w_ada", [D, 6 * D], F32, kind="ExternalInput").ap()
    cf_d = dt_("cf", [128, CF_N], F32, kind="ExternalInput").ap()
    cb_d = dt_("cb", [128, CB_N], F32, kind="ExternalInput").ap()
    imt_d = dt_("imt", [128, 1024], F32, kind="ExternalInput").ap()
    wsm_d = dt_("wsm", [128, 1216], F32, kind="ExternalInput").ap()
    winA_d = dt_("w_inA", [D, NA], F32, kind="ExternalInput").ap()
    winB_d = dt_("w_inB", [D, NB], F32, kind="ExternalInput").ap()
    w1_d = dt_("cmp_w1", [2, 2048, 128], F32, kind="ExternalInput").ap()
    b2v_d = dt_("b2v", [64], F32, kind="ExternalInput").ap()
    wout_d = dt_("w_out", [D, D], F32, kind="ExternalInput").ap()
    wgu_d = dt_("w_gate_up", [22, 128, 2048], F32, kind="ExternalInput").ap()
    wdn_d = dt_("w_down", [DFF, D], F32, kind="ExternalInput").ap()
    out_d = dt_("out", [2, S, D], F32, kind="ExternalOutput").ap()
    dbg_d = dt_("dbg", [128, dbg_cols], F32, kind="ExternalOutput").ap() if dbg_cols else None

    with ExitStack() as es:
        cx = Ctx(nc, es)
        TOT = 206 * 1024
        big = es.enter_context(nc.sbuf_tensor("big", [128, TOT // 2], BF16))
        mem = Mem(big)
        PS = [Buf(es.enter_context(nc.psum_tensor("ps%d" % i, [128, 512], F32))[:], "ps%d" % i) for i in range(8)]

        def psb(i):
            return PS[i].ap.bitcast(BF16)

        KB = 1024
        o = 0
        CFB = mem.view("cf", o, [128, CF_N], F32); o += CF_N * 4
        CBB = mem.view("cb", o, [128, CB_N], BF16); o += CB_N * 2
        MSK = mem.view("msk", o, [128, 8, 512], BF16); o += 8 * 512 * 2
        CMN = mem.view("cmn", o, [128, 2048], BF16); o += 2048 * 2
        ONESF = mem.view("onesf", o, [128, 128], F32); o += 512
        MODC = mem.view("modc", o, [128, 48, 2], F32); o += 384
        DERV = mem.view("derv", o, [128, 6, 8, 2], F32); o += 384
        CACT = mem.view("cact", o, [128, 8, 2], F32); o += 64
        SMALL = mem.view("small", o, [128, 64], F32); o += 256
        G1 = mem.view("G1", o, [128, 1024], F32); o += 4096
        G2 = mem.view("G2", o, [128, 1024], F32); o += 4096
        assert o <= 44 * KB, o
        R_OT = 44 * KB
        R_HT = 76 * KB
        R_WIN = 108 * KB
        R_PH = 152 * KB
        OT = mem.view("OT", R_OT, [128, 8, 2048], BF16)
        HT = mem.view("HT", R_HT, [128, 8, 2048], BF16)

        ident_f = CFB[:, CF_ID:CF_ID + 128]
        ident_b = CBB[:, CB_ID:CB_ID + 128]
        ropeC = CFB[:, CF_C:CF_C + S]
        ropeS = CFB[:, CF_S:CF_S + S]
        vec = lambda c0, c1: CFB[:, CF_V + c0:CF_V + c1]

        def dbg_dump(ap, col0, ncols, rd):
            if dbg_d is not None:
                cx.dma("pool", dbg_d[0:ap.shape[0], col0:col0 + ncols], ap, reads=[rd])

        cx.dma("sp", CFB[:], cf_d, writes=[CFB])
        cx.dma("pool", CBB[:], cb_d, writes=[CBB])
        cx.dma("sp", CACT[:], cT_d, writes=[CACT])
        cx.op("pool", "memset", MSK[:], 0.0, writes=[MSK])
        for k in range(4):
            cx.op("pool", "affine_select", out=MSK[:, k, :], in_=MSK[:, k, :], pattern=[[-1, 512]],
                  compare_op=ALU.is_gt, fill=1.0, base=128 * k, channel_multiplier=1, reads=[MSK], writes=[MSK])
        for k in range(1, 5):
            cx.op("pool", "affine_select", out=MSK[:, 3 + k, :], in_=MSK[:, 3 + k, :], pattern=[[1, 512]],
                  compare_op=ALU.is_ge, fill=1.0, base=-512 + 128 * k, channel_multiplier=-1, reads=[MSK], writes=[MSK])
        cx.op("pool", "memset", CMN[:], 0.0, writes=[CMN])
        cx.op("pool", "affine_select", out=CMN[:], in_=CMN[:], pattern=[[-1, 2048]],
              compare_op=ALU.is_gt, fill=1.0, base=31, channel_multiplier=16, reads=[CMN], writes=[CMN])
        cx.op("pool", "memset", ONESF[:], 1.0, writes=[ONESF])
        cx.op("act", "activation", out=CACT[:], in_=CACT[:], func=AF.Silu, reads=[CACT], writes=[CACT])
        WA = [mem.view("wa0", R_HT, [128, 8, 1024], F32), mem.view("wa1", R_WIN, [128, 8, 1024], F32)]
        wada_v = wada_d.rearrange("(kc p) n -> p kc n", p=128)
        for v in range(6):
            wb = WA[v % 2]
            for kc in range(8):
                cx.dma("sp", wb[:, kc, :], wada_v[:, kc, 1024 * v:1024 * v + 1024], writes=[wb])
            for fc in range(8):
                col = (v * 8 + fc) * 2
                for kc in range(8):
                    cx.op("pe", "matmul", PS[0][:, col:col + 2], wb[:, kc, 128 * fc:128 * fc + 128], CACT[:, kc, :],
                          start=(kc == 0), stop=(kc == 7), reads=[wb, CACT], writes=[PS[0]])
        cx.op("dve", "tensor_tensor", out=MODC[:], in0=PS[0][:, 0:96].rearrange("p (a b) -> p a b", b=2),
              in1=vec(0, 48).unsqueeze(2).to_broadcast([128, 48, 2]), op=ALU.add, reads=[PS[0], CFB], writes=[MODC])
        def gb(c0):
            return vec(c0, c0 + 8).unsqueeze(2).to_broadcast([128, 8, 2])
        cx.op("dve", "scalar_tensor_tensor", out=DERV[:, 0], in0=MODC[:, 8:16, :], scalar=1.0, in1=gb(48),
              op0=ALU.add, op1=ALU.mult, reads=[MODC, CFB], writes=[DERV])
        cx.op("dve", "tensor_copy", out=DERV[:, 1], in_=MODC[:, 0:8, :], reads=[MODC], writes=[DERV])
        cx.op("dve", "scalar_tensor_tensor", out=DERV[:, 2], in0=MODC[:, 32:40, :], scalar=1.0, in1=gb(64),
              op0=ALU.add, op1=ALU.mult, reads=[MODC, CFB], writes=[DERV])
        cx.op("dve", "tensor_copy", out=DERV[:, 3], in_=MODC[:, 24:32, :], reads=[MODC], writes=[DERV])
        cx.op("dve", "tensor_tensor", out=DERV[:, 4], in0=MODC[:, 16:24, :], in1=gb(56), op=ALU.mult,
              reads=[MODC, CFB], writes=[DERV])
        cx.op("dve", "tensor_tensor", out=DERV[:, 5], in0=MODC[:, 40:48, :], in1=gb(72), op=ALU.mult,
              reads=[MODC, CFB], writes=[DERV])
        cx.barrier()

        for seq in range(n_seq):
            if seq > 0:
                cx.dma("sp", CFB[:, CF_C:CF_C + 2 * S], cf_d[:, CF_C:CF_C + 2 * S], writes=[CFB])
            WIN = mem.view("win", R_WIN, [128, 8, NA], BF16)
            cx.dma("pool", WIN[:], winA_d.rearrange("(kc p) n -> p kc n", p=128), writes=[WIN])
            DG = mem.view("dg", R_PH, [128, 128], F32)
            for gi, GT_ in ((4, G1), (5, G2)):
                for fc in range(8):
                    cx.op("dve", "tensor_scalar", out=DG[:], in0=ident_f, scalar1=DERV[:, gi, fc, seq:seq + 1],
                          scalar2=None, op0=ALU.mult, reads=[CFB, DERV], writes=[DG])
                    pb = PS[fc // 4]
                    cx.op("pe", "matmul", pb[:, 128 * (fc % 4):128 * (fc % 4) + 128], ONESF[:], DG[:],
                          start=True, stop=True, reads=[ONESF, DG], writes=[pb])
                    if fc % 4 == 3:
                        cx.op("act", "copy", out=GT_[:, 512 * (fc // 4):512 * (fc // 4) + 512], in_=pb[:],
                              reads=[pb], writes=[GT_])
            cx.barrier()
            XIN = [mem.view("xin%d" % i, R_PH + 4 * KB * i, [128, 1024], F32) for i in range(2)]
            XS = [mem.view("xs%d" % i, R_PH + 8 * KB + 4 * KB * i, [128, 1024], F32) for i in range(2)]
            JUNK = mem.view("junk", R_PH + 16 * KB, [128, 1024], F32)
            SS = [mem.view("ss%d" % i, R_PH + 20 * KB + 64 * i, [128, 4], F32) for i in range(2)]
            for i in range(NT):
                xi, xs, ss = XIN[i % 2], XS[i % 2], SS[i % 2]
                cx.dma("sp", xi[:], x_d[seq, 128 * i:128 * i + 128, :], writes=[xi])
                cx.op("act", "activation", out=JUNK[:], in_=xi[:], func=AF.Square, accum_out=ss[:, 0:1],
                      reads=[xi], writes=[JUNK, ss])
                cx.op("act", "activation", out=ss[:, 1:2], in_=ss[:, 0:1], func=AF.Sqrt, scale=1.0 / D, bias=1e-6,
                      reads=[ss], writes=[ss])
                cx.op("dve", "reciprocal", out=ss[:, 2:3], in_=ss[:, 1:2], reads=[ss], writes=[ss])
                cx.op("dve", "tensor_scalar", out=xs[:], in0=xi[:], scalar1=ss[:, 2:3], scalar2=None, op0=ALU.mult,
                      reads=[xi, ss], writes=[xs])
                for fc in range(8):
                    pb = PS[2 * (i % 2) + fc // 4]
                    cx.op("pe", "transpose", pb[:, 128 * (fc % 4):128 * (fc % 4) + 128],
                          xs[:, 128 * fc:128 * fc + 128], ident_f, reads=[xs, CFB], writes=[pb])
                for fc in range(8):
                    pb = PS[2 * (i % 2) + fc // 4]
                    cx.op("act", "activation", out=HT[:, fc, 128 * i:128 * i + 128],
                          in_=pb[:, 128 * (fc % 4):128 * (fc % 4) + 128], func=AF.Identity,
                          scale=DERV[:, 0, fc, seq:seq + 1], bias=DERV[:, 1, fc, seq:seq + 1],
                          reads=[pb, DERV], writes=[HT])
            cx.barrier()

            o = R_PH
            QN = mem.view("QN", o, [128, 4, 2048], BF16); o += 16 * KB
            KS = mem.view("KS", o, [128, 2, 2048], BF16); o += 8 * KB
            KW = mem.view("KW", o, [128, 2, 2048], BF16); o += 8 * KB
            KCR = mem.view("KCR", o, [128, 2048], BF16); o += 4 * KB
            VCR = mem.view("VCR", o, [128, 2048], BF16); o += 4 * KB
            VT = mem.view("VT", o, [128, 16, 4, 65], BF16); o += 8320
            GT = mem.view("GT", o, [128, 16, 24], F32); o += 1536
            o_T1 = o
            T1 = mem.view("T1", o, [128, 512], F32); o += 2048
            T2 = mem.view("T2", o, [128, 512], F32); o += 2048
            assert o <= TOT, o
            ucnt = [0]

            def proj_unit(WINb, ca, cb_, M, tc, dst_ap, dstbuf):
                u = ucnt[0]; ucnt[0] += 1
                pa, pb = PS[2 * (u % 2)], PS[2 * (u % 2) + 1]
                for kc in range(8):
                    cx.op("pe", "matmul", pa[0:M, :], WINb[:, kc, ca:ca + M], HT[:, kc, 512 * tc:512 * tc + 512],
                          start=(kc == 0), stop=(kc == 7), reads=[WINb, HT], writes=[pa])
                if cb_ is None:
                    cx.op("act", "copy", out=dst_ap, in_=pa[0:M, :], reads=[pa], writes=[dstbuf])
                    return
                for kc in range(8):
                    cx.op("pe", "matmul", pb[0:M, :], WINb[:, kc, cb_:cb_ + M], HT[:, kc, 512 * tc:512 * tc + 512],
                          start=(kc == 0), stop=(kc == 7), reads=[WINb, HT], writes=[pb])
                cx.op("dve", "tensor_tensor", out=T1[0:M, :], in0=pa[0:M, :], in1=ropeC[0:M, 512 * tc:512 * tc + 512],
                      op=ALU.mult, reads=[pa, CFB], writes=[T1])
                cx.op("dve", "tensor_tensor", out=T2[0:M, :], in0=pb[0:M, :], in1=ropeS[0:M, 512 * tc:512 * tc + 512],
                      op=ALU.mult, reads=[pb, CFB], writes=[T2])
                cx.op("pool", "tensor_tensor", out=dst_ap, in0=T1[0:M, :], in1=T2[0:M, :], op=ALU.add,
                      reads=[T1, T2], writes=[dstbuf])

            cx.op("pool", "memset", VT[:, :, :, 64:65], 1.0, writes=[VT])
            for tc in range(4):
                sl = slice(512 * tc, 512 * tc + 512)
                for j in range(4):
                    proj_unit(WIN, 256 * j, 256 * j + 128, 128, tc, QN[:, j, sl], QN)
                proj_unit(WIN, 1024, 1152, 128, tc, KCR[:, sl], KCR)
                proj_unit(WIN, 1280, None, 128, tc, VCR[:, sl], VCR)
                for g in range(2):
                    proj_unit(WIN, 1408 + 256 * g, 1536 + 256 * g, 128, tc, KS[:, g, sl], KS)
                    proj_unit(WIN, 1920 + 256 * g, 2048 + 256 * g, 128, tc, KW[:, g, sl], KW)
            for i in range(NT):
                pb = PS[4 + i % 2]
                for kc in range(8):
                    cx.op("pe", "matmul", pb[:, 0:280], HT[:, kc, 128 * i:128 * i + 128], WIN[:, kc, NA_FM:NA],
                          start=(kc == 0), stop=(kc == 7), reads=[HT, WIN], writes=[pb])
                cx.op("act", "copy", out=VT[:, i, :, 0:64], in_=pb[:, 0:256].rearrange("p (a b) -> p a b", a=4),
                      reads=[pb], writes=[VT])
                cx.op("act", "activation", out=GT[:, i, :], in_=pb[:, 256:280], func=AF.Sigmoid, reads=[pb], writes=[GT])
            cx.barrier()

            o = R_WIN
            W1 = mem.view("W1", o, [128, 2, 32, 128], BF16); o += 16 * KB
            WSM = mem.view("WSM", o, [128, 1216], BF16); o += 2432
            IMT = mem.view("IMT", o, [128, 2, 16, 32], F32); o += 4096
            PT = []
            for i in range(4):
                PT.append(mem.view("PT%d" % i, o, [128, 512], BF16)); o += 1024
            XG = mem.view("XG", o, [128, 128], F32); o += 512
            UU = mem.view("UU", o, [128, 128], F32); o += 512
            HIDT = mem.view("HIDT", o, [128, 128], BF16); o += 256
            KCT = mem.view("KCT", o, [128, 2, 128], BF16); o += 512
            VCX = mem.view("VCX", o, [128, 2, 98], BF16); o += 392
            B2V = mem.view("B2V", o, [128, 64], F32); o += 256
            BIAS1 = mem.view("BIAS1", o, [128, 2], F32); o += 8
            OA = mem.view("OA", o, [128, 4, 512], F32); o += 8192
            OAB = mem.view("OAB", o_T1, [128, 4, 512], BF16)
            IMPS = []
            for g in range(2):
                IMPS.append(mem.view("IMP%d" % g, o, [128, 4, 32], F32)); o += 512
            TMPI = mem.view("TMPI", o, [128, 4, 32], F32); o += 512
            IMPM = mem.view("IMPM", o, [128, 4, 32], F32); o += 512
            TOP8 = mem.view("TOP8", o, [128, 4, 8], F32); o += 128
            NSELB = mem.view("NSELB", o, [128, 4, 32], BF16); o += 256
            NSELT = mem.view("NSELT", o, [128, 2, 512], BF16); o += 2048
            TMPO = mem.view("TMPO", o, [128, 4, 64], F32); o += 1024
            assert o <= R_PH, o
            RINV = Buf(SMALL[:, 0:4], "RINV")
            COEF = Buf(SMALL[:, 4:8], "COEF")

            for kv in range(2):
                src = w1_d[kv].rearrange("(l d) j -> d l j", d=64)
                cx.dma("pool", W1[0:64, kv], src, writes=[W1])
                cx.dma("pool", W1[64:128, kv], src, writes=[W1])
            cx.dma("pool", WSM[:], wsm_d, writes=[WSM])
            cx.dma("sp", IMT[:].rearrange("p a b c -> p (a b c)"), imt_d, writes=[IMT])
            cx.dma("sp", B2V[:], b2v_d.partition_broadcast(128), writes=[B2V])
            cx.op("pool", "memset", HIDT[:], 0.0, writes=[HIDT])
            cx.op("pool", "memset", NSELT[:], 0.0, writes=[NSELT])
            cx.op("pool", "memset", VCX[:, :, 64:65], 1.0, writes=[VCX])
            for g in range(2):
                cx.op("pool", "tensor_copy", out=VCX[:, g, 65:97], in_=CBB[:, CB_OV:CB_OV + 32], reads=[CBB], writes=[VCX])
            for kv in range(2):
                for l in range(32):
                    cx.op("pe", "matmul", PS[6][:, kv:kv + 1], W1[0:64, kv, l, :], CBB[0:64, CB_PE + 32 * kv + l:CB_PE + 32 * kv + l + 1],
                          start=(l == 0), stop=(l == 31), reads=[W1, CBB], writes=[PS[6]])
            cx.op("dve", "tensor_tensor", out=BIAS1[:], in0=PS[6][:, 0:2], in1=vec(80, 82), op=ALU.add,
                  reads=[PS[6], CFB], writes=[BIAS1])
            for kv in range(2):
                for g in range(2):
                    srcb = KCR if kv == 0 else VCR
                    hps = PS[4 + g]
                    for l in range(32):
                        cx.op("pe", "matmul", hps[:, 0:127], W1[64 * g:64 * g + 64, kv, l, :],
                              srcb[64 * g:64 * g + 64, l:l + 2017:16], start=(l == 0), stop=(l == 31),
                              reads=[W1, srcb], writes=[hps])
                    cx.op("act", "activation", out=XG[:, 0:127], in_=hps[:, 0:127], func=AF.Identity,
                          bias=BIAS1[:, kv:kv + 1], reads=[hps, BIAS1], writes=[XG])
                    cx.op("dve", "tensor_tensor", out=UU[:, 0:127], in0=XG[:, 0:127], in1=XG[:, 0:127], op=ALU.mult,
                          reads=[XG], writes=[UU])
                    cx.op("dve", "tensor_scalar", out=UU[:, 0:127], in0=UU[:, 0:127], scalar1=0.044715, scalar2=1.0,
                          op0=ALU.mult, op1=ALU.add, reads=[UU], writes=[UU])
                    cx.op("dve", "tensor_tensor", out=UU[:, 0:127], in0=UU[:, 0:127], in1=XG[:, 0:127], op=ALU.mult,
                          reads=[UU, XG], writes=[UU])
                    cx.op("act", "activation", out=UU[:, 0:127], in_=UU[:, 0:127], func=AF.Sigmoid, scale=1.5957691216057308,
                          reads=[UU], writes=[UU])
                    cx.op("dve", "tensor_tensor", out=HIDT[:, 0:127], in0=XG[:, 0:127], in1=UU[:, 0:127], op=ALU.mult,
                          reads=[XG, UU], writes=[HIDT])
                    if kv == 0:
                        cx.op("pe", "matmul", PS[6][:, 0:128], WSM[:, 1024:1152], HIDT[:], start=True, stop=True,
                              reads=[WSM, HIDT], writes=[PS[6]])
                        cx.op("act", "activation", out=KCT[:, g, :], in_=PS[6][:, 0:128], func=AF.Identity,
                              bias=vec(82, 83), reads=[PS[6], CFB], writes=[KCT])
                    else:
                        cx.op("pe", "matmul", PS[6][:, 0:64], HIDT[:], WSM[:, 1152:1216], start=True, stop=True,
                              reads=[WSM, HIDT], writes=[PS[6]])
                        cx.op("dve", "tensor_tensor", out=VCX[:, g, 0:64], in0=PS[6][:, 0:64], in1=B2V[:], op=ALU.add,
                              reads=[PS[6], B2V], writes=[VCX])

            pipe = {"pend": [], "u": 0, "job": 0}
            SCB = [PS[0], PS[1], PS[4], PS[5]]
            grp = []

            def unit(kT, qT, extras, V, acc, ncols, first, rk, rq, rv, after=None, mmask=None, ex128=(), last=False):
                u = pipe["u"]; pipe["u"] += 1
                grp.append(dict(u=u, kT=kT, qT=qT, ex=extras, ex128=ex128, V=V, acc=acc, ncols=ncols, first=first,
                                rk=rk, rq=rq, rv=rv, after=after, mmask=mmask, last=last))
                if len(grp) == (1 if 'G1' in _DBG else 2):
                    emit_group()

            def emit_group():
                if not grp:
                    return
                for d in grp:
                    sbk = SCB[d["u"] % 4]
                    nex = len(d["ex"]) + len(d["ex128"])
                    cx.op("pe", "matmul", sbk[:], d["kT"], d["qT"], start=True, stop=(nex == 0),
                          reads=[d["rk"], d["rq"]], writes=[sbk])
                for d in grp:
                    sbk = SCB[d["u"] % 4]
                    nex = len(d["ex"]) + len(d["ex128"])
                    for n_, (l_, r_, c0, c1, rd) in enumerate(d["ex"]):
                        cx.op("pe", "matmul", sbk[:, c0:c1], l_, r_, start=False, stop=(n_ == nex - 1), reads=rd, writes=[sbk])
                for d in grp:
                    sbk = SCB[d["u"] % 4]
                    nex = len(d["ex"]) + len(d["ex128"])
                    for n_, (l_, r_, c0, c1, rd) in enumerate(d["ex128"]):
                        cx.op("pe", "matmul", sbk[:, c0:c1], l_, r_, start=False, stop=(len(d["ex"]) + n_ == nex - 1),
                              reads=rd, writes=[sbk])
                for pvf in pipe["pend"]:
                    pvf()
                pipe["pend"] = []
                for d in grp:
                    sbk = SCB[d["u"] % 4]
                    pt = PT[d["u"] % 4]
                    cx.op("act", "activation", out=pt[:], in_=sbk[:], func=AF.Exp, scale=0.125, reads=[sbk], writes=[pt])
                    if d["mmask"] is not None and 'NM' not in _DBG:
                        cx.op("dve", "tensor_tensor", out=pt[:], in0=pt[:], in1=d["mmask"][0], op=ALU.mult,
                              reads=[pt, d["mmask"][1]], writes=[pt])

                    def pv(d=d, pt=pt):
                        for j in range(4):
                            cx.op("pe", "matmul", d["acc"][:, d["ncols"] * j:d["ncols"] * j + d["ncols"]],
                                  pt[:, 128 * j:128 * j + 128], d["V"], start=(d["first"] and j == 0),
                                  stop=(True if 'ST' in _DBG else (d["last"] and j == 3)), reads=[pt, d["rv"]], writes=[d["acc"]])
                        if d["after"] is not None:
                            d["after"]()
                    pipe["pend"].append(pv)
                grp.clear()

            def flush():
                emit_group()
                for pvf in pipe["pend"]:
                    pvf()
                pipe["pend"] = []

            def next_acc():
                a = PS[2 + pipe["job"] % 2]
                pipe["job"] += 1
                return a

            def nsa_final(acc, ncols, c, h, br, first_branch):
                accv = acc[:, 0:4 * ncols].rearrange("p (j n) -> p j n", j=4)

                def f():
                    cx.op("dve", "tensor_scalar", out=RINV[:], in0=accv[:, :, 64], scalar1=1e-30, scalar2=None,
                          op0=ALU.max, reads=[acc], writes=[RINV])
                    cx.op("dve", "reciprocal", out=RINV[:], in_=RINV[:], reads=[RINV], writes=[RINV])
                    if br == 0:
                        fi = (h % 4 == 0)
                        IMP = IMPS[h // 4]
                        dst = IMP if fi else TMPI
                        cx.op("dve", "tensor_tensor", out=dst[:], in0=accv[:, :, 65:97],
                              in1=RINV[:].unsqueeze(2).to_broadcast([128, 4, 32]), op=ALU.mult,
                              reads=[acc, RINV], writes=[dst])
                        if not fi:
                            cx.op("pool", "tensor_tensor", out=IMP[:], in0=IMP[:], in1=TMPI[:], op=ALU.add,
                                  reads=[IMP, TMPI], writes=[IMP])
                    cx.op("dve", "tensor_tensor", out=COEF[:], in0=RINV[:], in1=GT[:, 4 * c:4 * c + 4, 8 * br + h],
                          op=ALU.mult, reads=[RINV, GT], writes=[COEF])
                    cb3 = COEF[:].unsqueeze(2).to_broadcast([128, 4, 64])
                    if first_branch:
                        cx.op("dve", "tensor_tensor", out=OA[:, :, 64 * h:64 * h + 64], in0=accv[:, :, 0:64], in1=cb3,
                              op=ALU.mult, reads=[acc, COEF], writes=[OA])
                    else:
                        cx.op("dve", "tensor_tensor", out=TMPO[:], in0=accv[:, :, 0:64], in1=cb3, op=ALU.mult,
                              reads=[acc, COEF], writes=[TMPO])
                        cx.op("pool", "tensor_tensor", out=OA[:, :, 64 * h:64 * h + 64], in0=OA[:, :, 64 * h:64 * h + 64],
                              in1=TMPO[:], op=ALU.add, reads=[OA, TMPO], writes=[OA])
                return f

            def to_OT(SRC, c, fc0):
                for fc in range(4):
                    for j in range(4):
                        col = ((fc % 2) * 4 + j) * 128
                        cx.op("pe", "transpose", psb(6 + fc // 2)[:, col:col + 128], SRC[:, j, 128 * fc:128 * fc + 128],
                              ident_b, reads=[SRC, CBB], writes=[PS[6 + fc // 2]])
                for fc in range(4):
                    cx.op("act", "copy", out=OT[:, fc0 + fc, 512 * c:512 * c + 512],
                          in_=psb(6 + fc // 2)[:, (fc % 2) * 512:(fc % 2) * 512 + 512], reads=[PS[6 + fc // 2]], writes=[OT])

            for c in range(4):
                qs = slice(512 * c, 512 * c + 512)
                for h in range(8):
                    g, b_ = h // 4, 64 * (h % 2)
                    acc = next_acc()
                    unit(KCT[b_:b_ + 64, g, :], QN[b_:b_ + 64, h // 2, qs],
                         [], VCX[:, g, 0:97], acc, 97, True,
                         KCT, QN, VCX, after=nsa_final(acc, 97, c, h, 0, True), mmask=(CMN[:, qs], CMN), last=True)
                for h in range(8):
                    g, b_ = h // 4, 64 * (h % 2)
                    acc = next_acc()
                    tiles = list(range(max(0, 4 * c - 4), 4 * c + 4))
                    for n_, i in enumerate(tiles):
                        mk = MSK[:, 3 + (4 * c - i), :] if i < 4 * c else MSK[:, i - 4 * c, :]
                        unit(KW[b_:b_ + 64, g, 128 * i:128 * i + 128], QN[b_:b_ + 64, h // 2, qs],
                             [], VT[:, i, 2 + g, :], acc, 65, n_ == 0, KW, QN, VT,
                             after=(nsa_final(acc, 65, c, h, 2, False) if n_ == len(tiles) - 1 else None), mmask=(mk, MSK),
                             last=(n_ == len(tiles) - 1))
                flush()
                for g in range(2):
                    IMPg = IMPS[g]
                    cx.op("dve", "tensor_tensor", out=IMPM[:], in0=IMPg[:], in1=IMT[:, 0, 4 * c:4 * c + 4, :], op=ALU.mult,
                          reads=[IMPg, IMT], writes=[IMPM])
                    cx.op("dve", "tensor_tensor", out=IMPM[:], in0=IMPM[:], in1=IMT[:, 1, 4 * c:4 * c + 4, :], op=ALU.add,
                          reads=[IMPM, IMT], writes=[IMPM])
                    for j in range(4):
                        cx.op("dve", "max", out=TOP8[:, j, :], in_=IMPM[:, j, :], reads=[IMPM], writes=[TOP8])
                    for j in range(4):
                        cx.op("dve", "tensor_scalar", out=NSELB[:, j, :], in0=IMPM[:, j, :], scalar1=TOP8[:, j, 7:8],
                              scalar2=NEG, op0=ALU.is_lt, op1=ALU.mult, reads=[IMPM, TOP8], writes=[NSELB])
                    for j in range(4):
                        cx.op("pe", "transpose", psb(6)[0:32, 128 * j:128 * j + 128], NSELB[:, j, :], ident_b,
                              reads=[NSELB, CBB], writes=[PS[6]])
                    cx.op("act", "copy", out=NSELT[0:32, g, :], in_=psb(6)[0:32, 0:512], reads=[PS[6]], writes=[NSELT])
                for h in range(8):
                    g, b_ = h // 4, 64 * (h % 2)
                    acc = next_acc()
                    tiles = list(range(0, 4 * c + 4))
                    for n_, i in enumerate(tiles):
                        KX = 32
                        ex = [(CBB[0:KX, CB_EX + 128 * i:CB_EX + 128 * i + 128], NSELT[0:KX, g, :], 0, 512, [CBB, NSELT])]
                        unit(KS[b_:b_ + 64, g, 128 * i:128 * i + 128], QN[b_:b_ + 64, h // 2, qs], ex,
                             VT[:, i, g, :], acc, 65, n_ == 0, KS, QN, VT,
                             after=(nsa_final(acc, 65, c, h, 1, False) if n_ == len(tiles) - 1 else None),
                             mmask=((MSK[:, i - 4 * c, :], MSK) if i >= 4 * c else None), last=(n_ == len(tiles) - 1))
                flush()
                cx.op("act", "copy", out=OAB[:], in_=OA[:], reads=[OA], writes=[OAB])
                to_OT(OAB, c, 0)
            cx.barrier()

            WINB = mem.view("winb", R_WIN, [128, 8, NB], BF16)
            cx.dma("pool", WINB[:], winB_d.rearrange("(kc p) n -> p kc n", p=128), writes=[WINB])
            o = R_PH
            QD = mem.view("QD", o, [128, 4, 2048], BF16); o += 16 * KB
            QI = mem.view("QI", o, [128, 4, 2048], BF16); o += 16 * KB
            KI = mem.view("KI", o, [128, 2048], BF16); o += 4 * KB
            CKT = mem.view("CKT", o, [128, 2048], BF16); o += 4 * KB
            KRT = mem.view("KRT", o, [128, 2048], BF16); o += 4 * KB
            WI = mem.view("WI", o, [128, 16, 8], F32); o += 512
            T1 = mem.view("T1", o, [128, 512], F32); o_JB = o; o += 2048
            T2 = mem.view("T2", o, [128, 512], F32); o += 2048
            CKN = []
            for i in range(2):
                CKN.append(mem.view("CKN%d" % i, o, [128, 128], F32)); o += 512
            o_RB = o
            assert o + 4096 + 128 <= TOT, o
            SSD = Buf(SMALL[:, 56:60], "SSD")
            for tc in range(4):
                sl = slice(512 * tc, 512 * tc + 512)
                for j in range(4):
                    proj_unit(WINB, 256 * j, 256 * j + 128, 128, tc, QD[:, j, sl], QD)
                for j in range(4):
                    proj_unit(WINB, 1024 + 256 * j, 1024 + 256 * j + 128, 128, tc, QI[:, j, sl], QI)
                proj_unit(WINB, 2048, 2176, 128, tc, KI[:, sl], KI)
                proj_unit(WINB, 2304, 2320, 16, tc, KRT[0:16, sl], KRT)
            for i in range(NT):
                pb = PS[4 + i % 2]
                ck = CKN[i % 2]
                for kc in range(8):
                    cx.op("pe", "matmul", pb[:, 0:136], HT[:, kc, 128 * i:128 * i + 128], WINB[:, kc, NB_FM:NB],
                          start=(kc == 0), stop=(kc == 7), reads=[HT, WINB], writes=[pb])
                cx.op("act", "activation", out=ck[:], in_=pb[:, 0:128], func=AF.Square, accum_out=SSD[:, 0:1],
                      reads=[pb], writes=[ck, SSD])
                cx.op("act", "activation", out=SSD[:, 1:2], in_=SSD[:, 0:1], func=AF.Sqrt, scale=1.0 / 128, bias=1e-6,
                      reads=[SSD], writes=[SSD])
                cx.op("dve", "reciprocal", out=SSD[:, 2:3], in_=SSD[:, 1:2], reads=[SSD], writes=[SSD])
                cx.op("dve", "tensor_scalar", out=ck[:], in0=pb[:, 0:128], scalar1=SSD[:, 2:3], scalar2=None, op0=ALU.mult,
                      reads=[pb, SSD], writes=[ck])
                cx.op("act", "mul", out=WI[:, i, :], in_=pb[:, 128:136], mul=float(8 ** -0.5 * 64 ** -0.5), reads=[pb], writes=[WI])
                cx.op("pe", "transpose", PS[6][:, 128 * (i % 4):128 * (i % 4) + 128], ck[:], ident_f, reads=[ck, CFB], writes=[PS[6]])
                if i % 4 == 3:
                    cx.op("act", "activation", out=CKT[:, 512 * (i // 4):512 * (i // 4) + 512], in_=PS[6][:], func=AF.Identity,
                          scale=vec(83, 84), reads=[PS[6], CFB], writes=[CKT])
            cx.barrier()

            o = R_WIN
            KHT = mem.view("KHT", o, [128, 4, 2048], BF16); o += 16 * KB
            VH = mem.view("VH", o, [128, 16, 8, 65], BF16); o += 16640
            WSM = mem.view("WSM", o, [128, 1216], BF16); o += 2432
            PT = []
            for i in range(4):
                PT.append(mem.view("PT%d" % i, o, [128, 512], BF16)); o += 1024
            ODB = mem.view("ODB", o, [128, 4, 512], BF16); o += 4096
            assert o <= R_PH, o
            NMS = [[mem.view("NMA%d" % j, R_HT + 3 * KB * j, [128, 1536], BF16) for j in range(4)],
                   [mem.view("NMB%d" % j, CF_C * 4 + 4 * KB * j, [128, 2048], BF16) for j in range(4)]]
            IB = [mem.view("IB%d" % j, R_HT + 12 * KB + 8 * KB * j, [128, 2048], F32) for j in range(2)]
            RB = [mem.view("RB%d" % j, o_RB + 2048 * j, [128, 512], F32) for j in range(2)]
            BS = []
            for n2, c0 in enumerate((8, 40)):
                BS.append(dict(LO=Buf(SMALL[:, c0:c0 + 1], "LO%d" % n2), MID=Buf(SMALL[:, c0 + 1:c0 + 2], "MID%d" % n2),
                               CNT=Buf(SMALL[:, c0 + 2:c0 + 3], "CNT%d" % n2), TMPS=Buf(SMALL[:, c0 + 3:c0 + 4], "TMPS%d" % n2),
                               W0=Buf(SMALL[:, c0 + 4:c0 + 5], "W0%d" % n2), MN=Buf(SMALL[:, c0 + 5:c0 + 6], "MN%d" % n2),
                               MX8=Buf(SMALL[:, c0 + 6:c0 + 14], "MX8%d" % n2),
                               WK=mem.view("WK%d" % n2, o_RB + 4096 + 64 * n2, [128, 16], F32),
                               JB=Buf(mem.view("JBx%d" % n2, o_JB + 2048 * n2, [128, 1024], BF16).ap.bitcast(mybir.dt.uint8), "JB%d" % n2)))
            cx.dma("pool", WSM[:], wsm_d, writes=[WSM])
            cx.op("pool", "memset", VH[:, :, :, 64:65], 1.0, writes=[VH])
            n_ = 0
            for j in range(4):
                for tc in range(4):
                    pb = PS[4 + n_ % 2]; n_ += 1
                    cx.op("pe", "matmul", pb[:], WSM[:, 128 * j:128 * j + 128], CKT[:, 512 * tc:512 * tc + 512],
                          start=True, stop=False, reads=[WSM, CKT], writes=[pb])
                    cx.op("pe", "matmul", pb[:], CBB[0:16, CB_SEL:CB_SEL + 128], KRT[0:16, 512 * tc:512 * tc + 512],
                          start=False, stop=True, reads=[CBB, KRT], writes=[pb])
                    cx.op("act", "copy", out=KHT[:, j, 512 * tc:512 * tc + 512], in_=pb[:], reads=[pb], writes=[KHT])
            for i in range(NT):
                pb = PS[4 + n_ % 2]; n_ += 1
                cx.op("pe", "matmul", pb[:], CKT[:, 128 * i:128 * i + 128], WSM[:, 512:1024], start=True, stop=True,
                      reads=[CKT, WSM], writes=[pb])
                cx.op("act", "copy", out=VH[:, i, :, 0:64], in_=pb[:].rearrange("p (h d) -> p h d", h=8), reads=[pb], writes=[VH])

            pipe["pend"] = []
            grp.clear()
            def idx_scores(c, j):
                T = 4 * c + j
                Wc = 512 * (c + 1)
                Wv = 128 * (T + 1)
                IBt = IB[T % 2]
                for sc in range(c + 1):
                    N = min(512, Wv - 512 * sc)
                    for h in range(8):
                        b_ = 64 * (h % 2)
                        L = PS[6 + lcnt[0] % 2]; lcnt[0] += 1
                        cx.op("pe", "matmul", L[:, 0:N], QI[b_:b_ + 64, h // 2, 128 * T:128 * T + 128],
                              KI[b_:b_ + 64, 512 * sc:512 * sc + N], start=True, stop=True, reads=[QI, KI], writes=[L])
                        if h == 0:
                            cx.op("dve", "tensor_scalar", out=IBt[:, 512 * sc:512 * sc + N], in0=L[:, 0:N], scalar1=0.0,
                                  scalar2=WI[:, T, h:h + 1], op0=ALU.max, op1=ALU.mult, reads=[L, WI], writes=[IBt])
                        else:
                            rb = RB[h % 2]
                            cx.op("dve", "tensor_scalar", out=rb[:, 0:N], in0=L[:, 0:N], scalar1=0.0,
                                  scalar2=WI[:, T, h:h + 1], op0=ALU.max, op1=ALU.mult, reads=[L, WI], writes=[rb])
                            cx.op("pool", "tensor_tensor", out=IBt[:, 512 * sc:512 * sc + N], in0=IBt[:, 512 * sc:512 * sc + N],
                                  in1=rb[:, 0:N], op=ALU.add, reads=[IBt, rb], writes=[IBt])
                MX8, MN = BS[j % 2]["MX8"], BS[j % 2]["MN"]
                if T >= 2:
                    cx.op("dve", "max", out=MX8[:], in_=IBt[:, 0:Wv], reads=[IBt], writes=[MX8])
                    cx.op("dve", "tensor_reduce", out=MN[:], in_=IBt[:, 0:Wv], axis=AX.X, op=ALU.min, reads=[IBt], writes=[MN])
                cx.op("pool", "affine_select", out=IBt[:, 128 * T:128 * T + 128], in_=IBt[:, 128 * T:128 * T + 128],
                      pattern=[[-1, 128]], compare_op=ALU.is_ge, fill=-3.0e38, base=0, channel_multiplier=1,
                      reads=[IBt], writes=[IBt])
                if Wv < Wc:
                    cx.op("pool", "memset", IBt[:, Wv:Wc], -3.0e38, writes=[IBt])

            def idx_bisect_pair(c, js):
                Wc = 512 * (c + 1)
                chains = []
                for j in js:
                    T = 4 * c + j
                    chains.append((j, T, 128 * (T + 1), IB[T % 2], BS[j % 2]))
                act_ = [ch for ch in chains if ch[1] >= 2]
                for (j, T, Wv, IBt, B) in chains:
                    if T >= 2:
                        cx.op("dve", "tensor_copy", out=B["LO"][:], in_=B["MN"][:], reads=[B["MN"]], writes=[B["LO"]])
                        cx.op("dve", "tensor_tensor", out=B["W0"][:], in0=B["MX8"][:, 0:1], in1=B["MN"][:], op=ALU.subtract,
                              reads=[B["MX8"], B["MN"]], writes=[B["W0"]])
                        cx.op("dve", "tensor_scalar", out=B["WK"][:, 0:BIS_ITERS], in0=CFB[:, CF_P2:CF_P2 + BIS_ITERS],
                              scalar1=B["W0"][:], scalar2=None, op0=ALU.mult, reads=[CFB, B["W0"]], writes=[B["WK"]])
                    else:
                        cx.op("dve", "memset", B["LO"][:], -1.0e30, writes=[B["LO"]])
                for k in range(BIS_ITERS):
                    for (j, T, Wv, IBt, B) in act_:
                        cx.op("dve", "tensor_tensor", out=B["MID"][:], in0=B["LO"][:], in1=B["WK"][:, k:k + 1], op=ALU.add,
                              reads=[B["LO"], B["WK"]], writes=[B["MID"]])
                    for (j, T, Wv, IBt, B) in act_:
                        cx.op("dve", "tensor_scalar", out=B["JB"][:, 0:Wv], in0=IBt[:, 0:Wv], scalar1=B["MID"][:], scalar2=0.0,
                              op0=ALU.is_ge, op1=ALU.add, accum_out=B["CNT"][:], reads=[IBt, B["MID"]], writes=[B["JB"], B["CNT"]])
                    for (j, T, Wv, IBt, B) in act_:
                        cx.op("dve", "tensor_scalar", out=B["TMPS"][:], in0=B["CNT"][:], scalar1=255.5, scalar2=B["WK"][:, k:k + 1],
                              op0=ALU.is_ge, op1=ALU.mult, reads=[B["CNT"], B["WK"]], writes=[B["TMPS"]])
                    for (j, T, Wv, IBt, B) in act_:
                        cx.op("dve", "tensor_tensor", out=B["LO"][:], in0=B["LO"][:], in1=B["TMPS"][:], op=ALU.add,
                              reads=[B["LO"], B["TMPS"]], writes=[B["LO"]])
                for (j, T, Wv, IBt, B) in chains:
                    nm = NMS[c % 2][j]
                    cx.op("dve", "tensor_scalar", out=nm[:, 0:Wc], in0=IBt[:, 0:Wc], scalar1=B["LO"][:], scalar2=NEG,
                          op0=ALU.is_lt, op1=ALU.mult, reads=[IBt, B["LO"]], writes=[nm])

            def idx_slices(c):
                return [lambda: idx_scores(c, 0), lambda: idx_scores(c, 1), lambda: idx_bisect_pair(c, (0, 1)), lambda: None,
                        lambda: idx_scores(c, 2), lambda: idx_scores(c, 3), lambda: idx_bisect_pair(c, (2, 3)), lambda: None]

            lcnt = [0]
            for f_ in idx_slices(0):
                f_()
            for c in range(4):
                qs = slice(512 * c, 512 * c + 512)
                NMc = NMS[c % 2]
                sl_next = idx_slices(c + 1) if c < 3 else []
                for h in range(8):
                    if sl_next:
                        sl_next[h]()
                    b_ = 64 * (h % 2)
                    acc = next_acc()
                    accv = acc[:, 0:260].rearrange("p (j n) -> p j n", j=4)

                    def fin(acc=acc, accv=accv, h=h):
                        cx.op("dve", "reciprocal", out=RINV[:], in_=accv[:, :, 64], reads=[acc], writes=[RINV])
                        cx.op("dve", "tensor_tensor", out=ODB[:, :, 64 * h:64 * h + 64], in0=accv[:, :, 0:64],
                              in1=RINV[:].unsqueeze(2).to_broadcast([128, 4, 64]), op=ALU.mult,
                              reads=[acc, RINV], writes=[ODB])
                    tiles = list(range(0, 4 * c + 4))
                    for q_, i in enumerate(tiles):
                        ex = [(NMc[j][:, 128 * i:128 * i + 128], ident_b, 128 * j, 128 * j + 128, [NMc[j], CBB]) for j in range(4)]
                        unit(KHT[b_:b_ + 64, h // 2, 128 * i:128 * i + 128], QD[b_:b_ + 64, h // 2, qs], [],
                             VH[:, i, h, :], acc, 65, q_ == 0, KHT, QD, VH, after=(fin if q_ == len(tiles) - 1 else None),
                             ex128=ex, last=(q_ == len(tiles) - 1))
                flush()
                to_OT(ODB, c, 4)
            cx.barrier()

            o = R_HT
            X1 = mem.view("X1", o, [128, 4, 1024], F32); o += 16 * KB
            H2T = mem.view("H2T", o, [128, 8, 512], BF16); o += 8 * KB
            ACTT = mem.view("ACTT", o, [128, 22, 512], BF16); o += 22 * KB
            WOUT = mem.view("WOUT", CF_C * 4, [128, 8, 1024], BF16)
            WG = []
            for i in range(3):
                WG.append(mem.view("WG%d" % i, o, [128, 8, 256], BF16)); o += 4 * KB
            WDN = mem.view("WDN", o, [128, 22, 1024], BF16); o += 44 * KB
            XIN = []
            for i in range(2):
                XIN.append(mem.view("xin%d" % i, o, [128, 1024], F32)); o += 4 * KB
            JUNK = mem.view("junk", o, [128, 1024], F32); o += 4 * KB
            XS = mem.view("xs", o, [128, 1024], F32); o += 4 * KB
            TMPY = mem.view("tmpy", o, [128, 1024], F32); o += 4 * KB
            SIL = []
            for i in range(2):
                SIL.append(mem.view("sil%d" % i, o, [128, 512], F32)); o += 2 * KB
            assert o <= TOT, o
            ST = Buf(SMALL[:, 44:52], "ST")
            cx.dma("pool", WDN[:], wdn_d.rearrange("(c p) n -> p c n", p=128), writes=[WDN])
            wout_v = wout_d.rearrange("(kc p) n -> p kc n", p=128)

            cx.dma("pool", WOUT[:], wout_v, writes=[WOUT])
            for gi in range(4):
                for j in range(4):
                    T = 4 * gi + j
                    xi = XIN[j % 2]
                    cx.dma("sp", xi[:], x_d[seq, 128 * T:128 * T + 128, :], writes=[xi])
                    yb = [PS[2 * (j % 2)], PS[2 * (j % 2) + 1]]
                    for n in range(2):
                        for kc in range(8):
                            cx.op("pe", "matmul", yb[n][:], OT[:, kc, 128 * T:128 * T + 128], WOUT[:, kc, 512 * n:512 * n + 512],
                                  start=(kc == 0), stop=(kc == 7), reads=[OT, WOUT], writes=[yb[n]])
                    for n in range(2):
                        cx.op("act", "activation", out=JUNK[:, 512 * n:512 * n + 512], in_=yb[n][:], func=AF.Square,
                              accum_out=ST[:, n:n + 1], reads=[yb[n]], writes=[JUNK, ST])
                    cx.op("dve", "tensor_tensor", out=ST[:, 2:3], in0=ST[:, 0:1], in1=ST[:, 1:2], op=ALU.add, reads=[ST], writes=[ST])
                    cx.op("act", "activation", out=ST[:, 3:4], in_=ST[:, 2:3], func=AF.Sqrt, scale=1.0 / D, bias=1e-6, reads=[ST], writes=[ST])
                    cx.op("dve", "reciprocal", out=ST[:, 4:5], in_=ST[:, 3:4], reads=[ST], writes=[ST])
                    for n in range(2):
                        cx.op("dve", "scalar_tensor_tensor", out=TMPY[:, 512 * n:512 * n + 512], in0=yb[n][:], scalar=ST[:, 4:5],
                              in1=G1[:, 512 * n:512 * n + 512], op0=ALU.mult, op1=ALU.mult, reads=[yb[n], ST, G1], writes=[TMPY])
                    cx.op("pool", "tensor_tensor", out=X1[:, j, :], in0=TMPY[:], in1=xi[:], op=ALU.add, reads=[TMPY, xi], writes=[X1])
                    cx.op("act", "activation", out=JUNK[:], in_=X1[:, j, :], func=AF.Square, accum_out=ST[:, 0:1],
                          reads=[X1], writes=[JUNK, ST])
                    cx.op("act", "activation", out=ST[:, 3:4], in_=ST[:, 0:1], func=AF.Sqrt, scale=1.0 / D, bias=1e-6, reads=[ST], writes=[ST])
                    cx.op("dve", "reciprocal", out=ST[:, 4:5], in_=ST[:, 3:4], reads=[ST], writes=[ST])
                    cx.op("dve", "tensor_scalar", out=XS[:], in0=X1[:, j, :], scalar1=ST[:, 4:5], scalar2=None, op0=ALU.mult,
                          reads=[X1, ST], writes=[XS])
                    for fc in range(8):
                        pb = PS[4 + fc // 4]
                        cx.op("pe", "transpose", pb[:, 128 * (fc % 4):128 * (fc % 4) + 128], XS[:, 128 * fc:128 * fc + 128],
                              ident_f, reads=[XS, CFB], writes=[pb])
                    for fc in range(8):
                        pb = PS[4 + fc // 4]
                        cx.op("act", "activation", out=H2T[:, fc, 128 * j:128 * j + 128],
                              in_=pb[:, 128 * (fc % 4):128 * (fc % 4) + 128], func=AF.Identity,
                              scale=DERV[:, 2, fc, seq:seq + 1], bias=DERV[:, 3, fc, seq:seq + 1],
                              reads=[pb, DERV], writes=[H2T])
                for ch in range(22):
                    wg = WG[ch % 3]
                    cx.dma("pool", wg[:].rearrange("p a b -> p (a b)"), wgu_d[ch], writes=[wg])
                    pg, pu = PS[2 * (ch % 2)], PS[2 * (ch % 2) + 1]
                    for kc in range(8):
                        cx.op("pe", "matmul", pg[:], wg[:, kc, 0:128], H2T[:, kc, :], start=(kc == 0), stop=(kc == 7),
                              reads=[wg, H2T], writes=[pg])
                    for kc in range(8):
                        cx.op("pe", "matmul", pu[:], wg[:, kc, 128:256], H2T[:, kc, :], start=(kc == 0), stop=(kc == 7),
                              reads=[wg, H2T], writes=[pu])
                    sl_ = SIL[ch % 2]
                    cx.op("act", "activation", out=sl_[:], in_=pg[:], func=AF.Silu, reads=[pg], writes=[sl_])
                    cx.op("dve", "tensor_tensor", out=ACTT[:, ch, :], in0=sl_[:], in1=pu[:], op=ALU.mult,
                          reads=[sl_, pu], writes=[ACTT])
                for j in range(4):
                    T = 4 * gi + j
                    zb = [PS[4 + 2 * (j % 2)], PS[5 + 2 * (j % 2)]]
                    for n in range(2):
                        for ch in range(22):
                            cx.op("pe", "matmul", zb[n][:], ACTT[:, ch, 128 * j:128 * j + 128], WDN[:, ch, 512 * n:512 * n + 512],
                                  start=(ch == 0), stop=(ch == 21), reads=[ACTT, WDN], writes=[zb[n]])
                    for n in range(2):
                        cx.op("act", "activation", out=JUNK[:, 512 * n:512 * n + 512], in_=zb[n][:], func=AF.Square,
                              accum_out=ST[:, n:n + 1], reads=[zb[n]], writes=[JUNK, ST])
                    cx.op("dve", "tensor_tensor", out=ST[:, 2:3], in0=ST[:, 0:1], in1=ST[:, 1:2], op=ALU.add, reads=[ST], writes=[ST])
                    cx.op("act", "activation", out=ST[:, 3:4], in_=ST[:, 2:3], func=AF.Sqrt, scale=1.0 / D, bias=1e-6, reads=[ST], writes=[ST])
                    cx.op("dve", "reciprocal", out=ST[:, 4:5], in_=ST[:, 3:4], reads=[ST], writes=[ST])
                    for n in range(2):
                        cx.op("dve", "scalar_tensor_tensor", out=TMPY[:, 512 * n:512 * n + 512], in0=zb[n][:], scalar=ST[:, 4:5],
                              in1=G2[:, 512 * n:512 * n + 512], op0=ALU.mult, op1=ALU.mult, reads=[zb[n], ST, G2], writes=[TMPY])
                    cx.op("pool", "tensor_tensor", out=XS[:], in0=TMPY[:], in1=X1[:, j, :], op=ALU.add, reads=[TMPY, X1], writes=[XS])
                    cx.dma("sp", out_d[seq, 128 * T:128 * T + 128, :], XS[:], reads=[XS])
            cx.barrier()

        cx.barrier()
        cx.finish()
    return nc


def _prep(inputs):
    inp = {k: np.asarray(v) for k, v in inputs.items()}
    cf, cb, imt, wsm = _host_consts(inp)
    A, B = _col_index()
    shared = {
        "w_ada": np.ascontiguousarray(inp['w_ada'][0]),
        "cf": cf, "cb": cb, "imt": imt, "wsm": wsm,
        "w_inA": np.ascontiguousarray(inp['w_in'][0][:, A]),
        "w_inB": np.ascontiguousarray(inp['w_in'][0][:, B]),
        "cmp_w1": np.ascontiguousarray(inp['cmp_w1'][0]),
        "b2v": np.ascontiguousarray(inp['cmp_b2'][0, 1]),
        "w_out": np.ascontiguousarray(inp['w_out'][0]),
        "w_gate_up": np.ascontiguousarray(
            inp['w_gate_up'][0].reshape(8, 128, 2, 22, 128).transpose(3, 1, 0, 2, 4).reshape(22, 128, 2048)),
        "w_down": np.ascontiguousarray(inp['w_down'][0]),
    }
    maps = []
    for c in range(8):
        m = dict(shared)
        m["x"] = np.ascontiguousarray(inp['x'][2 * c:2 * c + 2])
        m["cT"] = np.ascontiguousarray(inp['c'][2 * c:2 * c + 2].T.reshape(8, 128, 2).transpose(1, 0, 2))
        maps.append(m)
    return maps


def kernel(**inputs):
    maps = _prep(inputs)
    nc = build()
    res = run_bass_kernel_spmd(nc, maps, core_ids=list(range(8)))
    return np.concatenate([r["out"] for r in res.results], axis=0).astype(np.float32)
```

```python
import numpy as np
import concourse.bass as bass
import concourse.mybir as mybir
from concourse.bass_utils import run_bass_kernel_spmd
from contextlib import ExitStack

F32 = mybir.dt.float32
BF16 = mybir.dt.bfloat16
ALU = mybir.AluOpType
AF = mybir.ActivationFunctionType
AX = mybir.AxisListType

S = 2048
D = 1024
NT = 16
DFF = 2816
NEG = -30000.0
IN_SPLITS = (512, 128, 128, 128, 128, 128, 128, 24, 512, 128, 16, 512, 64, 8)
NAMES = ['nq', 'nkc', 'nvc', 'nks', 'nvs', 'nkw', 'nvw', 'ngate', 'dq', 'dckv', 'dkr', 'iq', 'ik', 'iw']
OFF = dict(zip(NAMES, np.cumsum((0,) + IN_SPLITS)[:-1]))
NA_FM = 19 * 128
NA = NA_FM + 280
NB_FM = 18 * 128 + 32
NB = NB_FM + 136
BIS_ITERS = 12
import os as _os
_DBG = _os.environ.get('KDBG', '')


class _E:
    def __init__(self, name, eng, sem):
        self.name, self.eng, self.sem, self.tick, self.waited = name, eng, sem, 0, {}


class _St:
    __slots__ = ("w", "r")

    def __init__(self):
        self.w = None
        self.r = {}


class Buf:
    def __init__(self, ap, key):
        self.ap = ap
        self.key = key

    def __getitem__(self, idx):
        return self.ap[idx]


class Ctx:
    def __init__(self, nc, es, n_dma_sems=8):
        self.nc, self.es = nc, es
        self.E = {}
        for name, eng in (("pe", nc.tensor), ("act", nc.scalar), ("dve", nc.vector),
                          ("pool", nc.gpsimd), ("sp", nc.sync)):
            self.E[name] = _E(name, eng, es.enter_context(nc.semaphore("s_" + name)))
        self.dsems = {q: [[es.enter_context(nc.semaphore("d_%s%d" % (q, i))), 0] for i in range(n_dma_sems)]
                      for q in ("sp", "pool")}
        self.dnext = {"sp": 0, "pool": 0}
        self.st = {}

    def _state(self, b):
        k = b.key if isinstance(b, Buf) else (b if isinstance(b, str) else b.name)
        s = self.st.get(k)
        if s is None:
            s = self.st[k] = _St()
        return s

    def _wait(self, E, dep):
        kind, tk = dep
        if kind == E.name and E.name == "pe":
            return
        if E.waited.get(kind, 0) >= tk:
            return
        sem = self.dsems[kind[0]][kind[1]][0] if isinstance(kind, tuple) else self.E[kind].sem
        E.eng.wait_ge(sem, tk)
        E.waited[kind] = tk

    def _deps(self, E, reads, writes):
        deps = []
        for b in reads:
            s = self._state(b)
            if s.w is not None:
                deps.append(s.w)
        for b in writes:
            s = self._state(b)
            if s.w is not None:
                deps.append(s.w)
            deps.extend(s.r.items())
        for d in deps:
            self._wait(E, d)

    def _mark(self, token, reads, writes):
        for b in reads:
            s = self._state(b)
            if s.r.get(token[0], 0) < token[1]:
                s.r[token[0]] = token[1]
        for b in writes:
            s = self._state(b)
            s.w = token
            s.r = {}

    def op(self, en, fn, *args, reads=(), writes=(), **kw):
        E = self.E[en]
        self._deps(E, reads, writes)
        ins = getattr(E.eng, fn)(*args, **kw)
        E.tick += 1
        ins.then_inc(E.sem, 1)
        self._mark((en, E.tick), reads, writes)
        return ins

    def dma(self, q, out, in_, reads=(), writes=(), **kw):
        E = self.E[q]
        self._deps(E, reads, writes)
        i = self.dnext[q]
        self.dnext[q] = (i + 1) % len(self.dsems[q])
        slot = self.dsems[q][i]
        kind = (q, i)
        if slot[1] > 0:
            self._wait(E, (kind, slot[1]))
        slot[1] += 16
        E.eng.dma_start(out=out, in_=in_, **kw).then_inc(slot[0], 16)
        self._mark((kind, slot[1]), reads, writes)

    def barrier(self):
        toks = [(n, e.tick) for n, e in self.E.items() if e.tick > 0]
        for q in self.dsems:
            for i, slot in enumerate(self.dsems[q]):
                if slot[1] > 0:
                    toks.append(((q, i), slot[1]))
        for n, e in self.E.items():
            for t in toks:
                if t[0] == n:
                    if n != "sp" and e.waited.get(n, 0) < t[1]:
                        e.eng.wait_ge(e.sem, t[1])
                        e.waited[n] = t[1]
                else:
                    self._wait(e, t)
        self.st = {}

    def finish(self):
        E = self.E["sp"]
        for q in self.dsems:
            for i, slot in enumerate(self.dsems[q]):
                if slot[1] > 0:
                    self._wait(E, ((q, i), slot[1]))


class Mem:
    def __init__(self, big):
        self.big = big

    def view(self, key, off, shape, dt, pbase=0):
        esz = 4 if dt == F32 else 2
        nel = int(np.prod(shape[1:]))
        assert off % 4 == 0
        a = off // 2
        n = nel * esz // 2
        ap = self.big[pbase:pbase + shape[0], a:a + n]
        if dt == F32:
            ap = ap.bitcast(F32)
        if len(shape) == 3:
            ap = ap.rearrange("p (a b) -> p a b", a=shape[1])
        elif len(shape) == 4:
            ap = ap.rearrange("p (a b c) -> p a b c", a=shape[1], b=shape[2])
        return Buf(ap, key)


def _swap_head(cols):
    c = np.array(cols).copy()
    c[0:8] = cols[8:16]
    c[8:16] = cols[0:8]
    return c


def _col_index():
    def head(base, h):
        return np.arange(base + 64 * h, base + 64 * h + 64)
    A = []
    for j in range(4):
        a = np.concatenate([head(OFF['nq'], 2 * j), head(OFF['nq'], 2 * j + 1)])
        b = np.concatenate([_swap_head(head(OFF['nq'], 2 * j)), _swap_head(head(OFF['nq'], 2 * j + 1))])
        A += [a, b]
    a = np.concatenate([head(OFF['nkc'], 0), head(OFF['nkc'], 1)])
    b = np.concatenate([_swap_head(head(OFF['nkc'], 0)), _swap_head(head(OFF['nkc'], 1))])
    A += [a, b]
    A += [np.arange(OFF['nvc'], OFF['nvc'] + 128)]
    for nm in ('nks', 'nkw'):
        for g in range(2):
            a = np.concatenate([head(OFF[nm], g), head(OFF[nm], g)])
            b = np.concatenate([_swap_head(head(OFF[nm], g)), _swap_head(head(OFF[nm], g))])
            A += [a, b]
    A += [np.arange(OFF['nvs'], OFF['nvs'] + 128), np.arange(OFF['nvw'], OFF['nvw'] + 128),
          np.arange(OFF['ngate'], OFF['ngate'] + 24)]
    A = np.concatenate(A)
    assert A.size == NA
    B = []
    for nm in ('dq', 'iq'):
        for j in range(4):
            a = np.concatenate([head(OFF[nm], 2 * j), head(OFF[nm], 2 * j + 1)])
            b = np.concatenate([_swap_head(head(OFF[nm], 2 * j)), _swap_head(head(OFF[nm], 2 * j + 1))])
            B += [a, b]
    a = np.concatenate([head(OFF['ik'], 0), head(OFF['ik'], 0)])
    b = np.concatenate([_swap_head(head(OFF['ik'], 0)), _swap_head(head(OFF['ik'], 0))])
    B += [a, b]
    kr = np.arange(OFF['dkr'], OFF['dkr'] + 16)
    B += [kr, _swap_head(kr)]
    B += [np.arange(OFF['dckv'], OFF['dckv'] + 128), np.arange(OFF['iw'], OFF['iw'] + 8)]
    B = np.concatenate(B)
    assert B.size == NB
    return A, B


CF_ID = 0
CF_C = 128
CF_S = CF_C + 2048
CF_V = CF_S + 2048
CF_P2 = CF_V + 84
CF_N = CF_P2 + BIS_ITERS
CB_ID = 0
CB_EX = 128
CB_OV = CB_EX + 2048
CB_PE = CB_OV + 32
CB_SEL = CB_PE + 64
CB_N = CB_SEL + 128


def _host_consts(inp):
    cf = np.zeros((128, CF_N), np.float32)
    cf[:, CF_ID:CF_ID + 128] = np.eye(128, dtype=np.float32)
    inv_freq = 1.0 / (np.float32(500000.0) ** (np.arange(0, 16, 2, dtype=np.float32) / np.float32(16)))
    ang = np.arange(S, dtype=np.float32)[:, None] * inv_freq[None, :].astype(np.float32)
    cos, sin = np.cos(ang).astype(np.float32), np.sin(ang).astype(np.float32)
    for p in range(128):
        j = p % 64
        if j < 16:
            cf[p, CF_C:CF_C + S] = cos[:, j % 8]
            cf[p, CF_S:CF_S + S] = (-1.0 if j < 8 else 1.0) * sin[:, j % 8]
        else:
            cf[p, CF_C:CF_C + S] = 1.0
    v = cf[:, CF_V:CF_V + 84]
    v[:, 0:48] = inp['b_ada'][0].reshape(48, 128).T
    v[:, 48:56] = inp['g_pre_mix'][0].reshape(8, 128).T
    v[:, 56:64] = inp['g_post_mix'][0].reshape(8, 128).T
    v[:, 64:72] = inp['g_pre_ffn'][0].reshape(8, 128).T
    v[:, 72:80] = inp['g_post_ffn'][0].reshape(8, 128).T
    v[:, 80] = inp['cmp_b1'][0, 0]
    v[:, 81] = inp['cmp_b1'][0, 1]
    v[:, 82] = np.concatenate([inp['cmp_b2'][0, 0], inp['cmp_b2'][0, 0]])
    v[:, 83] = inp['g_kv_norm'][0]
    cf[:, CF_P2:CF_P2 + BIS_ITERS] = (0.5 ** np.arange(1, BIS_ITERS + 1, dtype=np.float64)).astype(np.float32)[None, :]
    cb = np.zeros((128, CB_N), np.float32)
    cb[:, CB_ID:CB_ID + 128] = np.eye(128, dtype=np.float32)
    for j in range(32):
        cb[j, CB_EX + 64 * j:CB_EX + 64 * j + 64] = 1.0
    ci = np.arange(127)[:, None] * 16
    sj = np.arange(32)[None, :] * 64
    cb[0:127, CB_OV:CB_OV + 32] = ((ci < sj + 64) & (ci + 32 > sj)).astype(np.float32)
    cb[0:64, CB_PE:CB_PE + 32] = inp['cmp_pe'][0, 0].T
    cb[0:64, CB_PE + 32:CB_PE + 64] = inp['cmp_pe'][0, 1].T
    for i in range(16):
        cb[i, CB_SEL + i] = 1.0
        cb[i, CB_SEL + 64 + i] = 1.0
    t = (np.arange(16)[None, :, None] * 128 + np.arange(128)[:, None, None])
    blk = t // 64
    j = np.arange(32)[None, None, :]
    visible = j <= blk
    forced = (j == 0) | (j == blk) | (j == blk - 1)
    mm = (visible & ~forced).astype(np.float32)
    ba = np.where(visible, np.where(forced, 1e6, 0.0), -1e30).astype(np.float32)
    imt = np.concatenate([mm.reshape(128, 512), ba.reshape(128, 512)], axis=1)
    wuk = np.zeros((128, 4, 128), np.float32)
    for h in range(8):
        wuk[:, h // 2, (h % 2) * 64 + 16:(h % 2) * 64 + 64] = inp['w_uk'][0, h]
    wuv = np.transpose(inp['w_uv'][0], (1, 0, 2)).reshape(128, 512)
    w2 = np.concatenate([inp['cmp_w2'][0, 0], inp['cmp_w2'][0, 0], inp['cmp_w2'][0, 1]], axis=1)
    wsm = np.concatenate([wuk.reshape(128, 512), wuv, w2], axis=1).astype(np.float32)
    return cf, cb, imt, wsm


def build(n_seq=2, stage=99, dbg_cols=0):
    nc = bass.Bass("TRN2", target_bir_lowering=False)
    dt_ = nc.dram_tensor
    x_d = dt_("x", [2, S, D], F32, kind="ExternalInput").ap()
    cT_d = dt_("cT", [128, 8, 2], F32, kind="ExternalInput").ap()
    wada_d = dt_("w_ada", [D, 6 * D], F32, kind="ExternalInput").ap()
    cf_d = dt_("cf", [128, CF_N], F32, kind="ExternalInput").ap()
    cb_d = dt_("cb", [128, CB_N], F32, kind="ExternalInput").ap()
    imt_d = dt_("imt", [128, 1024], F32, kind="ExternalInput").ap()
    wsm_d = dt_("wsm", [128, 1216], F32, kind="ExternalInput").ap()
    winA_d = dt_("w_inA", [D, NA], F32, kind="ExternalInput").ap()
    winB_d = dt_("w_inB", [D, NB], F32, kind="ExternalInput").ap()
    w1_d = dt_("cmp_w1", [2, 2048, 128], F32, kind="ExternalInput").ap()
    b2v_d = dt_("b2v", [64], F32, kind="ExternalInput").ap()
    wout_d = dt_("w_out", [D, D], F32, kind="ExternalInput").ap()
    wgu_d = dt_("w_gate_up", [22, 128, 2048], F32, kind="ExternalInput").ap()
    wdn_d = dt_("w_down", [DFF, D], F32, kind="ExternalInput").ap()
    out_d = dt_("out", [2, S, D], F32, kind="ExternalOutput").ap()
    wgubf_d = dt_("wgu_bf16", [22, 128, 2048], BF16, kind="Internal").ap()
    dbg_d = dt_("dbg", [128, dbg_cols], F32, kind="ExternalOutput").ap() if dbg_cols else None

    with ExitStack() as es:
        cx = Ctx(nc, es)
        TOT = 206 * 1024
        big = es.enter_context(nc.sbuf_tensor("big", [128, TOT // 2], BF16))
        mem = Mem(big)
        PS = [Buf(es.enter_context(nc.psum_tensor("ps%d" % i, [128, 512], F32))[:], "ps%d" % i) for i in range(8)]

        def psb(i):
            return PS[i].ap.bitcast(BF16)

        KB = 1024
        o = 0
        CFB = mem.view("cf", o, [128, CF_N], F32); o += CF_N * 4
        CBB = mem.view("cb", o, [128, CB_N], BF16); o += CB_N * 2
        MSK = mem.view("msk", o, [128, 8, 512], BF16); o += 8 * 512 * 2
        CMN = mem.view("cmn", o, [128, 2048], BF16); o += 2048 * 2
        ONESF = mem.view("onesf", o, [128, 128], F32); o += 512
        MODC = mem.view("modc", o, [128, 48, 2], F32); o += 384
        DERV = mem.view("derv", o, [128, 6, 8, 2], F32); o += 384
        CACT = mem.view("cact", o, [128, 8, 2], F32); o += 64
        SMALL = mem.view("small", o, [128, 64], F32); o += 256
        G1 = mem.view("G1", o, [128, 1024], F32); o += 4096
        G2 = mem.view("G2", o, [128, 1024], F32); o += 4096
        assert o <= 44 * KB, o
        R_OT = 44 * KB
        R_HT = 76 * KB
        R_WIN = 108 * KB
        R_PH = 152 * KB
        OT = mem.view("OT", R_OT, [128, 8, 2048], BF16)
        HT = mem.view("HT", R_HT, [128, 8, 2048], BF16)

        ident_f = CFB[:, CF_ID:CF_ID + 128]
        ident_b = CBB[:, CB_ID:CB_ID + 128]
        ropeC = CFB[:, CF_C:CF_C + S]
        ropeS = CFB[:, CF_S:CF_S + S]
        vec = lambda c0, c1: CFB[:, CF_V + c0:CF_V + c1]

        def dbg_dump(ap, col0, ncols, rd):
            if dbg_d is not None:
                cx.dma("pool", dbg_d[0:ap.shape[0], col0:col0 + ncols], ap, reads=[rd])

        cx.dma("sp", CFB[:], cf_d, writes=[CFB])
        cx.dma("pool", CBB[:], cb_d, writes=[CBB])
        cx.dma("sp", CACT[:], cT_d, writes=[CACT])
        cx.op("pool", "memset", MSK[:], 0.0, writes=[MSK])
        for k in range(4):
            cx.op("pool", "affine_select", out=MSK[:, k, :], in_=MSK[:, k, :], pattern=[[-1, 512]],
                  compare_op=ALU.is_gt, fill=1.0, base=128 * k, channel_multiplier=1, reads=[MSK], writes=[MSK])
        for k in range(1, 5):
            cx.op("pool", "affine_select", out=MSK[:, 3 + k, :], in_=MSK[:, 3 + k, :], pattern=[[1, 512]],
                  compare_op=ALU.is_ge, fill=1.0, base=-512 + 128 * k, channel_multiplier=-1, reads=[MSK], writes=[MSK])
        cx.op("pool", "memset", CMN[:], 0.0, writes=[CMN])
        cx.op("pool", "affine_select", out=CMN[:], in_=CMN[:], pattern=[[-1, 2048]],
              compare_op=ALU.is_gt, fill=1.0, base=31, channel_multiplier=16, reads=[CMN], writes=[CMN])
        cx.op("pool", "memset", ONESF[:], 1.0, writes=[ONESF])
        cx.op("act", "activation", out=CACT[:], in_=CACT[:], func=AF.Silu, reads=[CACT], writes=[CACT])
        WA = [mem.view("wa0", R_HT, [128, 8, 1024], F32), mem.view("wa1", R_WIN, [128, 8, 1024], F32)]
        wada_v = wada_d.rearrange("(kc p) n -> p kc n", p=128)
        for v in range(6):
            wb = WA[v % 2]
            for kc in range(8):
                cx.dma("sp", wb[:, kc, :], wada_v[:, kc, 1024 * v:1024 * v + 1024], writes=[wb])
            for fc in range(8):
                col = (v * 8 + fc) * 2
                for kc in range(8):
                    cx.op("pe", "matmul", PS[0][:, col:col + 2], wb[:, kc, 128 * fc:128 * fc + 128], CACT[:, kc, :],
                          start=(kc == 0), stop=(kc == 7), reads=[wb, CACT], writes=[PS[0]])
        cx.op("dve", "tensor_tensor", out=MODC[:], in0=PS[0][:, 0:96].rearrange("p (a b) -> p a b", b=2),
              in1=vec(0, 48).unsqueeze(2).to_broadcast([128, 48, 2]), op=ALU.add, reads=[PS[0], CFB], writes=[MODC])
        def gb(c0):
            return vec(c0, c0 + 8).unsqueeze(2).to_broadcast([128, 8, 2])
        cx.op("dve", "scalar_tensor_tensor", out=DERV[:, 0], in0=MODC[:, 8:16, :], scalar=1.0, in1=gb(48),
              op0=ALU.add, op1=ALU.mult, reads=[MODC, CFB], writes=[DERV])
        cx.op("dve", "tensor_copy", out=DERV[:, 1], in_=MODC[:, 0:8, :], reads=[MODC], writes=[DERV])
        cx.op("dve", "scalar_tensor_tensor", out=DERV[:, 2], in0=MODC[:, 32:40, :], scalar=1.0, in1=gb(64),
              op0=ALU.add, op1=ALU.mult, reads=[MODC, CFB], writes=[DERV])
        cx.op("dve", "tensor_copy", out=DERV[:, 3], in_=MODC[:, 24:32, :], reads=[MODC], writes=[DERV])
        cx.op("dve", "tensor_tensor", out=DERV[:, 4], in0=MODC[:, 16:24, :], in1=gb(56), op=ALU.mult,
              reads=[MODC, CFB], writes=[DERV])
        cx.op("dve", "tensor_tensor", out=DERV[:, 5], in0=MODC[:, 40:48, :], in1=gb(72), op=ALU.mult,
              reads=[MODC, CFB], writes=[DERV])
        cx.barrier()

        cvsems = []
        cv_waited = [False]
        for seq in range(n_seq):
            if seq > 0:
                cx.dma("sp", CFB[:, CF_C:CF_C + 2 * S], cf_d[:, CF_C:CF_C + 2 * S], writes=[CFB])
            WIN = mem.view("win", R_WIN, [128, 8, NA], BF16)
            cx.dma("pool", WIN[:], winA_d.rearrange("(kc p) n -> p kc n", p=128), writes=[WIN])
            DG = mem.view("dg", R_PH, [128, 128], F32)
            for gi, GT_ in ((4, G1), (5, G2)):
                for fc in range(8):
                    cx.op("dve", "tensor_scalar", out=DG[:], in0=ident_f, scalar1=DERV[:, gi, fc, seq:seq + 1],
                          scalar2=None, op0=ALU.mult, reads=[CFB, DERV], writes=[DG])
                    pb = PS[fc // 4]
                    cx.op("pe", "matmul", pb[:, 128 * (fc % 4):128 * (fc % 4) + 128], ONESF[:], DG[:],
                          start=True, stop=True, reads=[ONESF, DG], writes=[pb])
                    if fc % 4 == 3:
                        cx.op("act", "copy", out=GT_[:, 512 * (fc // 4):512 * (fc // 4) + 512], in_=pb[:],
                              reads=[pb], writes=[GT_])
            cx.barrier()
            XIN = [mem.view("xin%d" % i, R_PH + 4 * KB * i, [128, 1024], F32) for i in range(2)]
            XS = [mem.view("xs%d" % i, R_PH + 8 * KB + 4 * KB * i, [128, 1024], F32) for i in range(2)]
            JUNK = mem.view("junk", R_PH + 16 * KB, [128, 1024], F32)
            SS = [mem.view("ss%d" % i, R_PH + 20 * KB + 64 * i, [128, 4], F32) for i in range(2)]
            for i in range(NT):
                xi, xs, ss = XIN[i % 2], XS[i % 2], SS[i % 2]
                cx.dma("sp", xi[:], x_d[seq, 128 * i:128 * i + 128, :], writes=[xi])
                cx.op("act", "activation", out=JUNK[:], in_=xi[:], func=AF.Square, accum_out=ss[:, 0:1],
                      reads=[xi], writes=[JUNK, ss])
                cx.op("act", "activation", out=ss[:, 1:2], in_=ss[:, 0:1], func=AF.Sqrt, scale=1.0 / D, bias=1e-6,
                      reads=[ss], writes=[ss])
                cx.op("dve", "reciprocal", out=ss[:, 2:3], in_=ss[:, 1:2], reads=[ss], writes=[ss])
                cx.op("dve", "tensor_scalar", out=xs[:], in0=xi[:], scalar1=ss[:, 2:3], scalar2=None, op0=ALU.mult,
                      reads=[xi, ss], writes=[xs])
                for fc in range(8):
                    pb = PS[2 * (i % 2) + fc // 4]
                    cx.op("pe", "transpose", pb[:, 128 * (fc % 4):128 * (fc % 4) + 128],
                          xs[:, 128 * fc:128 * fc + 128], ident_f, reads=[xs, CFB], writes=[pb])
                for fc in range(8):
                    pb = PS[2 * (i % 2) + fc // 4]
                    cx.op("act", "activation", out=HT[:, fc, 128 * i:128 * i + 128],
                          in_=pb[:, 128 * (fc % 4):128 * (fc % 4) + 128], func=AF.Identity,
                          scale=DERV[:, 0, fc, seq:seq + 1], bias=DERV[:, 1, fc, seq:seq + 1],
                          reads=[pb, DERV], writes=[HT])
            cx.barrier()

            o = R_PH
            QN = mem.view("QN", o, [128, 4, 2048], BF16); o += 16 * KB
            KS = mem.view("KS", o, [128, 2, 2048], BF16); o += 8 * KB
            KW = mem.view("KW", o, [128, 2, 2048], BF16); o += 8 * KB
            KCR = mem.view("KCR", o, [128, 2048], BF16); o += 4 * KB
            VCR = mem.view("VCR", o, [128, 2048], BF16); o += 4 * KB
            VT = mem.view("VT", o, [128, 16, 4, 65], BF16); o += 8320
            GT = mem.view("GT", o, [128, 16, 24], F32); o += 1536
            o_T1 = o
            T1 = mem.view("T1", o, [128, 512], F32); o += 2048
            T2 = mem.view("T2", o, [128, 512], F32); o += 2048
            assert o <= TOT, o
            ucnt = [0]

            def proj_unit(WINb, ca, cb_, M, tc, dst_ap, dstbuf):
                u = ucnt[0]; ucnt[0] += 1
                pa, pb = PS[2 * (u % 2)], PS[2 * (u % 2) + 1]
                for kc in range(8):
                    cx.op("pe", "matmul", pa[0:M, :], WINb[:, kc, ca:ca + M], HT[:, kc, 512 * tc:512 * tc + 512],
                          start=(kc == 0), stop=(kc == 7), reads=[WINb, HT], writes=[pa])
                if cb_ is None:
                    cx.op("act", "copy", out=dst_ap, in_=pa[0:M, :], reads=[pa], writes=[dstbuf])
                    return
                for kc in range(8):
                    cx.op("pe", "matmul", pb[0:M, :], WINb[:, kc, cb_:cb_ + M], HT[:, kc, 512 * tc:512 * tc + 512],
                          start=(kc == 0), stop=(kc == 7), reads=[WINb, HT], writes=[pb])
                cx.op("dve", "tensor_tensor", out=T1[0:M, :], in0=pa[0:M, :], in1=ropeC[0:M, 512 * tc:512 * tc + 512],
                      op=ALU.mult, reads=[pa, CFB], writes=[T1])
                cx.op("dve", "tensor_tensor", out=T2[0:M, :], in0=pb[0:M, :], in1=ropeS[0:M, 512 * tc:512 * tc + 512],
                      op=ALU.mult, reads=[pb, CFB], writes=[T2])
                cx.op("pool", "tensor_tensor", out=dst_ap, in0=T1[0:M, :], in1=T2[0:M, :], op=ALU.add,
                      reads=[T1, T2], writes=[dstbuf])

            cx.op("pool", "memset", VT[:, :, :, 64:65], 1.0, writes=[VT])
            for tc in range(4):
                sl = slice(512 * tc, 512 * tc + 512)
                for j in range(4):
                    proj_unit(WIN, 256 * j, 256 * j + 128, 128, tc, QN[:, j, sl], QN)
                proj_unit(WIN, 1024, 1152, 128, tc, KCR[:, sl], KCR)
                proj_unit(WIN, 1280, None, 128, tc, VCR[:, sl], VCR)
                for g in range(2):
                    proj_unit(WIN, 1408 + 256 * g, 1536 + 256 * g, 128, tc, KS[:, g, sl], KS)
                    proj_unit(WIN, 1920 + 256 * g, 2048 + 256 * g, 128, tc, KW[:, g, sl], KW)
            for i in range(NT):
                pb = PS[4 + i % 2]
                for kc in range(8):
                    cx.op("pe", "matmul", pb[:, 0:280], HT[:, kc, 128 * i:128 * i + 128], WIN[:, kc, NA_FM:NA],
                          start=(kc == 0), stop=(kc == 7), reads=[HT, WIN], writes=[pb])
                cx.op("act", "copy", out=VT[:, i, :, 0:64], in_=pb[:, 0:256].rearrange("p (a b) -> p a b", a=4),
                      reads=[pb], writes=[VT])
                cx.op("act", "activation", out=GT[:, i, :], in_=pb[:, 256:280], func=AF.Sigmoid, reads=[pb], writes=[GT])
            cx.barrier()

            o = R_WIN
            W1 = mem.view("W1", o, [128, 2, 32, 128], BF16); o += 16 * KB
            WSM = mem.view("WSM", o, [128, 1216], BF16); o += 2432
            IMT = mem.view("IMT", o, [128, 2, 16, 32], F32); o += 4096
            PT = []
            for i in range(4):
                PT.append(mem.view("PT%d" % i, o, [128, 512], BF16)); o += 1024
            XG = mem.view("XG", o, [128, 128], F32); o += 512
            UU = mem.view("UU", o, [128, 128], F32); o += 512
            HIDT = mem.view("HIDT", o, [128, 128], BF16); o += 256
            KCT = mem.view("KCT", o, [128, 2, 128], BF16); o += 512
            VCX = mem.view("VCX", o, [128, 2, 98], BF16); o += 392
            B2V = mem.view("B2V", o, [128, 64], F32); o += 256
            BIAS1 = mem.view("BIAS1", o, [128, 2], F32); o += 8
            OA = mem.view("OA", o, [128, 4, 512], F32); o += 8192
            OAB = mem.view("OAB", o_T1, [128, 4, 512], BF16)
            IMPS = []
            for g in range(2):
                IMPS.append(mem.view("IMP%d" % g, o, [128, 4, 32], F32)); o += 512
            TMPI = mem.view("TMPI", o, [128, 4, 32], F32); o += 512
            IMPM = mem.view("IMPM", o, [128, 4, 32], F32); o += 512
            TOP8 = mem.view("TOP8", o, [128, 4, 8], F32); o += 128
            NSELB = mem.view("NSELB", o, [128, 4, 32], BF16); o += 256
            NSELT = mem.view("NSELT", o, [128, 2, 512], BF16); o += 2048
            TMPO = mem.view("TMPO", o, [128, 4, 64], F32); o += 1024
            assert o <= R_PH, o
            RINV = Buf(SMALL[:, 0:4], "RINV")
            COEF = Buf(SMALL[:, 4:8], "COEF")

            for kv in range(2):
                src = w1_d[kv].rearrange("(l d) j -> d l j", d=64)
                cx.dma("pool", W1[0:64, kv], src, writes=[W1])
                cx.dma("pool", W1[64:128, kv], src, writes=[W1])
            cx.dma("pool", WSM[:], wsm_d, writes=[WSM])
            cx.dma("sp", IMT[:].rearrange("p a b c -> p (a b c)"), imt_d, writes=[IMT])
            cx.dma("sp", B2V[:], b2v_d.partition_broadcast(128), writes=[B2V])
            if seq == 0:
                for i in range(11):
                    sem_cv = es.enter_context(nc.semaphore("cv%d" % i))
                    cvsems.append(sem_cv)
                    nc.gpsimd.dma_start(out=wgubf_d[2 * i:2 * i + 2].rearrange("c p n -> (c p) n"),
                                        in_=wgu_d[2 * i:2 * i + 2].rearrange("c p n -> (c p) n")).then_inc(sem_cv, 16)
            cx.op("pool", "memset", HIDT[:], 0.0, writes=[HIDT])
            cx.op("pool", "memset", NSELT[:], 0.0, writes=[NSELT])
            cx.op("pool", "memset", VCX[:, :, 64:65], 1.0, writes=[VCX])
            for g in range(2):
                cx.op("pool", "tensor_copy", out=VCX[:, g, 65:97], in_=CBB[:, CB_OV:CB_OV + 32], reads=[CBB], writes=[VCX])
            for kv in range(2):
                for l in range(32):
                    cx.op("pe", "matmul", PS[6][:, kv:kv + 1], W1[0:64, kv, l, :], CBB[0:64, CB_PE + 32 * kv + l:CB_PE + 32 * kv + l + 1],
                          start=(l == 0), stop=(l == 31), reads=[W1, CBB], writes=[PS[6]])
            cx.op("dve", "tensor_tensor", out=BIAS1[:], in0=PS[6][:, 0:2], in1=vec(80, 82), op=ALU.add,
                  reads=[PS[6], CFB], writes=[BIAS1])
            for kv in range(2):
                for g in range(2):
                    srcb = KCR if kv == 0 else VCR
                    hps = PS[4 + g]
                    for l in range(32):
                        cx.op("pe", "matmul", hps[:, 0:127], W1[64 * g:64 * g + 64, kv, l, :],
                              srcb[64 * g:64 * g + 64, l:l + 2017:16], start=(l == 0), stop=(l == 31),
                              reads=[W1, srcb], writes=[hps])
                    cx.op("act", "activation", out=XG[:, 0:127], in_=hps[:, 0:127], func=AF.Identity,
                          bias=BIAS1[:, kv:kv + 1], reads=[hps, BIAS1], writes=[XG])
                    cx.op("dve", "tensor_tensor", out=UU[:, 0:127], in0=XG[:, 0:127], in1=XG[:, 0:127], op=ALU.mult,
                          reads=[XG], writes=[UU])
                    cx.op("dve", "tensor_scalar", out=UU[:, 0:127], in0=UU[:, 0:127], scalar1=0.044715, scalar2=1.0,
                          op0=ALU.mult, op1=ALU.add, reads=[UU], writes=[UU])
                    cx.op("dve", "tensor_tensor", out=UU[:, 0:127], in0=UU[:, 0:127], in1=XG[:, 0:127], op=ALU.mult,
                          reads=[UU, XG], writes=[UU])
                    cx.op("act", "activation", out=UU[:, 0:127], in_=UU[:, 0:127], func=AF.Sigmoid, scale=1.5957691216057308,
                          reads=[UU], writes=[UU])
                    cx.op("dve", "tensor_tensor", out=HIDT[:, 0:127], in0=XG[:, 0:127], in1=UU[:, 0:127], op=ALU.mult,
                          reads=[XG, UU], writes=[HIDT])
                    if kv == 0:
                        cx.op("pe", "matmul", PS[6][:, 0:128], WSM[:, 1024:1152], HIDT[:], start=True, stop=True,
                              reads=[WSM, HIDT], writes=[PS[6]])
                        cx.op("act", "activation", out=KCT[:, g, :], in_=PS[6][:, 0:128], func=AF.Identity,
                              bias=vec(82, 83), reads=[PS[6], CFB], writes=[KCT])
                    else:
                        cx.op("pe", "matmul", PS[6][:, 0:64], HIDT[:], WSM[:, 1152:1216], start=True, stop=True,
                              reads=[WSM, HIDT], writes=[PS[6]])
                        cx.op("dve", "tensor_tensor", out=VCX[:, g, 0:64], in0=PS[6][:, 0:64], in1=B2V[:], op=ALU.add,
                              reads=[PS[6], B2V], writes=[VCX])

            pipe = {"pend": [], "u": 0, "job": 0}
            SCB = [PS[0], PS[1], PS[4], PS[5]]
            grp = []

            def unit(kT, qT, extras, V, acc, ncols, first, rk, rq, rv, after=None, mmask=None, ex128=(), last=False):
                u = pipe["u"]; pipe["u"] += 1
                grp.append(dict(u=u, kT=kT, qT=qT, ex=extras, ex128=ex128, V=V, acc=acc, ncols=ncols, first=first,
                                rk=rk, rq=rq, rv=rv, after=after, mmask=mmask, last=last))
                if len(grp) == (1 if 'G1' in _DBG else 2):
                    emit_group()

            def emit_group():
                if not grp:
                    return
                for d in grp:
                    sbk = SCB[d["u"] % 4]
                    nex = len(d["ex"]) + len(d["ex128"])
                    cx.op("pe", "matmul", sbk[:], d["kT"], d["qT"], start=True, stop=(nex == 0),
                          reads=[d["rk"], d["rq"]], writes=[sbk])
                for d in grp:
                    sbk = SCB[d["u"] % 4]
                    nex = len(d["ex"]) + len(d["ex128"])
                    for n_, (l_, r_, c0, c1, rd) in enumerate(d["ex"]):
                        cx.op("pe", "matmul", sbk[:, c0:c1], l_, r_, start=False, stop=(n_ == nex - 1), reads=rd, writes=[sbk])
                for d in grp:
                    sbk = SCB[d["u"] % 4]
                    nex = len(d["ex"]) + len(d["ex128"])
                    for n_, (l_, r_, c0, c1, rd) in enumerate(d["ex128"]):
                        cx.op("pe", "matmul", sbk[:, c0:c1], l_, r_, start=False, stop=(len(d["ex"]) + n_ == nex - 1),
                              reads=rd, writes=[sbk])
                for pvf in pipe["pend"]:
                    pvf()
                pipe["pend"] = []
                for d in grp:
                    sbk = SCB[d["u"] % 4]
                    pt = PT[d["u"] % 4]
                    cx.op("act", "activation", out=pt[:], in_=sbk[:], func=AF.Exp, scale=0.125, reads=[sbk], writes=[pt])
                    if d["mmask"] is not None and 'NM' not in _DBG:
                        cx.op("dve", "tensor_tensor", out=pt[:], in0=pt[:], in1=d["mmask"][0], op=ALU.mult,
                              reads=[pt, d["mmask"][1]], writes=[pt])

                    def pv(d=d, pt=pt):
                        for j in range(4):
                            cx.op("pe", "matmul", d["acc"][:, d["ncols"] * j:d["ncols"] * j + d["ncols"]],
                                  pt[:, 128 * j:128 * j + 128], d["V"], start=(d["first"] and j == 0),
                                  stop=(True if 'ST' in _DBG else (d["last"] and j == 3)), reads=[pt, d["rv"]], writes=[d["acc"]])
                        if d["after"] is not None:
                            d["after"]()
                    pipe["pend"].append(pv)
                grp.clear()

            def flush():
                emit_group()
                for pvf in pipe["pend"]:
                    pvf()
                pipe["pend"] = []

            def next_acc():
                a = PS[2 + pipe["job"] % 2]
                pipe["job"] += 1
                return a

            def nsa_final(acc, ncols, c, h, br, first_branch):
                accv = acc[:, 0:4 * ncols].rearrange("p (j n) -> p j n", j=4)

                def f():
                    cx.op("dve", "tensor_scalar", out=RINV[:], in0=accv[:, :, 64], scalar1=1e-30, scalar2=None,
                          op0=ALU.max, reads=[acc], writes=[RINV])
                    cx.op("dve", "reciprocal", out=RINV[:], in_=RINV[:], reads=[RINV], writes=[RINV])
                    if br == 0:
                        fi = (h % 4 == 0)
                        IMP = IMPS[h // 4]
                        dst = IMP if fi else TMPI
                        cx.op("dve", "tensor_tensor", out=dst[:], in0=accv[:, :, 65:97],
                              in1=RINV[:].unsqueeze(2).to_broadcast([128, 4, 32]), op=ALU.mult,
                              reads=[acc, RINV], writes=[dst])
                        if not fi:
                            cx.op("pool", "tensor_tensor", out=IMP[:], in0=IMP[:], in1=TMPI[:], op=ALU.add,
                                  reads=[IMP, TMPI], writes=[IMP])
                    cx.op("dve", "tensor_tensor", out=COEF[:], in0=RINV[:], in1=GT[:, 4 * c:4 * c + 4, 8 * br + h],
                          op=ALU.mult, reads=[RINV, GT], writes=[COEF])
                    cb3 = COEF[:].unsqueeze(2).to_broadcast([128, 4, 64])
                    if first_branch:
                        cx.op("dve", "tensor_tensor", out=OA[:, :, 64 * h:64 * h + 64], in0=accv[:, :, 0:64], in1=cb3,
                              op=ALU.mult, reads=[acc, COEF], writes=[OA])
                    else:
                        cx.op("dve", "tensor_tensor", out=TMPO[:], in0=accv[:, :, 0:64], in1=cb3, op=ALU.mult,
                              reads=[acc, COEF], writes=[TMPO])
                        cx.op("pool", "tensor_tensor", out=OA[:, :, 64 * h:64 * h + 64], in0=OA[:, :, 64 * h:64 * h + 64],
                              in1=TMPO[:], op=ALU.add, reads=[OA, TMPO], writes=[OA])
                return f

            def to_OT(SRC, c, fc0):
                for fc in range(4):
                    for j in range(4):
                        col = ((fc % 2) * 4 + j) * 128
                        cx.op("pe", "transpose", psb(6 + fc // 2)[:, col:col + 128], SRC[:, j, 128 * fc:128 * fc + 128],
                              ident_b, reads=[SRC, CBB], writes=[PS[6 + fc // 2]])
                for fc in range(4):
                    cx.op("act", "copy", out=OT[:, fc0 + fc, 512 * c:512 * c + 512],
                          in_=psb(6 + fc // 2)[:, (fc % 2) * 512:(fc % 2) * 512 + 512], reads=[PS[6 + fc // 2]], writes=[OT])

            for c in range(4):
                qs = slice(512 * c, 512 * c + 512)
                for h in range(8):
                    g, b_ = h // 4, 64 * (h % 2)
                    acc = next_acc()
                    unit(KCT[b_:b_ + 64, g, :], QN[b_:b_ + 64, h // 2, qs],
                         [], VCX[:, g, 0:97], acc, 97, True,
                         KCT, QN, VCX, after=nsa_final(acc, 97, c, h, 0, True), mmask=(CMN[:, qs], CMN), last=True)
                for h in range(8):
                    g, b_ = h // 4, 64 * (h % 2)
                    acc = next_acc()
                    tiles = list(range(max(0, 4 * c - 4), 4 * c + 4))
                    for n_, i in enumerate(tiles):
                        mk = MSK[:, 3 + (4 * c - i), :] if i < 4 * c else MSK[:, i - 4 * c, :]
                        unit(KW[b_:b_ + 64, g, 128 * i:128 * i + 128], QN[b_:b_ + 64, h // 2, qs],
                             [], VT[:, i, 2 + g, :], acc, 65, n_ == 0, KW, QN, VT,
                             after=(nsa_final(acc, 65, c, h, 2, False) if n_ == len(tiles) - 1 else None), mmask=(mk, MSK),
                             last=(n_ == len(tiles) - 1))
                flush()
                for g in range(2):
                    IMPg = IMPS[g]
                    cx.op("dve", "tensor_tensor", out=IMPM[:], in0=IMPg[:], in1=IMT[:, 0, 4 * c:4 * c + 4, :], op=ALU.mult,
                          reads=[IMPg, IMT], writes=[IMPM])
                    cx.op("dve", "tensor_tensor", out=IMPM[:], in0=IMPM[:], in1=IMT[:, 1, 4 * c:4 * c + 4, :], op=ALU.add,
                          reads=[IMPM, IMT], writes=[IMPM])
                    for j in range(4):
                        cx.op("dve", "max", out=TOP8[:, j, :], in_=IMPM[:, j, :], reads=[IMPM], writes=[TOP8])
                    for j in range(4):
                        cx.op("dve", "tensor_scalar", out=NSELB[:, j, :], in0=IMPM[:, j, :], scalar1=TOP8[:, j, 7:8],
                              scalar2=NEG, op0=ALU.is_lt, op1=ALU.mult, reads=[IMPM, TOP8], writes=[NSELB])
                    for j in range(4):
                        cx.op("pe", "transpose", psb(6)[0:32, 128 * j:128 * j + 128], NSELB[:, j, :], ident_b,
                              reads=[NSELB, CBB], writes=[PS[6]])
                    cx.op("act", "copy", out=NSELT[0:32, g, :], in_=psb(6)[0:32, 0:512], reads=[PS[6]], writes=[NSELT])
                for h in range(8):
                    g, b_ = h // 4, 64 * (h % 2)
                    acc = next_acc()
                    tiles = list(range(0, 4 * c + 4))
                    for n_, i in enumerate(tiles):
                        KX = 32
                        ex = [(CBB[0:KX, CB_EX + 128 * i:CB_EX + 128 * i + 128], NSELT[0:KX, g, :], 0, 512, [CBB, NSELT])]
                        unit(KS[b_:b_ + 64, g, 128 * i:128 * i + 128], QN[b_:b_ + 64, h // 2, qs], ex,
                             VT[:, i, g, :], acc, 65, n_ == 0, KS, QN, VT,
                             after=(nsa_final(acc, 65, c, h, 1, False) if n_ == len(tiles) - 1 else None),
                             mmask=((MSK[:, i - 4 * c, :], MSK) if i >= 4 * c else None), last=(n_ == len(tiles) - 1))
                flush()
                cx.op("act", "copy", out=OAB[:], in_=OA[:], reads=[OA], writes=[OAB])
                to_OT(OAB, c, 0)
            cx.barrier()

            WINB = mem.view("winb", R_WIN, [128, 8, NB], BF16)
            cx.dma("pool", WINB[:], winB_d.rearrange("(kc p) n -> p kc n", p=128), writes=[WINB])
            o = R_PH
            QD = mem.view("QD", o, [128, 4, 2048], BF16); o += 16 * KB
            QI = mem.view("QI", o, [128, 4, 2048], BF16); o += 16 * KB
            KI = mem.view("KI", o, [128, 2048], BF16); o += 4 * KB
            CKT = mem.view("CKT", o, [128, 2048], BF16); o += 4 * KB
            KRT = mem.view("KRT", o, [128, 2048], BF16); o += 4 * KB
            WI = mem.view("WI", o, [128, 16, 8], F32); o += 512
            T1 = mem.view("T1", o, [128, 512], F32); o_JB = o; o += 2048
            T2 = mem.view("T2", o, [128, 512], F32); o += 2048
            CKN = []
            for i in range(2):
                CKN.append(mem.view("CKN%d" % i, o, [128, 128], F32)); o += 512
            o_RB = o
            assert o + 4096 + 128 <= TOT, o
            SSD = Buf(SMALL[:, 56:60], "SSD")
            for tc in range(4):
                sl = slice(512 * tc, 512 * tc + 512)
                for j in range(4):
                    proj_unit(WINB, 256 * j, 256 * j + 128, 128, tc, QD[:, j, sl], QD)
                for j in range(4):
                    proj_unit(WINB, 1024 + 256 * j, 1024 + 256 * j + 128, 128, tc, QI[:, j, sl], QI)
                proj_unit(WINB, 2048, 2176, 128, tc, KI[:, sl], KI)
                proj_unit(WINB, 2304, 2320, 16, tc, KRT[0:16, sl], KRT)
            for i in range(NT):
                pb = PS[4 + i % 2]
                ck = CKN[i % 2]
                for kc in range(8):
                    cx.op("pe", "matmul", pb[:, 0:136], HT[:, kc, 128 * i:128 * i + 128], WINB[:, kc, NB_FM:NB],
                          start=(kc == 0), stop=(kc == 7), reads=[HT, WINB], writes=[pb])
                cx.op("act", "activation", out=ck[:], in_=pb[:, 0:128], func=AF.Square, accum_out=SSD[:, 0:1],
                      reads=[pb], writes=[ck, SSD])
                cx.op("act", "activation", out=SSD[:, 1:2], in_=SSD[:, 0:1], func=AF.Sqrt, scale=1.0 / 128, bias=1e-6,
                      reads=[SSD], writes=[SSD])
                cx.op("dve", "reciprocal", out=SSD[:, 2:3], in_=SSD[:, 1:2], reads=[SSD], writes=[SSD])
                cx.op("dve", "tensor_scalar", out=ck[:], in0=pb[:, 0:128], scalar1=SSD[:, 2:3], scalar2=None, op0=ALU.mult,
                      reads=[pb, SSD], writes=[ck])
                cx.op("act", "mul", out=WI[:, i, :], in_=pb[:, 128:136], mul=float(8 ** -0.5 * 64 ** -0.5), reads=[pb], writes=[WI])
                cx.op("pe", "transpose", PS[6][:, 128 * (i % 4):128 * (i % 4) + 128], ck[:], ident_f, reads=[ck, CFB], writes=[PS[6]])
                if i % 4 == 3:
                    cx.op("act", "activation", out=CKT[:, 512 * (i // 4):512 * (i // 4) + 512], in_=PS[6][:], func=AF.Identity,
                          scale=vec(83, 84), reads=[PS[6], CFB], writes=[CKT])
            cx.barrier()

            o = R_WIN
            KHT = mem.view("KHT", o, [128, 4, 2048], BF16); o += 16 * KB
            VH = mem.view("VH", o, [128, 16, 8, 65], BF16); o += 16640
            WSM = mem.view("WSM", o, [128, 1216], BF16); o += 2432
            PT = []
            for i in range(4):
                PT.append(mem.view("PT%d" % i, o, [128, 512], BF16)); o += 1024
            ODB = mem.view("ODB", o, [128, 4, 512], BF16); o += 4096
            assert o <= R_PH, o
            NMS = [[mem.view("NMA%d" % j, R_HT + 3 * KB * j, [128, 1536], BF16) for j in range(4)],
                   [mem.view("NMB%d" % j, CF_C * 4 + 4 * KB * j, [128, 2048], BF16) for j in range(4)]]
            IB = [mem.view("IB%d" % j, R_HT + 12 * KB + 8 * KB * j, [128, 2048], F32) for j in range(2)]
            RB = [mem.view("RB%d" % j, o_RB + 2048 * j, [128, 512], F32) for j in range(2)]
            BS = []
            for n2, c0 in enumerate((8, 40)):
                BS.append(dict(LO=Buf(SMALL[:, c0:c0 + 1], "LO%d" % n2), MID=Buf(SMALL[:, c0 + 1:c0 + 2], "MID%d" % n2),
                               CNT=Buf(SMALL[:, c0 + 2:c0 + 3], "CNT%d" % n2), TMPS=Buf(SMALL[:, c0 + 3:c0 + 4], "TMPS%d" % n2),
                               W0=Buf(SMALL[:, c0 + 4:c0 + 5], "W0%d" % n2), MN=Buf(SMALL[:, c0 + 5:c0 + 6], "MN%d" % n2),
                               MX8=Buf(SMALL[:, c0 + 6:c0 + 14], "MX8%d" % n2),
                               WK=mem.view("WK%d" % n2, o_RB + 4096 + 64 * n2, [128, 16], F32),
                               JB=Buf(mem.view("JBx%d" % n2, o_JB + 2048 * n2, [128, 1024], BF16).ap.bitcast(mybir.dt.uint8), "JB%d" % n2)))
            cx.dma("pool", WSM[:], wsm_d, writes=[WSM])
            cx.op("pool", "memset", VH[:, :, :, 64:65], 1.0, writes=[VH])
            n_ = 0
            for j in range(4):
                for tc in range(4):
                    pb = PS[4 + n_ % 2]; n_ += 1
                    cx.op("pe", "matmul", pb[:], WSM[:, 128 * j:128 * j + 128], CKT[:, 512 * tc:512 * tc + 512],
                          start=True, stop=False, reads=[WSM, CKT], writes=[pb])
                    cx.op("pe", "matmul", pb[:], CBB[0:16, CB_SEL:CB_SEL + 128], KRT[0:16, 512 * tc:512 * tc + 512],
                          start=False, stop=True, reads=[CBB, KRT], writes=[pb])
                    cx.op("act", "copy", out=KHT[:, j, 512 * tc:512 * tc + 512], in_=pb[:], reads=[pb], writes=[KHT])
            for i in range(NT):
                pb = PS[4 + n_ % 2]; n_ += 1
                cx.op("pe", "matmul", pb[:], CKT[:, 128 * i:128 * i + 128], WSM[:, 512:1024], start=True, stop=True,
                      reads=[CKT, WSM], writes=[pb])
                cx.op("act", "copy", out=VH[:, i, :, 0:64], in_=pb[:].rearrange("p (h d) -> p h d", h=8), reads=[pb], writes=[VH])

            pipe["pend"] = []
            grp.clear()
            def idx_scores(c, j):
                T = 4 * c + j
                Wc = 512 * (c + 1)
                Wv = 128 * (T + 1)
                IBt = IB[T % 2]
                for sc in range(c + 1):
                    N = min(512, Wv - 512 * sc)
                    for h in range(8):
                        b_ = 64 * (h % 2)
                        L = PS[6 + lcnt[0] % 2]; lcnt[0] += 1
                        cx.op("pe", "matmul", L[:, 0:N], QI[b_:b_ + 64, h // 2, 128 * T:128 * T + 128],
                              KI[b_:b_ + 64, 512 * sc:512 * sc + N], start=True, stop=True, reads=[QI, KI], writes=[L])
                        if h == 0:
                            cx.op("dve", "tensor_scalar", out=IBt[:, 512 * sc:512 * sc + N], in0=L[:, 0:N], scalar1=0.0,
                                  scalar2=WI[:, T, h:h + 1], op0=ALU.max, op1=ALU.mult, reads=[L, WI], writes=[IBt])
                        else:
                            rb = RB[h % 2]
                            cx.op("dve", "tensor_scalar", out=rb[:, 0:N], in0=L[:, 0:N], scalar1=0.0,
                                  scalar2=WI[:, T, h:h + 1], op0=ALU.max, op1=ALU.mult, reads=[L, WI], writes=[rb])
                            cx.op("pool", "tensor_tensor", out=IBt[:, 512 * sc:512 * sc + N], in0=IBt[:, 512 * sc:512 * sc + N],
                                  in1=rb[:, 0:N], op=ALU.add, reads=[IBt, rb], writes=[IBt])
                MX8, MN = BS[j % 2]["MX8"], BS[j % 2]["MN"]
                if T >= 2:
                    cx.op("dve", "max", out=MX8[:], in_=IBt[:, 0:Wv], reads=[IBt], writes=[MX8])
                    cx.op("dve", "tensor_reduce", out=MN[:], in_=IBt[:, 0:Wv], axis=AX.X, op=ALU.min, reads=[IBt], writes=[MN])
                cx.op("pool", "affine_select", out=IBt[:, 128 * T:128 * T + 128], in_=IBt[:, 128 * T:128 * T + 128],
                      pattern=[[-1, 128]], compare_op=ALU.is_ge, fill=-3.0e38, base=0, channel_multiplier=1,
                      reads=[IBt], writes=[IBt])
                if Wv < Wc:
                    cx.op("pool", "memset", IBt[:, Wv:Wc], -3.0e38, writes=[IBt])

            def idx_bisect_pair(c, js):
                Wc = 512 * (c + 1)
                chains = []
                for j in js:
                    T = 4 * c + j
                    chains.append((j, T, 128 * (T + 1), IB[T % 2], BS[j % 2]))
                act_ = [ch for ch in chains if ch[1] >= 2]
                for (j, T, Wv, IBt, B) in chains:
                    if T >= 2:
                        cx.op("dve", "tensor_copy", out=B["LO"][:], in_=B["MN"][:], reads=[B["MN"]], writes=[B["LO"]])
                        cx.op("dve", "tensor_tensor", out=B["W0"][:], in0=B["MX8"][:, 0:1], in1=B["MN"][:], op=ALU.subtract,
                              reads=[B["MX8"], B["MN"]], writes=[B["W0"]])
                        cx.op("dve", "tensor_scalar", out=B["WK"][:, 0:BIS_ITERS], in0=CFB[:, CF_P2:CF_P2 + BIS_ITERS],
                              scalar1=B["W0"][:], scalar2=None, op0=ALU.mult, reads=[CFB, B["W0"]], writes=[B["WK"]])
                    else:
                        cx.op("dve", "memset", B["LO"][:], -1.0e30, writes=[B["LO"]])
                for k in range(BIS_ITERS):
                    for (j, T, Wv, IBt, B) in act_:
                        cx.op("dve", "tensor_tensor", out=B["MID"][:], in0=B["LO"][:], in1=B["WK"][:, k:k + 1], op=ALU.add,
                              reads=[B["LO"], B["WK"]], writes=[B["MID"]])
                    for (j, T, Wv, IBt, B) in act_:
                        cx.op("dve", "tensor_scalar", out=B["JB"][:, 0:Wv], in0=IBt[:, 0:Wv], scalar1=B["MID"][:], scalar2=0.0,
                              op0=ALU.is_ge, op1=ALU.add, accum_out=B["CNT"][:], reads=[IBt, B["MID"]], writes=[B["JB"], B["CNT"]])
                    for (j, T, Wv, IBt, B) in act_:
                        cx.op("dve", "tensor_scalar", out=B["TMPS"][:], in0=B["CNT"][:], scalar1=255.5, scalar2=B["WK"][:, k:k + 1],
                              op0=ALU.is_ge, op1=ALU.mult, reads=[B["CNT"], B["WK"]], writes=[B["TMPS"]])
                    for (j, T, Wv, IBt, B) in act_:
                        cx.op("dve", "tensor_tensor", out=B["LO"][:], in0=B["LO"][:], in1=B["TMPS"][:], op=ALU.add,
                              reads=[B["LO"], B["TMPS"]], writes=[B["LO"]])
                for (j, T, Wv, IBt, B) in chains:
                    nm = NMS[c % 2][j]
                    cx.op("dve", "tensor_scalar", out=nm[:, 0:Wc], in0=IBt[:, 0:Wc], scalar1=B["LO"][:], scalar2=NEG,
                          op0=ALU.is_lt, op1=ALU.mult, reads=[IBt, B["LO"]], writes=[nm])

            def idx_slices(c):
                return [lambda: idx_scores(c, 0), lambda: idx_scores(c, 1), lambda: idx_bisect_pair(c, (0, 1)), lambda: None,
                        lambda: idx_scores(c, 2), lambda: idx_scores(c, 3), lambda: idx_bisect_pair(c, (2, 3)), lambda: None]

            lcnt = [0]
            for f_ in idx_slices(0):
                f_()
            for c in range(4):
                qs = slice(512 * c, 512 * c + 512)
                NMc = NMS[c % 2]
                sl_next = idx_slices(c + 1) if c < 3 else []
                for h in range(8):
                    if sl_next:
                        sl_next[h]()
                    b_ = 64 * (h % 2)
                    acc = next_acc()
                    accv = acc[:, 0:260].rearrange("p (j n) -> p j n", j=4)

                    def fin(acc=acc, accv=accv, h=h):
                        cx.op("dve", "reciprocal", out=RINV[:], in_=accv[:, :, 64], reads=[acc], writes=[RINV])
                        cx.op("dve", "tensor_tensor", out=ODB[:, :, 64 * h:64 * h + 64], in0=accv[:, :, 0:64],
                              in1=RINV[:].unsqueeze(2).to_broadcast([128, 4, 64]), op=ALU.mult,
                              reads=[acc, RINV], writes=[ODB])
                    tiles = list(range(0, 4 * c + 4))
                    for q_, i in enumerate(tiles):
                        ex = [(NMc[j][:, 128 * i:128 * i + 128], ident_b, 128 * j, 128 * j + 128, [NMc[j], CBB]) for j in range(4)]
                        unit(KHT[b_:b_ + 64, h // 2, 128 * i:128 * i + 128], QD[b_:b_ + 64, h // 2, qs], [],
                             VH[:, i, h, :], acc, 65, q_ == 0, KHT, QD, VH, after=(fin if q_ == len(tiles) - 1 else None),
                             ex128=ex, last=(q_ == len(tiles) - 1))
                flush()
                to_OT(ODB, c, 4)
            cx.barrier()

            o = R_HT
            X1 = mem.view("X1", o, [128, 4, 1024], F32); o += 16 * KB
            H2T = mem.view("H2T", o, [128, 8, 512], BF16); o += 8 * KB
            ACTT = mem.view("ACTT", o, [128, 22, 512], BF16); o += 22 * KB
            WOUT = mem.view("WOUT", CF_C * 4, [128, 8, 1024], BF16)
            WG = []
            for i in range(3):
                WG.append(mem.view("WG%d" % i, o, [128, 8, 256], BF16)); o += 4 * KB
            WDN = mem.view("WDN", o, [128, 22, 1024], BF16); o += 44 * KB
            XIN = []
            for i in range(2):
                XIN.append(mem.view("xin%d" % i, o, [128, 1024], F32)); o += 4 * KB
            JUNK = mem.view("junk", o, [128, 1024], F32); o += 4 * KB
            XS = mem.view("xs", o, [128, 1024], F32); o += 4 * KB
            TMPY = mem.view("tmpy", o, [128, 1024], F32); o += 4 * KB
            SIL = []
            for i in range(2):
                SIL.append(mem.view("sil%d" % i, o, [128, 512], F32)); o += 2 * KB
            assert o <= TOT, o
            ST = Buf(SMALL[:, 44:52], "ST")
            cx.dma("pool", WDN[:], wdn_d.rearrange("(c p) n -> p c n", p=128), writes=[WDN])
            wout_v = wout_d.rearrange("(kc p) n -> p kc n", p=128)

            cx.dma("pool", WOUT[:], wout_v, writes=[WOUT])
            for gi in range(4):
                for j in range(4):
                    T = 4 * gi + j
                    xi = XIN[j % 2]
                    cx.dma("sp", xi[:], x_d[seq, 128 * T:128 * T + 128, :], writes=[xi])
                    yb = [PS[2 * (j % 2)], PS[2 * (j % 2) + 1]]
                    for n in range(2):
                        for kc in range(8):
                            cx.op("pe", "matmul", yb[n][:], OT[:, kc, 128 * T:128 * T + 128], WOUT[:, kc, 512 * n:512 * n + 512],
                                  start=(kc == 0), stop=(kc == 7), reads=[OT, WOUT], writes=[yb[n]])
                    for n in range(2):
                        cx.op("act", "activation", out=JUNK[:, 512 * n:512 * n + 512], in_=yb[n][:], func=AF.Square,
                              accum_out=ST[:, n:n + 1], reads=[yb[n]], writes=[JUNK, ST])
                    cx.op("dve", "tensor_tensor", out=ST[:, 2:3], in0=ST[:, 0:1], in1=ST[:, 1:2], op=ALU.add, reads=[ST], writes=[ST])
                    cx.op("act", "activation", out=ST[:, 3:4], in_=ST[:, 2:3], func=AF.Sqrt, scale=1.0 / D, bias=1e-6, reads=[ST], writes=[ST])
                    cx.op("dve", "reciprocal", out=ST[:, 4:5], in_=ST[:, 3:4], reads=[ST], writes=[ST])
                    for n in range(2):
                        cx.op("dve", "scalar_tensor_tensor", out=TMPY[:, 512 * n:512 * n + 512], in0=yb[n][:], scalar=ST[:, 4:5],
                              in1=G1[:, 512 * n:512 * n + 512], op0=ALU.mult, op1=ALU.mult, reads=[yb[n], ST, G1], writes=[TMPY])
                    cx.op("dve", "tensor_tensor", out=X1[:, j, :], in0=TMPY[:], in1=xi[:], op=ALU.add, reads=[TMPY, xi], writes=[X1])
                    cx.op("act", "activation", out=JUNK[:], in_=X1[:, j, :], func=AF.Square, accum_out=ST[:, 0:1],
                          reads=[X1], writes=[JUNK, ST])
                    cx.op("act", "activation", out=ST[:, 3:4], in_=ST[:, 0:1], func=AF.Sqrt, scale=1.0 / D, bias=1e-6, reads=[ST], writes=[ST])
                    cx.op("dve", "reciprocal", out=ST[:, 4:5], in_=ST[:, 3:4], reads=[ST], writes=[ST])
                    cx.op("dve", "tensor_scalar", out=XS[:], in0=X1[:, j, :], scalar1=ST[:, 4:5], scalar2=None, op0=ALU.mult,
                          reads=[X1, ST], writes=[XS])
                    for fc in range(8):
                        pb = PS[4 + fc // 4]
                        cx.op("pe", "transpose", pb[:, 128 * (fc % 4):128 * (fc % 4) + 128], XS[:, 128 * fc:128 * fc + 128],
                              ident_f, reads=[XS, CFB], writes=[pb])
                    for fc in range(8):
                        pb = PS[4 + fc // 4]
                        cx.op("act", "activation", out=H2T[:, fc, 128 * j:128 * j + 128],
                              in_=pb[:, 128 * (fc % 4):128 * (fc % 4) + 128], func=AF.Identity,
                              scale=DERV[:, 2, fc, seq:seq + 1], bias=DERV[:, 3, fc, seq:seq + 1],
                              reads=[pb, DERV], writes=[H2T])
                for ch in range(22):
                    wg = WG[ch % 3]
                    if not cv_waited[0]:
                        for sem_cv in cvsems:
                            nc.sync.wait_ge(sem_cv, 16)
                        cv_waited[0] = True
                    cx.dma("sp", wg[:].rearrange("p a b -> p (a b)"), wgubf_d[ch], writes=[wg])
                    pg, pu = PS[2 * (ch % 2)], PS[2 * (ch % 2) + 1]
                    for kc in range(8):
                        cx.op("pe", "matmul", pg[:], wg[:, kc, 0:128], H2T[:, kc, :], start=(kc == 0), stop=(kc == 7),
                              reads=[wg, H2T], writes=[pg])
                    for kc in range(8):
                        cx.op("pe", "matmul", pu[:], wg[:, kc, 128:256], H2T[:, kc, :], start=(kc == 0), stop=(kc == 7),
                              reads=[wg, H2T], writes=[pu])
                    sl_ = SIL[ch % 2]
                    cx.op("act", "activation", out=sl_[:], in_=pg[:], func=AF.Silu, reads=[pg], writes=[sl_])
                    cx.op("dve", "tensor_tensor", out=ACTT[:, ch, :], in0=sl_[:], in1=pu[:], op=ALU.mult,
                          reads=[sl_, pu], writes=[ACTT])
                for j in range(4):
                    T = 4 * gi + j
                    zb = [PS[4 + 2 * (j % 2)], PS[5 + 2 * (j % 2)]]
                    for n in range(2):
                        for ch in range(22):
                            cx.op("pe", "matmul", zb[n][:], ACTT[:, ch, 128 * j:128 * j + 128], WDN[:, ch, 512 * n:512 * n + 512],
                                  start=(ch == 0), stop=(ch == 21), reads=[ACTT, WDN], writes=[zb[n]])
                    for n in range(2):
                        cx.op("act", "activation", out=JUNK[:, 512 * n:512 * n + 512], in_=zb[n][:], func=AF.Square,
                              accum_out=ST[:, n:n + 1], reads=[zb[n]], writes=[JUNK, ST])
                    cx.op("dve", "tensor_tensor", out=ST[:, 2:3], in0=ST[:, 0:1], in1=ST[:, 1:2], op=ALU.add, reads=[ST], writes=[ST])
                    cx.op("act", "activation", out=ST[:, 3:4], in_=ST[:, 2:3], func=AF.Sqrt, scale=1.0 / D, bias=1e-6, reads=[ST], writes=[ST])
                    cx.op("dve", "reciprocal", out=ST[:, 4:5], in_=ST[:, 3:4], reads=[ST], writes=[ST])
                    for n in range(2):
                        cx.op("dve", "scalar_tensor_tensor", out=TMPY[:, 512 * n:512 * n + 512], in0=zb[n][:], scalar=ST[:, 4:5],
                              in1=G2[:, 512 * n:512 * n + 512], op0=ALU.mult, op1=ALU.mult, reads=[zb[n], ST, G2], writes=[TMPY])
                    cx.op("dve", "tensor_tensor", out=XS[:], in0=TMPY[:], in1=X1[:, j, :], op=ALU.add, reads=[TMPY, X1], writes=[XS])
                    cx.dma("sp", out_d[seq, 128 * T:128 * T + 128, :], XS[:], reads=[XS])
            cx.barrier()

        cx.barrier()
        cx.finish()
    return nc


def _prep(inputs):
    inp = {k: np.asarray(v) for k, v in inputs.items()}
    cf, cb, imt, wsm = _host_consts(inp)
    A, B = _col_index()
    shared = {
        "w_ada": np.ascontiguousarray(inp['w_ada'][0]),
        "cf": cf, "cb": cb, "imt": imt, "wsm": wsm,
        "w_inA": np.ascontiguousarray(inp['w_in'][0][:, A]),
        "w_inB": np.ascontiguousarray(inp['w_in'][0][:, B]),
        "cmp_w1": np.ascontiguousarray(inp['cmp_w1'][0]),
        "b2v": np.ascontiguousarray(inp['cmp_b2'][0, 1]),
        "w_out": np.ascontiguousarray(inp['w_out'][0]),
        "w_gate_up": np.ascontiguousarray(
            inp['w_gate_up'][0].reshape(8, 128, 2, 22, 128).transpose(3, 1, 0, 2, 4).reshape(22, 128, 2048)),
        "w_down": np.ascontiguousarray(inp['w_down'][0]),
    }
    maps = []
    for c in range(8):
        m = dict(shared)
        m["x"] = np.ascontiguousarray(inp['x'][2 * c:2 * c + 2])
        m["cT"] = np.ascontiguousarray(inp['c'][2 * c:2 * c + 2].T.reshape(8, 128, 2).transpose(1, 0, 2))
        maps.append(m)
    return maps


def kernel(**inputs):
    maps = _prep(inputs)
    nc = build()
    res = run_bass_kernel_spmd(nc, maps, core_ids=list(range(8)))
    return np.concatenate([r["out"] for r in res.results], axis=0).astype(np.float32)
```

```python
import numpy as np
import concourse.bass as bass
import concourse.mybir as mybir
from concourse.bass_utils import run_bass_kernel_spmd
from contextlib import ExitStack

F32 = mybir.dt.float32
BF16 = mybir.dt.bfloat16
ALU = mybir.AluOpType
AF = mybir.ActivationFunctionType
AX = mybir.AxisListType

S = 2048
D = 1024
NT = 16
DFF = 2816
NEG = -30000.0
IN_SPLITS = (512, 128, 128, 128, 128, 128, 128, 24, 512, 128, 16, 512, 64, 8)
NAMES = ['nq', 'nkc', 'nvc', 'nks', 'nvs', 'nkw', 'nvw', 'ngate', 'dq', 'dckv', 'dkr', 'iq', 'ik', 'iw']
OFF = dict(zip(NAMES, np.cumsum((0,) + IN_SPLITS)[:-1]))
NA_FM = 19 * 128
NA = NA_FM + 280
NB_FM = 18 * 128 + 32
NB = NB_FM + 136
BIS_ITERS = 12
import os as _os
_DBG = _os.environ.get('KDBG', '')


class _E:
    def __init__(self, name, eng, sem):
        self.name, self.eng, self.sem, self.tick, self.waited = name, eng, sem, 0, {}


class _St:
    __slots__ = ("w", "r")

    def __init__(self):
        self.w = None
        self.r = {}


class Buf:
    def __init__(self, ap, key):
        self.ap = ap
        self.key = key

    def __getitem__(self, idx):
        return self.ap[idx]


class Ctx:
    def __init__(self, nc, es, n_dma_sems=8):
        self.nc, self.es = nc, es
        self.E = {}
        for name, eng in (("pe", nc.tensor), ("act", nc.scalar), ("dve", nc.vector),
                          ("pool", nc.gpsimd), ("sp", nc.sync)):
            self.E[name] = _E(name, eng, es.enter_context(nc.semaphore("s_" + name)))
        self.dsems = {q: [[es.enter_context(nc.semaphore("d_%s%d" % (q, i))), 0] for i in range(n_dma_sems)]
                      for q in ("sp", "pool")}
        self.dnext = {"sp": 0, "pool": 0}
        self.st = {}

    def _state(self, b):
        k = b.key if isinstance(b, Buf) else (b if isinstance(b, str) else b.name)
        s = self.st.get(k)
        if s is None:
            s = self.st[k] = _St()
        return s

    def _wait(self, E, dep):
        kind, tk = dep
        if kind == E.name and E.name == "pe":
            return
        if E.waited.get(kind, 0) >= tk:
            return
        sem = self.dsems[kind[0]][kind[1]][0] if isinstance(kind, tuple) else self.E[kind].sem
        E.eng.wait_ge(sem, tk)
        E.waited[kind] = tk

    def _deps(self, E, reads, writes):
        deps = []
        for b in reads:
            s = self._state(b)
            if s.w is not None:
                deps.append(s.w)
        for b in writes:
            s = self._state(b)
            if s.w is not None:
                deps.append(s.w)
            deps.extend(s.r.items())
        for d in deps:
            self._wait(E, d)

    def _mark(self, token, reads, writes):
        for b in reads:
            s = self._state(b)
            if s.r.get(token[0], 0) < token[1]:
                s.r[token[0]] = token[1]
        for b in writes:
            s = self._state(b)
            s.w = token
            s.r = {}

    def op(self, en, fn, *args, reads=(), writes=(), **kw):
        E = self.E[en]
        self._deps(E, reads, writes)
        ins = getattr(E.eng, fn)(*args, **kw)
        E.tick += 1
        ins.then_inc(E.sem, 1)
        self._mark((en, E.tick), reads, writes)
        return ins

    def dma(self, q, out, in_, reads=(), writes=(), **kw):
        E = self.E[q]
        self._deps(E, reads, writes)
        i = self.dnext[q]
        self.dnext[q] = (i + 1) % len(self.dsems[q])
        slot = self.dsems[q][i]
        kind = (q, i)
        if slot[1] > 0:
            self._wait(E, (kind, slot[1]))
        slot[1] += 16
        E.eng.dma_start(out=out, in_=in_, **kw).then_inc(slot[0], 16)
        self._mark((kind, slot[1]), reads, writes)

    def barrier(self):
        toks = [(n, e.tick) for n, e in self.E.items() if e.tick > 0]
        for q in self.dsems:
            for i, slot in enumerate(self.dsems[q]):
                if slot[1] > 0:
                    toks.append(((q, i), slot[1]))
        for n, e in self.E.items():
            for t in toks:
                if t[0] == n:
                    if n != "sp" and e.waited.get(n, 0) < t[1]:
                        e.eng.wait_ge(e.sem, t[1])
                        e.waited[n] = t[1]
                else:
                    self._wait(e, t)
        self.st = {}

    def finish(self):
        E = self.E["sp"]
        for q in self.dsems:
            for i, slot in enumerate(self.dsems[q]):
                if slot[1] > 0:
                    self._wait(E, ((q, i), slot[1]))


class Mem:
    def __init__(self, big):
        self.big = big

    def view(self, key, off, shape, dt, pbase=0):
        esz = 4 if dt == F32 else 2
        nel = int(np.prod(shape[1:]))
        assert off % 4 == 0
        a = off // 2
        n = nel * esz // 2
        ap = self.big[pbase:pbase + shape[0], a:a + n]
        if dt == F32:
            ap = ap.bitcast(F32)
        if len(shape) == 3:
            ap = ap.rearrange("p (a b) -> p a b", a=shape[1])
        elif len(shape) == 4:
            ap = ap.rearrange("p (a b c) -> p a b c", a=shape[1], b=shape[2])
        return Buf(ap, key)


def _swap_head(cols):
    c = np.array(cols).copy()
    c[0:8] = cols[8:16]
    c[8:16] = cols[0:8]
    return c


def _col_index():
    def head(base, h):
        return np.arange(base + 64 * h, base + 64 * h + 64)
    A = []
    for j in range(4):
        a = np.concatenate([head(OFF['nq'], 2 * j), head(OFF['nq'], 2 * j + 1)])
        b = np.concatenate([_swap_head(head(OFF['nq'], 2 * j)), _swap_head(head(OFF['nq'], 2 * j + 1))])
        A += [a, b]
    a = np.concatenate([head(OFF['nkc'], 0), head(OFF['nkc'], 1)])
    b = np.concatenate([_swap_head(head(OFF['nkc'], 0)), _swap_head(head(OFF['nkc'], 1))])
    A += [a, b]
    A += [np.arange(OFF['nvc'], OFF['nvc'] + 128)]
    for nm in ('nks', 'nkw'):
        for g in range(2):
            a = np.concatenate([head(OFF[nm], g), head(OFF[nm], g)])
            b = np.concatenate([_swap_head(head(OFF[nm], g)), _swap_head(head(OFF[nm], g))])
            A += [a, b]
    A += [np.arange(OFF['nvs'], OFF['nvs'] + 128), np.arange(OFF['nvw'], OFF['nvw'] + 128),
          np.arange(OFF['ngate'], OFF['ngate'] + 24)]
    A = np.concatenate(A)
    assert A.size == NA
    B = []
    for nm in ('dq', 'iq'):
        for j in range(4):
            a = np.concatenate([head(OFF[nm], 2 * j), head(OFF[nm], 2 * j + 1)])
            b = np.concatenate([_swap_head(head(OFF[nm], 2 * j)), _swap_head(head(OFF[nm], 2 * j + 1))])
            B += [a, b]
    a = np.concatenate([head(OFF['ik'], 0), head(OFF['ik'], 0)])
    b = np.concatenate([_swap_head(head(OFF['ik'], 0)), _swap_head(head(OFF['ik'], 0))])
    B += [a, b]
    kr = np.arange(OFF['dkr'], OFF['dkr'] + 16)
    B += [kr, _swap_head(kr)]
    B += [np.arange(OFF['dckv'], OFF['dckv'] + 128), np.arange(OFF['iw'], OFF['iw'] + 8)]
    B = np.concatenate(B)
    assert B.size == NB
    return A, B


CF_ID = 0
CF_C = 128
CF_S = CF_C + 2048
CF_V = CF_S + 2048
CF_P2 = CF_V + 84
CF_N = CF_P2 + BIS_ITERS
CB_ID = 0
CB_EX = 128
CB_OV = CB_EX + 2048
CB_PE = CB_OV + 32
CB_SEL = CB_PE + 64
CB_N = CB_SEL + 128


def _host_consts(inp):
    cf = np.zeros((128, CF_N), np.float32)
    cf[:, CF_ID:CF_ID + 128] = np.eye(128, dtype=np.float32)
    inv_freq = 1.0 / (np.float32(500000.0) ** (np.arange(0, 16, 2, dtype=np.float32) / np.float32(16)))
    ang = np.arange(S, dtype=np.float32)[:, None] * inv_freq[None, :].astype(np.float32)
    cos, sin = np.cos(ang).astype(np.float32), np.sin(ang).astype(np.float32)
    for p in range(128):
        j = p % 64
        if j < 16:
            cf[p, CF_C:CF_C + S] = cos[:, j % 8]
            cf[p, CF_S:CF_S + S] = (-1.0 if j < 8 else 1.0) * sin[:, j % 8]
        else:
            cf[p, CF_C:CF_C + S] = 1.0
    v = cf[:, CF_V:CF_V + 84]
    v[:, 0:48] = inp['b_ada'][0].reshape(48, 128).T
    v[:, 48:56] = inp['g_pre_mix'][0].reshape(8, 128).T
    v[:, 56:64] = inp['g_post_mix'][0].reshape(8, 128).T
    v[:, 64:72] = inp['g_pre_ffn'][0].reshape(8, 128).T
    v[:, 72:80] = inp['g_post_ffn'][0].reshape(8, 128).T
    v[:, 80] = inp['cmp_b1'][0, 0]
    v[:, 81] = inp['cmp_b1'][0, 1]
    v[:, 82] = np.concatenate([inp['cmp_b2'][0, 0], inp['cmp_b2'][0, 0]])
    v[:, 83] = inp['g_kv_norm'][0]
    cf[:, CF_P2:CF_P2 + BIS_ITERS] = (0.5 ** np.arange(1, BIS_ITERS + 1, dtype=np.float64)).astype(np.float32)[None, :]
    cb = np.zeros((128, CB_N), np.float32)
    cb[:, CB_ID:CB_ID + 128] = np.eye(128, dtype=np.float32)
    for j in range(32):
        cb[j, CB_EX + 64 * j:CB_EX + 64 * j + 64] = 1.0
    ci = np.arange(127)[:, None] * 16
    sj = np.arange(32)[None, :] * 64
    cb[0:127, CB_OV:CB_OV + 32] = ((ci < sj + 64) & (ci + 32 > sj)).astype(np.float32)
    cb[0:64, CB_PE:CB_PE + 32] = inp['cmp_pe'][0, 0].T
    cb[0:64, CB_PE + 32:CB_PE + 64] = inp['cmp_pe'][0, 1].T
    for i in range(16):
        cb[i, CB_SEL + i] = 1.0
        cb[i, CB_SEL + 64 + i] = 1.0
    t = (np.arange(16)[None, :, None] * 128 + np.arange(128)[:, None, None])
    blk = t // 64
    j = np.arange(32)[None, None, :]
    visible = j <= blk
    forced = (j == 0) | (j == blk) | (j == blk - 1)
    mm = (visible & ~forced).astype(np.float32)
    ba = np.where(visible, np.where(forced, 1e6, 0.0), -1e30).astype(np.float32)
    imt = np.concatenate([mm.reshape(128, 512), ba.reshape(128, 512)], axis=1)
    wuk = np.zeros((128, 4, 128), np.float32)
    for h in range(8):
        wuk[:, h // 2, (h % 2) * 64 + 16:(h % 2) * 64 + 64] = inp['w_uk'][0, h]
    wuv = np.transpose(inp['w_uv'][0], (1, 0, 2)).reshape(128, 512)
    w2 = np.concatenate([inp['cmp_w2'][0, 0], inp['cmp_w2'][0, 0], inp['cmp_w2'][0, 1]], axis=1)
    wsm = np.concatenate([wuk.reshape(128, 512), wuv, w2], axis=1).astype(np.float32)
    return cf, cb, imt, wsm


def build(n_seq=2, stage=99, dbg_cols=0):
    nc = bass.Bass("TRN2", target_bir_lowering=False)
    dt_ = nc.dram_tensor
    x_d = dt_("x", [2, S, D], F32, kind="ExternalInput").ap()
    cT_d = dt_("cT", [128, 8, 2], F32, kind="ExternalInput").ap()
    wada_d = dt_("w_ada", [D, 6 * D], F32, kind="ExternalInput").ap()
    cf_d = dt_("cf", [128, CF_N], F32, kind="ExternalInput").ap()
    cb_d = dt_("cb", [128, CB_N], F32, kind="ExternalInput").ap()
    imt_d = dt_("imt", [128, 1024], F32, kind="ExternalInput").ap()
    wsm_d = dt_("wsm", [128, 1216], F32, kind="ExternalInput").ap()
    winA_d = dt_("w_inA", [D, NA], F32, kind="ExternalInput").ap()
    winB_d = dt_("w_inB", [D, NB], F32, kind="ExternalInput").ap()
    w1_d = dt_("cmp_w1", [2, 2048, 128], F32, kind="ExternalInput").ap()
    b2v_d = dt_("b2v", [64], F32, kind="ExternalInput").ap()
    wout_d = dt_("w_out", [D, D], F32, kind="ExternalInput").ap()
    wgu_d = dt_("w_gate_up", [22, 128, 2048], F32, kind="ExternalInput").ap()
    wdn_d = dt_("w_down", [DFF, D], F32, kind="ExternalInput").ap()
    out_d = dt_("out", [2, S, D], F32, kind="ExternalOutput").ap()
    wgubf_d = dt_("wgu_bf16", [22, 128, 2048], BF16, kind="Internal").ap()
    dbg_d = dt_("dbg", [128, dbg_cols], F32, kind="ExternalOutput").ap() if dbg_cols else None

    with ExitStack() as es:
        cx = Ctx(nc, es)
        TOT = 206 * 1024
        big = es.enter_context(nc.sbuf_tensor("big", [128, TOT // 2], BF16))
        mem = Mem(big)
        PS = [Buf(es.enter_context(nc.psum_tensor("ps%d" % i, [128, 512], F32))[:], "ps%d" % i) for i in range(8)]

        def psb(i):
            return PS[i].ap.bitcast(BF16)

        KB = 1024
        o = 0
        CFB = mem.view("cf", o, [128, CF_N], F32); o += CF_N * 4
        CBB = mem.view("cb", o, [128, CB_N], BF16); o += CB_N * 2
        MSK = mem.view("msk", o, [128, 8, 512], BF16); o += 8 * 512 * 2
        CMN = mem.view("cmn", o, [128, 2048], BF16); o += 2048 * 2
        ONESF = mem.view("onesf", o, [128, 128], F32); o += 512
        MODC = mem.view("modc", o, [128, 48, 2], F32); o += 384
        DERV = mem.view("derv", o, [128, 6, 8, 2], F32); o += 384
        CACT = mem.view("cact", o, [128, 8, 2], F32); o += 64
        SMALL = mem.view("small", o, [128, 64], F32); o += 256
        G1 = mem.view("G1", o, [128, 1024], F32); o += 4096
        G2 = mem.view("G2", o, [128, 1024], F32); o += 4096
        assert o <= 44 * KB, o
        R_OT = 44 * KB
        R_HT = 76 * KB
        R_WIN = 108 * KB
        R_PH = 152 * KB
        OT = mem.view("OT", R_OT, [128, 8, 2048], BF16)
        HT = mem.view("HT", R_HT, [128, 8, 2048], BF16)

        ident_f = CFB[:, CF_ID:CF_ID + 128]
        ident_b = CBB[:, CB_ID:CB_ID + 128]
        ropeC = CFB[:, CF_C:CF_C + S]
        ropeS = CFB[:, CF_S:CF_S + S]
        vec = lambda c0, c1: CFB[:, CF_V + c0:CF_V + c1]

        def dbg_dump(ap, col0, ncols, rd):
            if dbg_d is not None:
                cx.dma("pool", dbg_d[0:ap.shape[0], col0:col0 + ncols], ap, reads=[rd])

        cx.dma("sp", CFB[:], cf_d, writes=[CFB])
        cx.dma("pool", CBB[:], cb_d, writes=[CBB])
        cx.dma("sp", CACT[:], cT_d, writes=[CACT])
        cx.op("pool", "memset", MSK[:], 0.0, writes=[MSK])
        for k in range(4):
            cx.op("pool", "affine_select", out=MSK[:, k, :], in_=MSK[:, k, :], pattern=[[-1, 512]],
                  compare_op=ALU.is_gt, fill=1.0, base=128 * k, channel_multiplier=1, reads=[MSK], writes=[MSK])
        for k in range(1, 5):
            cx.op("pool", "affine_select", out=MSK[:, 3 + k, :], in_=MSK[:, 3 + k, :], pattern=[[1, 512]],
                  compare_op=ALU.is_ge, fill=1.0, base=-512 + 128 * k, channel_multiplier=-1, reads=[MSK], writes=[MSK])
        cx.op("pool", "memset", CMN[:], 0.0, writes=[CMN])
        cx.op("pool", "affine_select", out=CMN[:], in_=CMN[:], pattern=[[-1, 2048]],
              compare_op=ALU.is_gt, fill=1.0, base=31, channel_multiplier=16, reads=[CMN], writes=[CMN])
        cx.op("pool", "memset", ONESF[:], 1.0, writes=[ONESF])
        cx.op("act", "activation", out=CACT[:], in_=CACT[:], func=AF.Silu, reads=[CACT], writes=[CACT])
        WA = [mem.view("wa0", R_HT, [128, 8, 1024], F32), mem.view("wa1", R_WIN, [128, 8, 1024], F32)]
        wada_v = wada_d.rearrange("(kc p) n -> p kc n", p=128)
        for v in range(6):
            wb = WA[v % 2]
            for kc in range(8):
                cx.dma("sp", wb[:, kc, :], wada_v[:, kc, 1024 * v:1024 * v + 1024], writes=[wb])
            for fc in range(8):
                col = (v * 8 + fc) * 2
                for kc in range(8):
                    cx.op("pe", "matmul", PS[0][:, col:col + 2], wb[:, kc, 128 * fc:128 * fc + 128], CACT[:, kc, :],
                          start=(kc == 0), stop=(kc == 7), reads=[wb, CACT], writes=[PS[0]])
        cx.op("dve", "tensor_tensor", out=MODC[:], in0=PS[0][:, 0:96].rearrange("p (a b) -> p a b", b=2),
              in1=vec(0, 48).unsqueeze(2).to_broadcast([128, 48, 2]), op=ALU.add, reads=[PS[0], CFB], writes=[MODC])
        def gb(c0):
            return vec(c0, c0 + 8).unsqueeze(2).to_broadcast([128, 8, 2])
        cx.op("dve", "scalar_tensor_tensor", out=DERV[:, 0], in0=MODC[:, 8:16, :], scalar=1.0, in1=gb(48),
              op0=ALU.add, op1=ALU.mult, reads=[MODC, CFB], writes=[DERV])
        cx.op("dve", "tensor_copy", out=DERV[:, 1], in_=MODC[:, 0:8, :], reads=[MODC], writes=[DERV])
        cx.op("dve", "scalar_tensor_tensor", out=DERV[:, 2], in0=MODC[:, 32:40, :], scalar=1.0, in1=gb(64),
              op0=ALU.add, op1=ALU.mult, reads=[MODC, CFB], writes=[DERV])
        cx.op("dve", "tensor_copy", out=DERV[:, 3], in_=MODC[:, 24:32, :], reads=[MODC], writes=[DERV])
        cx.op("dve", "tensor_tensor", out=DERV[:, 4], in0=MODC[:, 16:24, :], in1=gb(56), op=ALU.mult,
              reads=[MODC, CFB], writes=[DERV])
        cx.op("dve", "tensor_tensor", out=DERV[:, 5], in0=MODC[:, 40:48, :], in1=gb(72), op=ALU.mult,
              reads=[MODC, CFB], writes=[DERV])
        cx.barrier()

        cvsems = []
        cv_waited = [False]
        for seq in range(n_seq):
            if seq > 0:
                cx.dma("sp", CFB[:, CF_C:CF_C + 2 * S], cf_d[:, CF_C:CF_C + 2 * S], writes=[CFB])
            WIN = mem.view("win", R_WIN, [128, 8, NA], BF16)
            cx.dma("pool", WIN[:], winA_d.rearrange("(kc p) n -> p kc n", p=128), writes=[WIN])
            DG = mem.view("dg", R_PH, [128, 128], F32)
            for gi, GT_ in ((4, G1), (5, G2)):
                for fc in range(8):
                    cx.op("dve", "tensor_scalar", out=DG[:], in0=ident_f, scalar1=DERV[:, gi, fc, seq:seq + 1],
                          scalar2=None, op0=ALU.mult, reads=[CFB, DERV], writes=[DG])
                    pb = PS[fc // 4]
                    cx.op("pe", "matmul", pb[:, 128 * (fc % 4):128 * (fc % 4) + 128], ONESF[:], DG[:],
                          start=True, stop=True, reads=[ONESF, DG], writes=[pb])
                    if fc % 4 == 3:
                        cx.op("act", "copy", out=GT_[:, 512 * (fc // 4):512 * (fc // 4) + 512], in_=pb[:],
                              reads=[pb], writes=[GT_])
            cx.barrier()
            XIN = [mem.view("xin%d" % i, R_PH + 4 * KB * i, [128, 1024], F32) for i in range(2)]
            XS = [mem.view("xs%d" % i, R_PH + 8 * KB + 4 * KB * i, [128, 1024], F32) for i in range(2)]
            JUNK = mem.view("junk", R_PH + 16 * KB, [128, 1024], F32)
            SS = [mem.view("ss%d" % i, R_PH + 20 * KB + 64 * i, [128, 4], F32) for i in range(2)]
            def p1_stats(i):
                xi, xs, ss = XIN[i % 2], XS[i % 2], SS[i % 2]
                cx.dma("sp", xi[:], x_d[seq, 128 * i:128 * i + 128, :], writes=[xi])
                cx.op("act", "activation", out=JUNK[:], in_=xi[:], func=AF.Square, accum_out=ss[:, 0:1],
                      reads=[xi], writes=[JUNK, ss])
                cx.op("act", "activation", out=ss[:, 1:2], in_=ss[:, 0:1], func=AF.Sqrt, scale=1.0 / D, bias=1e-6,
                      reads=[ss], writes=[ss])
                cx.op("dve", "reciprocal", out=ss[:, 2:3], in_=ss[:, 1:2], reads=[ss], writes=[ss])
                cx.op("dve", "tensor_scalar", out=xs[:], in0=xi[:], scalar1=ss[:, 2:3], scalar2=None, op0=ALU.mult,
                      reads=[xi, ss], writes=[xs])

            p1_stats(0)
            for i in range(NT):
                xs = XS[i % 2]
                for fc in range(8):
                    pb = PS[2 * (i % 2) + fc // 4]
                    cx.op("pe", "transpose", pb[:, 128 * (fc % 4):128 * (fc % 4) + 128],
                          xs[:, 128 * fc:128 * fc + 128], ident_f, reads=[xs, CFB], writes=[pb])
                if i + 1 < NT:
                    p1_stats(i + 1)
                for fc in range(8):
                    pb = PS[2 * (i % 2) + fc // 4]
                    if fc < 4:
                        cx.op("act", "activation", out=HT[:, fc, 128 * i:128 * i + 128],
                              in_=pb[:, 128 * (fc % 4):128 * (fc % 4) + 128], func=AF.Identity,
                              scale=DERV[:, 0, fc, seq:seq + 1], bias=DERV[:, 1, fc, seq:seq + 1],
                              reads=[pb, DERV], writes=[HT])
                    else:
                        cx.op("dve", "tensor_scalar", out=HT[:, fc, 128 * i:128 * i + 128],
                              in0=pb[:, 128 * (fc % 4):128 * (fc % 4) + 128],
                              scalar1=DERV[:, 0, fc, seq:seq + 1], scalar2=DERV[:, 1, fc, seq:seq + 1],
                              op0=ALU.mult, op1=ALU.add, reads=[pb, DERV], writes=["HTd"])
            cx.barrier()

            o = R_PH
            QN = mem.view("QN", o, [128, 4, 2048], BF16); o += 16 * KB
            KS = mem.view("KS", o, [128, 2, 2048], BF16); o += 8 * KB
            KW = mem.view("KW", o, [128, 2, 2048], BF16); o += 8 * KB
            KCR = mem.view("KCR", o, [128, 2048], BF16); o += 4 * KB
            VCR = mem.view("VCR", o, [128, 2048], BF16); o += 4 * KB
            VT = mem.view("VT", o, [128, 16, 4, 65], BF16); o += 8320
            GT = mem.view("GT", o, [128, 16, 24], F32); o += 1536
            o_T1 = o
            T1 = mem.view("T1", o, [128, 512], F32); o += 2048
            T2 = mem.view("T2", o, [128, 512], F32); o += 2048
            assert o <= TOT, o
            ucnt = [0]

            def proj_unit(WINb, ca, cb_, M, tc, dst_ap, dstbuf):
                u = ucnt[0]; ucnt[0] += 1
                pa, pb = PS[2 * (u % 2)], PS[2 * (u % 2) + 1]
                for kc in range(8):
                    cx.op("pe", "matmul", pa[0:M, :], WINb[:, kc, ca:ca + M], HT[:, kc, 512 * tc:512 * tc + 512],
                          start=(kc == 0), stop=(kc == 7), reads=[WINb, HT], writes=[pa])
                if cb_ is None:
                    cx.op("act", "copy", out=dst_ap, in_=pa[0:M, :], reads=[pa], writes=[dstbuf])
                    return
                for kc in range(8):
                    cx.op("pe", "matmul", pb[0:M, :], WINb[:, kc, cb_:cb_ + M], HT[:, kc, 512 * tc:512 * tc + 512],
                          start=(kc == 0), stop=(kc == 7), reads=[WINb, HT], writes=[pb])
                cx.op("dve", "tensor_tensor", out=T1[0:M, :], in0=pa[0:M, :], in1=ropeC[0:M, 512 * tc:512 * tc + 512],
                      op=ALU.mult, reads=[pa, CFB], writes=[T1])
                cx.op("dve", "tensor_tensor", out=T2[0:M, :], in0=pb[0:M, :], in1=ropeS[0:M, 512 * tc:512 * tc + 512],
                      op=ALU.mult, reads=[pb, CFB], writes=[T2])
                cx.op("pool", "tensor_tensor", out=dst_ap, in0=T1[0:M, :], in1=T2[0:M, :], op=ALU.add,
                      reads=[T1, T2], writes=[dstbuf])

            cx.op("pool", "memset", VT[:, :, :, 64:65], 1.0, writes=[VT])
            for tc in range(4):
                sl = slice(512 * tc, 512 * tc + 512)
                for j in range(4):
                    proj_unit(WIN, 256 * j, 256 * j + 128, 128, tc, QN[:, j, sl], QN)
                proj_unit(WIN, 1024, 1152, 128, tc, KCR[:, sl], KCR)
                proj_unit(WIN, 1280, None, 128, tc, VCR[:, sl], VCR)
                for g in range(2):
                    proj_unit(WIN, 1408 + 256 * g, 1536 + 256 * g, 128, tc, KS[:, g, sl], KS)
                    proj_unit(WIN, 1920 + 256 * g, 2048 + 256 * g, 128, tc, KW[:, g, sl], KW)
            for i in range(NT):
                pb = PS[4 + i % 2]
                for kc in range(8):
                    cx.op("pe", "matmul", pb[:, 0:280], HT[:, kc, 128 * i:128 * i + 128], WIN[:, kc, NA_FM:NA],
                          start=(kc == 0), stop=(kc == 7), reads=[HT, WIN], writes=[pb])
                cx.op("act", "copy", out=VT[:, i, :, 0:64], in_=pb[:, 0:256].rearrange("p (a b) -> p a b", a=4),
                      reads=[pb], writes=[VT])
                cx.op("act", "activation", out=GT[:, i, :], in_=pb[:, 256:280], func=AF.Sigmoid, reads=[pb], writes=[GT])
            cx.barrier()

            o = R_WIN
            W1 = mem.view("W1", o, [128, 2, 32, 128], BF16); o += 16 * KB
            WSM = mem.view("WSM", o, [128, 1216], BF16); o += 2432
            IMT = mem.view("IMT", o, [128, 2, 16, 32], F32); o += 4096
            PT = []
            for i in range(4):
                PT.append(mem.view("PT%d" % i, o, [128, 512], BF16)); o += 1024
            XG = mem.view("XG", o, [128, 128], F32); o += 512
            UU = mem.view("UU", o, [128, 128], F32); o += 512
            HIDT = mem.view("HIDT", o, [128, 128], BF16); o += 256
            KCT = mem.view("KCT", o, [128, 2, 128], BF16); o += 512
            VCX = mem.view("VCX", o, [128, 2, 98], BF16); o += 392
            B2V = mem.view("B2V", o, [128, 64], F32); o += 256
            BIAS1 = mem.view("BIAS1", o, [128, 2], F32); o += 8
            OA = mem.view("OA", o, [128, 4, 512], F32); o += 8192
            OAB = mem.view("OAB", o_T1, [128, 4, 512], BF16)
            IMPS = []
            for g in range(2):
                IMPS.append(mem.view("IMP%d" % g, o, [128, 4, 32], F32)); o += 512
            TMPI = mem.view("TMPI", o, [128, 4, 32], F32); o += 512
            IMPM = mem.view("IMPM", o, [128, 4, 32], F32); o += 512
            TOP8 = mem.view("TOP8", o, [128, 4, 8], F32); o += 128
            NSELB = mem.view("NSELB", o, [128, 4, 32], BF16); o += 256
            NSELT = mem.view("NSELT", o, [128, 2, 512], BF16); o += 2048
            TMPO = mem.view("TMPO", o, [128, 4, 64], F32); o += 1024
            assert o <= R_PH, o
            RINV = Buf(SMALL[:, 0:4], "RINV")
            COEF = Buf(SMALL[:, 4:8], "COEF")

            for kv in range(2):
                src = w1_d[kv].rearrange("(l d) j -> d l j", d=64)
                cx.dma("pool", W1[0:64, kv], src, writes=[W1])
                cx.dma("pool", W1[64:128, kv], src, writes=[W1])
            cx.dma("pool", WSM[:], wsm_d, writes=[WSM])
            cx.dma("sp", IMT[:].rearrange("p a b c -> p (a b c)"), imt_d, writes=[IMT])
            cx.dma("sp", B2V[:], b2v_d.partition_broadcast(128), writes=[B2V])
            if seq == 0:
                for i in range(11):
                    sem_cv = es.enter_context(nc.semaphore("cv%d" % i))
                    cvsems.append(sem_cv)
                    nc.gpsimd.dma_start(out=wgubf_d[2 * i:2 * i + 2].rearrange("c p n -> (c p) n"),
                                        in_=wgu_d[2 * i:2 * i + 2].rearrange("c p n -> (c p) n")).then_inc(sem_cv, 16)
            cx.op("pool", "memset", HIDT[:], 0.0, writes=[HIDT])
            cx.op("pool", "memset", NSELT[:], 0.0, writes=[NSELT])
            cx.op("pool", "memset", VCX[:, :, 64:65], 1.0, writes=[VCX])
            for g in range(2):
                cx.op("pool", "tensor_copy", out=VCX[:, g, 65:97], in_=CBB[:, CB_OV:CB_OV + 32], reads=[CBB], writes=[VCX])
            for kv in range(2):
                for l in range(32):
                    cx.op("pe", "matmul", PS[6][:, kv:kv + 1], W1[0:64, kv, l, :], CBB[0:64, CB_PE + 32 * kv + l:CB_PE + 32 * kv + l + 1],
                          start=(l == 0), stop=(l == 31), reads=[W1, CBB], writes=[PS[6]])
            cx.op("dve", "tensor_tensor", out=BIAS1[:], in0=PS[6][:, 0:2], in1=vec(80, 82), op=ALU.add,
                  reads=[PS[6], CFB], writes=[BIAS1])
            for kv in range(2):
                for g in range(2):
                    srcb = KCR if kv == 0 else VCR
                    hps = PS[4 + g]
                    for l in range(32):
                        cx.op("pe", "matmul", hps[:, 0:127], W1[64 * g:64 * g + 64, kv, l, :],
                              srcb[64 * g:64 * g + 64, l:l + 2017:16], start=(l == 0), stop=(l == 31),
                              reads=[W1, srcb], writes=[hps])
                    cx.op("act", "activation", out=XG[:, 0:127], in_=hps[:, 0:127], func=AF.Identity,
                          bias=BIAS1[:, kv:kv + 1], reads=[hps, BIAS1], writes=[XG])
                    cx.op("dve", "tensor_tensor", out=UU[:, 0:127], in0=XG[:, 0:127], in1=XG[:, 0:127], op=ALU.mult,
                          reads=[XG], writes=[UU])
                    cx.op("dve", "tensor_scalar", out=UU[:, 0:127], in0=UU[:, 0:127], scalar1=0.044715, scalar2=1.0,
                          op0=ALU.mult, op1=ALU.add, reads=[UU], writes=[UU])
                    cx.op("dve", "tensor_tensor", out=UU[:, 0:127], in0=UU[:, 0:127], in1=XG[:, 0:127], op=ALU.mult,
                          reads=[UU, XG], writes=[UU])
                    cx.op("act", "activation", out=UU[:, 0:127], in_=UU[:, 0:127], func=AF.Sigmoid, scale=1.5957691216057308,
                          reads=[UU], writes=[UU])
                    cx.op("dve", "tensor_tensor", out=HIDT[:, 0:127], in0=XG[:, 0:127], in1=UU[:, 0:127], op=ALU.mult,
                          reads=[XG, UU], writes=[HIDT])
                    if kv == 0:
                        cx.op("pe", "matmul", PS[6][:, 0:128], WSM[:, 1024:1152], HIDT[:], start=True, stop=True,
                              reads=[WSM, HIDT], writes=[PS[6]])
                        cx.op("act", "activation", out=KCT[:, g, :], in_=PS[6][:, 0:128], func=AF.Identity,
                              bias=vec(82, 83), reads=[PS[6], CFB], writes=[KCT])
                    else:
                        cx.op("pe", "matmul", PS[6][:, 0:64], HIDT[:], WSM[:, 1152:1216], start=True, stop=True,
                              reads=[WSM, HIDT], writes=[PS[6]])
                        cx.op("dve", "tensor_tensor", out=VCX[:, g, 0:64], in0=PS[6][:, 0:64], in1=B2V[:], op=ALU.add,
                              reads=[PS[6], B2V], writes=[VCX])

            pipe = {"pend": [], "u": 0, "job": 0}
            SCB = [PS[0], PS[1], PS[4], PS[5]]
            grp = []

            def unit(kT, qT, extras, V, acc, ncols, first, rk, rq, rv, after=None, mmask=None, ex128=(), last=False):
                u = pipe["u"]; pipe["u"] += 1
                grp.append(dict(u=u, kT=kT, qT=qT, ex=extras, ex128=ex128, V=V, acc=acc, ncols=ncols, first=first,
                                rk=rk, rq=rq, rv=rv, after=after, mmask=mmask, last=last))
                if len(grp) == (1 if 'G1' in _DBG else 2):
                    emit_group()

            def emit_group():
                if not grp:
                    return
                for d in grp:
                    sbk = SCB[d["u"] % 4]
                    nex = len(d["ex"]) + len(d["ex128"])
                    cx.op("pe", "matmul", sbk[:], d["kT"], d["qT"], start=True, stop=(nex == 0),
                          reads=[d["rk"], d["rq"]], writes=[sbk])
                for d in grp:
                    sbk = SCB[d["u"] % 4]
                    nex = len(d["ex"]) + len(d["ex128"])
                    for n_, (l_, r_, c0, c1, rd) in enumerate(d["ex"]):
                        cx.op("pe", "matmul", sbk[:, c0:c1], l_, r_, start=False, stop=(n_ == nex - 1), reads=rd, writes=[sbk])
                for d in grp:
                    sbk = SCB[d["u"] % 4]
                    nex = len(d["ex"]) + len(d["ex128"])
                    for n_, (l_, r_, c0, c1, rd) in enumerate(d["ex128"]):
                        cx.op("pe", "matmul", sbk[:, c0:c1], l_, r_, start=False, stop=(len(d["ex"]) + n_ == nex - 1),
                              reads=rd, writes=[sbk])
                for pvf in pipe["pend"]:
                    pvf()
                pipe["pend"] = []
                for d in grp:
                    sbk = SCB[d["u"] % 4]
                    pt = PT[d["u"] % 4]
                    cx.op("act", "activation", out=pt[:], in_=sbk[:], func=AF.Exp, scale=0.125, reads=[sbk], writes=[pt])
                    if d["mmask"] is not None and 'NM' not in _DBG:
                        cx.op("dve", "tensor_tensor", out=pt[:], in0=pt[:], in1=d["mmask"][0], op=ALU.mult,
                              reads=[pt, d["mmask"][1]], writes=[pt])

                    def pv(d=d, pt=pt):
                        for j in range(4):
                            cx.op("pe", "matmul", d["acc"][:, d["ncols"] * j:d["ncols"] * j + d["ncols"]],
                                  pt[:, 128 * j:128 * j + 128], d["V"], start=(d["first"] and j == 0),
                                  stop=(True if 'ST' in _DBG else (d["last"] and j == 3)), reads=[pt, d["rv"]], writes=[d["acc"]])
                        if d["after"] is not None:
                            d["after"]()
                    pipe["pend"].append(pv)
                grp.clear()

            def flush():
                emit_group()
                for pvf in pipe["pend"]:
                    pvf()
                pipe["pend"] = []

            def next_acc():
                a = PS[2 + pipe["job"] % 2]
                pipe["job"] += 1
                return a

            def nsa_final(acc, ncols, c, h, br, first_branch):
                accv = acc[:, 0:4 * ncols].rearrange("p (j n) -> p j n", j=4)

                def f():
                    cx.op("dve", "tensor_scalar", out=RINV[:], in0=accv[:, :, 64], scalar1=1e-30, scalar2=None,
                          op0=ALU.max, reads=[acc], writes=[RINV])
                    cx.op("dve", "reciprocal", out=RINV[:], in_=RINV[:], reads=[RINV], writes=[RINV])
                    if br == 0:
                        fi = (h % 4 == 0)
                        IMP = IMPS[h // 4]
                        dst = IMP if fi else TMPI
                        cx.op("dve", "tensor_tensor", out=dst[:], in0=accv[:, :, 65:97],
                              in1=RINV[:].unsqueeze(2).to_broadcast([128, 4, 32]), op=ALU.mult,
                              reads=[acc, RINV], writes=[dst])
                        if not fi:
                            cx.op("pool", "tensor_tensor", out=IMP[:], in0=IMP[:], in1=TMPI[:], op=ALU.add,
                                  reads=[IMP, TMPI], writes=[IMP])
                    cx.op("dve", "tensor_tensor", out=COEF[:], in0=RINV[:], in1=GT[:, 4 * c:4 * c + 4, 8 * br + h],
                          op=ALU.mult, reads=[RINV, GT], writes=[COEF])
                    cb3 = COEF[:].unsqueeze(2).to_broadcast([128, 4, 64])
                    if first_branch:
                        cx.op("dve", "tensor_tensor", out=OA[:, :, 64 * h:64 * h + 64], in0=accv[:, :, 0:64], in1=cb3,
                              op=ALU.mult, reads=[acc, COEF], writes=[OA])
                    else:
                        cx.op("dve", "tensor_tensor", out=TMPO[:], in0=accv[:, :, 0:64], in1=cb3, op=ALU.mult,
                              reads=[acc, COEF], writes=[TMPO])
                        cx.op("pool", "tensor_tensor", out=OA[:, :, 64 * h:64 * h + 64], in0=OA[:, :, 64 * h:64 * h + 64],
                              in1=TMPO[:], op=ALU.add, reads=[OA, TMPO], writes=[OA])
                return f

            def to_OT(SRC, c, fc0):
                for fc in range(4):
                    for j in range(4):
                        col = ((fc % 2) * 4 + j) * 128
                        cx.op("pe", "transpose", psb(6 + fc // 2)[:, col:col + 128], SRC[:, j, 128 * fc:128 * fc + 128],
                              ident_b, reads=[SRC, CBB], writes=[PS[6 + fc // 2]])
                for fc in range(4):
                    cx.op("act", "copy", out=OT[:, fc0 + fc, 512 * c:512 * c + 512],
                          in_=psb(6 + fc // 2)[:, (fc % 2) * 512:(fc % 2) * 512 + 512], reads=[PS[6 + fc // 2]], writes=[OT])

            for c in range(4):
                qs = slice(512 * c, 512 * c + 512)
                for h in range(8):
                    g, b_ = h // 4, 64 * (h % 2)
                    acc = next_acc()
                    unit(KCT[b_:b_ + 64, g, :], QN[b_:b_ + 64, h // 2, qs],
                         [], VCX[:, g, 0:97], acc, 97, True,
                         KCT, QN, VCX, after=nsa_final(acc, 97, c, h, 0, True), mmask=(CMN[:, qs], CMN), last=True)
                for h in range(8):
                    g, b_ = h // 4, 64 * (h % 2)
                    acc = next_acc()
                    tiles = list(range(max(0, 4 * c - 4), 4 * c + 4))
                    for n_, i in enumerate(tiles):
                        mk = MSK[:, 3 + (4 * c - i), :] if i < 4 * c else MSK[:, i - 4 * c, :]
                        unit(KW[b_:b_ + 64, g, 128 * i:128 * i + 128], QN[b_:b_ + 64, h // 2, qs],
                             [], VT[:, i, 2 + g, :], acc, 65, n_ == 0, KW, QN, VT,
                             after=(nsa_final(acc, 65, c, h, 2, False) if n_ == len(tiles) - 1 else None), mmask=(mk, MSK),
                             last=(n_ == len(tiles) - 1))
                flush()
                for g in range(2):
                    IMPg = IMPS[g]
                    cx.op("dve", "tensor_tensor", out=IMPM[:], in0=IMPg[:], in1=IMT[:, 0, 4 * c:4 * c + 4, :], op=ALU.mult,
                          reads=[IMPg, IMT], writes=[IMPM])
                    cx.op("dve", "tensor_tensor", out=IMPM[:], in0=IMPM[:], in1=IMT[:, 1, 4 * c:4 * c + 4, :], op=ALU.add,
                          reads=[IMPM, IMT], writes=[IMPM])
                    for j in range(4):
                        cx.op("dve", "max", out=TOP8[:, j, :], in_=IMPM[:, j, :], reads=[IMPM], writes=[TOP8])
                    for j in range(4):
                        cx.op("dve", "tensor_scalar", out=NSELB[:, j, :], in0=IMPM[:, j, :], scalar1=TOP8[:, j, 7:8],
                              scalar2=NEG, op0=ALU.is_lt, op1=ALU.mult, reads=[IMPM, TOP8], writes=[NSELB])
                    for j in range(4):
                        cx.op("pe", "transpose", psb(6)[0:32, 128 * j:128 * j + 128], NSELB[:, j, :], ident_b,
                              reads=[NSELB, CBB], writes=[PS[6]])
                    cx.op("act", "copy", out=NSELT[0:32, g, :], in_=psb(6)[0:32, 0:512], reads=[PS[6]], writes=[NSELT])
                for h in range(8):
                    g, b_ = h // 4, 64 * (h % 2)
                    acc = next_acc()
                    tiles = list(range(0, 4 * c + 4))
                    for n_, i in enumerate(tiles):
                        KX = 32
                        ex = [(CBB[0:KX, CB_EX + 128 * i:CB_EX + 128 * i + 128], NSELT[0:KX, g, :], 0, 512, [CBB, NSELT])]
                        unit(KS[b_:b_ + 64, g, 128 * i:128 * i + 128], QN[b_:b_ + 64, h // 2, qs], ex,
                             VT[:, i, g, :], acc, 65, n_ == 0, KS, QN, VT,
                             after=(nsa_final(acc, 65, c, h, 1, False) if n_ == len(tiles) - 1 else None),
                             mmask=((MSK[:, i - 4 * c, :], MSK) if i >= 4 * c else None), last=(n_ == len(tiles) - 1))
                flush()
                cx.op("act", "copy", out=OAB[:], in_=OA[:], reads=[OA], writes=[OAB])
                to_OT(OAB, c, 0)
            cx.barrier()

            WINB = mem.view("winb", R_WIN, [128, 8, NB], BF16)
            cx.dma("pool", WINB[:], winB_d.rearrange("(kc p) n -> p kc n", p=128), writes=[WINB])
            o = R_PH
            QD = mem.view("QD", o, [128, 4, 2048], BF16); o += 16 * KB
            QI = mem.view("QI", o, [128, 4, 2048], BF16); o += 16 * KB
            KI = mem.view("KI", o, [128, 2048], BF16); o += 4 * KB
            CKT = mem.view("CKT", o, [128, 2048], BF16); o += 4 * KB
            KRT = mem.view("KRT", o, [128, 2048], BF16); o += 4 * KB
            WI = mem.view("WI", o, [128, 16, 8], F32); o += 512
            T1 = mem.view("T1", o, [128, 512], F32); o_JB = o; o += 2048
            T2 = mem.view("T2", o, [128, 512], F32); o += 2048
            CKN = []
            for i in range(2):
                CKN.append(mem.view("CKN%d" % i, o, [128, 128], F32)); o += 512
            o_RB = o
            assert o + 4096 + 128 <= TOT, o
            SSD = Buf(SMALL[:, 56:60], "SSD")
            for tc in range(4):
                sl = slice(512 * tc, 512 * tc + 512)
                for j in range(4):
                    proj_unit(WINB, 256 * j, 256 * j + 128, 128, tc, QD[:, j, sl], QD)
                for j in range(4):
                    proj_unit(WINB, 1024 + 256 * j, 1024 + 256 * j + 128, 128, tc, QI[:, j, sl], QI)
                proj_unit(WINB, 2048, 2176, 128, tc, KI[:, sl], KI)
                proj_unit(WINB, 2304, 2320, 16, tc, KRT[0:16, sl], KRT)
            for i in range(NT):
                pb = PS[4 + i % 2]
                ck = CKN[i % 2]
                for kc in range(8):
                    cx.op("pe", "matmul", pb[:, 0:136], HT[:, kc, 128 * i:128 * i + 128], WINB[:, kc, NB_FM:NB],
                          start=(kc == 0), stop=(kc == 7), reads=[HT, WINB], writes=[pb])
                cx.op("act", "activation", out=ck[:], in_=pb[:, 0:128], func=AF.Square, accum_out=SSD[:, 0:1],
                      reads=[pb], writes=[ck, SSD])
                cx.op("act", "activation", out=SSD[:, 1:2], in_=SSD[:, 0:1], func=AF.Sqrt, scale=1.0 / 128, bias=1e-6,
                      reads=[SSD], writes=[SSD])
                cx.op("dve", "reciprocal", out=SSD[:, 2:3], in_=SSD[:, 1:2], reads=[SSD], writes=[SSD])
                cx.op("dve", "tensor_scalar", out=ck[:], in0=pb[:, 0:128], scalar1=SSD[:, 2:3], scalar2=None, op0=ALU.mult,
                      reads=[pb, SSD], writes=[ck])
                cx.op("act", "mul", out=WI[:, i, :], in_=pb[:, 128:136], mul=float(8 ** -0.5 * 64 ** -0.5), reads=[pb], writes=[WI])
                cx.op("pe", "transpose", PS[6][:, 128 * (i % 4):128 * (i % 4) + 128], ck[:], ident_f, reads=[ck, CFB], writes=[PS[6]])
                if i % 4 == 3:
                    cx.op("act", "activation", out=CKT[:, 512 * (i // 4):512 * (i // 4) + 512], in_=PS[6][:], func=AF.Identity,
                          scale=vec(83, 84), reads=[PS[6], CFB], writes=[CKT])
            cx.barrier()

            o = R_WIN
            KHT = mem.view("KHT", o, [128, 4, 2048], BF16); o += 16 * KB
            VH = mem.view("VH", o, [128, 16, 8, 65], BF16); o += 16640
            WSM = mem.view("WSM", o, [128, 1216], BF16); o += 2432
            PT = []
            for i in range(4):
                PT.append(mem.view("PT%d" % i, o, [128, 512], BF16)); o += 1024
            ODB = mem.view("ODB", o, [128, 4, 512], BF16); o += 4096
            assert o <= R_PH, o
            NMS = [[mem.view("NMA%d" % j, R_HT + 3 * KB * j, [128, 1536], BF16) for j in range(4)],
                   [mem.view("NMB%d" % j, CF_C * 4 + 4 * KB * j, [128, 2048], BF16) for j in range(4)]]
            IB = [mem.view("IB%d" % j, R_HT + 12 * KB + 8 * KB * j, [128, 2048], F32) for j in range(2)]
            RB = [mem.view("RB%d" % j, o_RB + 2048 * j, [128, 512], F32) for j in range(2)]
            BS = []
            for n2, c0 in enumerate((8, 40)):
                BS.append(dict(LO=Buf(SMALL[:, c0:c0 + 1], "LO%d" % n2), MID=Buf(SMALL[:, c0 + 1:c0 + 2], "MID%d" % n2),
                               CNT=Buf(SMALL[:, c0 + 2:c0 + 3], "CNT%d" % n2), TMPS=Buf(SMALL[:, c0 + 3:c0 + 4], "TMPS%d" % n2),
                               W0=Buf(SMALL[:, c0 + 4:c0 + 5], "W0%d" % n2), MN=Buf(SMALL[:, c0 + 5:c0 + 6], "MN%d" % n2),
                               MX8=Buf(SMALL[:, c0 + 6:c0 + 14], "MX8%d" % n2),
                               WK=mem.view("WK%d" % n2, o_RB + 4096 + 64 * n2, [128, 16], F32),
                               JB=Buf(mem.view("JBx%d" % n2, o_JB + 2048 * n2, [128, 1024], BF16).ap.bitcast(mybir.dt.uint8), "JB%d" % n2)))
            cx.dma("pool", WSM[:], wsm_d, writes=[WSM])
            cx.op("pool", "memset", VH[:, :, :, 64:65], 1.0, writes=[VH])
            n_ = 0
            for j in range(4):
                for tc in range(4):
                    pb = PS[4 + n_ % 2]; n_ += 1
                    cx.op("pe", "matmul", pb[:], WSM[:, 128 * j:128 * j + 128], CKT[:, 512 * tc:512 * tc + 512],
                          start=True, stop=False, reads=[WSM, CKT], writes=[pb])
                    cx.op("pe", "matmul", pb[:], CBB[0:16, CB_SEL:CB_SEL + 128], KRT[0:16, 512 * tc:512 * tc + 512],
                          start=False, stop=True, reads=[CBB, KRT], writes=[pb])
                    cx.op("act", "copy", out=KHT[:, j, 512 * tc:512 * tc + 512], in_=pb[:], reads=[pb], writes=[KHT])
            for i in range(NT):
                pb = PS[4 + n_ % 2]; n_ += 1
                cx.op("pe", "matmul", pb[:], CKT[:, 128 * i:128 * i + 128], WSM[:, 512:1024], start=True, stop=True,
                      reads=[CKT, WSM], writes=[pb])
                cx.op("act", "copy", out=VH[:, i, :, 0:64], in_=pb[:].rearrange("p (h d) -> p h d", h=8), reads=[pb], writes=[VH])

            pipe["pend"] = []
            grp.clear()
            def idx_scores(c, j):
                T = 4 * c + j
                Wc = 512 * (c + 1)
                Wv = 128 * (T + 1)
                IBt = IB[T % 2]
                for sc in range(c + 1):
                    N = min(512, Wv - 512 * sc)
                    for h in range(8):
                        b_ = 64 * (h % 2)
                        L = PS[6 + lcnt[0] % 2]; lcnt[0] += 1
                        cx.op("pe", "matmul", L[:, 0:N], QI[b_:b_ + 64, h // 2, 128 * T:128 * T + 128],
                              KI[b_:b_ + 64, 512 * sc:512 * sc + N], start=True, stop=True, reads=[QI, KI], writes=[L])
                        if h == 0:
                            cx.op("dve", "tensor_scalar", out=IBt[:, 512 * sc:512 * sc + N], in0=L[:, 0:N], scalar1=0.0,
                                  scalar2=WI[:, T, h:h + 1], op0=ALU.max, op1=ALU.mult, reads=[L, WI], writes=[IBt])
                        else:
                            rb = RB[h % 2]
                            cx.op("dve", "tensor_scalar", out=rb[:, 0:N], in0=L[:, 0:N], scalar1=0.0,
                                  scalar2=WI[:, T, h:h + 1], op0=ALU.max, op1=ALU.mult, reads=[L, WI], writes=[rb])
                            cx.op("pool", "tensor_tensor", out=IBt[:, 512 * sc:512 * sc + N], in0=IBt[:, 512 * sc:512 * sc + N],
                                  in1=rb[:, 0:N], op=ALU.add, reads=[IBt, rb], writes=[IBt])
                MX8, MN = BS[j % 2]["MX8"], BS[j % 2]["MN"]
                if T >= 2:
                    cx.op("dve", "max", out=MX8[:], in_=IBt[:, 0:Wv], reads=[IBt], writes=[MX8])
                    cx.op("dve", "tensor_reduce", out=MN[:], in_=IBt[:, 0:Wv], axis=AX.X, op=ALU.min, reads=[IBt], writes=[MN])
                cx.op("pool", "affine_select", out=IBt[:, 128 * T:128 * T + 128], in_=IBt[:, 128 * T:128 * T + 128],
                      pattern=[[-1, 128]], compare_op=ALU.is_ge, fill=-3.0e38, base=0, channel_multiplier=1,
                      reads=[IBt], writes=[IBt])
                if Wv < Wc:
                    cx.op("pool", "memset", IBt[:, Wv:Wc], -3.0e38, writes=[IBt])

            def idx_bisect_pair(c, js):
                Wc = 512 * (c + 1)
                chains = []
                for j in js:
                    T = 4 * c + j
                    chains.append((j, T, 128 * (T + 1), IB[T % 2], BS[j % 2]))
                act_ = [ch for ch in chains if ch[1] >= 2]
                for (j, T, Wv, IBt, B) in chains:
                    if T >= 2:
                        cx.op("dve", "tensor_copy", out=B["LO"][:], in_=B["MN"][:], reads=[B["MN"]], writes=[B["LO"]])
                        cx.op("dve", "tensor_tensor", out=B["W0"][:], in0=B["MX8"][:, 0:1], in1=B["MN"][:], op=ALU.subtract,
                              reads=[B["MX8"], B["MN"]], writes=[B["W0"]])
                        cx.op("dve", "tensor_scalar", out=B["WK"][:, 0:BIS_ITERS], in0=CFB[:, CF_P2:CF_P2 + BIS_ITERS],
                              scalar1=B["W0"][:], scalar2=None, op0=ALU.mult, reads=[CFB, B["W0"]], writes=[B["WK"]])
                    else:
                        cx.op("dve", "memset", B["LO"][:], -1.0e30, writes=[B["LO"]])
                for k in range(BIS_ITERS):
                    for (j, T, Wv, IBt, B) in act_:
                        cx.op("dve", "tensor_tensor", out=B["MID"][:], in0=B["LO"][:], in1=B["WK"][:, k:k + 1], op=ALU.add,
                              reads=[B["LO"], B["WK"]], writes=[B["MID"]])
                    for (j, T, Wv, IBt, B) in act_:
                        cx.op("dve", "tensor_scalar", out=B["JB"][:, 0:Wv], in0=IBt[:, 0:Wv], scalar1=B["MID"][:], scalar2=0.0,
                              op0=ALU.is_ge, op1=ALU.add, accum_out=B["CNT"][:], reads=[IBt, B["MID"]], writes=[B["JB"], B["CNT"]])
                    for (j, T, Wv, IBt, B) in act_:
                        cx.op("dve", "tensor_scalar", out=B["TMPS"][:], in0=B["CNT"][:], scalar1=255.5, scalar2=B["WK"][:, k:k + 1],
                              op0=ALU.is_ge, op1=ALU.mult, reads=[B["CNT"], B["WK"]], writes=[B["TMPS"]])
                    for (j, T, Wv, IBt, B) in act_:
                        cx.op("dve", "tensor_tensor", out=B["LO"][:], in0=B["LO"][:], in1=B["TMPS"][:], op=ALU.add,
                              reads=[B["LO"], B["TMPS"]], writes=[B["LO"]])
                for (j, T, Wv, IBt, B) in chains:
                    nm = NMS[c % 2][j]
                    cx.op("dve", "tensor_scalar", out=nm[:, 0:Wc], in0=IBt[:, 0:Wc], scalar1=B["LO"][:], scalar2=NEG,
                          op0=ALU.is_lt, op1=ALU.mult, reads=[IBt, B["LO"]], writes=[nm])

            def idx_slices(c):
                return [lambda: idx_scores(c, 0), lambda: idx_scores(c, 1), lambda: idx_bisect_pair(c, (0, 1)), lambda: None,
                        lambda: idx_scores(c, 2), lambda: idx_scores(c, 3), lambda: idx_bisect_pair(c, (2, 3)), lambda: None]

            lcnt = [0]
            for f_ in idx_slices(0):
                f_()
            for c in range(4):
                qs = slice(512 * c, 512 * c + 512)
                NMc = NMS[c % 2]
                sl_next = idx_slices(c + 1) if c < 3 else []
                for h in range(8):
                    if sl_next:
                        sl_next[h]()
                    b_ = 64 * (h % 2)
                    acc = next_acc()
                    accv = acc[:, 0:260].rearrange("p (j n) -> p j n", j=4)

                    def fin(acc=acc, accv=accv, h=h):
                        cx.op("dve", "reciprocal", out=RINV[:], in_=accv[:, :, 64], reads=[acc], writes=[RINV])
                        cx.op("dve", "tensor_tensor", out=ODB[:, :, 64 * h:64 * h + 64], in0=accv[:, :, 0:64],
                              in1=RINV[:].unsqueeze(2).to_broadcast([128, 4, 64]), op=ALU.mult,
                              reads=[acc, RINV], writes=[ODB])
                    tiles = list(range(0, 4 * c + 4))
                    for q_, i in enumerate(tiles):
                        ex = [(NMc[j][:, 128 * i:128 * i + 128], ident_b, 128 * j, 128 * j + 128, [NMc[j], CBB]) for j in range(4)]
                        unit(KHT[b_:b_ + 64, h // 2, 128 * i:128 * i + 128], QD[b_:b_ + 64, h // 2, qs], [],
                             VH[:, i, h, :], acc, 65, q_ == 0, KHT, QD, VH, after=(fin if q_ == len(tiles) - 1 else None),
                             ex128=ex, last=(q_ == len(tiles) - 1))
                flush()
                to_OT(ODB, c, 4)
            cx.barrier()

            o = R_HT
            X1 = mem.view("X1", o, [128, 4, 1024], F32); o += 16 * KB
            H2T = mem.view("H2T", o, [128, 8, 512], BF16); o += 8 * KB
            ACTT = mem.view("ACTT", o, [128, 22, 512], BF16); o += 22 * KB
            WOUT = mem.view("WOUT", CF_C * 4, [128, 8, 1024], BF16)
            WG = []
            for i in range(3):
                WG.append(mem.view("WG%d" % i, o, [128, 8, 256], BF16)); o += 4 * KB
            WDN = mem.view("WDN", o, [128, 22, 1024], BF16); o += 44 * KB
            XIN = []
            for i in range(2):
                XIN.append(mem.view("xin%d" % i, o, [128, 1024], F32)); o += 4 * KB
            JUNK = mem.view("junk", o, [128, 1024], F32); o += 4 * KB
            XS = mem.view("xs", o, [128, 1024], F32); o += 4 * KB
            TMPY = mem.view("tmpy", o, [128, 1024], F32); o += 4 * KB
            SIL = []
            for i in range(2):
                SIL.append(mem.view("sil%d" % i, o, [128, 512], F32)); o += 2 * KB
            assert o <= TOT, o
            ST = Buf(SMALL[:, 44:52], "ST")
            cx.dma("pool", WDN[:], wdn_d.rearrange("(c p) n -> p c n", p=128), writes=[WDN])
            wout_v = wout_d.rearrange("(kc p) n -> p kc n", p=128)

            cx.dma("pool", WOUT[:], wout_v, writes=[WOUT])
            for gi in range(4):
                for j in range(4):
                    T = 4 * gi + j
                    xi = XIN[j % 2]
                    cx.dma("sp", xi[:], x_d[seq, 128 * T:128 * T + 128, :], writes=[xi])
                    yb = [PS[2 * (j % 2)], PS[2 * (j % 2) + 1]]
                    for n in range(2):
                        for kc in range(8):
                            cx.op("pe", "matmul", yb[n][:], OT[:, kc, 128 * T:128 * T + 128], WOUT[:, kc, 512 * n:512 * n + 512],
                                  start=(kc == 0), stop=(kc == 7), reads=[OT, WOUT], writes=[yb[n]])
                    for n in range(2):
                        cx.op("act", "activation", out=JUNK[:, 512 * n:512 * n + 512], in_=yb[n][:], func=AF.Square,
                              accum_out=ST[:, n:n + 1], reads=[yb[n]], writes=[JUNK, ST])
                    cx.op("dve", "tensor_tensor", out=ST[:, 2:3], in0=ST[:, 0:1], in1=ST[:, 1:2], op=ALU.add, reads=[ST], writes=[ST])
                    cx.op("act", "activation", out=ST[:, 3:4], in_=ST[:, 2:3], func=AF.Sqrt, scale=1.0 / D, bias=1e-6, reads=[ST], writes=[ST])
                    cx.op("dve", "reciprocal", out=ST[:, 4:5], in_=ST[:, 3:4], reads=[ST], writes=[ST])
                    for n in range(2):
                        cx.op("dve", "scalar_tensor_tensor", out=TMPY[:, 512 * n:512 * n + 512], in0=yb[n][:], scalar=ST[:, 4:5],
                              in1=G1[:, 512 * n:512 * n + 512], op0=ALU.mult, op1=ALU.mult, reads=[yb[n], ST, G1], writes=[TMPY])
                    cx.op("dve", "tensor_tensor", out=X1[:, j, :], in0=TMPY[:], in1=xi[:], op=ALU.add, reads=[TMPY, xi], writes=[X1])
                    cx.op("act", "activation", out=JUNK[:], in_=X1[:, j, :], func=AF.Square, accum_out=ST[:, 0:1],
                          reads=[X1], writes=[JUNK, ST])
                    cx.op("act", "activation", out=ST[:, 3:4], in_=ST[:, 0:1], func=AF.Sqrt, scale=1.0 / D, bias=1e-6, reads=[ST], writes=[ST])
                    cx.op("dve", "reciprocal", out=ST[:, 4:5], in_=ST[:, 3:4], reads=[ST], writes=[ST])
                    cx.op("dve", "tensor_scalar", out=XS[:], in0=X1[:, j, :], scalar1=ST[:, 4:5], scalar2=None, op0=ALU.mult,
                          reads=[X1, ST], writes=[XS])
                    for fc in range(8):
                        pb = PS[4 + fc // 4]
                        cx.op("pe", "transpose", pb[:, 128 * (fc % 4):128 * (fc % 4) + 128], XS[:, 128 * fc:128 * fc + 128],
                              ident_f, reads=[XS, CFB], writes=[pb])
                    for fc in range(8):
                        pb = PS[4 + fc // 4]
                        cx.op("act", "activation", out=H2T[:, fc, 128 * j:128 * j + 128],
                              in_=pb[:, 128 * (fc % 4):128 * (fc % 4) + 128], func=AF.Identity,
                              scale=DERV[:, 2, fc, seq:seq + 1], bias=DERV[:, 3, fc, seq:seq + 1],
                              reads=[pb, DERV], writes=[H2T])
                for ch in range(22):
                    wg = WG[ch % 3]
                    if not cv_waited[0]:
                        for sem_cv in cvsems:
                            nc.sync.wait_ge(sem_cv, 16)
                        cv_waited[0] = True
                    cx.dma("sp", wg[:].rearrange("p a b -> p (a b)"), wgubf_d[ch], writes=[wg])
                    pg, pu = PS[2 * (ch % 2)], PS[2 * (ch % 2) + 1]
                    for kc in range(8):
                        cx.op("pe", "matmul", pg[:], wg[:, kc, 0:128], H2T[:, kc, :], start=(kc == 0), stop=(kc == 7),
                              reads=[wg, H2T], writes=[pg])
                    for kc in range(8):
                        cx.op("pe", "matmul", pu[:], wg[:, kc, 128:256], H2T[:, kc, :], start=(kc == 0), stop=(kc == 7),
                              reads=[wg, H2T], writes=[pu])
                    sl_ = SIL[ch % 2]
                    cx.op("act", "activation", out=sl_[:], in_=pg[:], func=AF.Silu, reads=[pg], writes=[sl_])
                    cx.op("dve", "tensor_tensor", out=ACTT[:, ch, :], in0=sl_[:], in1=pu[:], op=ALU.mult,
                          reads=[sl_, pu], writes=[ACTT])
                for j in range(4):
                    T = 4 * gi + j
                    zb = [PS[4 + 2 * (j % 2)], PS[5 + 2 * (j % 2)]]
                    for n in range(2):
                        for ch in range(22):
                            cx.op("pe", "matmul", zb[n][:], ACTT[:, ch, 128 * j:128 * j + 128], WDN[:, ch, 512 * n:512 * n + 512],
                                  start=(ch == 0), stop=(ch == 21), reads=[ACTT, WDN], writes=[zb[n]])
                    for n in range(2):
                        cx.op("act", "activation", out=JUNK[:, 512 * n:512 * n + 512], in_=zb[n][:], func=AF.Square,
                              accum_out=ST[:, n:n + 1], reads=[zb[n]], writes=[JUNK, ST])
                    cx.op("dve", "tensor_tensor", out=ST[:, 2:3], in0=ST[:, 0:1], in1=ST[:, 1:2], op=ALU.add, reads=[ST], writes=[ST])
                    cx.op("act", "activation", out=ST[:, 3:4], in_=ST[:, 2:3], func=AF.Sqrt, scale=1.0 / D, bias=1e-6, reads=[ST], writes=[ST])
                    cx.op("dve", "reciprocal", out=ST[:, 4:5], in_=ST[:, 3:4], reads=[ST], writes=[ST])
                    for n in range(2):
                        cx.op("dve", "scalar_tensor_tensor", out=TMPY[:, 512 * n:512 * n + 512], in0=zb[n][:], scalar=ST[:, 4:5],
                              in1=G2[:, 512 * n:512 * n + 512], op0=ALU.mult, op1=ALU.mult, reads=[zb[n], ST, G2], writes=[TMPY])
                    cx.op("dve", "tensor_tensor", out=XS[:], in0=TMPY[:], in1=X1[:, j, :], op=ALU.add, reads=[TMPY, X1], writes=[XS])
                    cx.dma("sp", out_d[seq, 128 * T:128 * T + 128, :], XS[:], reads=[XS])
            cx.barrier()

        cx.barrier()
        cx.finish()
    return nc


def _prep(inputs):
    inp = {k: np.asarray(v) for k, v in inputs.items()}
    cf, cb, imt, wsm = _host_consts(inp)
    A, B = _col_index()
    shared = {
        "w_ada": np.ascontiguousarray(inp['w_ada'][0]),
        "cf": cf, "cb": cb, "imt": imt, "wsm": wsm,
        "w_inA": np.ascontiguousarray(inp['w_in'][0][:, A]),
        "w_inB": np.ascontiguousarray(inp['w_in'][0][:, B]),
        "cmp_w1": np.ascontiguousarray(inp['cmp_w1'][0]),
        "b2v": np.ascontiguousarray(inp['cmp_b2'][0, 1]),
        "w_out": np.ascontiguousarray(inp['w_out'][0]),
        "w_gate_up": np.ascontiguousarray(
            inp['w_gate_up'][0].reshape(8, 128, 2, 22, 128).transpose(3, 1, 0, 2, 4).reshape(22, 128, 2048)),
        "w_down": np.ascontiguousarray(inp['w_down'][0]),
    }
    maps = []
    for c in range(8):
        m = dict(shared)
        m["x"] = np.ascontiguousarray(inp['x'][2 * c:2 * c + 2])
        m["cT"] = np.ascontiguousarray(inp['c'][2 * c:2 * c + 2].T.reshape(8, 128, 2).transpose(1, 0, 2))
        maps.append(m)
    return maps


def kernel(**inputs):
    maps = _prep(inputs)
    nc = build()
    res = run_bass_kernel_spmd(nc, maps, core_ids=list(range(8)))
    return np.concatenate([r["out"] for r in res.results], axis=0).astype(np.float32)
```

```python
import numpy as np
import concourse.bass as bass
import concourse.mybir as mybir
from concourse.bass_utils import run_bass_kernel_spmd
from contextlib import ExitStack

F32 = mybir.dt.float32
BF16 = mybir.dt.bfloat16
ALU = mybir.AluOpType
AF = mybir.ActivationFunctionType
AX = mybir.AxisListType

S = 2048
D = 1024
NT = 16
DFF = 2816
NEG = -30000.0
IN_SPLITS = (512, 128, 128, 128, 128, 128, 128, 24, 512, 128, 16, 512, 64, 8)
NAMES = ['nq', 'nkc', 'nvc', 'nks', 'nvs', 'nkw', 'nvw', 'ngate', 'dq', 'dckv', 'dkr', 'iq', 'ik', 'iw']
OFF = dict(zip(NAMES, np.cumsum((0,) + IN_SPLITS)[:-1]))
NA_FM = 19 * 128
NA = NA_FM + 280
NB_FM = 18 * 128 + 32
NB = NB_FM + 136
BIS_ITERS = 12
import os as _os
_DBG = _os.environ.get('KDBG', '')


class _E:
    def __init__(self, name, eng, sem):
        self.name, self.eng, self.sem, self.tick, self.waited = name, eng, sem, 0, {}


class _St:
    __slots__ = ("w", "r")

    def __init__(self):
        self.w = None
        self.r = {}


class Buf:
    def __init__(self, ap, key):
        self.ap = ap
        self.key = key

    def __getitem__(self, idx):
        return self.ap[idx]


class Ctx:
    def __init__(self, nc, es, n_dma_sems=8):
        self.nc, self.es = nc, es
        self.E = {}
        for name, eng in (("pe", nc.tensor), ("act", nc.scalar), ("dve", nc.vector),
                          ("pool", nc.gpsimd), ("sp", nc.sync)):
            self.E[name] = _E(name, eng, es.enter_context(nc.semaphore("s_" + name)))
        self.dsems = {q: [[es.enter_context(nc.semaphore("d_%s%d" % (q, i))), 0] for i in range(n_dma_sems)]
                      for q in ("sp", "pool")}
        self.dnext = {"sp": 0, "pool": 0}
        self.st = {}

    def _state(self, b):
        k = b.key if isinstance(b, Buf) else (b if isinstance(b, str) else b.name)
        s = self.st.get(k)
        if s is None:
            s = self.st[k] = _St()
        return s

    def _wait(self, E, dep):
        kind, tk = dep
        if kind == E.name and E.name == "pe":
            return
        if E.waited.get(kind, 0) >= tk:
            return
        sem = self.dsems[kind[0]][kind[1]][0] if isinstance(kind, tuple) else self.E[kind].sem
        E.eng.wait_ge(sem, tk)
        E.waited[kind] = tk

    def _deps(self, E, reads, writes):
        deps = []
        for b in reads:
            s = self._state(b)
            if s.w is not None:
                deps.append(s.w)
        for b in writes:
            s = self._state(b)
            if s.w is not None:
                deps.append(s.w)
            deps.extend(s.r.items())
        for d in deps:
            self._wait(E, d)

    def _mark(self, token, reads, writes):
        for b in reads:
            s = self._state(b)
            if s.r.get(token[0], 0) < token[1]:
                s.r[token[0]] = token[1]
        for b in writes:
            s = self._state(b)
            s.w = token
            s.r = {}

    def op(self, en, fn, *args, reads=(), writes=(), **kw):
        E = self.E[en]
        self._deps(E, reads, writes)
        ins = getattr(E.eng, fn)(*args, **kw)
        E.tick += 1
        ins.then_inc(E.sem, 1)
        self._mark((en, E.tick), reads, writes)
        return ins

    def dma(self, q, out, in_, reads=(), writes=(), **kw):
        E = self.E[q]
        self._deps(E, reads, writes)
        i = self.dnext[q]
        self.dnext[q] = (i + 1) % len(self.dsems[q])
        slot = self.dsems[q][i]
        kind = (q, i)
        if slot[1] > 0:
            self._wait(E, (kind, slot[1]))
        slot[1] += 16
        E.eng.dma_start(out=out, in_=in_, **kw).then_inc(slot[0], 16)
        self._mark((kind, slot[1]), reads, writes)

    def barrier(self):
        toks = [(n, e.tick) for n, e in self.E.items() if e.tick > 0]
        for q in self.dsems:
            for i, slot in enumerate(self.dsems[q]):
                if slot[1] > 0:
                    toks.append(((q, i), slot[1]))
        for n, e in self.E.items():
            for t in toks:
                if t[0] == n:
                    if n != "sp" and e.waited.get(n, 0) < t[1]:
                        e.eng.wait_ge(e.sem, t[1])
                        e.waited[n] = t[1]
                else:
                    self._wait(e, t)
        self.st = {}

    def finish(self):
        E = self.E["sp"]
        for q in self.dsems:
            for i, slot in enumerate(self.dsems[q]):
                if slot[1] > 0:
                    self._wait(E, ((q, i), slot[1]))


class Mem:
    def __init__(self, big):
        self.big = big

    def view(self, key, off, shape, dt, pbase=0):
        esz = 4 if dt == F32 else 2
        nel = int(np.prod(shape[1:]))
        assert off % 4 == 0
        a = off // 2
        n = nel * esz // 2
        ap = self.big[pbase:pbase + shape[0], a:a + n]
        if dt == F32:
            ap = ap.bitcast(F32)
        if len(shape) == 3:
            ap = ap.rearrange("p (a b) -> p a b", a=shape[1])
        elif len(shape) == 4:
            ap = ap.rearrange("p (a b c) -> p a b c", a=shape[1], b=shape[2])
        return Buf(ap, key)


def _swap_head(cols):
    c = np.array(cols).copy()
    c[0:8] = cols[8:16]
    c[8:16] = cols[0:8]
    return c


def _col_index():
    def head(base, h):
        return np.arange(base + 64 * h, base + 64 * h + 64)
    A = []
    for j in range(4):
        a = np.concatenate([head(OFF['nq'], 2 * j), head(OFF['nq'], 2 * j + 1)])
        b = np.concatenate([_swap_head(head(OFF['nq'], 2 * j)), _swap_head(head(OFF['nq'], 2 * j + 1))])
        A += [a, b]
    a = np.concatenate([head(OFF['nkc'], 0), head(OFF['nkc'], 1)])
    b = np.concatenate([_swap_head(head(OFF['nkc'], 0)), _swap_head(head(OFF['nkc'], 1))])
    A += [a, b]
    A += [np.arange(OFF['nvc'], OFF['nvc'] + 128)]
    for nm in ('nks', 'nkw'):
        for g in range(2):
            a = np.concatenate([head(OFF[nm], g), head(OFF[nm], g)])
            b = np.concatenate([_swap_head(head(OFF[nm], g)), _swap_head(head(OFF[nm], g))])
            A += [a, b]
    A += [np.arange(OFF['nvs'], OFF['nvs'] + 128), np.arange(OFF['nvw'], OFF['nvw'] + 128),
          np.arange(OFF['ngate'], OFF['ngate'] + 24)]
    A = np.concatenate(A)
    assert A.size == NA
    B = []
    for nm in ('dq', 'iq'):
        for j in range(4):
            a = np.concatenate([head(OFF[nm], 2 * j), head(OFF[nm], 2 * j + 1)])
            b = np.concatenate([_swap_head(head(OFF[nm], 2 * j)), _swap_head(head(OFF[nm], 2 * j + 1))])
            B += [a, b]
    a = np.concatenate([head(OFF['ik'], 0), head(OFF['ik'], 0)])
    b = np.concatenate([_swap_head(head(OFF['ik'], 0)), _swap_head(head(OFF['ik'], 0))])
    B += [a, b]
    kr = np.arange(OFF['dkr'], OFF['dkr'] + 16)
    B += [kr, _swap_head(kr)]
    B += [np.arange(OFF['dckv'], OFF['dckv'] + 128), np.arange(OFF['iw'], OFF['iw'] + 8)]
    B = np.concatenate(B)
    assert B.size == NB
    return A, B


CF_ID = 0
CF_C = 128
CF_S = CF_C + 2048
CF_V = CF_S + 2048
CF_P2 = CF_V + 84
CF_N = CF_P2 + BIS_ITERS
CB_ID = 0
CB_EX = 128
CB_OV = CB_EX + 2048
CB_PE = CB_OV + 32
CB_SEL = CB_PE + 64
CB_N = CB_SEL + 128


def _host_consts(inp):
    cf = np.zeros((128, CF_N), np.float32)
    cf[:, CF_ID:CF_ID + 128] = np.eye(128, dtype=np.float32)
    inv_freq = 1.0 / (np.float32(500000.0) ** (np.arange(0, 16, 2, dtype=np.float32) / np.float32(16)))
    ang = np.arange(S, dtype=np.float32)[:, None] * inv_freq[None, :].astype(np.float32)
    cos, sin = np.cos(ang).astype(np.float32), np.sin(ang).astype(np.float32)
    for p in range(128):
        j = p % 64
        if j < 16:
            cf[p, CF_C:CF_C + S] = cos[:, j % 8]
            cf[p, CF_S:CF_S + S] = (-1.0 if j < 8 else 1.0) * sin[:, j % 8]
        else:
            cf[p, CF_C:CF_C + S] = 1.0
    v = cf[:, CF_V:CF_V + 84]
    v[:, 0:48] = inp['b_ada'][0].reshape(48, 128).T
    v[:, 48:56] = inp['g_pre_mix'][0].reshape(8, 128).T
    v[:, 56:64] = inp['g_post_mix'][0].reshape(8, 128).T
    v[:, 64:72] = inp['g_pre_ffn'][0].reshape(8, 128).T
    v[:, 72:80] = inp['g_post_ffn'][0].reshape(8, 128).T
    v[:, 80] = inp['cmp_b1'][0, 0]
    v[:, 81] = inp['cmp_b1'][0, 1]
    v[:, 82] = np.concatenate([inp['cmp_b2'][0, 0], inp['cmp_b2'][0, 0]])
    v[:, 83] = inp['g_kv_norm'][0]
    cf[:, CF_P2:CF_P2 + BIS_ITERS] = (0.5 ** np.arange(1, BIS_ITERS + 1, dtype=np.float64)).astype(np.float32)[None, :]
    cb = np.zeros((128, CB_N), np.float32)
    cb[:, CB_ID:CB_ID + 128] = np.eye(128, dtype=np.float32)
    for j in range(32):
        cb[j, CB_EX + 64 * j:CB_EX + 64 * j + 64] = 1.0
    ci = np.arange(127)[:, None] * 16
    sj = np.arange(32)[None, :] * 64
    cb[0:127, CB_OV:CB_OV + 32] = ((ci < sj + 64) & (ci + 32 > sj)).astype(np.float32)
    cb[0:64, CB_PE:CB_PE + 32] = inp['cmp_pe'][0, 0].T
    cb[0:64, CB_PE + 32:CB_PE + 64] = inp['cmp_pe'][0, 1].T
    for i in range(16):
        cb[i, CB_SEL + i] = 1.0
        cb[i, CB_SEL + 64 + i] = 1.0
    t = (np.arange(16)[None, :, None] * 128 + np.arange(128)[:, None, None])
    blk = t // 64
    j = np.arange(32)[None, None, :]
    visible = j <= blk
    forced = (j == 0) | (j == blk) | (j == blk - 1)
    mm = (visible & ~forced).astype(np.float32)
    ba = np.where(visible, np.where(forced, 1e6, 0.0), -1e30).astype(np.float32)
    imt = np.concatenate([mm.reshape(128, 512), ba.reshape(128, 512)], axis=1)
    wuk = np.zeros((128, 4, 128), np.float32)
    for h in range(8):
        wuk[:, h // 2, (h % 2) * 64 + 16:(h % 2) * 64 + 64] = inp['w_uk'][0, h]
    wuv = np.transpose(inp['w_uv'][0], (1, 0, 2)).reshape(128, 512)
    w2 = np.concatenate([inp['cmp_w2'][0, 0], inp['cmp_w2'][0, 0], inp['cmp_w2'][0, 1]], axis=1)
    wsm = np.concatenate([wuk.reshape(128, 512), wuv, w2], axis=1).astype(np.float32)
    return cf, cb, imt, wsm


def build(n_seq=2, stage=99, dbg_cols=0):
    nc = bass.Bass("TRN2", target_bir_lowering=False)
    dt_ = nc.dram_tensor
    x_d = dt_("x", [2, S, D], F32, kind="ExternalInput").ap()
    cT_d = dt_("cT", [128, 8, 2], F32, kind="ExternalInput").ap()
    wada_d = dt_("w_ada", [D, 6 * D], F32, kind="ExternalInput").ap()
    cf_d = dt_("cf", [128, CF_N], F32, kind="ExternalInput").ap()
    cb_d = dt_("cb", [128, CB_N], F32, kind="ExternalInput").ap()
    imt_d = dt_("imt", [128, 1024], F32, kind="ExternalInput").ap()
    wsm_d = dt_("wsm", [128, 1216], F32, kind="ExternalInput").ap()
    winA_d = dt_("w_inA", [D, NA], F32, kind="ExternalInput").ap()
    winB_d = dt_("w_inB", [D, NB], F32, kind="ExternalInput").ap()
    w1_d = dt_("cmp_w1", [2, 2048, 128], F32, kind="ExternalInput").ap()
    b2v_d = dt_("b2v", [64], F32, kind="ExternalInput").ap()
    wout_d = dt_("w_out", [D, D], F32, kind="ExternalInput").ap()
    wgu_d = dt_("w_gate_up", [22, 128, 2048], F32, kind="ExternalInput").ap()
    wdn_d = dt_("w_down", [DFF, D], F32, kind="ExternalInput").ap()
    out_d = dt_("out", [2, S, D], F32, kind="ExternalOutput").ap()
    wgubf_d = dt_("wgu_bf16", [22, 128, 2048], BF16, kind="Internal").ap()
    wdnbf_d = dt_("wdn_bf16", [DFF, D], BF16, kind="Internal").ap()
    woutbf_d = dt_("wout_bf16", [D, D], BF16, kind="Internal").ap()
    winAbf_d = dt_("winA_bf16", [D, NA], BF16, kind="Internal").ap()
    winBbf_d = dt_("winB_bf16", [D, NB], BF16, kind="Internal").ap()
    dbg_d = dt_("dbg", [128, dbg_cols], F32, kind="ExternalOutput").ap() if dbg_cols else None

    with ExitStack() as es:
        cx = Ctx(nc, es)
        TOT = 206 * 1024
        big = es.enter_context(nc.sbuf_tensor("big", [128, TOT // 2], BF16))
        mem = Mem(big)
        PS = [Buf(es.enter_context(nc.psum_tensor("ps%d" % i, [128, 512], F32))[:], "ps%d" % i) for i in range(8)]

        def psb(i):
            return PS[i].ap.bitcast(BF16)

        KB = 1024
        o = 0
        CFB = mem.view("cf", o, [128, CF_N], F32); o += CF_N * 4
        CBB = mem.view("cb", o, [128, CB_N], BF16); o += CB_N * 2
        MSK = mem.view("msk", o, [128, 8, 512], BF16); o += 8 * 512 * 2
        CMN = mem.view("cmn", o, [128, 2048], BF16); o += 2048 * 2
        ONESF = mem.view("onesf", o, [128, 128], F32); o += 512
        MODC = mem.view("modc", o, [128, 48, 2], F32); o += 384
        DERV = mem.view("derv", o, [128, 6, 8, 2], F32); o += 384
        CACT = mem.view("cact", o, [128, 8, 2], F32); o += 64
        SMALL = mem.view("small", o, [128, 64], F32); o += 256
        G1 = mem.view("G1", o, [128, 1024], F32); o += 4096
        G2 = mem.view("G2", o, [128, 1024], F32); o += 4096
        assert o <= 44 * KB, o
        R_OT = 44 * KB
        R_HT = 76 * KB
        R_WIN = 108 * KB
        R_PH = 152 * KB
        OT = mem.view("OT", R_OT, [128, 8, 2048], BF16)
        HT = mem.view("HT", R_HT, [128, 8, 2048], BF16)

        ident_f = CFB[:, CF_ID:CF_ID + 128]
        ident_b = CBB[:, CB_ID:CB_ID + 128]
        ropeC = CFB[:, CF_C:CF_C + S]
        ropeS = CFB[:, CF_S:CF_S + S]
        vec = lambda c0, c1: CFB[:, CF_V + c0:CF_V + c1]

        def dbg_dump(ap, col0, ncols, rd):
            if dbg_d is not None:
                cx.dma("pool", dbg_d[0:ap.shape[0], col0:col0 + ncols], ap, reads=[rd])

        cx.dma("sp", CFB[:], cf_d, writes=[CFB])
        cx.dma("pool", CBB[:], cb_d, writes=[CBB])
        cx.dma("sp", CACT[:], cT_d, writes=[CACT])
        cx.op("pool", "memset", MSK[:], 0.0, writes=[MSK])
        for k in range(4):
            cx.op("pool", "affine_select", out=MSK[:, k, :], in_=MSK[:, k, :], pattern=[[-1, 512]],
                  compare_op=ALU.is_gt, fill=1.0, base=128 * k, channel_multiplier=1, reads=[MSK], writes=[MSK])
        for k in range(1, 5):
            cx.op("pool", "affine_select", out=MSK[:, 3 + k, :], in_=MSK[:, 3 + k, :], pattern=[[1, 512]],
                  compare_op=ALU.is_ge, fill=1.0, base=-512 + 128 * k, channel_multiplier=-1, reads=[MSK], writes=[MSK])
        cx.op("pool", "memset", CMN[:], 0.0, writes=[CMN])
        cx.op("pool", "affine_select", out=CMN[:], in_=CMN[:], pattern=[[-1, 2048]],
              compare_op=ALU.is_gt, fill=1.0, base=31, channel_multiplier=16, reads=[CMN], writes=[CMN])
        cx.op("pool", "memset", ONESF[:], 1.0, writes=[ONESF])
        cx.op("act", "activation", out=CACT[:], in_=CACT[:], func=AF.Silu, reads=[CACT], writes=[CACT])
        WA = [mem.view("wa0", R_HT, [128, 8, 1024], F32), mem.view("wa1", R_WIN, [128, 8, 1024], F32)]
        wada_v = wada_d.rearrange("(kc p) n -> p kc n", p=128)
        for v in range(6):
            wb = WA[v % 2]
            for kc in range(8):
                cx.dma("sp", wb[:, kc, :], wada_v[:, kc, 1024 * v:1024 * v + 1024], writes=[wb])
            for fc in range(8):
                col = (v * 8 + fc) * 2
                for kc in range(8):
                    cx.op("pe", "matmul", PS[0][:, col:col + 2], wb[:, kc, 128 * fc:128 * fc + 128], CACT[:, kc, :],
                          start=(kc == 0), stop=(kc == 7), reads=[wb, CACT], writes=[PS[0]])
        cx.op("dve", "tensor_tensor", out=MODC[:], in0=PS[0][:, 0:96].rearrange("p (a b) -> p a b", b=2),
              in1=vec(0, 48).unsqueeze(2).to_broadcast([128, 48, 2]), op=ALU.add, reads=[PS[0], CFB], writes=[MODC])
        def gb(c0):
            return vec(c0, c0 + 8).unsqueeze(2).to_broadcast([128, 8, 2])
        cx.op("dve", "scalar_tensor_tensor", out=DERV[:, 0], in0=MODC[:, 8:16, :], scalar=1.0, in1=gb(48),
              op0=ALU.add, op1=ALU.mult, reads=[MODC, CFB], writes=[DERV])
        cx.op("dve", "tensor_copy", out=DERV[:, 1], in_=MODC[:, 0:8, :], reads=[MODC], writes=[DERV])
        cx.op("dve", "scalar_tensor_tensor", out=DERV[:, 2], in0=MODC[:, 32:40, :], scalar=1.0, in1=gb(64),
              op0=ALU.add, op1=ALU.mult, reads=[MODC, CFB], writes=[DERV])
        cx.op("dve", "tensor_copy", out=DERV[:, 3], in_=MODC[:, 24:32, :], reads=[MODC], writes=[DERV])
        cx.op("dve", "tensor_tensor", out=DERV[:, 4], in0=MODC[:, 16:24, :], in1=gb(56), op=ALU.mult,
              reads=[MODC, CFB], writes=[DERV])
        cx.op("dve", "tensor_tensor", out=DERV[:, 5], in0=MODC[:, 40:48, :], in1=gb(72), op=ALU.mult,
              reads=[MODC, CFB], writes=[DERV])
        cx.barrier()

        cvsems = []
        cv_waited = [False]
        for seq in range(n_seq):
            if seq > 0:
                cx.dma("sp", CFB[:, CF_C:CF_C + 2 * S], cf_d[:, CF_C:CF_C + 2 * S], writes=[CFB])
            WIN = mem.view("win", R_WIN, [128, 8, NA], BF16)
            if seq == 0:
                cx.dma("pool", WIN[:], winA_d.rearrange("(kc p) n -> p kc n", p=128), writes=[WIN])
            else:
                cx.dma("sp", WIN[:], winAbf_d.rearrange("(kc p) n -> p kc n", p=128), writes=[WIN])
            DG = mem.view("dg", R_PH, [128, 128], F32)
            for gi, GT_ in ((4, G1), (5, G2)):
                for fc in range(8):
                    cx.op("dve", "tensor_scalar", out=DG[:], in0=ident_f, scalar1=DERV[:, gi, fc, seq:seq + 1],
                          scalar2=None, op0=ALU.mult, reads=[CFB, DERV], writes=[DG])
                    pb = PS[fc // 4]
                    cx.op("pe", "matmul", pb[:, 128 * (fc % 4):128 * (fc % 4) + 128], ONESF[:], DG[:],
                          start=True, stop=True, reads=[ONESF, DG], writes=[pb])
                    if fc % 4 == 3:
                        cx.op("act", "copy", out=GT_[:, 512 * (fc // 4):512 * (fc // 4) + 512], in_=pb[:],
                              reads=[pb], writes=[GT_])
            cx.barrier()
            XIN = [mem.view("xin%d" % i, R_PH + 4 * KB * i, [128, 1024], F32) for i in range(2)]
            XS = [mem.view("xs%d" % i, R_PH + 8 * KB + 4 * KB * i, [128, 1024], F32) for i in range(2)]
            JUNK = mem.view("junk", R_PH + 16 * KB, [128, 1024], F32)
            SS = [mem.view("ss%d" % i, R_PH + 20 * KB + 64 * i, [128, 4], F32) for i in range(2)]
            def p1_stats(i):
                xi, xs, ss = XIN[i % 2], XS[i % 2], SS[i % 2]
                cx.dma("sp", xi[:], x_d[seq, 128 * i:128 * i + 128, :], writes=[xi])
                cx.op("act", "activation", out=JUNK[:], in_=xi[:], func=AF.Square, accum_out=ss[:, 0:1],
                      reads=[xi], writes=[JUNK, ss])
                cx.op("act", "activation", out=ss[:, 1:2], in_=ss[:, 0:1], func=AF.Sqrt, scale=1.0 / D, bias=1e-6,
                      reads=[ss], writes=[ss])
                cx.op("dve", "reciprocal", out=ss[:, 2:3], in_=ss[:, 1:2], reads=[ss], writes=[ss])
                cx.op("dve", "tensor_scalar", out=xs[:], in0=xi[:], scalar1=ss[:, 2:3], scalar2=None, op0=ALU.mult,
                      reads=[xi, ss], writes=[xs])

            p1_stats(0)
            for i in range(NT):
                xs = XS[i % 2]
                for fc in range(8):
                    pb = PS[2 * (i % 2) + fc // 4]
                    cx.op("pe", "transpose", pb[:, 128 * (fc % 4):128 * (fc % 4) + 128],
                          xs[:, 128 * fc:128 * fc + 128], ident_f, reads=[xs, CFB], writes=[pb])
                if i + 1 < NT:
                    p1_stats(i + 1)
                for fc in range(8):
                    pb = PS[2 * (i % 2) + fc // 4]
                    if fc < 4:
                        cx.op("act", "activation", out=HT[:, fc, 128 * i:128 * i + 128],
                              in_=pb[:, 128 * (fc % 4):128 * (fc % 4) + 128], func=AF.Identity,
                              scale=DERV[:, 0, fc, seq:seq + 1], bias=DERV[:, 1, fc, seq:seq + 1],
                              reads=[pb, DERV], writes=[HT])
                    else:
                        cx.op("dve", "tensor_scalar", out=HT[:, fc, 128 * i:128 * i + 128],
                              in0=pb[:, 128 * (fc % 4):128 * (fc % 4) + 128],
                              scalar1=DERV[:, 0, fc, seq:seq + 1], scalar2=DERV[:, 1, fc, seq:seq + 1],
                              op0=ALU.mult, op1=ALU.add, reads=[pb, DERV], writes=["HTd"])
            cx.barrier()

            o = R_PH
            QN = mem.view("QN", o, [128, 4, 2048], BF16); o += 16 * KB
            KS = mem.view("KS", o, [128, 2, 2048], BF16); o += 8 * KB
            KW = mem.view("KW", o, [128, 2, 2048], BF16); o += 8 * KB
            KCR = mem.view("KCR", o, [128, 2048], BF16); o += 4 * KB
            VCR = mem.view("VCR", o, [128, 2048], BF16); o += 4 * KB
            VT = mem.view("VT", o, [128, 16, 4, 65], BF16); o += 8320
            GT = mem.view("GT", o, [128, 16, 24], F32); o += 1536
            o_T1 = o
            T1 = mem.view("T1", o, [128, 512], F32); o += 2048
            T2 = mem.view("T2", o, [128, 512], F32); o += 2048
            assert o <= TOT, o
            TL = [[T1], [T2]]
            ucnt = [0]

            def proj_unit(WINb, ca, cb_, M, tc, dst_ap, dstbuf):
                u = ucnt[0]; ucnt[0] += 1
                pa, pb = PS[2 * (u % 2)], PS[2 * (u % 2) + 1]
                for kc in range(8):
                    cx.op("pe", "matmul", pa[0:M, :], WINb[:, kc, ca:ca + M], HT[:, kc, 512 * tc:512 * tc + 512],
                          start=(kc == 0), stop=(kc == 7), reads=[WINb, HT], writes=[pa])
                if cb_ is None:
                    cx.op("act", "copy", out=dst_ap, in_=pa[0:M, :], reads=[pa], writes=[dstbuf])
                    return
                for kc in range(8):
                    cx.op("pe", "matmul", pb[0:M, :], WINb[:, kc, cb_:cb_ + M], HT[:, kc, 512 * tc:512 * tc + 512],
                          start=(kc == 0), stop=(kc == 7), reads=[WINb, HT], writes=[pb])
                T1, T2 = TL[0][u % len(TL[0])], TL[1][u % len(TL[1])]
                cx.op("dve", "tensor_tensor", out=T1[0:M, :], in0=pa[0:M, :], in1=ropeC[0:M, 512 * tc:512 * tc + 512],
                      op=ALU.mult, reads=[pa, CFB], writes=[T1])
                cx.op("dve", "tensor_tensor", out=T2[0:M, :], in0=pb[0:M, :], in1=ropeS[0:M, 512 * tc:512 * tc + 512],
                      op=ALU.mult, reads=[pb, CFB], writes=[T2])
                cx.op("pool", "tensor_tensor", out=dst_ap, in0=T1[0:M, :], in1=T2[0:M, :], op=ALU.add,
                      reads=[T1, T2], writes=[dstbuf])

            cx.op("pool", "memset", VT[:, :, :, 64:65], 1.0, writes=[VT])
            for tc in range(4):
                sl = slice(512 * tc, 512 * tc + 512)
                for j in range(4):
                    proj_unit(WIN, 256 * j, 256 * j + 128, 128, tc, QN[:, j, sl], QN)
                proj_unit(WIN, 1024, 1152, 128, tc, KCR[:, sl], KCR)
                proj_unit(WIN, 1280, None, 128, tc, VCR[:, sl], VCR)
                for g in range(2):
                    proj_unit(WIN, 1408 + 256 * g, 1536 + 256 * g, 128, tc, KS[:, g, sl], KS)
                    proj_unit(WIN, 1920 + 256 * g, 2048 + 256 * g, 128, tc, KW[:, g, sl], KW)
            for i in range(NT):
                pb = PS[4 + i % 2]
                for kc in range(8):
                    cx.op("pe", "matmul", pb[:, 0:280], HT[:, kc, 128 * i:128 * i + 128], WIN[:, kc, NA_FM:NA],
                          start=(kc == 0), stop=(kc == 7), reads=[HT, WIN], writes=[pb])
                cx.op("act", "copy", out=VT[:, i, :, 0:64], in_=pb[:, 0:256].rearrange("p (a b) -> p a b", a=4),
                      reads=[pb], writes=[VT])
                cx.op("act", "activation", out=GT[:, i, :], in_=pb[:, 256:280], func=AF.Sigmoid, reads=[pb], writes=[GT])
            cx.barrier()

            o = R_WIN
            W1 = mem.view("W1", o, [128, 2, 32, 128], BF16); o += 16 * KB
            WSM = mem.view("WSM", o, [128, 1216], BF16); o += 2432
            IMT = mem.view("IMT", o, [128, 2, 16, 32], F32); o += 4096
            PT = []
            for i in range(4):
                PT.append(mem.view("PT%d" % i, o, [128, 512], BF16)); o += 1024
            XG = mem.view("XG", o, [128, 128], F32); o += 512
            UU = mem.view("UU", o, [128, 128], F32); o += 512
            HIDT = mem.view("HIDT", o, [128, 128], BF16); o += 256
            KCT = mem.view("KCT", o, [128, 2, 128], BF16); o += 512
            VCX = mem.view("VCX", o, [128, 2, 98], BF16); o += 392
            B2V = mem.view("B2V", o, [128, 64], F32); o += 256
            BIAS1 = mem.view("BIAS1", o, [128, 2], F32); o += 8
            OA = mem.view("OA", o, [128, 4, 512], F32); o += 8192
            OAB = mem.view("OAB", o_T1, [128, 4, 512], BF16)
            IMPS = []
            for g in range(2):
                IMPS.append(mem.view("IMP%d" % g, o, [128, 4, 32], F32)); o += 512
            TMPI = mem.view("TMPI", o, [128, 4, 32], F32); o += 512
            IMPM = mem.view("IMPM", o, [128, 4, 32], F32); o += 512
            TOP8 = mem.view("TOP8", o, [128, 4, 8], F32); o += 128
            NSELB = mem.view("NSELB", o, [128, 4, 32], BF16); o += 256
            NSELT = mem.view("NSELT", o, [128, 2, 512], BF16); o += 2048
            TMPO = mem.view("TMPO", o, [128, 4, 64], F32); o += 1024
            assert o <= R_PH, o
            RINV = Buf(SMALL[:, 0:4], "RINV")
            COEF = Buf(SMALL[:, 4:8], "COEF")

            for kv in range(2):
                src = w1_d[kv].rearrange("(l d) j -> d l j", d=64)
                cx.dma("pool", W1[0:64, kv], src, writes=[W1])
                cx.dma("pool", W1[64:128, kv], src, writes=[W1])
            cx.dma("pool", WSM[:], wsm_d, writes=[WSM])
            cx.dma("sp", IMT[:].rearrange("p a b c -> p (a b c)"), imt_d, writes=[IMT])
            cx.dma("sp", B2V[:], b2v_d.partition_broadcast(128), writes=[B2V])
            if seq == 0:
                for i in range(11):
                    sem_cv = es.enter_context(nc.semaphore("cv%d" % i))
                    cvsems.append(sem_cv)
                    nc.gpsimd.dma_start(out=wgubf_d[2 * i:2 * i + 2].rearrange("c p n -> (c p) n"),
                                        in_=wgu_d[2 * i:2 * i + 2].rearrange("c p n -> (c p) n")).then_inc(sem_cv, 16)
                cv_list = [(wdnbf_d[704 * i:704 * i + 704, :], wdn_d[704 * i:704 * i + 704, :]) for i in range(4)]
                cv_list += [(woutbf_d, wout_d)]
                if n_seq > 1:
                    cv_list += [(winAbf_d, winA_d), (winBbf_d, winB_d)]
                for i, (dst_, src_) in enumerate(cv_list):
                    sem_cv = es.enter_context(nc.semaphore("cw%d" % i))
                    cvsems.append(sem_cv)
                    nc.gpsimd.dma_start(out=dst_, in_=src_).then_inc(sem_cv, 16)
            cx.op("pool", "memset", HIDT[:], 0.0, writes=[HIDT])
            cx.op("pool", "memset", NSELT[:], 0.0, writes=[NSELT])
            cx.op("pool", "memset", VCX[:, :, 64:65], 1.0, writes=[VCX])
            for g in range(2):
                cx.op("pool", "tensor_copy", out=VCX[:, g, 65:97], in_=CBB[:, CB_OV:CB_OV + 32], reads=[CBB], writes=[VCX])
            for kv in range(2):
                for l in range(32):
                    cx.op("pe", "matmul", PS[6][:, kv:kv + 1], W1[0:64, kv, l, :], CBB[0:64, CB_PE + 32 * kv + l:CB_PE + 32 * kv + l + 1],
                          start=(l == 0), stop=(l == 31), reads=[W1, CBB], writes=[PS[6]])
            cx.op("dve", "tensor_tensor", out=BIAS1[:], in0=PS[6][:, 0:2], in1=vec(80, 82), op=ALU.add,
                  reads=[PS[6], CFB], writes=[BIAS1])
            for kv in range(2):
                for g in range(2):
                    srcb = KCR if kv == 0 else VCR
                    hps = PS[4 + g]
                    for l in range(32):
                        cx.op("pe", "matmul", hps[:, 0:127], W1[64 * g:64 * g + 64, kv, l, :],
                              srcb[64 * g:64 * g + 64, l:l + 2017:16], start=(l == 0), stop=(l == 31),
                              reads=[W1, srcb], writes=[hps])
                    cx.op("act", "activation", out=XG[:, 0:127], in_=hps[:, 0:127], func=AF.Identity,
                          bias=BIAS1[:, kv:kv + 1], reads=[hps, BIAS1], writes=[XG])
                    cx.op("dve", "tensor_tensor", out=UU[:, 0:127], in0=XG[:, 0:127], in1=XG[:, 0:127], op=ALU.mult,
                          reads=[XG], writes=[UU])
                    cx.op("dve", "tensor_scalar", out=UU[:, 0:127], in0=UU[:, 0:127], scalar1=0.044715, scalar2=1.0,
                          op0=ALU.mult, op1=ALU.add, reads=[UU], writes=[UU])
                    cx.op("dve", "tensor_tensor", out=UU[:, 0:127], in0=UU[:, 0:127], in1=XG[:, 0:127], op=ALU.mult,
                          reads=[UU, XG], writes=[UU])
                    cx.op("act", "activation", out=UU[:, 0:127], in_=UU[:, 0:127], func=AF.Sigmoid, scale=1.5957691216057308,
                          reads=[UU], writes=[UU])
                    cx.op("dve", "tensor_tensor", out=HIDT[:, 0:127], in0=XG[:, 0:127], in1=UU[:, 0:127], op=ALU.mult,
                          reads=[XG, UU], writes=[HIDT])
                    if kv == 0:
                        cx.op("pe", "matmul", PS[6][:, 0:128], WSM[:, 1024:1152], HIDT[:], start=True, stop=True,
                              reads=[WSM, HIDT], writes=[PS[6]])
                        cx.op("act", "activation", out=KCT[:, g, :], in_=PS[6][:, 0:128], func=AF.Identity,
                              bias=vec(82, 83), reads=[PS[6], CFB], writes=[KCT])
                    else:
                        cx.op("pe", "matmul", PS[6][:, 0:64], HIDT[:], WSM[:, 1152:1216], start=True, stop=True,
                              reads=[WSM, HIDT], writes=[PS[6]])
                        cx.op("dve", "tensor_tensor", out=VCX[:, g, 0:64], in0=PS[6][:, 0:64], in1=B2V[:], op=ALU.add,
                              reads=[PS[6], B2V], writes=[VCX])

            pipe = {"pend": [], "u": 0, "job": 0}
            SCB = [PS[0], PS[1], PS[4], PS[5]]
            grp = []

            def unit(kT, qT, extras, V, acc, ncols, first, rk, rq, rv, after=None, mmask=None, ex128=(), last=False):
                u = pipe["u"]; pipe["u"] += 1
                grp.append(dict(u=u, kT=kT, qT=qT, ex=extras, ex128=ex128, V=V, acc=acc, ncols=ncols, first=first,
                                rk=rk, rq=rq, rv=rv, after=after, mmask=mmask, last=last))
                if len(grp) == (1 if 'G1' in _DBG else 2):
                    emit_group()

            def emit_group():
                if not grp:
                    return
                for d in grp:
                    sbk = SCB[d["u"] % 4]
                    nex = len(d["ex"]) + len(d["ex128"])
                    cx.op("pe", "matmul", sbk[:], d["kT"], d["qT"], start=True, stop=(nex == 0),
                          reads=[d["rk"], d["rq"]], writes=[sbk])
                for d in grp:
                    sbk = SCB[d["u"] % 4]
                    nex = len(d["ex"]) + len(d["ex128"])
                    for n_, (l_, r_, c0, c1, rd) in enumerate(d["ex"]):
                        cx.op("pe", "matmul", sbk[:, c0:c1], l_, r_, start=False, stop=(n_ == nex - 1), reads=rd, writes=[sbk])
                for d in grp:
                    sbk = SCB[d["u"] % 4]
                    nex = len(d["ex"]) + len(d["ex128"])
                    for n_, (l_, r_, c0, c1, rd) in enumerate(d["ex128"]):
                        cx.op("pe", "matmul", sbk[:, c0:c1], l_, r_, start=False, stop=(len(d["ex"]) + n_ == nex - 1),
                              reads=rd, writes=[sbk])
                for pvf in pipe["pend"]:
                    pvf()
                pipe["pend"] = []
                for d in grp:
                    sbk = SCB[d["u"] % 4]
                    pt = PT[d["u"] % 4]
                    cx.op("act", "activation", out=pt[:], in_=sbk[:], func=AF.Exp, scale=0.125, reads=[sbk], writes=[pt])
                    if d["mmask"] is not None and 'NM' not in _DBG:
                        cx.op("dve", "tensor_tensor", out=pt[:], in0=pt[:], in1=d["mmask"][0], op=ALU.mult,
                              reads=[pt, d["mmask"][1]], writes=[pt])

                    def pv(d=d, pt=pt):
                        for j in range(4):
                            cx.op("pe", "matmul", d["acc"][:, d["ncols"] * j:d["ncols"] * j + d["ncols"]],
                                  pt[:, 128 * j:128 * j + 128], d["V"], start=(d["first"] and j == 0),
                                  stop=(True if 'ST' in _DBG else (d["last"] and j == 3)), reads=[pt, d["rv"]], writes=[d["acc"]])
                        if d["after"] is not None:
                            d["after"]()
                    pipe["pend"].append(pv)
                grp.clear()

            def flush():
                emit_group()
                for pvf in pipe["pend"]:
                    pvf()
                pipe["pend"] = []

            def next_acc():
                a = PS[2 + pipe["job"] % 2]
                pipe["job"] += 1
                return a

            def nsa_final(acc, ncols, c, h, br, first_branch):
                accv = acc[:, 0:4 * ncols].rearrange("p (j n) -> p j n", j=4)

                def f():
                    cx.op("dve", "tensor_scalar", out=RINV[:], in0=accv[:, :, 64], scalar1=1e-30, scalar2=None,
                          op0=ALU.max, reads=[acc], writes=[RINV])
                    cx.op("dve", "reciprocal", out=RINV[:], in_=RINV[:], reads=[RINV], writes=[RINV])
                    if br == 0:
                        fi = (h % 4 == 0)
                        IMP = IMPS[h // 4]
                        dst = IMP if fi else TMPI
                        cx.op("dve", "tensor_tensor", out=dst[:], in0=accv[:, :, 65:97],
                              in1=RINV[:].unsqueeze(2).to_broadcast([128, 4, 32]), op=ALU.mult,
                              reads=[acc, RINV], writes=[dst])
                        if not fi:
                            cx.op("pool", "tensor_tensor", out=IMP[:], in0=IMP[:], in1=TMPI[:], op=ALU.add,
                                  reads=[IMP, TMPI], writes=[IMP])
                    cx.op("dve", "tensor_tensor", out=COEF[:], in0=RINV[:], in1=GT[:, 4 * c:4 * c + 4, 8 * br + h],
                          op=ALU.mult, reads=[RINV, GT], writes=[COEF])
                    cb3 = COEF[:].unsqueeze(2).to_broadcast([128, 4, 64])
                    if first_branch:
                        cx.op("dve", "tensor_tensor", out=OA[:, :, 64 * h:64 * h + 64], in0=accv[:, :, 0:64], in1=cb3,
                              op=ALU.mult, reads=[acc, COEF], writes=[OA])
                    else:
                        cx.op("dve", "tensor_tensor", out=TMPO[:], in0=accv[:, :, 0:64], in1=cb3, op=ALU.mult,
                              reads=[acc, COEF], writes=[TMPO])
                        cx.op("pool", "tensor_tensor", out=OA[:, :, 64 * h:64 * h + 64], in0=OA[:, :, 64 * h:64 * h + 64],
                              in1=TMPO[:], op=ALU.add, reads=[OA, TMPO], writes=[OA])
                return f

            def to_OT(SRC, c, fc0):
                for fc in range(4):
                    for j in range(4):
                        col = ((fc % 2) * 4 + j) * 128
                        cx.op("pe", "transpose", psb(6 + fc // 2)[:, col:col + 128], SRC[:, j, 128 * fc:128 * fc + 128],
                              ident_b, reads=[SRC, CBB], writes=[PS[6 + fc // 2]])
                for fc in range(4):
                    cx.op("act", "copy", out=OT[:, fc0 + fc, 512 * c:512 * c + 512],
                          in_=psb(6 + fc // 2)[:, (fc % 2) * 512:(fc % 2) * 512 + 512], reads=[PS[6 + fc // 2]], writes=[OT])

            for c in range(4):
                qs = slice(512 * c, 512 * c + 512)
                for h in range(8):
                    g, b_ = h // 4, 64 * (h % 2)
                    acc = next_acc()
                    unit(KCT[b_:b_ + 64, g, :], QN[b_:b_ + 64, h // 2, qs],
                         [], VCX[:, g, 0:97], acc, 97, True,
                         KCT, QN, VCX, after=nsa_final(acc, 97, c, h, 0, True), mmask=(CMN[:, qs], CMN), last=True)
                for h in range(8):
                    g, b_ = h // 4, 64 * (h % 2)
                    acc = next_acc()
                    tiles = list(range(max(0, 4 * c - 4), 4 * c + 4))
                    for n_, i in enumerate(tiles):
                        mk = MSK[:, 3 + (4 * c - i), :] if i < 4 * c else MSK[:, i - 4 * c, :]
                        unit(KW[b_:b_ + 64, g, 128 * i:128 * i + 128], QN[b_:b_ + 64, h // 2, qs],
                             [], VT[:, i, 2 + g, :], acc, 65, n_ == 0, KW, QN, VT,
                             after=(nsa_final(acc, 65, c, h, 2, False) if n_ == len(tiles) - 1 else None), mmask=(mk, MSK),
                             last=(n_ == len(tiles) - 1))
                flush()
                for g in range(2):
                    IMPg = IMPS[g]
                    cx.op("dve", "tensor_tensor", out=IMPM[:], in0=IMPg[:], in1=IMT[:, 0, 4 * c:4 * c + 4, :], op=ALU.mult,
                          reads=[IMPg, IMT], writes=[IMPM])
                    cx.op("dve", "tensor_tensor", out=IMPM[:], in0=IMPM[:], in1=IMT[:, 1, 4 * c:4 * c + 4, :], op=ALU.add,
                          reads=[IMPM, IMT], writes=[IMPM])
                    for j in range(4):
                        cx.op("dve", "max", out=TOP8[:, j, :], in_=IMPM[:, j, :], reads=[IMPM], writes=[TOP8])
                    for j in range(4):
                        cx.op("dve", "tensor_scalar", out=NSELB[:, j, :], in0=IMPM[:, j, :], scalar1=TOP8[:, j, 7:8],
                              scalar2=NEG, op0=ALU.is_lt, op1=ALU.mult, reads=[IMPM, TOP8], writes=[NSELB])
                    for j in range(4):
                        cx.op("pe", "transpose", psb(6)[0:32, 128 * j:128 * j + 128], NSELB[:, j, :], ident_b,
                              reads=[NSELB, CBB], writes=[PS[6]])
                    cx.op("act", "copy", out=NSELT[0:32, g, :], in_=psb(6)[0:32, 0:512], reads=[PS[6]], writes=[NSELT])
                for h in range(8):
                    g, b_ = h // 4, 64 * (h % 2)
                    acc = next_acc()
                    tiles = list(range(0, 4 * c + 4))
                    for n_, i in enumerate(tiles):
                        KX = 32
                        ex = [(CBB[0:KX, CB_EX + 128 * i:CB_EX + 128 * i + 128], NSELT[0:KX, g, :], 0, 512, [CBB, NSELT])]
                        unit(KS[b_:b_ + 64, g, 128 * i:128 * i + 128], QN[b_:b_ + 64, h // 2, qs], ex,
                             VT[:, i, g, :], acc, 65, n_ == 0, KS, QN, VT,
                             after=(nsa_final(acc, 65, c, h, 1, False) if n_ == len(tiles) - 1 else None),
                             mmask=((MSK[:, i - 4 * c, :], MSK) if i >= 4 * c else None), last=(n_ == len(tiles) - 1))
                flush()
                cx.op("act", "copy", out=OAB[:], in_=OA[:], reads=[OA], writes=[OAB])
                to_OT(OAB, c, 0)
            cx.barrier()

            WINB = mem.view("winb", R_WIN, [128, 8, NB], BF16)
            if seq == 0:
                cx.dma("pool", WINB[:], winB_d.rearrange("(kc p) n -> p kc n", p=128), writes=[WINB])
            else:
                cx.dma("sp", WINB[:], winBbf_d.rearrange("(kc p) n -> p kc n", p=128), writes=[WINB])
            o = R_PH
            QD = mem.view("QD", o, [128, 4, 2048], BF16); o += 16 * KB
            QI = mem.view("QI", o, [128, 4, 2048], BF16); o += 16 * KB
            KI = mem.view("KI", o, [128, 2048], BF16); o += 4 * KB
            CKT = mem.view("CKT", o, [128, 2048], BF16); o += 4 * KB
            KRT = mem.view("KRT", o, [128, 2048], BF16); o += 4 * KB
            WI = mem.view("WI", o, [128, 16, 8], F32); o += 512
            T1 = mem.view("T1", o, [128, 512], F32); o_JB = o; o += 2048
            T2 = mem.view("T2", o, [128, 512], F32); o += 2048
            CKN = []
            for i in range(2):
                CKN.append(mem.view("CKN%d" % i, o, [128, 128], F32)); o += 512
            o_RB = o
            assert o + 4096 + 128 <= TOT, o
            TL[0] = [T1, mem.view("T1b", o_RB, [128, 512], F32)]
            TL[1] = [T2, mem.view("T2b", o_RB + 2048, [128, 512], F32)]
            SSD = Buf(SMALL[:, 56:60], "SSD")
            for tc in range(4):
                sl = slice(512 * tc, 512 * tc + 512)
                for j in range(4):
                    proj_unit(WINB, 256 * j, 256 * j + 128, 128, tc, QD[:, j, sl], QD)
                for j in range(4):
                    proj_unit(WINB, 1024 + 256 * j, 1024 + 256 * j + 128, 128, tc, QI[:, j, sl], QI)
                proj_unit(WINB, 2048, 2176, 128, tc, KI[:, sl], KI)
                proj_unit(WINB, 2304, 2320, 16, tc, KRT[0:16, sl], KRT)
            for i in range(NT):
                pb = PS[4 + i % 2]
                ck = CKN[i % 2]
                for kc in range(8):
                    cx.op("pe", "matmul", pb[:, 0:136], HT[:, kc, 128 * i:128 * i + 128], WINB[:, kc, NB_FM:NB],
                          start=(kc == 0), stop=(kc == 7), reads=[HT, WINB], writes=[pb])
                cx.op("act", "activation", out=ck[:], in_=pb[:, 0:128], func=AF.Square, accum_out=SSD[:, 0:1],
                      reads=[pb], writes=[ck, SSD])
                cx.op("act", "activation", out=SSD[:, 1:2], in_=SSD[:, 0:1], func=AF.Sqrt, scale=1.0 / 128, bias=1e-6,
                      reads=[SSD], writes=[SSD])
                cx.op("dve", "reciprocal", out=SSD[:, 2:3], in_=SSD[:, 1:2], reads=[SSD], writes=[SSD])
                cx.op("dve", "tensor_scalar", out=ck[:], in0=pb[:, 0:128], scalar1=SSD[:, 2:3], scalar2=None, op0=ALU.mult,
                      reads=[pb, SSD], writes=[ck])
                cx.op("act", "mul", out=WI[:, i, :], in_=pb[:, 128:136], mul=float(8 ** -0.5 * 64 ** -0.5), reads=[pb], writes=[WI])
                cx.op("pe", "transpose", PS[6][:, 128 * (i % 4):128 * (i % 4) + 128], ck[:], ident_f, reads=[ck, CFB], writes=[PS[6]])
                if i % 4 == 3:
                    cx.op("act", "activation", out=CKT[:, 512 * (i // 4):512 * (i // 4) + 512], in_=PS[6][:], func=AF.Identity,
                          scale=vec(83, 84), reads=[PS[6], CFB], writes=[CKT])
            cx.barrier()

            o = R_WIN
            KHT = mem.view("KHT", o, [128, 4, 2048], BF16); o += 16 * KB
            VH = mem.view("VH", o, [128, 16, 8, 65], BF16); o += 16640
            WSM = mem.view("WSM", o, [128, 1216], BF16); o += 2432
            PT = []
            for i in range(4):
                PT.append(mem.view("PT%d" % i, o, [128, 512], BF16)); o += 1024
            ODB = mem.view("ODB", o, [128, 4, 512], BF16); o += 4096
            assert o <= R_PH, o
            NMS = [[mem.view("NMA%d" % j, R_HT + 3 * KB * j, [128, 1536], BF16) for j in range(4)],
                   [mem.view("NMB%d" % j, CF_C * 4 + 4 * KB * j, [128, 2048], BF16) for j in range(4)]]
            IB = [mem.view("IB%d" % j, R_HT + 12 * KB + 8 * KB * j, [128, 2048], F32) for j in range(2)]
            RB = [mem.view("RB%d" % j, o_RB + 2048 * j, [128, 512], F32) for j in range(2)]
            BS = []
            for n2, c0 in enumerate((8, 40)):
                BS.append(dict(LO=Buf(SMALL[:, c0:c0 + 1], "LO%d" % n2), MID=Buf(SMALL[:, c0 + 1:c0 + 2], "MID%d" % n2),
                               CNT=Buf(SMALL[:, c0 + 2:c0 + 3], "CNT%d" % n2), TMPS=Buf(SMALL[:, c0 + 3:c0 + 4], "TMPS%d" % n2),
                               W0=Buf(SMALL[:, c0 + 4:c0 + 5], "W0%d" % n2), MN=Buf(SMALL[:, c0 + 5:c0 + 6], "MN%d" % n2),
                               MX8=Buf(SMALL[:, c0 + 6:c0 + 14], "MX8%d" % n2),
                               WK=mem.view("WK%d" % n2, o_RB + 4096 + 64 * n2, [128, 16], F32),
                               JB=Buf(mem.view("JBx%d" % n2, o_JB + 2048 * n2, [128, 1024], BF16).ap.bitcast(mybir.dt.uint8), "JB%d" % n2)))
            cx.dma("pool", WSM[:], wsm_d, writes=[WSM])
            cx.op("pool", "memset", VH[:, :, :, 64:65], 1.0, writes=[VH])
            n_ = 0
            for j in range(4):
                for tc in range(4):
                    pb = PS[4 + n_ % 2]; n_ += 1
                    cx.op("pe", "matmul", pb[:], WSM[:, 128 * j:128 * j + 128], CKT[:, 512 * tc:512 * tc + 512],
                          start=True, stop=False, reads=[WSM, CKT], writes=[pb])
                    cx.op("pe", "matmul", pb[:], CBB[0:16, CB_SEL:CB_SEL + 128], KRT[0:16, 512 * tc:512 * tc + 512],
                          start=False, stop=True, reads=[CBB, KRT], writes=[pb])
                    cx.op("act", "copy", out=KHT[:, j, 512 * tc:512 * tc + 512], in_=pb[:], reads=[pb], writes=[KHT])
            for i in range(NT):
                pb = PS[4 + n_ % 2]; n_ += 1
                cx.op("pe", "matmul", pb[:], CKT[:, 128 * i:128 * i + 128], WSM[:, 512:1024], start=True, stop=True,
                      reads=[CKT, WSM], writes=[pb])
                cx.op("act", "copy", out=VH[:, i, :, 0:64], in_=pb[:].rearrange("p (h d) -> p h d", h=8), reads=[pb], writes=[VH])

            pipe["pend"] = []
            grp.clear()
            def idx_scores(c, j):
                T = 4 * c + j
                Wc = 512 * (c + 1)
                Wv = 128 * (T + 1)
                IBt = IB[T % 2]
                for sc in range(c + 1):
                    N = min(512, Wv - 512 * sc)
                    for h in range(8):
                        b_ = 64 * (h % 2)
                        L = PS[6 + lcnt[0] % 2]; lcnt[0] += 1
                        cx.op("pe", "matmul", L[:, 0:N], QI[b_:b_ + 64, h // 2, 128 * T:128 * T + 128],
                              KI[b_:b_ + 64, 512 * sc:512 * sc + N], start=True, stop=True, reads=[QI, KI], writes=[L])
                        if h == 0:
                            cx.op("dve", "tensor_scalar", out=IBt[:, 512 * sc:512 * sc + N], in0=L[:, 0:N], scalar1=0.0,
                                  scalar2=WI[:, T, h:h + 1], op0=ALU.max, op1=ALU.mult, reads=[L, WI], writes=[IBt])
                        else:
                            rb = RB[h % 2]
                            cx.op("dve", "tensor_scalar", out=rb[:, 0:N], in0=L[:, 0:N], scalar1=0.0,
                                  scalar2=WI[:, T, h:h + 1], op0=ALU.max, op1=ALU.mult, reads=[L, WI], writes=[rb])
                            cx.op("pool", "tensor_tensor", out=IBt[:, 512 * sc:512 * sc + N], in0=IBt[:, 512 * sc:512 * sc + N],
                                  in1=rb[:, 0:N], op=ALU.add, reads=[IBt, rb], writes=[IBt])
                MX8, MN = BS[j % 2]["MX8"], BS[j % 2]["MN"]
                if T >= 2:
                    cx.op("dve", "max", out=MX8[:], in_=IBt[:, 0:Wv], reads=[IBt], writes=[MX8])
                    cx.op("dve", "tensor_reduce", out=MN[:], in_=IBt[:, 0:Wv], axis=AX.X, op=ALU.min, reads=[IBt], writes=[MN])
                cx.op("pool", "affine_select", out=IBt[:, 128 * T:128 * T + 128], in_=IBt[:, 128 * T:128 * T + 128],
                      pattern=[[-1, 128]], compare_op=ALU.is_ge, fill=-3.0e38, base=0, channel_multiplier=1,
                      reads=[IBt], writes=[IBt])
                if Wv < Wc:
                    cx.op("pool", "memset", IBt[:, Wv:Wc], -3.0e38, writes=[IBt])

            def idx_bisect_pair(c, js):
                Wc = 512 * (c + 1)
                chains = []
                for j in js:
                    T = 4 * c + j
                    chains.append((j, T, 128 * (T + 1), IB[T % 2], BS[j % 2]))
                act_ = [ch for ch in chains if ch[1] >= 2]
                for (j, T, Wv, IBt, B) in chains:
                    if T >= 2:
                        cx.op("dve", "tensor_copy", out=B["LO"][:], in_=B["MN"][:], reads=[B["MN"]], writes=[B["LO"]])
                        cx.op("dve", "tensor_tensor", out=B["W0"][:], in0=B["MX8"][:, 0:1], in1=B["MN"][:], op=ALU.subtract,
                              reads=[B["MX8"], B["MN"]], writes=[B["W0"]])
                        cx.op("dve", "tensor_scalar", out=B["WK"][:, 0:BIS_ITERS], in0=CFB[:, CF_P2:CF_P2 + BIS_ITERS],
                              scalar1=B["W0"][:], scalar2=None, op0=ALU.mult, reads=[CFB, B["W0"]], writes=[B["WK"]])
                    else:
                        cx.op("dve", "memset", B["LO"][:], -1.0e30, writes=[B["LO"]])
                for k in range(BIS_ITERS):
                    for (j, T, Wv, IBt, B) in act_:
                        cx.op("dve", "tensor_tensor", out=B["MID"][:], in0=B["LO"][:], in1=B["WK"][:, k:k + 1], op=ALU.add,
                              reads=[B["LO"], B["WK"]], writes=[B["MID"]])
                    for (j, T, Wv, IBt, B) in act_:
                        cx.op("dve", "tensor_scalar", out=B["JB"][:, 0:Wv], in0=IBt[:, 0:Wv], scalar1=B["MID"][:], scalar2=0.0,
                              op0=ALU.is_ge, op1=ALU.add, accum_out=B["CNT"][:], reads=[IBt, B["MID"]], writes=[B["JB"], B["CNT"]])
                    for (j, T, Wv, IBt, B) in act_:
                        cx.op("dve", "tensor_scalar", out=B["TMPS"][:], in0=B["CNT"][:], scalar1=255.5, scalar2=B["WK"][:, k:k + 1],
                              op0=ALU.is_ge, op1=ALU.mult, reads=[B["CNT"], B["WK"]], writes=[B["TMPS"]])
                    for (j, T, Wv, IBt, B) in act_:
                        cx.op("dve", "tensor_tensor", out=B["LO"][:], in0=B["LO"][:], in1=B["TMPS"][:], op=ALU.add,
                              reads=[B["LO"], B["TMPS"]], writes=[B["LO"]])
                for (j, T, Wv, IBt, B) in chains:
                    nm = NMS[c % 2][j]
                    cx.op("dve", "tensor_scalar", out=nm[:, 0:Wc], in0=IBt[:, 0:Wc], scalar1=B["LO"][:], scalar2=NEG,
                          op0=ALU.is_lt, op1=ALU.mult, reads=[IBt, B["LO"]], writes=[nm])

            def idx_slices(c):
                return [lambda: idx_scores(c, 0), lambda: idx_scores(c, 1), lambda: idx_bisect_pair(c, (0, 1)), lambda: None,
                        lambda: idx_scores(c, 2), lambda: idx_scores(c, 3), lambda: idx_bisect_pair(c, (2, 3)), lambda: None]

            lcnt = [0]
            for f_ in idx_slices(0):
                f_()
            for c in range(4):
                qs = slice(512 * c, 512 * c + 512)
                NMc = NMS[c % 2]
                sl_next = idx_slices(c + 1) if c < 3 else []
                for h in range(8):
                    if sl_next:
                        sl_next[h]()
                    b_ = 64 * (h % 2)
                    acc = next_acc()
                    accv = acc[:, 0:260].rearrange("p (j n) -> p j n", j=4)

                    def fin(acc=acc, accv=accv, h=h):
                        cx.op("dve", "reciprocal", out=RINV[:], in_=accv[:, :, 64], reads=[acc], writes=[RINV])
                        cx.op("dve", "tensor_tensor", out=ODB[:, :, 64 * h:64 * h + 64], in0=accv[:, :, 0:64],
                              in1=RINV[:].unsqueeze(2).to_broadcast([128, 4, 64]), op=ALU.mult,
                              reads=[acc, RINV], writes=[ODB])
                    tiles = list(range(0, 4 * c + 4))
                    for q_, i in enumerate(tiles):
                        ex = [(NMc[j][:, 128 * i:128 * i + 128], ident_b, 128 * j, 128 * j + 128, [NMc[j], CBB]) for j in range(4)]
                        unit(KHT[b_:b_ + 64, h // 2, 128 * i:128 * i + 128], QD[b_:b_ + 64, h // 2, qs], [],
                             VH[:, i, h, :], acc, 65, q_ == 0, KHT, QD, VH, after=(fin if q_ == len(tiles) - 1 else None),
                             ex128=ex, last=(q_ == len(tiles) - 1))
                flush()
                to_OT(ODB, c, 4)
            cx.barrier()

            o = R_HT
            X1 = mem.view("X1", o, [128, 4, 1024], F32); o += 16 * KB
            H2T = mem.view("H2T", o, [128, 8, 512], BF16); o += 8 * KB
            ACTT = mem.view("ACTT", o, [128, 22, 512], BF16); o += 22 * KB
            WOUT = mem.view("WOUT", CF_C * 4, [128, 8, 1024], BF16)
            WG = []
            for i in range(3):
                WG.append(mem.view("WG%d" % i, o, [128, 8, 256], BF16)); o += 4 * KB
            WDN = mem.view("WDN", o, [128, 22, 1024], BF16); o += 44 * KB
            XIN = []
            for i in range(2):
                XIN.append(mem.view("xin%d" % i, o, [128, 1024], F32)); o += 4 * KB
            JUNK = mem.view("junk", o, [128, 1024], F32); o += 4 * KB
            XS = mem.view("xs", o, [128, 1024], F32); o += 4 * KB
            TMPY = mem.view("tmpy", o, [128, 1024], F32); o += 4 * KB
            SIL = []
            for i in range(2):
                SIL.append(mem.view("sil%d" % i, o, [128, 512], F32)); o += 2 * KB
            assert o <= TOT, o
            ST = Buf(SMALL[:, 44:52], "ST")
            if not cv_waited[0]:
                for sem_cv in cvsems:
                    nc.sync.wait_ge(sem_cv, 16)
                cv_waited[0] = True
            cx.dma("sp", WDN[:], wdnbf_d.rearrange("(c p) n -> p c n", p=128), writes=[WDN])
            wout_v = woutbf_d.rearrange("(kc p) n -> p kc n", p=128)

            cx.dma("sp", WOUT[:], wout_v, writes=[WOUT])
            for gi in range(4):
                for j in range(4):
                    T = 4 * gi + j
                    xi = XIN[j % 2]
                    cx.dma("sp", xi[:], x_d[seq, 128 * T:128 * T + 128, :], writes=[xi])
                    yb = [PS[2 * (j % 2)], PS[2 * (j % 2) + 1]]
                    for n in range(2):
                        for kc in range(8):
                            cx.op("pe", "matmul", yb[n][:], OT[:, kc, 128 * T:128 * T + 128], WOUT[:, kc, 512 * n:512 * n + 512],
                                  start=(kc == 0), stop=(kc == 7), reads=[OT, WOUT], writes=[yb[n]])
                    for n in range(2):
                        cx.op("act", "activation", out=JUNK[:, 512 * n:512 * n + 512], in_=yb[n][:], func=AF.Square,
                              accum_out=ST[:, n:n + 1], reads=[yb[n]], writes=[JUNK, ST])
                    cx.op("dve", "tensor_tensor", out=ST[:, 2:3], in0=ST[:, 0:1], in1=ST[:, 1:2], op=ALU.add, reads=[ST], writes=[ST])
                    cx.op("act", "activation", out=ST[:, 3:4], in_=ST[:, 2:3], func=AF.Sqrt, scale=1.0 / D, bias=1e-6, reads=[ST], writes=[ST])
                    cx.op("dve", "reciprocal", out=ST[:, 4:5], in_=ST[:, 3:4], reads=[ST], writes=[ST])
                    for n in range(2):
                        cx.op("dve", "scalar_tensor_tensor", out=TMPY[:, 512 * n:512 * n + 512], in0=yb[n][:], scalar=ST[:, 4:5],
                              in1=G1[:, 512 * n:512 * n + 512], op0=ALU.mult, op1=ALU.mult, reads=[yb[n], ST, G1], writes=[TMPY])
                    cx.op("dve", "tensor_tensor", out=X1[:, j, :], in0=TMPY[:], in1=xi[:], op=ALU.add, reads=[TMPY, xi], writes=[X1])
                    cx.op("act", "activation", out=JUNK[:], in_=X1[:, j, :], func=AF.Square, accum_out=ST[:, 0:1],
                          reads=[X1], writes=[JUNK, ST])
                    cx.op("act", "activation", out=ST[:, 3:4], in_=ST[:, 0:1], func=AF.Sqrt, scale=1.0 / D, bias=1e-6, reads=[ST], writes=[ST])
                    cx.op("dve", "reciprocal", out=ST[:, 4:5], in_=ST[:, 3:4], reads=[ST], writes=[ST])
                    cx.op("dve", "tensor_scalar", out=XS[:], in0=X1[:, j, :], scalar1=ST[:, 4:5], scalar2=None, op0=ALU.mult,
                          reads=[X1, ST], writes=[XS])
                    for fc in range(8):
                        pb = PS[4 + fc // 4]
                        cx.op("pe", "transpose", pb[:, 128 * (fc % 4):128 * (fc % 4) + 128], XS[:, 128 * fc:128 * fc + 128],
                              ident_f, reads=[XS, CFB], writes=[pb])
                    for fc in range(8):
                        pb = PS[4 + fc // 4]
                        cx.op("act", "activation", out=H2T[:, fc, 128 * j:128 * j + 128],
                              in_=pb[:, 128 * (fc % 4):128 * (fc % 4) + 128], func=AF.Identity,
                              scale=DERV[:, 2, fc, seq:seq + 1], bias=DERV[:, 3, fc, seq:seq + 1],
                              reads=[pb, DERV], writes=[H2T])
                for ch in range(22):
                    wg = WG[ch % 3]
                    if not cv_waited[0]:
                        for sem_cv in cvsems:
                            nc.sync.wait_ge(sem_cv, 16)
                        cv_waited[0] = True
                    cx.dma("sp", wg[:].rearrange("p a b -> p (a b)"), wgubf_d[ch], writes=[wg])
                    pg, pu = PS[2 * (ch % 2)], PS[2 * (ch % 2) + 1]
                    for kc in range(8):
                        cx.op("pe", "matmul", pg[:], wg[:, kc, 0:128], H2T[:, kc, :], start=(kc == 0), stop=(kc == 7),
                              reads=[wg, H2T], writes=[pg])
                    for kc in range(8):
                        cx.op("pe", "matmul", pu[:], wg[:, kc, 128:256], H2T[:, kc, :], start=(kc == 0), stop=(kc == 7),
                              reads=[wg, H2T], writes=[pu])
                    sl_ = SIL[ch % 2]
                    cx.op("act", "activation", out=sl_[:], in_=pg[:], func=AF.Silu, reads=[pg], writes=[sl_])
                    cx.op("dve", "tensor_tensor", out=ACTT[:, ch, :], in0=sl_[:], in1=pu[:], op=ALU.mult,
                          reads=[sl_, pu], writes=[ACTT])
                for j in range(4):
                    T = 4 * gi + j
                    zb = [PS[4 + 2 * (j % 2)], PS[5 + 2 * (j % 2)]]
                    for n in range(2):
                        for ch in range(22):
                            cx.op("pe", "matmul", zb[n][:], ACTT[:, ch, 128 * j:128 * j + 128], WDN[:, ch, 512 * n:512 * n + 512],
                                  start=(ch == 0), stop=(ch == 21), reads=[ACTT, WDN], writes=[zb[n]])
                    for n in range(2):
                        cx.op("act", "activation", out=JUNK[:, 512 * n:512 * n + 512], in_=zb[n][:], func=AF.Square,
                              accum_out=ST[:, n:n + 1], reads=[zb[n]], writes=[JUNK, ST])
                    cx.op("dve", "tensor_tensor", out=ST[:, 2:3], in0=ST[:, 0:1], in1=ST[:, 1:2], op=ALU.add, reads=[ST], writes=[ST])
                    cx.op("act", "activation", out=ST[:, 3:4], in_=ST[:, 2:3], func=AF.Sqrt, scale=1.0 / D, bias=1e-6, reads=[ST], writes=[ST])
                    cx.op("dve", "reciprocal", out=ST[:, 4:5], in_=ST[:, 3:4], reads=[ST], writes=[ST])
                    for n in range(2):
                        cx.op("dve", "scalar_tensor_tensor", out=TMPY[:, 512 * n:512 * n + 512], in0=zb[n][:], scalar=ST[:, 4:5],
                              in1=G2[:, 512 * n:512 * n + 512], op0=ALU.mult, op1=ALU.mult, reads=[zb[n], ST, G2], writes=[TMPY])
                    cx.op("dve", "tensor_tensor", out=XS[:], in0=TMPY[:], in1=X1[:, j, :], op=ALU.add, reads=[TMPY, X1], writes=[XS])
                    cx.dma("sp", out_d[seq, 128 * T:128 * T + 128, :], XS[:], reads=[XS])
            cx.barrier()

        cx.barrier()
        cx.finish()
    return nc


def _prep(inputs):
    inp = {k: np.asarray(v) for k, v in inputs.items()}
    cf, cb, imt, wsm = _host_consts(inp)
    A, B = _col_index()
    shared = {
        "w_ada": np.ascontiguousarray(inp['w_ada'][0]),
        "cf": cf, "cb": cb, "imt": imt, "wsm": wsm,
        "w_inA": np.ascontiguousarray(inp['w_in'][0][:, A]),
        "w_inB": np.ascontiguousarray(inp['w_in'][0][:, B]),
        "cmp_w1": np.ascontiguousarray(inp['cmp_w1'][0]),
        "b2v": np.ascontiguousarray(inp['cmp_b2'][0, 1]),
        "w_out": np.ascontiguousarray(inp['w_out'][0]),
        "w_gate_up": np.ascontiguousarray(
            inp['w_gate_up'][0].reshape(8, 128, 2, 22, 128).transpose(3, 1, 0, 2, 4).reshape(22, 128, 2048)),
        "w_down": np.ascontiguousarray(inp['w_down'][0]),
    }
    maps = []
    for c in range(8):
        m = dict(shared)
        m["x"] = np.ascontiguousarray(inp['x'][2 * c:2 * c + 2])
        m["cT"] = np.ascontiguousarray(inp['c'][2 * c:2 * c + 2].T.reshape(8, 128, 2).transpose(1, 0, 2))
        maps.append(m)
    return maps


def kernel(**inputs):
    maps = _prep(inputs)
    nc = build()
    res = run_bass_kernel_spmd(nc, maps, core_ids=list(range(8)))
    return np.concatenate([r["out"] for r in res.results], axis=0).astype(np.float32)
```

```python
import numpy as np
import concourse.bass as bass
import concourse.mybir as mybir
from concourse.bass_utils import run_bass_kernel_spmd
from contextlib import ExitStack

F32 = mybir.dt.float32
BF16 = mybir.dt.bfloat16
ALU = mybir.AluOpType
AF = mybir.ActivationFunctionType
AX = mybir.AxisListType

S = 2048
D = 1024
NT = 16
DFF = 2816
NEG = -30000.0
IN_SPLITS = (512, 128, 128, 128, 128, 128, 128, 24, 512, 128, 16, 512, 64, 8)
NAMES = ['nq', 'nkc', 'nvc', 'nks', 'nvs', 'nkw', 'nvw', 'ngate', 'dq', 'dckv', 'dkr', 'iq', 'ik', 'iw']
OFF = dict(zip(NAMES, np.cumsum((0,) + IN_SPLITS)[:-1]))
NA_FM = 19 * 128
NA = NA_FM + 280
NB_FM = 18 * 128 + 32
NB = NB_FM + 136
BIS_ITERS = 12
import os as _os
_DBG = _os.environ.get('KDBG', '')


class _E:
    def __init__(self, name, eng, sem):
        self.name, self.eng, self.sem, self.tick, self.waited = name, eng, sem, 0, {}


class _St:
    __slots__ = ("w", "r")

    def __init__(self):
        self.w = None
        self.r = {}


class Buf:
    def __init__(self, ap, key):
        self.ap = ap
        self.key = key

    def __getitem__(self, idx):
        return self.ap[idx]


class Ctx:
    def __init__(self, nc, es, n_dma_sems=8):
        self.nc, self.es = nc, es
        self.E = {}
        for name, eng in (("pe", nc.tensor), ("act", nc.scalar), ("dve", nc.vector),
                          ("pool", nc.gpsimd), ("sp", nc.sync)):
            self.E[name] = _E(name, eng, es.enter_context(nc.semaphore("s_" + name)))
        self.dsems = {q: [[es.enter_context(nc.semaphore("d_%s%d" % (q, i))), 0] for i in range(n_dma_sems)]
                      for q in ("sp", "pool")}
        self.dnext = {"sp": 0, "pool": 0}
        self.st = {}

    def _state(self, b):
        k = b.key if isinstance(b, Buf) else (b if isinstance(b, str) else b.name)
        s = self.st.get(k)
        if s is None:
            s = self.st[k] = _St()
        return s

    def _wait(self, E, dep):
        kind, tk = dep
        if kind == E.name and E.name == "pe":
            return
        if E.waited.get(kind, 0) >= tk:
            return
        sem = self.dsems[kind[0]][kind[1]][0] if isinstance(kind, tuple) else self.E[kind].sem
        E.eng.wait_ge(sem, tk)
        E.waited[kind] = tk

    def _deps(self, E, reads, writes):
        deps = []
        for b in reads:
            s = self._state(b)
            if s.w is not None:
                deps.append(s.w)
        for b in writes:
            s = self._state(b)
            if s.w is not None:
                deps.append(s.w)
            deps.extend(s.r.items())
        for d in deps:
            self._wait(E, d)

    def _mark(self, token, reads, writes):
        for b in reads:
            s = self._state(b)
            if s.r.get(token[0], 0) < token[1]:
                s.r[token[0]] = token[1]
        for b in writes:
            s = self._state(b)
            s.w = token
            s.r = {}

    def op(self, en, fn, *args, reads=(), writes=(), **kw):
        E = self.E[en]
        self._deps(E, reads, writes)
        ins = getattr(E.eng, fn)(*args, **kw)
        E.tick += 1
        ins.then_inc(E.sem, 1)
        self._mark((en, E.tick), reads, writes)
        return ins

    def dma(self, q, out, in_, reads=(), writes=(), **kw):
        E = self.E[q]
        self._deps(E, reads, writes)
        i = self.dnext[q]
        self.dnext[q] = (i + 1) % len(self.dsems[q])
        slot = self.dsems[q][i]
        kind = (q, i)
        if slot[1] > 0:
            self._wait(E, (kind, slot[1]))
        slot[1] += 16
        E.eng.dma_start(out=out, in_=in_, **kw).then_inc(slot[0], 16)
        self._mark((kind, slot[1]), reads, writes)

    def barrier(self):
        toks = [(n, e.tick) for n, e in self.E.items() if e.tick > 0]
        for q in self.dsems:
            for i, slot in enumerate(self.dsems[q]):
                if slot[1] > 0:
                    toks.append(((q, i), slot[1]))
        for n, e in self.E.items():
            for t in toks:
                if t[0] == n:
                    if n != "sp" and e.waited.get(n, 0) < t[1]:
                        e.eng.wait_ge(e.sem, t[1])
                        e.waited[n] = t[1]
                else:
                    self._wait(e, t)
        self.st = {}

    def finish(self):
        E = self.E["sp"]
        for q in self.dsems:
            for i, slot in enumerate(self.dsems[q]):
                if slot[1] > 0:
                    self._wait(E, ((q, i), slot[1]))


class Mem:
    def __init__(self, big):
        self.big = big

    def view(self, key, off, shape, dt, pbase=0):
        esz = 4 if dt == F32 else 2
        nel = int(np.prod(shape[1:]))
        assert off % 4 == 0
        a = off // 2
        n = nel * esz // 2
        ap = self.big[pbase:pbase + shape[0], a:a + n]
        if dt == F32:
            ap = ap.bitcast(F32)
        if len(shape) == 3:
            ap = ap.rearrange("p (a b) -> p a b", a=shape[1])
        elif len(shape) == 4:
            ap = ap.rearrange("p (a b c) -> p a b c", a=shape[1], b=shape[2])
        return Buf(ap, key)


def _swap_head(cols):
    c = np.array(cols).copy()
    c[0:8] = cols[8:16]
    c[8:16] = cols[0:8]
    return c


def _col_index():
    def head(base, h):
        return np.arange(base + 64 * h, base + 64 * h + 64)
    A = []
    for j in range(4):
        a = np.concatenate([head(OFF['nq'], 2 * j), head(OFF['nq'], 2 * j + 1)])
        b = np.concatenate([_swap_head(head(OFF['nq'], 2 * j)), _swap_head(head(OFF['nq'], 2 * j + 1))])
        A += [a, b]
    a = np.concatenate([head(OFF['nkc'], 0), head(OFF['nkc'], 1)])
    b = np.concatenate([_swap_head(head(OFF['nkc'], 0)), _swap_head(head(OFF['nkc'], 1))])
    A += [a, b]
    A += [np.arange(OFF['nvc'], OFF['nvc'] + 128)]
    for nm in ('nks', 'nkw'):
        for g in range(2):
            a = np.concatenate([head(OFF[nm], g), head(OFF[nm], g)])
            b = np.concatenate([_swap_head(head(OFF[nm], g)), _swap_head(head(OFF[nm], g))])
            A += [a, b]
    A += [np.arange(OFF['nvs'], OFF['nvs'] + 128), np.arange(OFF['nvw'], OFF['nvw'] + 128),
          np.arange(OFF['ngate'], OFF['ngate'] + 24)]
    A = np.concatenate(A)
    assert A.size == NA
    B = []
    for nm in ('dq', 'iq'):
        for j in range(4):
            a = np.concatenate([head(OFF[nm], 2 * j), head(OFF[nm], 2 * j + 1)])
            b = np.concatenate([_swap_head(head(OFF[nm], 2 * j)), _swap_head(head(OFF[nm], 2 * j + 1))])
            B += [a, b]
    a = np.concatenate([head(OFF['ik'], 0), head(OFF['ik'], 0)])
    b = np.concatenate([_swap_head(head(OFF['ik'], 0)), _swap_head(head(OFF['ik'], 0))])
    B += [a, b]
    kr = np.arange(OFF['dkr'], OFF['dkr'] + 16)
    B += [kr, _swap_head(kr)]
    B += [np.arange(OFF['dckv'], OFF['dckv'] + 128), np.arange(OFF['iw'], OFF['iw'] + 8)]
    B = np.concatenate(B)
    assert B.size == NB
    return A, B


CF_ID = 0
CF_C = 128
CF_S = CF_C + 2048
CF_V = CF_S + 2048
CF_P2 = CF_V + 84
CF_N = CF_P2 + BIS_ITERS
CB_ID = 0
CB_EX = 128
CB_OV = CB_EX + 2048
CB_PE = CB_OV + 32
CB_SEL = CB_PE + 64
CB_N = CB_SEL + 128


def _host_consts(inp):
    cf = np.zeros((128, CF_N), np.float32)
    cf[:, CF_ID:CF_ID + 128] = np.eye(128, dtype=np.float32)
    inv_freq = 1.0 / (np.float32(500000.0) ** (np.arange(0, 16, 2, dtype=np.float32) / np.float32(16)))
    ang = np.arange(S, dtype=np.float32)[:, None] * inv_freq[None, :].astype(np.float32)
    cos, sin = np.cos(ang).astype(np.float32), np.sin(ang).astype(np.float32)
    for p in range(128):
        j = p % 64
        if j < 16:
            cf[p, CF_C:CF_C + S] = cos[:, j % 8]
            cf[p, CF_S:CF_S + S] = (-1.0 if j < 8 else 1.0) * sin[:, j % 8]
        else:
            cf[p, CF_C:CF_C + S] = 1.0
    v = cf[:, CF_V:CF_V + 84]
    v[:, 0:48] = inp['b_ada'][0].reshape(48, 128).T
    v[:, 48:56] = inp['g_pre_mix'][0].reshape(8, 128).T
    v[:, 56:64] = inp['g_post_mix'][0].reshape(8, 128).T
    v[:, 64:72] = inp['g_pre_ffn'][0].reshape(8, 128).T
    v[:, 72:80] = inp['g_post_ffn'][0].reshape(8, 128).T
    v[:, 80] = inp['cmp_b1'][0, 0]
    v[:, 81] = inp['cmp_b1'][0, 1]
    v[:, 82] = np.concatenate([inp['cmp_b2'][0, 0], inp['cmp_b2'][0, 0]])
    v[:, 83] = inp['g_kv_norm'][0]
    cf[:, CF_P2:CF_P2 + BIS_ITERS] = (0.5 ** np.arange(1, BIS_ITERS + 1, dtype=np.float64)).astype(np.float32)[None, :]
    cb = np.zeros((128, CB_N), np.float32)
    cb[:, CB_ID:CB_ID + 128] = np.eye(128, dtype=np.float32)
    for j in range(32):
        cb[j, CB_EX + 64 * j:CB_EX + 64 * j + 64] = 1.0
    ci = np.arange(127)[:, None] * 16
    sj = np.arange(32)[None, :] * 64
    cb[0:127, CB_OV:CB_OV + 32] = ((ci < sj + 64) & (ci + 32 > sj)).astype(np.float32)
    cb[0:64, CB_PE:CB_PE + 32] = inp['cmp_pe'][0, 0].T
    cb[0:64, CB_PE + 32:CB_PE + 64] = inp['cmp_pe'][0, 1].T
    for i in range(16):
        cb[i, CB_SEL + i] = 1.0
        cb[i, CB_SEL + 64 + i] = 1.0
    t = (np.arange(16)[None, :, None] * 128 + np.arange(128)[:, None, None])
    blk = t // 64
    j = np.arange(32)[None, None, :]
    visible = j <= blk
    forced = (j == 0) | (j == blk) | (j == blk - 1)
    mm = (visible & ~forced).astype(np.float32)
    ba = np.where(visible, np.where(forced, 1e6, 0.0), -1e30).astype(np.float32)
    imt = np.concatenate([mm.reshape(128, 512), ba.reshape(128, 512)], axis=1)
    wuk = np.zeros((128, 4, 128), np.float32)
    for h in range(8):
        wuk[:, h // 2, (h % 2) * 64 + 16:(h % 2) * 64 + 64] = inp['w_uk'][0, h]
    wuv = np.transpose(inp['w_uv'][0], (1, 0, 2)).reshape(128, 512)
    w2 = np.concatenate([inp['cmp_w2'][0, 0], inp['cmp_w2'][0, 0], inp['cmp_w2'][0, 1]], axis=1)
    wsm = np.concatenate([wuk.reshape(128, 512), wuv, w2], axis=1).astype(np.float32)
    return cf, cb, imt, wsm


def build(n_seq=2, stage=99, dbg_cols=0):
    nc = bass.Bass("TRN2", target_bir_lowering=False)
    dt_ = nc.dram_tensor
    x_d = dt_("x", [2, S, D], F32, kind="ExternalInput").ap()
    cT_d = dt_("cT", [128, 8, 2], F32, kind="ExternalInput").ap()
    wada_d = dt_("w_ada", [D, 6 * D], F32, kind="ExternalInput").ap()
    cf_d = dt_("cf", [128, CF_N], F32, kind="ExternalInput").ap()
    cb_d = dt_("cb", [128, CB_N], F32, kind="ExternalInput").ap()
    imt_d = dt_("imt", [128, 1024], F32, kind="ExternalInput").ap()
    wsm_d = dt_("wsm", [128, 1216], F32, kind="ExternalInput").ap()
    winA_d = dt_("w_inA", [D, NA], F32, kind="ExternalInput").ap()
    winB_d = dt_("w_inB", [D, NB], F32, kind="ExternalInput").ap()
    w1_d = dt_("cmp_w1", [2, 2048, 128], F32, kind="ExternalInput").ap()
    b2v_d = dt_("b2v", [64], F32, kind="ExternalInput").ap()
    wout_d = dt_("w_out", [D, D], F32, kind="ExternalInput").ap()
    wgu_d = dt_("w_gate_up", [22, 128, 2048], F32, kind="ExternalInput").ap()
    wdn_d = dt_("w_down", [DFF, D], F32, kind="ExternalInput").ap()
    out_d = dt_("out", [2, S, D], F32, kind="ExternalOutput").ap()
    wgubf_d = dt_("wgu_bf16", [22, 128, 2048], BF16, kind="Internal").ap()
    wdnbf_d = dt_("wdn_bf16", [DFF, D], BF16, kind="Internal").ap()
    woutbf_d = dt_("wout_bf16", [D, D], BF16, kind="Internal").ap()
    winAbf_d = dt_("winA_bf16", [D, NA], BF16, kind="Internal").ap()
    winBbf_d = dt_("winB_bf16", [D, NB], BF16, kind="Internal").ap()
    dbg_d = dt_("dbg", [128, dbg_cols], F32, kind="ExternalOutput").ap() if dbg_cols else None

    with ExitStack() as es:
        cx = Ctx(nc, es)
        TOT = 206 * 1024
        big = es.enter_context(nc.sbuf_tensor("big", [128, TOT // 2], BF16))
        mem = Mem(big)
        PS = [Buf(es.enter_context(nc.psum_tensor("ps%d" % i, [128, 512], F32))[:], "ps%d" % i) for i in range(8)]

        def psb(i):
            return PS[i].ap.bitcast(BF16)

        KB = 1024
        o = 0
        CFB = mem.view("cf", o, [128, CF_N], F32); o += CF_N * 4
        CBB = mem.view("cb", o, [128, CB_N], BF16); o += CB_N * 2
        MSK = mem.view("msk", o, [128, 8, 512], BF16); o += 8 * 512 * 2
        CMN = mem.view("cmn", o, [128, 2048], BF16); o += 2048 * 2
        ONESF = mem.view("onesf", o, [128, 128], F32); o += 512
        MODC = mem.view("modc", o, [128, 48, 2], F32); o += 384
        DERV = mem.view("derv", o, [128, 6, 8, 2], F32); o += 384
        CACT = mem.view("cact", o, [128, 8, 2], F32); o += 64
        SMALL = mem.view("small", o, [128, 64], F32); o += 256
        G1 = mem.view("G1", o, [128, 1024], F32); o += 4096
        G2 = mem.view("G2", o, [128, 1024], F32); o += 4096
        assert o <= 44 * KB, o
        R_OT = 44 * KB
        R_HT = 76 * KB
        R_WIN = 108 * KB
        R_PH = 152 * KB
        OT = mem.view("OT", R_OT, [128, 8, 2048], BF16)
        HT = mem.view("HT", R_HT, [128, 8, 2048], BF16)

        ident_f = CFB[:, CF_ID:CF_ID + 128]
        ident_b = CBB[:, CB_ID:CB_ID + 128]
        ropeC = CFB[:, CF_C:CF_C + S]
        ropeS = CFB[:, CF_S:CF_S + S]
        vec = lambda c0, c1: CFB[:, CF_V + c0:CF_V + c1]

        def dbg_dump(ap, col0, ncols, rd):
            if dbg_d is not None:
                cx.dma("pool", dbg_d[0:ap.shape[0], col0:col0 + ncols], ap, reads=[rd])

        cx.dma("sp", CFB[:], cf_d, writes=[CFB])
        cx.dma("pool", CBB[:], cb_d, writes=[CBB])
        cx.dma("sp", CACT[:], cT_d, writes=[CACT])
        cx.op("pool", "memset", MSK[:], 0.0, writes=[MSK])
        for k in range(4):
            cx.op("pool", "affine_select", out=MSK[:, k, :], in_=MSK[:, k, :], pattern=[[-1, 512]],
                  compare_op=ALU.is_gt, fill=1.0, base=128 * k, channel_multiplier=1, reads=[MSK], writes=[MSK])
        for k in range(1, 5):
            cx.op("pool", "affine_select", out=MSK[:, 3 + k, :], in_=MSK[:, 3 + k, :], pattern=[[1, 512]],
                  compare_op=ALU.is_ge, fill=1.0, base=-512 + 128 * k, channel_multiplier=-1, reads=[MSK], writes=[MSK])
        cx.op("pool", "memset", CMN[:], 0.0, writes=[CMN])
        cx.op("pool", "affine_select", out=CMN[:], in_=CMN[:], pattern=[[-1, 2048]],
              compare_op=ALU.is_gt, fill=1.0, base=31, channel_multiplier=16, reads=[CMN], writes=[CMN])
        cx.op("pool", "memset", ONESF[:], 1.0, writes=[ONESF])
        cx.op("act", "activation", out=CACT[:], in_=CACT[:], func=AF.Silu, reads=[CACT], writes=[CACT])
        WA = [mem.view("wa0", R_HT, [128, 8, 1024], F32), mem.view("wa1", R_WIN, [128, 8, 1024], F32)]
        wada_v = wada_d.rearrange("(kc p) n -> p kc n", p=128)
        for v in range(6):
            wb = WA[v % 2]
            for kc in range(8):
                cx.dma("sp", wb[:, kc, :], wada_v[:, kc, 1024 * v:1024 * v + 1024], writes=[wb])
            for fc in range(8):
                col = (v * 8 + fc) * 2
                for kc in range(8):
                    cx.op("pe", "matmul", PS[0][:, col:col + 2], wb[:, kc, 128 * fc:128 * fc + 128], CACT[:, kc, :],
                          start=(kc == 0), stop=(kc == 7), reads=[wb, CACT], writes=[PS[0]])
        cx.op("dve", "tensor_tensor", out=MODC[:], in0=PS[0][:, 0:96].rearrange("p (a b) -> p a b", b=2),
              in1=vec(0, 48).unsqueeze(2).to_broadcast([128, 48, 2]), op=ALU.add, reads=[PS[0], CFB], writes=[MODC])
        def gb(c0):
            return vec(c0, c0 + 8).unsqueeze(2).to_broadcast([128, 8, 2])
        cx.op("dve", "scalar_tensor_tensor", out=DERV[:, 0], in0=MODC[:, 8:16, :], scalar=1.0, in1=gb(48),
              op0=ALU.add, op1=ALU.mult, reads=[MODC, CFB], writes=[DERV])
        cx.op("dve", "tensor_copy", out=DERV[:, 1], in_=MODC[:, 0:8, :], reads=[MODC], writes=[DERV])
        cx.op("dve", "scalar_tensor_tensor", out=DERV[:, 2], in0=MODC[:, 32:40, :], scalar=1.0, in1=gb(64),
              op0=ALU.add, op1=ALU.mult, reads=[MODC, CFB], writes=[DERV])
        cx.op("dve", "tensor_copy", out=DERV[:, 3], in_=MODC[:, 24:32, :], reads=[MODC], writes=[DERV])
        cx.op("dve", "tensor_tensor", out=DERV[:, 4], in0=MODC[:, 16:24, :], in1=gb(56), op=ALU.mult,
              reads=[MODC, CFB], writes=[DERV])
        cx.op("dve", "tensor_tensor", out=DERV[:, 5], in0=MODC[:, 40:48, :], in1=gb(72), op=ALU.mult,
              reads=[MODC, CFB], writes=[DERV])
        cx.barrier()

        cvsems = []
        cv_waited = [False]
        for seq in range(n_seq):
            if seq > 0:
                cx.dma("sp", CFB[:, CF_C:CF_C + 2 * S], cf_d[:, CF_C:CF_C + 2 * S], writes=[CFB])
            WIN = mem.view("win", R_WIN, [128, 8, NA], BF16)
            if seq == 0:
                cx.dma("pool", WIN[:], winA_d.rearrange("(kc p) n -> p kc n", p=128), writes=[WIN])
            else:
                cx.dma("sp", WIN[:], winAbf_d.rearrange("(kc p) n -> p kc n", p=128), writes=[WIN])
            DG = mem.view("dg", R_PH, [128, 128], F32)
            for gi, GT_ in ((4, G1), (5, G2)):
                for fc in range(8):
                    cx.op("dve", "tensor_scalar", out=DG[:], in0=ident_f, scalar1=DERV[:, gi, fc, seq:seq + 1],
                          scalar2=None, op0=ALU.mult, reads=[CFB, DERV], writes=[DG])
                    pb = PS[fc // 4]
                    cx.op("pe", "matmul", pb[:, 128 * (fc % 4):128 * (fc % 4) + 128], ONESF[:], DG[:],
                          start=True, stop=True, reads=[ONESF, DG], writes=[pb])
                    if fc % 4 == 3:
                        cx.op("act", "copy", out=GT_[:, 512 * (fc // 4):512 * (fc // 4) + 512], in_=pb[:],
                              reads=[pb], writes=[GT_])
            cx.barrier()
            XIN = [mem.view("xin%d" % i, R_PH + 4 * KB * i, [128, 1024], F32) for i in range(2)]
            XS = [mem.view("xs%d" % i, R_PH + 8 * KB + 4 * KB * i, [128, 1024], F32) for i in range(2)]
            JUNK = mem.view("junk", R_PH + 16 * KB, [128, 1024], F32)
            SS = [mem.view("ss%d" % i, R_PH + 20 * KB + 64 * i, [128, 4], F32) for i in range(2)]
            def p1_stats(i):
                xi, xs, ss = XIN[i % 2], XS[i % 2], SS[i % 2]
                cx.dma("sp", xi[:], x_d[seq, 128 * i:128 * i + 128, :], writes=[xi])
                cx.op("act", "activation", out=JUNK[:], in_=xi[:], func=AF.Square, accum_out=ss[:, 0:1],
                      reads=[xi], writes=[JUNK, ss])
                cx.op("act", "activation", out=ss[:, 1:2], in_=ss[:, 0:1], func=AF.Sqrt, scale=1.0 / D, bias=1e-6,
                      reads=[ss], writes=[ss])
                cx.op("dve", "reciprocal", out=ss[:, 2:3], in_=ss[:, 1:2], reads=[ss], writes=[ss])
                cx.op("dve", "tensor_scalar", out=xs[:], in0=xi[:], scalar1=ss[:, 2:3], scalar2=None, op0=ALU.mult,
                      reads=[xi, ss], writes=[xs])

            p1_stats(0)
            for i in range(NT):
                xs = XS[i % 2]
                for fc in range(8):
                    pb = PS[2 * (i % 2) + fc // 4]
                    cx.op("pe", "transpose", pb[:, 128 * (fc % 4):128 * (fc % 4) + 128],
                          xs[:, 128 * fc:128 * fc + 128], ident_f, reads=[xs, CFB], writes=[pb])
                if i + 1 < NT:
                    p1_stats(i + 1)
                for fc in range(8):
                    pb = PS[2 * (i % 2) + fc // 4]
                    if fc < 4:
                        cx.op("act", "activation", out=HT[:, fc, 128 * i:128 * i + 128],
                              in_=pb[:, 128 * (fc % 4):128 * (fc % 4) + 128], func=AF.Identity,
                              scale=DERV[:, 0, fc, seq:seq + 1], bias=DERV[:, 1, fc, seq:seq + 1],
                              reads=[pb, DERV], writes=[HT])
                    else:
                        cx.op("dve", "tensor_scalar", out=HT[:, fc, 128 * i:128 * i + 128],
                              in0=pb[:, 128 * (fc % 4):128 * (fc % 4) + 128],
                              scalar1=DERV[:, 0, fc, seq:seq + 1], scalar2=DERV[:, 1, fc, seq:seq + 1],
                              op0=ALU.mult, op1=ALU.add, reads=[pb, DERV], writes=["HTd"])
            cx.barrier()

            o = R_PH
            QN = mem.view("QN", o, [128, 4, 2048], BF16); o += 16 * KB
            KS = mem.view("KS", o, [128, 2, 2048], BF16); o += 8 * KB
            KW = mem.view("KW", o, [128, 2, 2048], BF16); o += 8 * KB
            KCR = mem.view("KCR", o, [128, 2048], BF16); o += 4 * KB
            VCR = mem.view("VCR", o, [128, 2048], BF16); o += 4 * KB
            VT = mem.view("VT", o, [128, 16, 4, 65], BF16); o += 8320
            GT = mem.view("GT", o, [128, 16, 24], F32); o += 1536
            o_T1 = o
            T1 = mem.view("T1", o, [128, 512], F32); o += 2048
            T2 = mem.view("T2", o, [128, 512], F32); o += 2048
            assert o <= TOT, o
            TL = [[T1], [T2]]
            ucnt = [0]

            def proj_unit(WINb, ca, cb_, M, tc, dst_ap, dstbuf):
                u = ucnt[0]; ucnt[0] += 1
                pa, pb = PS[2 * (u % 2)], PS[2 * (u % 2) + 1]
                for kc in range(8):
                    cx.op("pe", "matmul", pa[0:M, :], WINb[:, kc, ca:ca + M], HT[:, kc, 512 * tc:512 * tc + 512],
                          start=(kc == 0), stop=(kc == 7), reads=[WINb, HT], writes=[pa])
                if cb_ is None:
                    cx.op("act", "copy", out=dst_ap, in_=pa[0:M, :], reads=[pa], writes=[dstbuf])
                    return
                for kc in range(8):
                    cx.op("pe", "matmul", pb[0:M, :], WINb[:, kc, cb_:cb_ + M], HT[:, kc, 512 * tc:512 * tc + 512],
                          start=(kc == 0), stop=(kc == 7), reads=[WINb, HT], writes=[pb])
                T1, T2 = TL[0][u % len(TL[0])], TL[1][u % len(TL[1])]
                cx.op("dve", "tensor_tensor", out=T1[0:M, :], in0=pa[0:M, :], in1=ropeC[0:M, 512 * tc:512 * tc + 512],
                      op=ALU.mult, reads=[pa, CFB], writes=[T1])
                cx.op("dve", "tensor_tensor", out=T2[0:M, :], in0=pb[0:M, :], in1=ropeS[0:M, 512 * tc:512 * tc + 512],
                      op=ALU.mult, reads=[pb, CFB], writes=[T2])
                cx.op("pool", "tensor_tensor", out=dst_ap, in0=T1[0:M, :], in1=T2[0:M, :], op=ALU.add,
                      reads=[T1, T2], writes=[dstbuf])

            cx.op("pool", "memset", VT[:, :, :, 64:65], 1.0, writes=[VT])
            for tc in range(4):
                sl = slice(512 * tc, 512 * tc + 512)
                for j in range(4):
                    proj_unit(WIN, 256 * j, 256 * j + 128, 128, tc, QN[:, j, sl], QN)
                proj_unit(WIN, 1024, 1152, 128, tc, KCR[:, sl], KCR)
                proj_unit(WIN, 1280, None, 128, tc, VCR[:, sl], VCR)
                for g in range(2):
                    proj_unit(WIN, 1408 + 256 * g, 1536 + 256 * g, 128, tc, KS[:, g, sl], KS)
                    proj_unit(WIN, 1920 + 256 * g, 2048 + 256 * g, 128, tc, KW[:, g, sl], KW)
            for i in range(NT):
                pb = PS[4 + i % 2]
                for kc in range(8):
                    cx.op("pe", "matmul", pb[:, 0:280], HT[:, kc, 128 * i:128 * i + 128], WIN[:, kc, NA_FM:NA],
                          start=(kc == 0), stop=(kc == 7), reads=[HT, WIN], writes=[pb])
                cx.op("act", "copy", out=VT[:, i, :, 0:64], in_=pb[:, 0:256].rearrange("p (a b) -> p a b", a=4),
                      reads=[pb], writes=[VT])
                cx.op("act", "activation", out=GT[:, i, :], in_=pb[:, 256:280], func=AF.Sigmoid, reads=[pb], writes=[GT])
            cx.barrier()

            o = R_WIN
            W1 = mem.view("W1", o, [128, 2, 32, 128], BF16); o += 16 * KB
            WSM = mem.view("WSM", o, [128, 1216], BF16); o += 2432
            IMT = mem.view("IMT", o, [128, 2, 16, 32], F32); o += 4096
            PT = []
            for i in range(4):
                PT.append(mem.view("PT%d" % i, o, [128, 512], BF16)); o += 1024
            XG = mem.view("XG", o, [128, 128], F32); o += 512
            UU = mem.view("UU", o, [128, 128], F32); o += 512
            HIDT = mem.view("HIDT", o, [128, 128], BF16); o += 256
            KCT = mem.view("KCT", o, [128, 2, 128], BF16); o += 512
            VCX = mem.view("VCX", o, [128, 2, 98], BF16); o += 392
            B2V = mem.view("B2V", o, [128, 64], F32); o += 256
            BIAS1 = mem.view("BIAS1", o, [128, 2], F32); o += 8
            OA = mem.view("OA", o, [128, 4, 512], F32); o += 8192
            OAB = mem.view("OAB", o_T1, [128, 4, 512], BF16)
            IMPS = []
            for g in range(2):
                IMPS.append(mem.view("IMP%d" % g, o, [128, 4, 32], F32)); o += 512
            TMPI = mem.view("TMPI", o, [128, 4, 32], F32); o += 512
            IMPM = mem.view("IMPM", o, [128, 4, 32], F32); o += 512
            TOP8 = mem.view("TOP8", o, [128, 4, 8], F32); o += 128
            NSELB = mem.view("NSELB", o, [128, 4, 32], BF16); o += 256
            NSELT = mem.view("NSELT", o, [128, 2, 512], BF16); o += 2048
            TMPO = mem.view("TMPO", o, [128, 4, 64], F32); o += 1024
            assert o <= R_PH, o
            RINV = Buf(SMALL[:, 0:4], "RINV")
            COEF = Buf(SMALL[:, 4:8], "COEF")

            for kv in range(2):
                src = w1_d[kv].rearrange("(l d) j -> d l j", d=64)
                cx.dma("pool", W1[0:64, kv], src, writes=[W1])
                cx.dma("pool", W1[64:128, kv], src, writes=[W1])
            cx.dma("pool", WSM[:], wsm_d, writes=[WSM])
            cx.dma("sp", IMT[:].rearrange("p a b c -> p (a b c)"), imt_d, writes=[IMT])
            cx.dma("sp", B2V[:], b2v_d.partition_broadcast(128), writes=[B2V])
            if seq == 0:
                for i in range(11):
                    sem_cv = es.enter_context(nc.semaphore("cv%d" % i))
                    cvsems.append(sem_cv)
                    nc.gpsimd.dma_start(out=wgubf_d[2 * i:2 * i + 2].rearrange("c p n -> (c p) n"),
                                        in_=wgu_d[2 * i:2 * i + 2].rearrange("c p n -> (c p) n")).then_inc(sem_cv, 16)
                cv_list = [(wdnbf_d[704 * i:704 * i + 704, :], wdn_d[704 * i:704 * i + 704, :]) for i in range(4)]
                cv_list += [(woutbf_d, wout_d)]
                if n_seq > 1:
                    cv_list += [(winAbf_d, winA_d), (winBbf_d, winB_d)]
                for i, (dst_, src_) in enumerate(cv_list):
                    sem_cv = es.enter_context(nc.semaphore("cw%d" % i))
                    cvsems.append(sem_cv)
                    nc.gpsimd.dma_start(out=dst_, in_=src_).then_inc(sem_cv, 16)
            cx.op("pool", "memset", HIDT[:], 0.0, writes=[HIDT])
            cx.op("pool", "memset", NSELT[:], 0.0, writes=[NSELT])
            cx.op("pool", "memset", VCX[:, :, 64:65], 1.0, writes=[VCX])
            for g in range(2):
                cx.op("pool", "tensor_copy", out=VCX[:, g, 65:97], in_=CBB[:, CB_OV:CB_OV + 32], reads=[CBB], writes=[VCX])
            for kv in range(2):
                for l in range(32):
                    cx.op("pe", "matmul", PS[6][:, kv:kv + 1], W1[0:64, kv, l, :], CBB[0:64, CB_PE + 32 * kv + l:CB_PE + 32 * kv + l + 1],
                          start=(l == 0), stop=(l == 31), reads=[W1, CBB], writes=[PS[6]])
            cx.op("dve", "tensor_tensor", out=BIAS1[:], in0=PS[6][:, 0:2], in1=vec(80, 82), op=ALU.add,
                  reads=[PS[6], CFB], writes=[BIAS1])
            for kv in range(2):
                for g in range(2):
                    srcb = KCR if kv == 0 else VCR
                    hps = PS[4 + g]
                    for l in range(32):
                        cx.op("pe", "matmul", hps[:, 0:127], W1[64 * g:64 * g + 64, kv, l, :],
                              srcb[64 * g:64 * g + 64, l:l + 2017:16], start=(l == 0), stop=(l == 31),
                              reads=[W1, srcb], writes=[hps])
                    cx.op("act", "activation", out=XG[:, 0:127], in_=hps[:, 0:127], func=AF.Identity,
                          bias=BIAS1[:, kv:kv + 1], reads=[hps, BIAS1], writes=[XG])
                    cx.op("dve", "tensor_tensor", out=UU[:, 0:127], in0=XG[:, 0:127], in1=XG[:, 0:127], op=ALU.mult,
                          reads=[XG], writes=[UU])
                    cx.op("dve", "tensor_scalar", out=UU[:, 0:127], in0=UU[:, 0:127], scalar1=0.044715, scalar2=1.0,
                          op0=ALU.mult, op1=ALU.add, reads=[UU], writes=[UU])
                    cx.op("dve", "tensor_tensor", out=UU[:, 0:127], in0=UU[:, 0:127], in1=XG[:, 0:127], op=ALU.mult,
                          reads=[UU, XG], writes=[UU])
                    cx.op("act", "activation", out=UU[:, 0:127], in_=UU[:, 0:127], func=AF.Sigmoid, scale=1.5957691216057308,
                          reads=[UU], writes=[UU])
                    cx.op("dve", "tensor_tensor", out=HIDT[:, 0:127], in0=XG[:, 0:127], in1=UU[:, 0:127], op=ALU.mult,
                          reads=[XG, UU], writes=[HIDT])
                    if kv == 0:
                        cx.op("pe", "matmul", PS[6][:, 0:128], WSM[:, 1024:1152], HIDT[:], start=True, stop=True,
                              reads=[WSM, HIDT], writes=[PS[6]])
                        cx.op("act", "activation", out=KCT[:, g, :], in_=PS[6][:, 0:128], func=AF.Identity,
                              bias=vec(82, 83), reads=[PS[6], CFB], writes=[KCT])
                    else:
                        cx.op("pe", "matmul", PS[6][:, 0:64], HIDT[:], WSM[:, 1152:1216], start=True, stop=True,
                              reads=[WSM, HIDT], writes=[PS[6]])
                        cx.op("dve", "tensor_tensor", out=VCX[:, g, 0:64], in0=PS[6][:, 0:64], in1=B2V[:], op=ALU.add,
                              reads=[PS[6], B2V], writes=[VCX])

            pipe = {"pend": [], "u": 0, "job": 0}
            SCB = [PS[0], PS[1], PS[4], PS[5]]
            grp = []

            def unit(kT, qT, extras, V, acc, ncols, first, rk, rq, rv, after=None, mmask=None, ex128=(), last=False):
                u = pipe["u"]; pipe["u"] += 1
                grp.append(dict(u=u, kT=kT, qT=qT, ex=extras, ex128=ex128, V=V, acc=acc, ncols=ncols, first=first,
                                rk=rk, rq=rq, rv=rv, after=after, mmask=mmask, last=last))
                if len(grp) == (1 if 'G1' in _DBG else 2):
                    emit_group()

            def emit_group():
                if not grp:
                    return
                for d in grp:
                    sbk = SCB[d["u"] % 4]
                    nex = len(d["ex"]) + len(d["ex128"])
                    cx.op("pe", "matmul", sbk[:], d["kT"], d["qT"], start=True, stop=(nex == 0),
                          reads=[d["rk"], d["rq"]], writes=[sbk])
                for d in grp:
                    sbk = SCB[d["u"] % 4]
                    nex = len(d["ex"]) + len(d["ex128"])
                    for n_, (l_, r_, c0, c1, rd) in enumerate(d["ex"]):
                        cx.op("pe", "matmul", sbk[:, c0:c1], l_, r_, start=False, stop=(n_ == nex - 1), reads=rd, writes=[sbk])
                for d in grp:
                    sbk = SCB[d["u"] % 4]
                    nex = len(d["ex"]) + len(d["ex128"])
                    for n_, (l_, r_, c0, c1, rd) in enumerate(d["ex128"]):
                        cx.op("pe", "matmul", sbk[:, c0:c1], l_, r_, start=False, stop=(len(d["ex"]) + n_ == nex - 1),
                              reads=rd, writes=[sbk])
                for pvf in pipe["pend"]:
                    pvf()
                pipe["pend"] = []
                for d in grp:
                    sbk = SCB[d["u"] % 4]
                    pt = PT[d["u"] % 4]
                    cx.op("act", "activation", out=pt[:], in_=sbk[:], func=AF.Exp, scale=0.125, reads=[sbk], writes=[pt])
                    if d["mmask"] is not None and 'NM' not in _DBG:
                        cx.op("dve", "tensor_tensor", out=pt[:], in0=pt[:], in1=d["mmask"][0], op=ALU.mult,
                              reads=[pt, d["mmask"][1]], writes=[pt])

                    def pv(d=d, pt=pt):
                        for j in range(4):
                            cx.op("pe", "matmul", d["acc"][:, d["ncols"] * j:d["ncols"] * j + d["ncols"]],
                                  pt[:, 128 * j:128 * j + 128], d["V"], start=(d["first"] and j == 0),
                                  stop=(True if 'ST' in _DBG else (d["last"] and j == 3)), reads=[pt, d["rv"]], writes=[d["acc"]])
                        if d["after"] is not None:
                            d["after"]()
                    pipe["pend"].append(pv)
                grp.clear()

            def flush():
                emit_group()
                for pvf in pipe["pend"]:
                    pvf()
                pipe["pend"] = []

            def next_acc():
                a = PS[2 + pipe["job"] % 2]
                pipe["job"] += 1
                return a

            def nsa_final(acc, ncols, c, h, br, first_branch):
                accv = acc[:, 0:4 * ncols].rearrange("p (j n) -> p j n", j=4)

                def f():
                    cx.op("dve", "tensor_scalar", out=RINV[:], in0=accv[:, :, 64], scalar1=1e-30, scalar2=None,
                          op0=ALU.max, reads=[acc], writes=[RINV])
                    cx.op("dve", "reciprocal", out=RINV[:], in_=RINV[:], reads=[RINV], writes=[RINV])
                    if br == 0:
                        fi = (h % 4 == 0)
                        IMP = IMPS[h // 4]
                        dst = IMP if fi else TMPI
                        cx.op("dve", "tensor_tensor", out=dst[:], in0=accv[:, :, 65:97],
                              in1=RINV[:].unsqueeze(2).to_broadcast([128, 4, 32]), op=ALU.mult,
                              reads=[acc, RINV], writes=[dst])
                        if not fi:
                            cx.op("pool", "tensor_tensor", out=IMP[:], in0=IMP[:], in1=TMPI[:], op=ALU.add,
                                  reads=[IMP, TMPI], writes=[IMP])
                    cx.op("dve", "tensor_tensor", out=COEF[:], in0=RINV[:], in1=GT[:, 4 * c:4 * c + 4, 8 * br + h],
                          op=ALU.mult, reads=[RINV, GT], writes=[COEF])
                    cb3 = COEF[:].unsqueeze(2).to_broadcast([128, 4, 64])
                    if first_branch:
                        cx.op("dve", "tensor_tensor", out=OA[:, :, 64 * h:64 * h + 64], in0=accv[:, :, 0:64], in1=cb3,
                              op=ALU.mult, reads=[acc, COEF], writes=[OA])
                    else:
                        cx.op("dve", "tensor_tensor", out=TMPO[:], in0=accv[:, :, 0:64], in1=cb3, op=ALU.mult,
                              reads=[acc, COEF], writes=[TMPO])
                        cx.op("pool", "tensor_tensor", out=OA[:, :, 64 * h:64 * h + 64], in0=OA[:, :, 64 * h:64 * h + 64],
                              in1=TMPO[:], op=ALU.add, reads=[OA, TMPO], writes=[OA])
                return f

            def to_OT(SRC, c, fc0):
                for fc in range(4):
                    for j in range(4):
                        col = ((fc % 2) * 4 + j) * 128
                        cx.op("pe", "transpose", psb(6 + fc // 2)[:, col:col + 128], SRC[:, j, 128 * fc:128 * fc + 128],
                              ident_b, reads=[SRC, CBB], writes=[PS[6 + fc // 2]])
                for fc in range(4):
                    cx.op("act", "copy", out=OT[:, fc0 + fc, 512 * c:512 * c + 512],
                          in_=psb(6 + fc // 2)[:, (fc % 2) * 512:(fc % 2) * 512 + 512], reads=[PS[6 + fc // 2]], writes=[OT])

            for c in range(4):
                qs = slice(512 * c, 512 * c + 512)
                for h in range(8):
                    g, b_ = h // 4, 64 * (h % 2)
                    acc = next_acc()
                    unit(KCT[b_:b_ + 64, g, :], QN[b_:b_ + 64, h // 2, qs],
                         [], VCX[:, g, 0:97], acc, 97, True,
                         KCT, QN, VCX, after=nsa_final(acc, 97, c, h, 0, True), mmask=(CMN[:, qs], CMN), last=True)
                for h in range(8):
                    g, b_ = h // 4, 64 * (h % 2)
                    acc = next_acc()
                    tiles = list(range(max(0, 4 * c - 4), 4 * c + 4))
                    for n_, i in enumerate(tiles):
                        mk = MSK[:, 3 + (4 * c - i), :] if i < 4 * c else MSK[:, i - 4 * c, :]
                        unit(KW[b_:b_ + 64, g, 128 * i:128 * i + 128], QN[b_:b_ + 64, h // 2, qs],
                             [], VT[:, i, 2 + g, :], acc, 65, n_ == 0, KW, QN, VT,
                             after=(nsa_final(acc, 65, c, h, 2, False) if n_ == len(tiles) - 1 else None), mmask=(mk, MSK),
                             last=(n_ == len(tiles) - 1))
                flush()
                for g in range(2):
                    IMPg = IMPS[g]
                    cx.op("dve", "tensor_tensor", out=IMPM[:], in0=IMPg[:], in1=IMT[:, 0, 4 * c:4 * c + 4, :], op=ALU.mult,
                          reads=[IMPg, IMT], writes=[IMPM])
                    cx.op("dve", "tensor_tensor", out=IMPM[:], in0=IMPM[:], in1=IMT[:, 1, 4 * c:4 * c + 4, :], op=ALU.add,
                          reads=[IMPM, IMT], writes=[IMPM])
                    for j in range(4):
                        cx.op("dve", "max", out=TOP8[:, j, :], in_=IMPM[:, j, :], reads=[IMPM], writes=[TOP8])
                    for j in range(4):
                        cx.op("dve", "tensor_scalar", out=NSELB[:, j, :], in0=IMPM[:, j, :], scalar1=TOP8[:, j, 7:8],
                              scalar2=NEG, op0=ALU.is_lt, op1=ALU.mult, reads=[IMPM, TOP8], writes=[NSELB])
                    for j in range(4):
                        cx.op("pe", "transpose", psb(6)[0:32, 128 * j:128 * j + 128], NSELB[:, j, :], ident_b,
                              reads=[NSELB, CBB], writes=[PS[6]])
                    cx.op("act", "copy", out=NSELT[0:32, g, :], in_=psb(6)[0:32, 0:512], reads=[PS[6]], writes=[NSELT])
                for h in range(8):
                    g, b_ = h // 4, 64 * (h % 2)
                    acc = next_acc()
                    tiles = list(range(0, 4 * c + 4))
                    for n_, i in enumerate(tiles):
                        KX = 32
                        ex = [(CBB[0:KX, CB_EX + 128 * i:CB_EX + 128 * i + 128], NSELT[0:KX, g, :], 0, 512, [CBB, NSELT])]
                        unit(KS[b_:b_ + 64, g, 128 * i:128 * i + 128], QN[b_:b_ + 64, h // 2, qs], ex,
                             VT[:, i, g, :], acc, 65, n_ == 0, KS, QN, VT,
                             after=(nsa_final(acc, 65, c, h, 1, False) if n_ == len(tiles) - 1 else None),
                             mmask=((MSK[:, i - 4 * c, :], MSK) if i >= 4 * c else None), last=(n_ == len(tiles) - 1))
                flush()
                cx.op("act", "copy", out=OAB[:], in_=OA[:], reads=[OA], writes=[OAB])
                to_OT(OAB, c, 0)
            cx.barrier()

            WINB = mem.view("winb", R_WIN, [128, 8, NB], BF16)
            if seq == 0:
                cx.dma("pool", WINB[:], winB_d.rearrange("(kc p) n -> p kc n", p=128), writes=[WINB])
            else:
                cx.dma("sp", WINB[:], winBbf_d.rearrange("(kc p) n -> p kc n", p=128), writes=[WINB])
            o = R_PH
            QD = mem.view("QD", o, [128, 4, 2048], BF16); o += 16 * KB
            QI = mem.view("QI", o, [128, 4, 2048], BF16); o += 16 * KB
            KI = mem.view("KI", o, [128, 2048], BF16); o += 4 * KB
            CKT = mem.view("CKT", o, [128, 2048], BF16); o += 4 * KB
            KRT = mem.view("KRT", o, [128, 2048], BF16); o += 4 * KB
            WI = mem.view("WI", o, [128, 16, 8], F32); o += 512
            T1 = mem.view("T1", o, [128, 512], F32); o_JB = o; o += 2048
            T2 = mem.view("T2", o, [128, 512], F32); o += 2048
            CKN = []
            for i in range(2):
                CKN.append(mem.view("CKN%d" % i, o, [128, 128], F32)); o += 512
            o_RB = o
            assert o + 4096 + 128 <= TOT, o
            TL[0] = [T1, mem.view("T1b", o_RB, [128, 512], F32)]
            TL[1] = [T2, mem.view("T2b", o_RB + 2048, [128, 512], F32)]
            SSD = Buf(SMALL[:, 56:60], "SSD")
            for tc in range(4):
                sl = slice(512 * tc, 512 * tc + 512)
                for j in range(4):
                    proj_unit(WINB, 256 * j, 256 * j + 128, 128, tc, QD[:, j, sl], QD)
                for j in range(4):
                    proj_unit(WINB, 1024 + 256 * j, 1024 + 256 * j + 128, 128, tc, QI[:, j, sl], QI)
                proj_unit(WINB, 2048, 2176, 128, tc, KI[:, sl], KI)
                proj_unit(WINB, 2304, 2320, 16, tc, KRT[0:16, sl], KRT)
            for i in range(NT):
                pb = PS[4 + i % 2]
                ck = CKN[i % 2]
                for kc in range(8):
                    cx.op("pe", "matmul", pb[:, 0:136], HT[:, kc, 128 * i:128 * i + 128], WINB[:, kc, NB_FM:NB],
                          start=(kc == 0), stop=(kc == 7), reads=[HT, WINB], writes=[pb])
                cx.op("act", "activation", out=ck[:], in_=pb[:, 0:128], func=AF.Square, accum_out=SSD[:, 0:1],
                      reads=[pb], writes=[ck, SSD])
                cx.op("act", "activation", out=SSD[:, 1:2], in_=SSD[:, 0:1], func=AF.Sqrt, scale=1.0 / 128, bias=1e-6,
                      reads=[SSD], writes=[SSD])
                cx.op("dve", "reciprocal", out=SSD[:, 2:3], in_=SSD[:, 1:2], reads=[SSD], writes=[SSD])
                cx.op("dve", "tensor_scalar", out=ck[:], in0=pb[:, 0:128], scalar1=SSD[:, 2:3], scalar2=None, op0=ALU.mult,
                      reads=[pb, SSD], writes=[ck])
                cx.op("act", "mul", out=WI[:, i, :], in_=pb[:, 128:136], mul=float(8 ** -0.5 * 64 ** -0.5), reads=[pb], writes=[WI])
                cx.op("pe", "transpose", PS[6][:, 128 * (i % 4):128 * (i % 4) + 128], ck[:], ident_f, reads=[ck, CFB], writes=[PS[6]])
                if i % 4 == 3:
                    cx.op("act", "activation", out=CKT[:, 512 * (i // 4):512 * (i // 4) + 512], in_=PS[6][:], func=AF.Identity,
                          scale=vec(83, 84), reads=[PS[6], CFB], writes=[CKT])
            cx.barrier()

            o = R_WIN
            KHT = mem.view("KHT", o, [128, 4, 2048], BF16); o += 16 * KB
            VH = mem.view("VH", o, [128, 16, 8, 65], BF16); o += 16640
            WSM = mem.view("WSM", o, [128, 1216], BF16); o += 2432
            PT = []
            for i in range(4):
                PT.append(mem.view("PT%d" % i, o, [128, 512], BF16)); o += 1024
            ODB = mem.view("ODB", o, [128, 4, 512], BF16); o += 4096
            assert o <= R_PH, o
            NMS = [[mem.view("NMA%d" % j, R_HT + 3 * KB * j, [128, 1536], BF16) for j in range(4)],
                   [mem.view("NMB%d" % j, CF_C * 4 + 4 * KB * j, [128, 2048], BF16) for j in range(4)]]
            IB = [mem.view("IB%d" % j, R_HT + 12 * KB + 8 * KB * j, [128, 2048], F32) for j in range(2)]
            RB = [mem.view("RB%d" % j, o_RB + 2048 * j, [128, 512], F32) for j in range(2)]
            BS = []
            for n2, c0 in enumerate((8, 40)):
                BS.append(dict(LO=Buf(SMALL[:, c0:c0 + 1], "LO%d" % n2), MID=Buf(SMALL[:, c0 + 1:c0 + 2], "MID%d" % n2),
                               CNT=Buf(SMALL[:, c0 + 2:c0 + 3], "CNT%d" % n2), TMPS=Buf(SMALL[:, c0 + 3:c0 + 4], "TMPS%d" % n2),
                               W0=Buf(SMALL[:, c0 + 4:c0 + 5], "W0%d" % n2), MN=Buf(SMALL[:, c0 + 5:c0 + 6], "MN%d" % n2),
                               MX8=Buf(SMALL[:, c0 + 6:c0 + 14], "MX8%d" % n2),
                               WK=mem.view("WK%d" % n2, o_RB + 4096 + 64 * n2, [128, 16], F32),
                               JB=Buf(mem.view("JBx%d" % n2, o_JB + 2048 * n2, [128, 1024], BF16).ap.bitcast(mybir.dt.uint8), "JB%d" % n2)))
            cx.dma("pool", WSM[:], wsm_d, writes=[WSM])
            cx.op("pool", "memset", VH[:, :, :, 64:65], 1.0, writes=[VH])
            n_ = 0
            for j in range(4):
                for tc in range(4):
                    pb = PS[4 + n_ % 2]; n_ += 1
                    cx.op("pe", "matmul", pb[:], WSM[:, 128 * j:128 * j + 128], CKT[:, 512 * tc:512 * tc + 512],
                          start=True, stop=False, reads=[WSM, CKT], writes=[pb])
                    cx.op("pe", "matmul", pb[:], CBB[0:16, CB_SEL:CB_SEL + 128], KRT[0:16, 512 * tc:512 * tc + 512],
                          start=False, stop=True, reads=[CBB, KRT], writes=[pb])
                    cx.op("act", "copy", out=KHT[:, j, 512 * tc:512 * tc + 512], in_=pb[:], reads=[pb], writes=[KHT])
            for i in range(NT):
                pb = PS[4 + n_ % 2]; n_ += 1
                cx.op("pe", "matmul", pb[:], CKT[:, 128 * i:128 * i + 128], WSM[:, 512:1024], start=True, stop=True,
                      reads=[CKT, WSM], writes=[pb])
                cx.op("act", "copy", out=VH[:, i, :, 0:64], in_=pb[:].rearrange("p (h d) -> p h d", h=8), reads=[pb], writes=[VH])

            pipe["pend"] = []
            grp.clear()
            def idx_scores(c, j):
                T = 4 * c + j
                Wc = 512 * (c + 1)
                Wv = 128 * (T + 1)
                IBt = IB[T % 2]
                for sc in range(c + 1):
                    N = min(512, Wv - 512 * sc)
                    for h in range(8):
                        b_ = 64 * (h % 2)
                        L = PS[6 + lcnt[0] % 2]; lcnt[0] += 1
                        cx.op("pe", "matmul", L[:, 0:N], QI[b_:b_ + 64, h // 2, 128 * T:128 * T + 128],
                              KI[b_:b_ + 64, 512 * sc:512 * sc + N], start=True, stop=True, reads=[QI, KI], writes=[L])
                        if h == 0:
                            cx.op("dve", "tensor_scalar", out=IBt[:, 512 * sc:512 * sc + N], in0=L[:, 0:N], scalar1=0.0,
                                  scalar2=WI[:, T, h:h + 1], op0=ALU.max, op1=ALU.mult, reads=[L, WI], writes=[IBt])
                        else:
                            rb = RB[h % 2]
                            cx.op("dve", "tensor_scalar", out=rb[:, 0:N], in0=L[:, 0:N], scalar1=0.0,
                                  scalar2=WI[:, T, h:h + 1], op0=ALU.max, op1=ALU.mult, reads=[L, WI], writes=[rb])
                            cx.op("pool", "tensor_tensor", out=IBt[:, 512 * sc:512 * sc + N], in0=IBt[:, 512 * sc:512 * sc + N],
                                  in1=rb[:, 0:N], op=ALU.add, reads=[IBt, rb], writes=[IBt])
                MX8, MN = BS[j % 2]["MX8"], BS[j % 2]["MN"]
                if T >= 2:
                    cx.op("dve", "max", out=MX8[:], in_=IBt[:, 0:Wv], reads=[IBt], writes=[MX8])
                    cx.op("dve", "tensor_reduce", out=MN[:], in_=IBt[:, 0:Wv], axis=AX.X, op=ALU.min, reads=[IBt], writes=[MN])
                cx.op("pool", "affine_select", out=IBt[:, 128 * T:128 * T + 128], in_=IBt[:, 128 * T:128 * T + 128],
                      pattern=[[-1, 128]], compare_op=ALU.is_ge, fill=-3.0e38, base=0, channel_multiplier=1,
                      reads=[IBt], writes=[IBt])
                if Wv < Wc:
                    cx.op("pool", "memset", IBt[:, Wv:Wc], -3.0e38, writes=[IBt])

            def idx_bisect_pair(c, js):
                Wc = 512 * (c + 1)
                chains = []
                for j in js:
                    T = 4 * c + j
                    chains.append((j, T, 128 * (T + 1), IB[T % 2], BS[j % 2]))
                act_ = [ch for ch in chains if ch[1] >= 2]
                for (j, T, Wv, IBt, B) in chains:
                    if T >= 2:
                        cx.op("dve", "tensor_copy", out=B["LO"][:], in_=B["MN"][:], reads=[B["MN"]], writes=[B["LO"]])
                        cx.op("dve", "tensor_tensor", out=B["W0"][:], in0=B["MX8"][:, 0:1], in1=B["MN"][:], op=ALU.subtract,
                              reads=[B["MX8"], B["MN"]], writes=[B["W0"]])
                        cx.op("dve", "tensor_scalar", out=B["WK"][:, 0:BIS_ITERS], in0=CFB[:, CF_P2:CF_P2 + BIS_ITERS],
                              scalar1=B["W0"][:], scalar2=None, op0=ALU.mult, reads=[CFB, B["W0"]], writes=[B["WK"]])
                    else:
                        cx.op("dve", "memset", B["LO"][:], -1.0e30, writes=[B["LO"]])
                for k in range(BIS_ITERS):
                    for (j, T, Wv, IBt, B) in act_:
                        cx.op("dve", "tensor_tensor", out=B["MID"][:], in0=B["LO"][:], in1=B["WK"][:, k:k + 1], op=ALU.add,
                              reads=[B["LO"], B["WK"]], writes=[B["MID"]])
                    for (j, T, Wv, IBt, B) in act_:
                        cx.op("dve", "tensor_scalar", out=B["JB"][:, 0:Wv], in0=IBt[:, 0:Wv], scalar1=B["MID"][:], scalar2=0.0,
                              op0=ALU.is_ge, op1=ALU.add, accum_out=B["CNT"][:], reads=[IBt, B["MID"]], writes=[B["JB"], B["CNT"]])
                    for (j, T, Wv, IBt, B) in act_:
                        cx.op("dve", "tensor_scalar", out=B["TMPS"][:], in0=B["CNT"][:], scalar1=255.5, scalar2=B["WK"][:, k:k + 1],
                              op0=ALU.is_ge, op1=ALU.mult, reads=[B["CNT"], B["WK"]], writes=[B["TMPS"]])
                    for (j, T, Wv, IBt, B) in act_:
                        cx.op("dve", "tensor_tensor", out=B["LO"][:], in0=B["LO"][:], in1=B["TMPS"][:], op=ALU.add,
                              reads=[B["LO"], B["TMPS"]], writes=[B["LO"]])
                for (j, T, Wv, IBt, B) in chains:
                    nm = NMS[c % 2][j]
                    cx.op("dve", "tensor_scalar", out=nm[:, 0:Wc], in0=IBt[:, 0:Wc], scalar1=B["LO"][:], scalar2=NEG,
                          op0=ALU.is_lt, op1=ALU.mult, reads=[IBt, B["LO"]], writes=[nm])

            def idx_slices(c):
                return [lambda: idx_scores(c, 0), lambda: idx_scores(c, 1), lambda: idx_bisect_pair(c, (0, 1)), lambda: None,
                        lambda: idx_scores(c, 2), lambda: idx_scores(c, 3), lambda: idx_bisect_pair(c, (2, 3)), lambda: None]

            lcnt = [0]
            for f_ in idx_slices(0):
                f_()
            for c in range(4):
                qs = slice(512 * c, 512 * c + 512)
                NMc = NMS[c % 2]
                sl_next = idx_slices(c + 1) if c < 3 else []
                for h in range(8):
                    if sl_next:
                        sl_next[h]()
                    b_ = 64 * (h % 2)
                    acc = next_acc()
                    accv = acc[:, 0:260].rearrange("p (j n) -> p j n", j=4)

                    def fin(acc=acc, accv=accv, h=h):
                        cx.op("dve", "reciprocal", out=RINV[:], in_=accv[:, :, 64], reads=[acc], writes=[RINV])
                        cx.op("dve", "tensor_tensor", out=ODB[:, :, 64 * h:64 * h + 64], in0=accv[:, :, 0:64],
                              in1=RINV[:].unsqueeze(2).to_broadcast([128, 4, 64]), op=ALU.mult,
                              reads=[acc, RINV], writes=[ODB])
                    tiles = list(range(0, 4 * c + 4))
                    for q_, i in enumerate(tiles):
                        ex = [(NMc[j][:, 128 * i:128 * i + 128], ident_b, 128 * j, 128 * j + 128, [NMc[j], CBB]) for j in range(4)]
                        unit(KHT[b_:b_ + 64, h // 2, 128 * i:128 * i + 128], QD[b_:b_ + 64, h // 2, qs], [],
                             VH[:, i, h, :], acc, 65, q_ == 0, KHT, QD, VH, after=(fin if q_ == len(tiles) - 1 else None),
                             ex128=ex, last=(q_ == len(tiles) - 1))
                flush()
                to_OT(ODB, c, 4)
            cx.barrier()

            o = R_HT
            X1 = mem.view("X1", o, [128, 4, 1024], F32); o += 16 * KB
            H2T = mem.view("H2T", o, [128, 8, 512], BF16); o += 8 * KB
            ACTT = mem.view("ACTT", o, [128, 22, 512], BF16); o += 22 * KB
            WOUT = mem.view("WOUT", CF_C * 4, [128, 8, 1024], BF16)
            WG = []
            for i in range(3):
                WG.append(mem.view("WG%d" % i, o, [128, 8, 256], BF16)); o += 4 * KB
            WDN = mem.view("WDN", o, [128, 22, 1024], BF16); o += 44 * KB
            XIN = []
            for i in range(2):
                XIN.append(mem.view("xin%d" % i, o, [128, 1024], F32)); o += 4 * KB
            JUNK = mem.view("junk", o, [128, 1024], F32); o += 4 * KB
            XS = mem.view("xs", o, [128, 1024], F32); o += 4 * KB
            TMPY = mem.view("tmpy", o, [128, 1024], F32); o += 4 * KB
            SIL = []
            for i in range(2):
                SIL.append(mem.view("sil%d" % i, o, [128, 512], F32)); o += 2 * KB
            assert o <= TOT, o
            ST = Buf(SMALL[:, 44:52], "ST")
            if not cv_waited[0]:
                for sem_cv in cvsems:
                    nc.sync.wait_ge(sem_cv, 16)
                cv_waited[0] = True
            cx.dma("sp", WDN[:], wdnbf_d.rearrange("(c p) n -> p c n", p=128), writes=[WDN])
            wout_v = woutbf_d.rearrange("(kc p) n -> p kc n", p=128)

            cx.dma("sp", WOUT[:], wout_v, writes=[WOUT])
            for gi in range(4):
                for j in range(4):
                    T = 4 * gi + j
                    xi = XIN[j % 2]
                    cx.dma("sp", xi[:], x_d[seq, 128 * T:128 * T + 128, :], writes=[xi])
                    yb = [PS[2 * (j % 2)], PS[2 * (j % 2) + 1]]
                    for n in range(2):
                        for kc in range(8):
                            cx.op("pe", "matmul", yb[n][:], OT[:, kc, 128 * T:128 * T + 128], WOUT[:, kc, 512 * n:512 * n + 512],
                                  start=(kc == 0), stop=(kc == 7), reads=[OT, WOUT], writes=[yb[n]])
                    for n in range(2):
                        cx.op("act", "activation", out=JUNK[:, 512 * n:512 * n + 512], in_=yb[n][:], func=AF.Square,
                              accum_out=ST[:, n:n + 1], reads=[yb[n]], writes=[JUNK, ST])
                    cx.op("dve", "tensor_tensor", out=ST[:, 2:3], in0=ST[:, 0:1], in1=ST[:, 1:2], op=ALU.add, reads=[ST], writes=[ST])
                    cx.op("act", "activation", out=ST[:, 3:4], in_=ST[:, 2:3], func=AF.Sqrt, scale=1.0 / D, bias=1e-6, reads=[ST], writes=[ST])
                    cx.op("dve", "reciprocal", out=ST[:, 4:5], in_=ST[:, 3:4], reads=[ST], writes=[ST])
                    for n in range(2):
                        cx.op("dve", "scalar_tensor_tensor", out=TMPY[:, 512 * n:512 * n + 512], in0=yb[n][:], scalar=ST[:, 4:5],
                              in1=G1[:, 512 * n:512 * n + 512], op0=ALU.mult, op1=ALU.mult, reads=[yb[n], ST, G1], writes=[TMPY])
                    cx.op("dve", "tensor_tensor", out=X1[:, j, :], in0=TMPY[:], in1=xi[:], op=ALU.add, reads=[TMPY, xi], writes=[X1])
                    cx.op("act", "activation", out=JUNK[:], in_=X1[:, j, :], func=AF.Square, accum_out=ST[:, 0:1],
                          reads=[X1], writes=[JUNK, ST])
                    cx.op("act", "activation", out=ST[:, 3:4], in_=ST[:, 0:1], func=AF.Sqrt, scale=1.0 / D, bias=1e-6, reads=[ST], writes=[ST])
                    cx.op("dve", "reciprocal", out=ST[:, 4:5], in_=ST[:, 3:4], reads=[ST], writes=[ST])
                    cx.op("dve", "tensor_scalar", out=XS[:], in0=X1[:, j, :], scalar1=ST[:, 4:5], scalar2=None, op0=ALU.mult,
                          reads=[X1, ST], writes=[XS])
                    for fc in range(8):
                        pb = PS[4 + fc // 4]
                        cx.op("pe", "transpose", pb[:, 128 * (fc % 4):128 * (fc % 4) + 128], XS[:, 128 * fc:128 * fc + 128],
                              ident_f, reads=[XS, CFB], writes=[pb])
                    for fc in range(8):
                        pb = PS[4 + fc // 4]
                        if fc < 4:
                            cx.op("act", "activation", out=H2T[:, fc, 128 * j:128 * j + 128],
                                  in_=pb[:, 128 * (fc % 4):128 * (fc % 4) + 128], func=AF.Identity,
                                  scale=DERV[:, 2, fc, seq:seq + 1], bias=DERV[:, 3, fc, seq:seq + 1],
                                  reads=[pb, DERV], writes=[H2T])
                        else:
                            cx.op("dve", "tensor_scalar", out=H2T[:, fc, 128 * j:128 * j + 128],
                                  in0=pb[:, 128 * (fc % 4):128 * (fc % 4) + 128],
                                  scalar1=DERV[:, 2, fc, seq:seq + 1], scalar2=DERV[:, 3, fc, seq:seq + 1],
                                  op0=ALU.mult, op1=ALU.add, reads=[pb, DERV], writes=[H2T])
                for ch in range(22):
                    wg = WG[ch % 3]
                    if not cv_waited[0]:
                        for sem_cv in cvsems:
                            nc.sync.wait_ge(sem_cv, 16)
                        cv_waited[0] = True
                    cx.dma("sp", wg[:].rearrange("p a b -> p (a b)"), wgubf_d[ch], writes=[wg])
                    pg, pu = PS[2 * (ch % 2)], PS[2 * (ch % 2) + 1]
                    for kc in range(8):
                        cx.op("pe", "matmul", pg[:], wg[:, kc, 0:128], H2T[:, kc, :], start=(kc == 0), stop=(kc == 7),
                              reads=[wg, H2T], writes=[pg])
                    for kc in range(8):
                        cx.op("pe", "matmul", pu[:], wg[:, kc, 128:256], H2T[:, kc, :], start=(kc == 0), stop=(kc == 7),
                              reads=[wg, H2T], writes=[pu])
                    sl_ = SIL[ch % 2]
                    cx.op("act", "activation", out=sl_[:], in_=pg[:], func=AF.Silu, reads=[pg], writes=[sl_])
                    cx.op("dve", "tensor_tensor", out=ACTT[:, ch, :], in0=sl_[:], in1=pu[:], op=ALU.mult,
                          reads=[sl_, pu], writes=[ACTT])
                for j in range(4):
                    T = 4 * gi + j
                    zb = [PS[4 + 2 * (j % 2)], PS[5 + 2 * (j % 2)]]
                    for n in range(2):
                        for ch in range(22):
                            cx.op("pe", "matmul", zb[n][:], ACTT[:, ch, 128 * j:128 * j + 128], WDN[:, ch, 512 * n:512 * n + 512],
                                  start=(ch == 0), stop=(ch == 21), reads=[ACTT, WDN], writes=[zb[n]])
                    for n in range(2):
                        cx.op("act", "activation", out=JUNK[:, 512 * n:512 * n + 512], in_=zb[n][:], func=AF.Square,
                              accum_out=ST[:, n:n + 1], reads=[zb[n]], writes=[JUNK, ST])
                    cx.op("dve", "tensor_tensor", out=ST[:, 2:3], in0=ST[:, 0:1], in1=ST[:, 1:2], op=ALU.add, reads=[ST], writes=[ST])
                    cx.op("act", "activation", out=ST[:, 3:4], in_=ST[:, 2:3], func=AF.Sqrt, scale=1.0 / D, bias=1e-6, reads=[ST], writes=[ST])
                    cx.op("dve", "reciprocal", out=ST[:, 4:5], in_=ST[:, 3:4], reads=[ST], writes=[ST])
                    for n in range(2):
                        cx.op("dve", "scalar_tensor_tensor", out=TMPY[:, 512 * n:512 * n + 512], in0=zb[n][:], scalar=ST[:, 4:5],
                              in1=G2[:, 512 * n:512 * n + 512], op0=ALU.mult, op1=ALU.mult, reads=[zb[n], ST, G2], writes=[TMPY])
                    cx.op("dve", "tensor_tensor", out=XS[:], in0=TMPY[:], in1=X1[:, j, :], op=ALU.add, reads=[TMPY, X1], writes=[XS])
                    cx.dma("sp", out_d[seq, 128 * T:128 * T + 128, :], XS[:], reads=[XS])
            cx.barrier()

        cx.barrier()
        cx.finish()
    return nc


def _prep(inputs):
    inp = {k: np.asarray(v) for k, v in inputs.items()}
    cf, cb, imt, wsm = _host_consts(inp)
    A, B = _col_index()
    shared = {
        "w_ada": np.ascontiguousarray(inp['w_ada'][0]),
        "cf": cf, "cb": cb, "imt": imt, "wsm": wsm,
        "w_inA": np.ascontiguousarray(inp['w_in'][0][:, A]),
        "w_inB": np.ascontiguousarray(inp['w_in'][0][:, B]),
        "cmp_w1": np.ascontiguousarray(inp['cmp_w1'][0]),
        "b2v": np.ascontiguousarray(inp['cmp_b2'][0, 1]),
        "w_out": np.ascontiguousarray(inp['w_out'][0]),
        "w_gate_up": np.ascontiguousarray(
            inp['w_gate_up'][0].reshape(8, 128, 2, 22, 128).transpose(3, 1, 0, 2, 4).reshape(22, 128, 2048)),
        "w_down": np.ascontiguousarray(inp['w_down'][0]),
    }
    maps = []
    for c in range(8):
        m = dict(shared)
        m["x"] = np.ascontiguousarray(inp['x'][2 * c:2 * c + 2])
        m["cT"] = np.ascontiguousarray(inp['c'][2 * c:2 * c + 2].T.reshape(8, 128, 2).transpose(1, 0, 2))
        maps.append(m)
    return maps


def kernel(**inputs):
    maps = _prep(inputs)
    nc = build()
    res = run_bass_kernel_spmd(nc, maps, core_ids=list(range(8)))
    return np.concatenate([r["out"] for r in res.results], axis=0).astype(np.float32)
```

```python
import numpy as np
import concourse.bass as bass
import concourse.mybir as mybir
from concourse.bass_utils import run_bass_kernel_spmd
from contextlib import ExitStack

F32 = mybir.dt.float32
BF16 = mybir.dt.bfloat16
ALU = mybir.AluOpType
AF = mybir.ActivationFunctionType
AX = mybir.AxisListType

S = 2048
D = 1024
NT = 16
DFF = 2816
NEG = -30000.0
IN_SPLITS = (512, 128, 128, 128, 128, 128, 128, 24, 512, 128, 16, 512, 64, 8)
NAMES = ['nq', 'nkc', 'nvc', 'nks', 'nvs', 'nkw', 'nvw', 'ngate', 'dq', 'dckv', 'dkr', 'iq', 'ik', 'iw']
OFF = dict(zip(NAMES, np.cumsum((0,) + IN_SPLITS)[:-1]))
NA_FM = 19 * 128
NA = NA_FM + 280
NB_FM = 18 * 128 + 32
NB = NB_FM + 136
BIS_ITERS = 12
import os as _os
_DBG = _os.environ.get('KDBG', '')


class _E:
    def __init__(self, name, eng, sem):
        self.name, self.eng, self.sem, self.tick, self.waited = name, eng, sem, 0, {}


class _St:
    __slots__ = ("w", "r")

    def __init__(self):
        self.w = None
        self.r = {}


class Buf:
    def __init__(self, ap, key):
        self.ap = ap
        self.key = key

    def __getitem__(self, idx):
        return self.ap[idx]


class Ctx:
    def __init__(self, nc, es, n_dma_sems=8):
        self.nc, self.es = nc, es
        self.E = {}
        for name, eng in (("pe", nc.tensor), ("act", nc.scalar), ("dve", nc.vector),
                          ("pool", nc.gpsimd), ("sp", nc.sync)):
            self.E[name] = _E(name, eng, es.enter_context(nc.semaphore("s_" + name)))
        self.dsems = {q: [[es.enter_context(nc.semaphore("d_%s%d" % (q, i))), 0] for i in range(n_dma_sems)]
                      for q in ("sp", "pool")}
        self.dnext = {"sp": 0, "pool": 0}
        self.st = {}

    def _state(self, b):
        k = b.key if isinstance(b, Buf) else (b if isinstance(b, str) else b.name)
        s = self.st.get(k)
        if s is None:
            s = self.st[k] = _St()
        return s

    def _wait(self, E, dep):
        kind, tk = dep
        if kind == E.name and E.name == "pe":
            return
        if E.waited.get(kind, 0) >= tk:
            return
        sem = self.dsems[kind[0]][kind[1]][0] if isinstance(kind, tuple) else self.E[kind].sem
        E.eng.wait_ge(sem, tk)
        E.waited[kind] = tk

    def _deps(self, E, reads, writes):
        deps = []
        for b in reads:
            s = self._state(b)
            if s.w is not None:
                deps.append(s.w)
        for b in writes:
            s = self._state(b)
            if s.w is not None:
                deps.append(s.w)
            deps.extend(s.r.items())
        for d in deps:
            self._wait(E, d)

    def _mark(self, token, reads, writes):
        for b in reads:
            s = self._state(b)
            if s.r.get(token[0], 0) < token[1]:
                s.r[token[0]] = token[1]
        for b in writes:
            s = self._state(b)
            s.w = token
            s.r = {}

    def op(self, en, fn, *args, reads=(), writes=(), **kw):
        E = self.E[en]
        self._deps(E, reads, writes)
        ins = getattr(E.eng, fn)(*args, **kw)
        E.tick += 1
        ins.then_inc(E.sem, 1)
        self._mark((en, E.tick), reads, writes)
        return ins

    def dma(self, q, out, in_, reads=(), writes=(), **kw):
        E = self.E[q]
        self._deps(E, reads, writes)
        i = self.dnext[q]
        self.dnext[q] = (i + 1) % len(self.dsems[q])
        slot = self.dsems[q][i]
        kind = (q, i)
        if slot[1] > 0:
            self._wait(E, (kind, slot[1]))
        slot[1] += 16
        E.eng.dma_start(out=out, in_=in_, **kw).then_inc(slot[0], 16)
        self._mark((kind, slot[1]), reads, writes)

    def barrier(self):
        toks = [(n, e.tick) for n, e in self.E.items() if e.tick > 0]
        for q in self.dsems:
            for i, slot in enumerate(self.dsems[q]):
                if slot[1] > 0:
                    toks.append(((q, i), slot[1]))
        for n, e in self.E.items():
            for t in toks:
                if t[0] == n:
                    if n != "sp" and e.waited.get(n, 0) < t[1]:
                        e.eng.wait_ge(e.sem, t[1])
                        e.waited[n] = t[1]
                else:
                    self._wait(e, t)
        self.st = {}

    def finish(self):
        E = self.E["sp"]
        for q in self.dsems:
            for i, slot in enumerate(self.dsems[q]):
                if slot[1] > 0:
                    self._wait(E, ((q, i), slot[1]))


class Mem:
    def __init__(self, big):
        self.big = big

    def view(self, key, off, shape, dt, pbase=0):
        esz = 4 if dt == F32 else 2
        nel = int(np.prod(shape[1:]))
        assert off % 4 == 0
        a = off // 2
        n = nel * esz // 2
        ap = self.big[pbase:pbase + shape[0], a:a + n]
        if dt == F32:
            ap = ap.bitcast(F32)
        if len(shape) == 3:
            ap = ap.rearrange("p (a b) -> p a b", a=shape[1])
        elif len(shape) == 4:
            ap = ap.rearrange("p (a b c) -> p a b c", a=shape[1], b=shape[2])
        return Buf(ap, key)


def _swap_head(cols):
    c = np.array(cols).copy()
    c[0:8] = cols[8:16]
    c[8:16] = cols[0:8]
    return c


def _col_index():
    def head(base, h):
        return np.arange(base + 64 * h, base + 64 * h + 64)
    A = []
    for j in range(4):
        a = np.concatenate([head(OFF['nq'], 2 * j), head(OFF['nq'], 2 * j + 1)])
        b = np.concatenate([_swap_head(head(OFF['nq'], 2 * j)), _swap_head(head(OFF['nq'], 2 * j + 1))])
        A += [a, b]
    a = np.concatenate([head(OFF['nkc'], 0), head(OFF['nkc'], 1)])
    b = np.concatenate([_swap_head(head(OFF['nkc'], 0)), _swap_head(head(OFF['nkc'], 1))])
    A += [a, b]
    A += [np.arange(OFF['nvc'], OFF['nvc'] + 128)]
    for nm in ('nks', 'nkw'):
        for g in range(2):
            a = np.concatenate([head(OFF[nm], g), head(OFF[nm], g)])
            b = np.concatenate([_swap_head(head(OFF[nm], g)), _swap_head(head(OFF[nm], g))])
            A += [a, b]
    A += [np.arange(OFF['nvs'], OFF['nvs'] + 128), np.arange(OFF['nvw'], OFF['nvw'] + 128),
          np.arange(OFF['ngate'], OFF['ngate'] + 24)]
    A = np.concatenate(A)
    assert A.size == NA
    B = []
    for nm in ('dq', 'iq'):
        for j in range(4):
            a = np.concatenate([head(OFF[nm], 2 * j), head(OFF[nm], 2 * j + 1)])
            b = np.concatenate([_swap_head(head(OFF[nm], 2 * j)), _swap_head(head(OFF[nm], 2 * j + 1))])
            B += [a, b]
    a = np.concatenate([head(OFF['ik'], 0), head(OFF['ik'], 0)])
    b = np.concatenate([_swap_head(head(OFF['ik'], 0)), _swap_head(head(OFF['ik'], 0))])
    B += [a, b]
    kr = np.arange(OFF['dkr'], OFF['dkr'] + 16)
    B += [kr, _swap_head(kr)]
    B += [np.arange(OFF['dckv'], OFF['dckv'] + 128), np.arange(OFF['iw'], OFF['iw'] + 8)]
    B = np.concatenate(B)
    assert B.size == NB
    return A, B


CF_ID = 0
CF_C = 128
CF_S = CF_C + 2048
CF_V = CF_S + 2048
CF_P2 = CF_V + 84
CF_N = CF_P2 + BIS_ITERS
CB_ID = 0
CB_EX = 128
CB_OV = CB_EX + 2048
CB_PE = CB_OV + 32
CB_SEL = CB_PE + 64
CB_N = CB_SEL + 128


def _host_consts(inp):
    cf = np.zeros((128, CF_N), np.float32)
    cf[:, CF_ID:CF_ID + 128] = np.eye(128, dtype=np.float32)
    inv_freq = 1.0 / (np.float32(500000.0) ** (np.arange(0, 16, 2, dtype=np.float32) / np.float32(16)))
    ang = np.arange(S, dtype=np.float32)[:, None] * inv_freq[None, :].astype(np.float32)
    cos, sin = np.cos(ang).astype(np.float32), np.sin(ang).astype(np.float32)
    for p in range(128):
        j = p % 64
        if j < 16:
            cf[p, CF_C:CF_C + S] = cos[:, j % 8]
            cf[p, CF_S:CF_S + S] = (-1.0 if j < 8 else 1.0) * sin[:, j % 8]
        else:
            cf[p, CF_C:CF_C + S] = 1.0
    v = cf[:, CF_V:CF_V + 84]
    v[:, 0:48] = inp['b_ada'][0].reshape(48, 128).T
    v[:, 48:56] = inp['g_pre_mix'][0].reshape(8, 128).T
    v[:, 56:64] = inp['g_post_mix'][0].reshape(8, 128).T
    v[:, 64:72] = inp['g_pre_ffn'][0].reshape(8, 128).T
    v[:, 72:80] = inp['g_post_ffn'][0].reshape(8, 128).T
    v[:, 80] = inp['cmp_b1'][0, 0]
    v[:, 81] = inp['cmp_b1'][0, 1]
    v[:, 82] = np.concatenate([inp['cmp_b2'][0, 0], inp['cmp_b2'][0, 0]])
    v[:, 83] = inp['g_kv_norm'][0]
    cf[:, CF_P2:CF_P2 + BIS_ITERS] = (0.5 ** np.arange(1, BIS_ITERS + 1, dtype=np.float64)).astype(np.float32)[None, :]
    cb = np.zeros((128, CB_N), np.float32)
    cb[:, CB_ID:CB_ID + 128] = np.eye(128, dtype=np.float32)
    for j in range(32):
        cb[j, CB_EX + 64 * j:CB_EX + 64 * j + 64] = 1.0
    ci = np.arange(127)[:, None] * 16
    sj = np.arange(32)[None, :] * 64
    cb[0:127, CB_OV:CB_OV + 32] = ((ci < sj + 64) & (ci + 32 > sj)).astype(np.float32)
    cb[0:64, CB_PE:CB_PE + 32] = inp['cmp_pe'][0, 0].T
    cb[0:64, CB_PE + 32:CB_PE + 64] = inp['cmp_pe'][0, 1].T
    for i in range(16):
        cb[i, CB_SEL + i] = 1.0
        cb[i, CB_SEL + 64 + i] = 1.0
    t = (np.arange(16)[None, :, None] * 128 + np.arange(128)[:, None, None])
    blk = t // 64
    j = np.arange(32)[None, None, :]
    visible = j <= blk
    forced = (j == 0) | (j == blk) | (j == blk - 1)
    mm = (visible & ~forced).astype(np.float32)
    ba = np.where(visible, np.where(forced, 1e6, 0.0), -1e30).astype(np.float32)
    imt = np.concatenate([mm.reshape(128, 512), ba.reshape(128, 512)], axis=1)
    wuk = np.zeros((128, 4, 128), np.float32)
    for h in range(8):
        wuk[:, h // 2, (h % 2) * 64 + 16:(h % 2) * 64 + 64] = inp['w_uk'][0, h]
    wuv = np.transpose(inp['w_uv'][0], (1, 0, 2)).reshape(128, 512)
    w2 = np.concatenate([inp['cmp_w2'][0, 0], inp['cmp_w2'][0, 0], inp['cmp_w2'][0, 1]], axis=1)
    wsm = np.concatenate([wuk.reshape(128, 512), wuv, w2], axis=1).astype(np.float32)
    return cf, cb, imt, wsm


def build(n_seq=2, stage=99, dbg_cols=0):
    nc = bass.Bass("TRN2", target_bir_lowering=False)
    dt_ = nc.dram_tensor
    x_d = dt_("x", [2, S, D], F32, kind="ExternalInput").ap()
    cT_d = dt_("cT", [128, 8, 2], F32, kind="ExternalInput").ap()
    wada_d = dt_("w_ada", [D, 6 * D], F32, kind="ExternalInput").ap()
    cf_d = dt_("cf", [128, CF_N], F32, kind="ExternalInput").ap()
    cb_d = dt_("cb", [128, CB_N], F32, kind="ExternalInput").ap()
    imt_d = dt_("imt", [128, 1024], F32, kind="ExternalInput").ap()
    wsm_d = dt_("wsm", [128, 1216], F32, kind="ExternalInput").ap()
    winA_d = dt_("w_inA", [D, NA], F32, kind="ExternalInput").ap()
    winB_d = dt_("w_inB", [D, NB], F32, kind="ExternalInput").ap()
    w1_d = dt_("cmp_w1", [2, 2048, 128], F32, kind="ExternalInput").ap()
    b2v_d = dt_("b2v", [64], F32, kind="ExternalInput").ap()
    wout_d = dt_("w_out", [D, D], F32, kind="ExternalInput").ap()
    wgu_d = dt_("w_gate_up", [22, 128, 2048], F32, kind="ExternalInput").ap()
    wdn_d = dt_("w_down", [DFF, D], F32, kind="ExternalInput").ap()
    out_d = dt_("out", [2, S, D], F32, kind="ExternalOutput").ap()
    wgubf_d = dt_("wgu_bf16", [22, 128, 2048], BF16, kind="Internal").ap()
    wdnbf_d = dt_("wdn_bf16", [DFF, D], BF16, kind="Internal").ap()
    woutbf_d = dt_("wout_bf16", [D, D], BF16, kind="Internal").ap()
    winAbf_d = dt_("winA_bf16", [D, NA], BF16, kind="Internal").ap()
    winBbf_d = dt_("winB_bf16", [D, NB], BF16, kind="Internal").ap()
    dbg_d = dt_("dbg", [128, dbg_cols], F32, kind="ExternalOutput").ap() if dbg_cols else None

    with ExitStack() as es:
        cx = Ctx(nc, es)
        TOT = 206 * 1024
        big = es.enter_context(nc.sbuf_tensor("big", [128, TOT // 2], BF16))
        mem = Mem(big)
        PS = [Buf(es.enter_context(nc.psum_tensor("ps%d" % i, [128, 512], F32))[:], "ps%d" % i) for i in range(8)]

        def psb(i):
            return PS[i].ap.bitcast(BF16)

        KB = 1024
        o = 0
        CFB = mem.view("cf", o, [128, CF_N], F32); o += CF_N * 4
        CBB = mem.view("cb", o, [128, CB_N], BF16); o += CB_N * 2
        MSK = mem.view("msk", o, [128, 8, 512], BF16); o += 8 * 512 * 2
        CMN = mem.view("cmn", o, [128, 2048], BF16); o += 2048 * 2
        ONESF = mem.view("onesf", o, [128, 128], F32); o += 512
        MODC = mem.view("modc", o, [128, 48, 2], F32); o += 384
        DERV = mem.view("derv", o, [128, 6, 8, 2], F32); o += 384
        CACT = mem.view("cact", o, [128, 8, 2], F32); o += 64
        SMALL = mem.view("small", o, [128, 64], F32); o += 256
        G1 = mem.view("G1", o, [128, 1024], F32); o += 4096
        G2 = mem.view("G2", o, [128, 1024], F32); o += 4096
        assert o <= 44 * KB, o
        R_OT = 44 * KB
        R_HT = 76 * KB
        R_WIN = 108 * KB
        R_PH = 152 * KB
        OT = mem.view("OT", R_OT, [128, 8, 2048], BF16)
        HT = mem.view("HT", R_HT, [128, 8, 2048], BF16)

        ident_f = CFB[:, CF_ID:CF_ID + 128]
        ident_b = CBB[:, CB_ID:CB_ID + 128]
        ropeC = CFB[:, CF_C:CF_C + S]
        ropeS = CFB[:, CF_S:CF_S + S]
        vec = lambda c0, c1: CFB[:, CF_V + c0:CF_V + c1]

        def dbg_dump(ap, col0, ncols, rd):
            if dbg_d is not None:
                cx.dma("pool", dbg_d[0:ap.shape[0], col0:col0 + ncols], ap, reads=[rd])

        cx.dma("sp", CFB[:], cf_d, writes=[CFB])
        cx.dma("pool", CBB[:], cb_d, writes=[CBB])
        cx.dma("sp", CACT[:], cT_d, writes=[CACT])
        cx.op("pool", "memset", MSK[:], 0.0, writes=[MSK])
        for k in range(4):
            cx.op("pool", "affine_select", out=MSK[:, k, :], in_=MSK[:, k, :], pattern=[[-1, 512]],
                  compare_op=ALU.is_gt, fill=1.0, base=128 * k, channel_multiplier=1, reads=[MSK], writes=[MSK])
        for k in range(1, 5):
            cx.op("pool", "affine_select", out=MSK[:, 3 + k, :], in_=MSK[:, 3 + k, :], pattern=[[1, 512]],
                  compare_op=ALU.is_ge, fill=1.0, base=-512 + 128 * k, channel_multiplier=-1, reads=[MSK], writes=[MSK])
        cx.op("pool", "memset", CMN[:], 0.0, writes=[CMN])
        cx.op("pool", "affine_select", out=CMN[:], in_=CMN[:], pattern=[[-1, 2048]],
              compare_op=ALU.is_gt, fill=1.0, base=31, channel_multiplier=16, reads=[CMN], writes=[CMN])
        cx.op("pool", "memset", ONESF[:], 1.0, writes=[ONESF])
        cx.op("act", "activation", out=CACT[:], in_=CACT[:], func=AF.Silu, reads=[CACT], writes=[CACT])
        WA = [mem.view("wa0", R_HT, [128, 8, 1024], F32), mem.view("wa1", R_WIN, [128, 8, 1024], F32)]
        wada_v = wada_d.rearrange("(kc p) n -> p kc n", p=128)
        for v in range(6):
            wb = WA[v % 2]
            for kc in range(8):
                cx.dma("sp", wb[:, kc, :], wada_v[:, kc, 1024 * v:1024 * v + 1024], writes=[wb])
            for fc in range(8):
                col = (v * 8 + fc) * 2
                for kc in range(8):
                    cx.op("pe", "matmul", PS[0][:, col:col + 2], wb[:, kc, 128 * fc:128 * fc + 128], CACT[:, kc, :],
                          start=(kc == 0), stop=(kc == 7), reads=[wb, CACT], writes=[PS[0]])
        cx.op("dve", "tensor_tensor", out=MODC[:], in0=PS[0][:, 0:96].rearrange("p (a b) -> p a b", b=2),
              in1=vec(0, 48).unsqueeze(2).to_broadcast([128, 48, 2]), op=ALU.add, reads=[PS[0], CFB], writes=[MODC])
        def gb(c0):
            return vec(c0, c0 + 8).unsqueeze(2).to_broadcast([128, 8, 2])
        cx.op("dve", "scalar_tensor_tensor", out=DERV[:, 0], in0=MODC[:, 8:16, :], scalar=1.0, in1=gb(48),
              op0=ALU.add, op1=ALU.mult, reads=[MODC, CFB], writes=[DERV])
        cx.op("dve", "tensor_copy", out=DERV[:, 1], in_=MODC[:, 0:8, :], reads=[MODC], writes=[DERV])
        cx.op("dve", "scalar_tensor_tensor", out=DERV[:, 2], in0=MODC[:, 32:40, :], scalar=1.0, in1=gb(64),
              op0=ALU.add, op1=ALU.mult, reads=[MODC, CFB], writes=[DERV])
        cx.op("dve", "tensor_copy", out=DERV[:, 3], in_=MODC[:, 24:32, :], reads=[MODC], writes=[DERV])
        cx.op("dve", "tensor_tensor", out=DERV[:, 4], in0=MODC[:, 16:24, :], in1=gb(56), op=ALU.mult,
              reads=[MODC, CFB], writes=[DERV])
        cx.op("dve", "tensor_tensor", out=DERV[:, 5], in0=MODC[:, 40:48, :], in1=gb(72), op=ALU.mult,
              reads=[MODC, CFB], writes=[DERV])
        cx.barrier()

        cvsems = []
        cv_waited = [False]
        for seq in range(n_seq):
            if seq > 0:
                cx.dma("sp", CFB[:, CF_C:CF_C + 2 * S], cf_d[:, CF_C:CF_C + 2 * S], writes=[CFB])
            WIN = mem.view("win", R_WIN, [128, 8, NA], BF16)
            if seq == 0:
                cx.dma("pool", WIN[:], winA_d.rearrange("(kc p) n -> p kc n", p=128), writes=[WIN])
            else:
                cx.dma("sp", WIN[:], winAbf_d.rearrange("(kc p) n -> p kc n", p=128), writes=[WIN])
            DG = mem.view("dg", R_PH, [128, 128], F32)
            for gi, GT_ in ((4, G1), (5, G2)):
                for fc in range(8):
                    cx.op("dve", "tensor_scalar", out=DG[:], in0=ident_f, scalar1=DERV[:, gi, fc, seq:seq + 1],
                          scalar2=None, op0=ALU.mult, reads=[CFB, DERV], writes=[DG])
                    pb = PS[fc // 4]
                    cx.op("pe", "matmul", pb[:, 128 * (fc % 4):128 * (fc % 4) + 128], ONESF[:], DG[:],
                          start=True, stop=True, reads=[ONESF, DG], writes=[pb])
                    if fc % 4 == 3:
                        cx.op("act", "copy", out=GT_[:, 512 * (fc // 4):512 * (fc // 4) + 512], in_=pb[:],
                              reads=[pb], writes=[GT_])
            cx.barrier()
            XIN = [mem.view("xin%d" % i, R_PH + 4 * KB * i, [128, 1024], F32) for i in range(2)]
            XS = [mem.view("xs%d" % i, R_PH + 8 * KB + 4 * KB * i, [128, 1024], F32) for i in range(2)]
            JUNK = mem.view("junk", R_PH + 16 * KB, [128, 1024], F32)
            SS = [mem.view("ss%d" % i, R_PH + 20 * KB + 64 * i, [128, 4], F32) for i in range(2)]
            def p1_stats(i):
                xi, xs, ss = XIN[i % 2], XS[i % 2], SS[i % 2]
                cx.dma("sp", xi[:], x_d[seq, 128 * i:128 * i + 128, :], writes=[xi])
                cx.op("act", "activation", out=JUNK[:], in_=xi[:], func=AF.Square, accum_out=ss[:, 0:1],
                      reads=[xi], writes=[JUNK, ss])
                cx.op("act", "activation", out=ss[:, 1:2], in_=ss[:, 0:1], func=AF.Sqrt, scale=1.0 / D, bias=1e-6,
                      reads=[ss], writes=[ss])
                cx.op("dve", "reciprocal", out=ss[:, 2:3], in_=ss[:, 1:2], reads=[ss], writes=[ss])
                cx.op("dve", "tensor_scalar", out=xs[:], in0=xi[:], scalar1=ss[:, 2:3], scalar2=None, op0=ALU.mult,
                      reads=[xi, ss], writes=[xs])

            p1_stats(0)
            for i in range(NT):
                xs = XS[i % 2]
                for fc in range(8):
                    pb = PS[2 * (i % 2) + fc // 4]
                    cx.op("pe", "transpose", pb[:, 128 * (fc % 4):128 * (fc % 4) + 128],
                          xs[:, 128 * fc:128 * fc + 128], ident_f, reads=[xs, CFB], writes=[pb])
                if i + 1 < NT:
                    p1_stats(i + 1)
                for fc in range(8):
                    pb = PS[2 * (i % 2) + fc // 4]
                    if fc < 4:
                        cx.op("act", "activation", out=HT[:, fc, 128 * i:128 * i + 128],
                              in_=pb[:, 128 * (fc % 4):128 * (fc % 4) + 128], func=AF.Identity,
                              scale=DERV[:, 0, fc, seq:seq + 1], bias=DERV[:, 1, fc, seq:seq + 1],
                              reads=[pb, DERV], writes=[HT])
                    else:
                        cx.op("dve", "tensor_scalar", out=HT[:, fc, 128 * i:128 * i + 128],
                              in0=pb[:, 128 * (fc % 4):128 * (fc % 4) + 128],
                              scalar1=DERV[:, 0, fc, seq:seq + 1], scalar2=DERV[:, 1, fc, seq:seq + 1],
                              op0=ALU.mult, op1=ALU.add, reads=[pb, DERV], writes=["HTd"])
            cx.barrier()

            o = R_PH
            QN = mem.view("QN", o, [128, 4, 2048], BF16); o += 16 * KB
            KS = mem.view("KS", o, [128, 2, 2048], BF16); o += 8 * KB
            KW = mem.view("KW", o, [128, 2, 2048], BF16); o += 8 * KB
            KCR = mem.view("KCR", o, [128, 2048], BF16); o += 4 * KB
            VCR = mem.view("VCR", o, [128, 2048], BF16); o += 4 * KB
            VT = mem.view("VT", o, [128, 16, 4, 65], BF16); o += 8320
            GT = mem.view("GT", o, [128, 16, 24], F32); o += 1536
            o_T1 = o
            T1 = mem.view("T1", o, [128, 512], F32); o += 2048
            T2 = mem.view("T2", o, [128, 512], F32); o += 2048
            assert o <= TOT, o
            TL = [[T1], [T2]]
            ucnt = [0]

            def proj_unit(WINb, ca, cb_, M, tc, dst_ap, dstbuf):
                u = ucnt[0]; ucnt[0] += 1
                pa, pb = PS[2 * (u % 2)], PS[2 * (u % 2) + 1]
                for kc in range(8):
                    cx.op("pe", "matmul", pa[0:M, :], WINb[:, kc, ca:ca + M], HT[:, kc, 512 * tc:512 * tc + 512],
                          start=(kc == 0), stop=(kc == 7), reads=[WINb, HT], writes=[pa])
                if cb_ is None:
                    cx.op("act", "copy", out=dst_ap, in_=pa[0:M, :], reads=[pa], writes=[dstbuf])
                    return
                for kc in range(8):
                    cx.op("pe", "matmul", pb[0:M, :], WINb[:, kc, cb_:cb_ + M], HT[:, kc, 512 * tc:512 * tc + 512],
                          start=(kc == 0), stop=(kc == 7), reads=[WINb, HT], writes=[pb])
                T1, T2 = TL[0][u % len(TL[0])], TL[1][u % len(TL[1])]
                cx.op("dve", "tensor_tensor", out=T1[0:M, :], in0=pa[0:M, :], in1=ropeC[0:M, 512 * tc:512 * tc + 512],
                      op=ALU.mult, reads=[pa, CFB], writes=[T1])
                cx.op("dve", "tensor_tensor", out=T2[0:M, :], in0=pb[0:M, :], in1=ropeS[0:M, 512 * tc:512 * tc + 512],
                      op=ALU.mult, reads=[pb, CFB], writes=[T2])
                cx.op("pool", "tensor_tensor", out=dst_ap, in0=T1[0:M, :], in1=T2[0:M, :], op=ALU.add,
                      reads=[T1, T2], writes=[dstbuf])

            cx.op("pool", "memset", VT[:, :, :, 64:65], 1.0, writes=[VT])
            for tc in range(4):
                sl = slice(512 * tc, 512 * tc + 512)
                for j in range(4):
                    proj_unit(WIN, 256 * j, 256 * j + 128, 128, tc, QN[:, j, sl], QN)
                proj_unit(WIN, 1024, 1152, 128, tc, KCR[:, sl], KCR)
                proj_unit(WIN, 1280, None, 128, tc, VCR[:, sl], VCR)
                for g in range(2):
                    proj_unit(WIN, 1408 + 256 * g, 1536 + 256 * g, 128, tc, KS[:, g, sl], KS)
                    proj_unit(WIN, 1920 + 256 * g, 2048 + 256 * g, 128, tc, KW[:, g, sl], KW)
            for i in range(NT):
                pb = PS[4 + i % 2]
                for kc in range(8):
                    cx.op("pe", "matmul", pb[:, 0:280], HT[:, kc, 128 * i:128 * i + 128], WIN[:, kc, NA_FM:NA],
                          start=(kc == 0), stop=(kc == 7), reads=[HT, WIN], writes=[pb])
                cx.op("act", "copy", out=VT[:, i, :, 0:64], in_=pb[:, 0:256].rearrange("p (a b) -> p a b", a=4),
                      reads=[pb], writes=[VT])
                cx.op("act", "activation", out=GT[:, i, :], in_=pb[:, 256:280], func=AF.Sigmoid, reads=[pb], writes=[GT])
            cx.barrier()

            o = R_WIN
            W1 = mem.view("W1", o, [128, 2, 32, 128], BF16); o += 16 * KB
            WSM = mem.view("WSM", o, [128, 1216], BF16); o += 2432
            IMT = mem.view("IMT", o, [128, 2, 16, 32], F32); o += 4096
            PT = []
            for i in range(4):
                PT.append(mem.view("PT%d" % i, o, [128, 512], BF16)); o += 1024
            XG = mem.view("XG", o, [128, 128], F32); o += 512
            UU = mem.view("UU", o, [128, 128], F32); o += 512
            HIDT = mem.view("HIDT", o, [128, 128], BF16); o += 256
            KCT = mem.view("KCT", o, [128, 2, 128], BF16); o += 512
            VCX = mem.view("VCX", o, [128, 2, 98], BF16); o += 392
            B2V = mem.view("B2V", o, [128, 64], F32); o += 256
            BIAS1 = mem.view("BIAS1", o, [128, 2], F32); o += 8
            OA = mem.view("OA", o, [128, 4, 512], F32); o += 8192
            OAB = mem.view("OAB", o_T1, [128, 4, 512], BF16)
            IMPS = []
            for g in range(2):
                IMPS.append(mem.view("IMP%d" % g, o, [128, 4, 32], F32)); o += 512
            TMPI = mem.view("TMPI", o, [128, 4, 32], F32); o += 512
            IMPM = mem.view("IMPM", o, [128, 4, 32], F32); o += 512
            TOP8 = mem.view("TOP8", o, [128, 4, 8], F32); o += 128
            NSELB = mem.view("NSELB", o, [128, 4, 32], BF16); o += 256
            NSELT = mem.view("NSELT", o, [128, 2, 512], BF16); o += 2048
            TMPO = mem.view("TMPO", o, [128, 4, 64], F32); o += 1024
            assert o <= R_PH, o
            RINV = Buf(SMALL[:, 0:4], "RINV")
            COEF = Buf(SMALL[:, 4:8], "COEF")

            for kv in range(2):
                src = w1_d[kv].rearrange("(l d) j -> d l j", d=64)
                cx.dma("pool", W1[0:64, kv], src, writes=[W1])
                cx.dma("pool", W1[64:128, kv], src, writes=[W1])
            cx.dma("pool", WSM[:], wsm_d, writes=[WSM])
            cx.dma("sp", IMT[:].rearrange("p a b c -> p (a b c)"), imt_d, writes=[IMT])
            cx.dma("sp", B2V[:], b2v_d.partition_broadcast(128), writes=[B2V])
            if seq == 0:
                for i in range(11):
                    sem_cv = es.enter_context(nc.semaphore("cv%d" % i))
                    cvsems.append(sem_cv)
                    nc.gpsimd.dma_start(out=wgubf_d[2 * i:2 * i + 2].rearrange("c p n -> (c p) n"),
                                        in_=wgu_d[2 * i:2 * i + 2].rearrange("c p n -> (c p) n")).then_inc(sem_cv, 16)
                cv_list = [(wdnbf_d[704 * i:704 * i + 704, :], wdn_d[704 * i:704 * i + 704, :]) for i in range(4)]
                cv_list += [(woutbf_d, wout_d)]
                if n_seq > 1:
                    cv_list += [(winAbf_d, winA_d), (winBbf_d, winB_d)]
                for i, (dst_, src_) in enumerate(cv_list):
                    sem_cv = es.enter_context(nc.semaphore("cw%d" % i))
                    cvsems.append(sem_cv)
                    nc.gpsimd.dma_start(out=dst_, in_=src_).then_inc(sem_cv, 16)
            cx.op("pool", "memset", HIDT[:], 0.0, writes=[HIDT])
            cx.op("pool", "memset", NSELT[:], 0.0, writes=[NSELT])
            cx.op("pool", "memset", VCX[:, :, 64:65], 1.0, writes=[VCX])
            for g in range(2):
                cx.op("pool", "tensor_copy", out=VCX[:, g, 65:97], in_=CBB[:, CB_OV:CB_OV + 32], reads=[CBB], writes=[VCX])
            for kv in range(2):
                for l in range(32):
                    cx.op("pe", "matmul", PS[6][:, kv:kv + 1], W1[0:64, kv, l, :], CBB[0:64, CB_PE + 32 * kv + l:CB_PE + 32 * kv + l + 1],
                          start=(l == 0), stop=(l == 31), reads=[W1, CBB], writes=[PS[6]])
            cx.op("dve", "tensor_tensor", out=BIAS1[:], in0=PS[6][:, 0:2], in1=vec(80, 82), op=ALU.add,
                  reads=[PS[6], CFB], writes=[BIAS1])
            for kv in range(2):
                for g in range(2):
                    srcb = KCR if kv == 0 else VCR
                    hps = PS[4 + g]
                    for l in range(32):
                        cx.op("pe", "matmul", hps[:, 0:127], W1[64 * g:64 * g + 64, kv, l, :],
                              srcb[64 * g:64 * g + 64, l:l + 2017:16], start=(l == 0), stop=(l == 31),
                              reads=[W1, srcb], writes=[hps])
                    cx.op("act", "activation", out=XG[:, 0:127], in_=hps[:, 0:127], func=AF.Identity,
                          bias=BIAS1[:, kv:kv + 1], reads=[hps, BIAS1], writes=[XG])
                    cx.op("dve", "tensor_tensor", out=UU[:, 0:127], in0=XG[:, 0:127], in1=XG[:, 0:127], op=ALU.mult,
                          reads=[XG], writes=[UU])
                    cx.op("dve", "tensor_scalar", out=UU[:, 0:127], in0=UU[:, 0:127], scalar1=0.044715, scalar2=1.0,
                          op0=ALU.mult, op1=ALU.add, reads=[UU], writes=[UU])
                    cx.op("dve", "tensor_tensor", out=UU[:, 0:127], in0=UU[:, 0:127], in1=XG[:, 0:127], op=ALU.mult,
                          reads=[UU, XG], writes=[UU])
                    cx.op("act", "activation", out=UU[:, 0:127], in_=UU[:, 0:127], func=AF.Sigmoid, scale=1.5957691216057308,
                          reads=[UU], writes=[UU])
                    cx.op("dve", "tensor_tensor", out=HIDT[:, 0:127], in0=XG[:, 0:127], in1=UU[:, 0:127], op=ALU.mult,
                          reads=[XG, UU], writes=[HIDT])
                    if kv == 0:
                        cx.op("pe", "matmul", PS[6][:, 0:128], WSM[:, 1024:1152], HIDT[:], start=True, stop=True,
                              reads=[WSM, HIDT], writes=[PS[6]])
                        cx.op("act", "activation", out=KCT[:, g, :], in_=PS[6][:, 0:128], func=AF.Identity,
                              bias=vec(82, 83), reads=[PS[6], CFB], writes=[KCT])
                    else:
                        cx.op("pe", "matmul", PS[6][:, 0:64], HIDT[:], WSM[:, 1152:1216], start=True, stop=True,
                              reads=[WSM, HIDT], writes=[PS[6]])
                        cx.op("dve", "tensor_tensor", out=VCX[:, g, 0:64], in0=PS[6][:, 0:64], in1=B2V[:], op=ALU.add,
                              reads=[PS[6], B2V], writes=[VCX])

            pipe = {"pend": [], "u": 0, "job": 0}
            SCB = [PS[0], PS[1], PS[4], PS[5]]
            grp = []

            def unit(kT, qT, extras, V, acc, ncols, first, rk, rq, rv, after=None, mmask=None, ex128=(), last=False):
                u = pipe["u"]; pipe["u"] += 1
                grp.append(dict(u=u, kT=kT, qT=qT, ex=extras, ex128=ex128, V=V, acc=acc, ncols=ncols, first=first,
                                rk=rk, rq=rq, rv=rv, after=after, mmask=mmask, last=last))
                if len(grp) == (1 if 'G1' in _DBG else 2):
                    emit_group()

            def emit_group():
                if not grp:
                    return
                for d in grp:
                    sbk = SCB[d["u"] % 4]
                    nex = len(d["ex"]) + len(d["ex128"])
                    cx.op("pe", "matmul", sbk[:], d["kT"], d["qT"], start=True, stop=(nex == 0),
                          reads=[d["rk"], d["rq"]], writes=[sbk])
                for d in grp:
                    sbk = SCB[d["u"] % 4]
                    nex = len(d["ex"]) + len(d["ex128"])
                    for n_, (l_, r_, c0, c1, rd) in enumerate(d["ex"]):
                        cx.op("pe", "matmul", sbk[:, c0:c1], l_, r_, start=False, stop=(n_ == nex - 1), reads=rd, writes=[sbk])
                for d in grp:
                    sbk = SCB[d["u"] % 4]
                    nex = len(d["ex"]) + len(d["ex128"])
                    for n_, (l_, r_, c0, c1, rd) in enumerate(d["ex128"]):
                        cx.op("pe", "matmul", sbk[:, c0:c1], l_, r_, start=False, stop=(len(d["ex"]) + n_ == nex - 1),
                              reads=rd, writes=[sbk])
                for pvf in pipe["pend"]:
                    pvf()
                pipe["pend"] = []
                for d in grp:
                    sbk = SCB[d["u"] % 4]
                    pt = PT[d["u"] % 4]
                    cx.op("act", "activation", out=pt[:], in_=sbk[:], func=AF.Exp, scale=0.125, reads=[sbk], writes=[pt])
                    if d["mmask"] is not None and 'NM' not in _DBG:
                        cx.op("dve", "tensor_tensor", out=pt[:], in0=pt[:], in1=d["mmask"][0], op=ALU.mult,
                              reads=[pt, d["mmask"][1]], writes=[pt])

                    def pv(d=d, pt=pt):
                        for j in range(4):
                            cx.op("pe", "matmul", d["acc"][:, d["ncols"] * j:d["ncols"] * j + d["ncols"]],
                                  pt[:, 128 * j:128 * j + 128], d["V"], start=(d["first"] and j == 0),
                                  stop=(True if 'ST' in _DBG else (d["last"] and j == 3)), reads=[pt, d["rv"]], writes=[d["acc"]])
                        if d["after"] is not None:
                            d["after"]()
                    pipe["pend"].append(pv)
                grp.clear()

            def flush():
                emit_group()
                for pvf in pipe["pend"]:
                    pvf()
                pipe["pend"] = []

            def next_acc():
                a = PS[2 + pipe["job"] % 2]
                pipe["job"] += 1
                return a

            def nsa_final(acc, ncols, c, h, br, first_branch):
                accv = acc[:, 0:4 * ncols].rearrange("p (j n) -> p j n", j=4)

                def f():
                    cx.op("dve", "tensor_scalar", out=RINV[:], in0=accv[:, :, 64], scalar1=1e-30, scalar2=None,
                          op0=ALU.max, reads=[acc], writes=[RINV])
                    cx.op("dve", "reciprocal", out=RINV[:], in_=RINV[:], reads=[RINV], writes=[RINV])
                    if br == 0:
                        fi = (h % 4 == 0)
                        IMP = IMPS[h // 4]
                        dst = IMP if fi else TMPI
                        cx.op("dve", "tensor_tensor", out=dst[:], in0=accv[:, :, 65:97],
                              in1=RINV[:].unsqueeze(2).to_broadcast([128, 4, 32]), op=ALU.mult,
                              reads=[acc, RINV], writes=[dst])
                        if not fi:
                            cx.op("pool", "tensor_tensor", out=IMP[:], in0=IMP[:], in1=TMPI[:], op=ALU.add,
                                  reads=[IMP, TMPI], writes=[IMP])
                    cx.op("dve", "tensor_tensor", out=COEF[:], in0=RINV[:], in1=GT[:, 4 * c:4 * c + 4, 8 * br + h],
                          op=ALU.mult, reads=[RINV, GT], writes=[COEF])
                    cb3 = COEF[:].unsqueeze(2).to_broadcast([128, 4, 64])
                    if first_branch:
                        cx.op("dve", "tensor_tensor", out=OA[:, :, 64 * h:64 * h + 64], in0=accv[:, :, 0:64], in1=cb3,
                              op=ALU.mult, reads=[acc, COEF], writes=[OA])
                    else:
                        cx.op("dve", "tensor_tensor", out=TMPO[:], in0=accv[:, :, 0:64], in1=cb3, op=ALU.mult,
                              reads=[acc, COEF], writes=[TMPO])
                        cx.op("pool", "tensor_tensor", out=OA[:, :, 64 * h:64 * h + 64], in0=OA[:, :, 64 * h:64 * h + 64],
                              in1=TMPO[:], op=ALU.add, reads=[OA, TMPO], writes=[OA])
                return f

            def to_OT(SRC, c, fc0):
                for fc in range(4):
                    for j in range(4):
                        col = ((fc % 2) * 4 + j) * 128
                        cx.op("pe", "transpose", psb(6 + fc // 2)[:, col:col + 128], SRC[:, j, 128 * fc:128 * fc + 128],
                              ident_b, reads=[SRC, CBB], writes=[PS[6 + fc // 2]])
                for fc in range(4):
                    cx.op("act", "copy", out=OT[:, fc0 + fc, 512 * c:512 * c + 512],
                          in_=psb(6 + fc // 2)[:, (fc % 2) * 512:(fc % 2) * 512 + 512], reads=[PS[6 + fc // 2]], writes=[OT])

            for c in range(4):
                qs = slice(512 * c, 512 * c + 512)
                for h in range(8):
                    g, b_ = h // 4, 64 * (h % 2)
                    acc = next_acc()
                    unit(KCT[b_:b_ + 64, g, :], QN[b_:b_ + 64, h // 2, qs],
                         [], VCX[:, g, 0:97], acc, 97, True,
                         KCT, QN, VCX, after=nsa_final(acc, 97, c, h, 0, True), mmask=(CMN[:, qs], CMN), last=True)
                for h in range(8):
                    g, b_ = h // 4, 64 * (h % 2)
                    acc = next_acc()
                    tiles = list(range(max(0, 4 * c - 4), 4 * c + 4))
                    for n_, i in enumerate(tiles):
                        mk = MSK[:, 3 + (4 * c - i), :] if i < 4 * c else MSK[:, i - 4 * c, :]
                        unit(KW[b_:b_ + 64, g, 128 * i:128 * i + 128], QN[b_:b_ + 64, h // 2, qs],
                             [], VT[:, i, 2 + g, :], acc, 65, n_ == 0, KW, QN, VT,
                             after=(nsa_final(acc, 65, c, h, 2, False) if n_ == len(tiles) - 1 else None), mmask=(mk, MSK),
                             last=(n_ == len(tiles) - 1))
                flush()
                for g in range(2):
                    IMPg = IMPS[g]
                    cx.op("dve", "tensor_tensor", out=IMPM[:], in0=IMPg[:], in1=IMT[:, 0, 4 * c:4 * c + 4, :], op=ALU.mult,
                          reads=[IMPg, IMT], writes=[IMPM])
                    cx.op("dve", "tensor_tensor", out=IMPM[:], in0=IMPM[:], in1=IMT[:, 1, 4 * c:4 * c + 4, :], op=ALU.add,
                          reads=[IMPM, IMT], writes=[IMPM])
                    for j in range(4):
                        cx.op("dve", "max", out=TOP8[:, j, :], in_=IMPM[:, j, :], reads=[IMPM], writes=[TOP8])
                    for j in range(4):
                        cx.op("dve", "tensor_scalar", out=NSELB[:, j, :], in0=IMPM[:, j, :], scalar1=TOP8[:, j, 7:8],
                              scalar2=NEG, op0=ALU.is_lt, op1=ALU.mult, reads=[IMPM, TOP8], writes=[NSELB])
                    for j in range(4):
                        cx.op("pe", "transpose", psb(6)[0:32, 128 * j:128 * j + 128], NSELB[:, j, :], ident_b,
                              reads=[NSELB, CBB], writes=[PS[6]])
                    cx.op("act", "copy", out=NSELT[0:32, g, :], in_=psb(6)[0:32, 0:512], reads=[PS[6]], writes=[NSELT])
                for h in range(8):
                    g, b_ = h // 4, 64 * (h % 2)
                    acc = next_acc()
                    tiles = list(range(0, 4 * c + 4))
                    for n_, i in enumerate(tiles):
                        KX = 32
                        ex = [(CBB[0:KX, CB_EX + 128 * i:CB_EX + 128 * i + 128], NSELT[0:KX, g, :], 0, 512, [CBB, NSELT])]
                        unit(KS[b_:b_ + 64, g, 128 * i:128 * i + 128], QN[b_:b_ + 64, h // 2, qs], ex,
                             VT[:, i, g, :], acc, 65, n_ == 0, KS, QN, VT,
                             after=(nsa_final(acc, 65, c, h, 1, False) if n_ == len(tiles) - 1 else None),
                             mmask=((MSK[:, i - 4 * c, :], MSK) if i >= 4 * c else None), last=(n_ == len(tiles) - 1))
                flush()
                cx.op("act", "copy", out=OAB[:], in_=OA[:], reads=[OA], writes=[OAB])
                to_OT(OAB, c, 0)
            cx.barrier()

            WINB = mem.view("winb", R_WIN, [128, 8, NB], BF16)
            if seq == 0:
                cx.dma("pool", WINB[:], winB_d.rearrange("(kc p) n -> p kc n", p=128), writes=[WINB])
            else:
                cx.dma("sp", WINB[:], winBbf_d.rearrange("(kc p) n -> p kc n", p=128), writes=[WINB])
            o = R_PH
            QD = mem.view("QD", o, [128, 4, 2048], BF16); o += 16 * KB
            QI = mem.view("QI", o, [128, 4, 2048], BF16); o += 16 * KB
            KI = mem.view("KI", o, [128, 2048], BF16); o += 4 * KB
            CKT = mem.view("CKT", o, [128, 2048], BF16); o += 4 * KB
            KRT = mem.view("KRT", o, [128, 2048], BF16); o += 4 * KB
            WI = mem.view("WI", o, [128, 16, 8], F32); o += 512
            T1 = mem.view("T1", o, [128, 512], F32); o_JB = o; o += 2048
            T2 = mem.view("T2", o, [128, 512], F32); o += 2048
            CKN = []
            for i in range(2):
                CKN.append(mem.view("CKN%d" % i, o, [128, 128], F32)); o += 512
            o_RB = o
            assert o + 4096 + 128 <= TOT, o
            TL[0] = [T1, mem.view("T1b", o_RB, [128, 512], F32)]
            TL[1] = [T2, mem.view("T2b", o_RB + 2048, [128, 512], F32)]
            SSD = Buf(SMALL[:, 56:60], "SSD")
            for tc in range(4):
                sl = slice(512 * tc, 512 * tc + 512)
                for j in range(4):
                    proj_unit(WINB, 256 * j, 256 * j + 128, 128, tc, QD[:, j, sl], QD)
                for j in range(4):
                    proj_unit(WINB, 1024 + 256 * j, 1024 + 256 * j + 128, 128, tc, QI[:, j, sl], QI)
                proj_unit(WINB, 2048, 2176, 128, tc, KI[:, sl], KI)
                proj_unit(WINB, 2304, 2320, 16, tc, KRT[0:16, sl], KRT)
            for i in range(NT):
                pb = PS[4 + i % 2]
                ck = CKN[i % 2]
                for kc in range(8):
                    cx.op("pe", "matmul", pb[:, 0:136], HT[:, kc, 128 * i:128 * i + 128], WINB[:, kc, NB_FM:NB],
                          start=(kc == 0), stop=(kc == 7), reads=[HT, WINB], writes=[pb])
                cx.op("act", "activation", out=ck[:], in_=pb[:, 0:128], func=AF.Square, accum_out=SSD[:, 0:1],
                      reads=[pb], writes=[ck, SSD])
                cx.op("act", "activation", out=SSD[:, 1:2], in_=SSD[:, 0:1], func=AF.Sqrt, scale=1.0 / 128, bias=1e-6,
                      reads=[SSD], writes=[SSD])
                cx.op("dve", "reciprocal", out=SSD[:, 2:3], in_=SSD[:, 1:2], reads=[SSD], writes=[SSD])
                cx.op("dve", "tensor_scalar", out=ck[:], in0=pb[:, 0:128], scalar1=SSD[:, 2:3], scalar2=None, op0=ALU.mult,
                      reads=[pb, SSD], writes=[ck])
                cx.op("act", "mul", out=WI[:, i, :], in_=pb[:, 128:136], mul=float(8 ** -0.5 * 64 ** -0.5), reads=[pb], writes=[WI])
                cx.op("pe", "transpose", PS[6][:, 128 * (i % 4):128 * (i % 4) + 128], ck[:], ident_f, reads=[ck, CFB], writes=[PS[6]])
                if i % 4 == 3:
                    cx.op("act", "activation", out=CKT[:, 512 * (i // 4):512 * (i // 4) + 512], in_=PS[6][:], func=AF.Identity,
                          scale=vec(83, 84), reads=[PS[6], CFB], writes=[CKT])
            cx.barrier()

            o = R_WIN
            KHT = mem.view("KHT", o, [128, 4, 2048], BF16); o += 16 * KB
            VH = mem.view("VH", o, [128, 16, 8, 65], BF16); o += 16640
            WSM = mem.view("WSM", o, [128, 1216], BF16); o += 2432
            PT = []
            for i in range(4):
                PT.append(mem.view("PT%d" % i, o, [128, 512], BF16)); o += 1024
            ODB = mem.view("ODB", o, [128, 4, 512], BF16); o += 4096
            assert o <= R_PH, o
            NMS = [[mem.view("NMA%d" % j, R_HT + 3 * KB * j, [128, 1536], BF16) for j in range(4)],
                   [mem.view("NMB%d" % j, CF_C * 4 + 4 * KB * j, [128, 2048], BF16) for j in range(4)]]
            IB = [mem.view("IB%d" % j, R_HT + 12 * KB + 8 * KB * j, [128, 2048], F32) for j in range(2)]
            RB = [mem.view("RB%d" % j, o_RB + 2048 * j, [128, 512], F32) for j in range(2)]
            BS = []
            for n2, c0 in enumerate((8, 40)):
                BS.append(dict(LO=Buf(SMALL[:, c0:c0 + 1], "LO%d" % n2), MID=Buf(SMALL[:, c0 + 1:c0 + 2], "MID%d" % n2),
                               CNT=Buf(SMALL[:, c0 + 2:c0 + 3], "CNT%d" % n2), TMPS=Buf(SMALL[:, c0 + 3:c0 + 4], "TMPS%d" % n2),
                               W0=Buf(SMALL[:, c0 + 4:c0 + 5], "W0%d" % n2), MN=Buf(SMALL[:, c0 + 5:c0 + 6], "MN%d" % n2),
                               MX8=Buf(SMALL[:, c0 + 6:c0 + 14], "MX8%d" % n2),
                               WK=mem.view("WK%d" % n2, o_RB + 4096 + 64 * n2, [128, 16], F32),
                               JB=Buf(mem.view("JBx%d" % n2, o_JB + 2048 * n2, [128, 1024], BF16).ap.bitcast(mybir.dt.uint8), "JB%d" % n2)))
            cx.dma("pool", WSM[:], wsm_d, writes=[WSM])
            cx.op("pool", "memset", VH[:, :, :, 64:65], 1.0, writes=[VH])
            n_ = 0
            for j in range(4):
                for tc in range(4):
                    pb = PS[4 + n_ % 2]; n_ += 1
                    cx.op("pe", "matmul", pb[:], WSM[:, 128 * j:128 * j + 128], CKT[:, 512 * tc:512 * tc + 512],
                          start=True, stop=False, reads=[WSM, CKT], writes=[pb])
                    cx.op("pe", "matmul", pb[:], CBB[0:16, CB_SEL:CB_SEL + 128], KRT[0:16, 512 * tc:512 * tc + 512],
                          start=False, stop=True, reads=[CBB, KRT], writes=[pb])
                    cx.op("act", "copy", out=KHT[:, j, 512 * tc:512 * tc + 512], in_=pb[:], reads=[pb], writes=[KHT])
            for i in range(NT):
                pb = PS[4 + n_ % 2]; n_ += 1
                cx.op("pe", "matmul", pb[:], CKT[:, 128 * i:128 * i + 128], WSM[:, 512:1024], start=True, stop=True,
                      reads=[CKT, WSM], writes=[pb])
                cx.op("act", "copy", out=VH[:, i, :, 0:64], in_=pb[:].rearrange("p (h d) -> p h d", h=8), reads=[pb], writes=[VH])

            pipe["pend"] = []
            grp.clear()
            def idx_scores(c, j):
                T = 4 * c + j
                Wc = 512 * (c + 1)
                Wv = 128 * (T + 1)
                IBt = IB[T % 2]
                for sc in range(c + 1):
                    N = min(512, Wv - 512 * sc)
                    for h in range(8):
                        b_ = 64 * (h % 2)
                        L = PS[6 + lcnt[0] % 2]; lcnt[0] += 1
                        cx.op("pe", "matmul", L[:, 0:N], QI[b_:b_ + 64, h // 2, 128 * T:128 * T + 128],
                              KI[b_:b_ + 64, 512 * sc:512 * sc + N], start=True, stop=True, reads=[QI, KI], writes=[L])
                        if h == 0:
                            cx.op("dve", "tensor_scalar", out=IBt[:, 512 * sc:512 * sc + N], in0=L[:, 0:N], scalar1=0.0,
                                  scalar2=WI[:, T, h:h + 1], op0=ALU.max, op1=ALU.mult, reads=[L, WI], writes=[IBt])
                        else:
                            rb = RB[h % 2]
                            cx.op("dve", "tensor_scalar", out=rb[:, 0:N], in0=L[:, 0:N], scalar1=0.0,
                                  scalar2=WI[:, T, h:h + 1], op0=ALU.max, op1=ALU.mult, reads=[L, WI], writes=[rb])
                            cx.op("pool", "tensor_tensor", out=IBt[:, 512 * sc:512 * sc + N], in0=IBt[:, 512 * sc:512 * sc + N],
                                  in1=rb[:, 0:N], op=ALU.add, reads=[IBt, rb], writes=[IBt])
                MX8, MN = BS[j % 2]["MX8"], BS[j % 2]["MN"]
                if T >= 2:
                    cx.op("dve", "max", out=MX8[:], in_=IBt[:, 0:Wv], reads=[IBt], writes=[MX8])
                    cx.op("dve", "tensor_reduce", out=MN[:], in_=IBt[:, 0:Wv], axis=AX.X, op=ALU.min, reads=[IBt], writes=[MN])
                cx.op("pool", "affine_select", out=IBt[:, 128 * T:128 * T + 128], in_=IBt[:, 128 * T:128 * T + 128],
                      pattern=[[-1, 128]], compare_op=ALU.is_ge, fill=-3.0e38, base=0, channel_multiplier=1,
                      reads=[IBt], writes=[IBt])
                if Wv < Wc:
                    cx.op("pool", "memset", IBt[:, Wv:Wc], -3.0e38, writes=[IBt])

            def idx_bisect_pair(c, js):
                Wc = 512 * (c + 1)
                chains = []
                for j in js:
                    T = 4 * c + j
                    chains.append((j, T, 128 * (T + 1), IB[T % 2], BS[j % 2]))
                act_ = [ch for ch in chains if ch[1] >= 2]
                for (j, T, Wv, IBt, B) in chains:
                    if T >= 2:
                        cx.op("dve", "tensor_copy", out=B["LO"][:], in_=B["MN"][:], reads=[B["MN"]], writes=[B["LO"]])
                        cx.op("dve", "tensor_tensor", out=B["W0"][:], in0=B["MX8"][:, 0:1], in1=B["MN"][:], op=ALU.subtract,
                              reads=[B["MX8"], B["MN"]], writes=[B["W0"]])
                        cx.op("dve", "tensor_scalar", out=B["WK"][:, 0:BIS_ITERS], in0=CFB[:, CF_P2:CF_P2 + BIS_ITERS],
                              scalar1=B["W0"][:], scalar2=None, op0=ALU.mult, reads=[CFB, B["W0"]], writes=[B["WK"]])
                    else:
                        cx.op("dve", "memset", B["LO"][:], -1.0e30, writes=[B["LO"]])
                for k in range(BIS_ITERS):
                    for (j, T, Wv, IBt, B) in act_:
                        cx.op("dve", "tensor_tensor", out=B["MID"][:], in0=B["LO"][:], in1=B["WK"][:, k:k + 1], op=ALU.add,
                              reads=[B["LO"], B["WK"]], writes=[B["MID"]])
                    for (j, T, Wv, IBt, B) in act_:
                        cx.op("dve", "tensor_scalar", out=B["JB"][:, 0:Wv], in0=IBt[:, 0:Wv], scalar1=B["MID"][:], scalar2=0.0,
                              op0=ALU.is_ge, op1=ALU.add, accum_out=B["CNT"][:], reads=[IBt, B["MID"]], writes=[B["JB"], B["CNT"]])
                    for (j, T, Wv, IBt, B) in act_:
                        cx.op("dve", "tensor_scalar", out=B["TMPS"][:], in0=B["CNT"][:], scalar1=255.5, scalar2=B["WK"][:, k:k + 1],
                              op0=ALU.is_ge, op1=ALU.mult, reads=[B["CNT"], B["WK"]], writes=[B["TMPS"]])
                    for (j, T, Wv, IBt, B) in act_:
                        cx.op("dve", "tensor_tensor", out=B["LO"][:], in0=B["LO"][:], in1=B["TMPS"][:], op=ALU.add,
                              reads=[B["LO"], B["TMPS"]], writes=[B["LO"]])
                for (j, T, Wv, IBt, B) in chains:
                    nm = NMS[c % 2][j]
                    cx.op("dve", "tensor_scalar", out=nm[:, 0:Wc], in0=IBt[:, 0:Wc], scalar1=B["LO"][:], scalar2=NEG,
                          op0=ALU.is_lt, op1=ALU.mult, reads=[IBt, B["LO"]], writes=[nm])

            def idx_slices(c):
                return [lambda: idx_scores(c, 0), lambda: idx_scores(c, 1), lambda: idx_bisect_pair(c, (0, 1)), lambda: None,
                        lambda: idx_scores(c, 2), lambda: idx_scores(c, 3), lambda: idx_bisect_pair(c, (2, 3)), lambda: None]

            lcnt = [0]
            for f_ in idx_slices(0):
                f_()
            for c in range(4):
                qs = slice(512 * c, 512 * c + 512)
                NMc = NMS[c % 2]
                sl_next = idx_slices(c + 1) if c < 3 else []
                for h in range(8):
                    if sl_next:
                        sl_next[h]()
                    b_ = 64 * (h % 2)
                    acc = next_acc()
                    accv = acc[:, 0:260].rearrange("p (j n) -> p j n", j=4)

                    def fin(acc=acc, accv=accv, h=h):
                        cx.op("dve", "reciprocal", out=RINV[:], in_=accv[:, :, 64], reads=[acc], writes=[RINV])
                        cx.op("dve", "tensor_tensor", out=ODB[:, :, 64 * h:64 * h + 64], in0=accv[:, :, 0:64],
                              in1=RINV[:].unsqueeze(2).to_broadcast([128, 4, 64]), op=ALU.mult,
                              reads=[acc, RINV], writes=[ODB])
                    tiles = list(range(0, 4 * c + 4))
                    for q_, i in enumerate(tiles):
                        ex = [(NMc[j][:, 128 * i:128 * i + 128], ident_b, 128 * j, 128 * j + 128, [NMc[j], CBB]) for j in range(4)]
                        unit(KHT[b_:b_ + 64, h // 2, 128 * i:128 * i + 128], QD[b_:b_ + 64, h // 2, qs], [],
                             VH[:, i, h, :], acc, 65, q_ == 0, KHT, QD, VH, after=(fin if q_ == len(tiles) - 1 else None),
                             ex128=ex, last=(q_ == len(tiles) - 1))
                flush()
                to_OT(ODB, c, 4)
            cx.barrier()

            o = R_HT
            X1 = mem.view("X1", o, [128, 4, 1024], F32); o += 16 * KB
            H2T = mem.view("H2T", o, [128, 8, 512], BF16); o += 8 * KB
            ACTT = mem.view("ACTT", o, [128, 22, 512], BF16); o += 22 * KB
            WOUT = mem.view("WOUT", CF_C * 4, [128, 8, 1024], BF16)
            WG = []
            for i in range(3):
                WG.append(mem.view("WG%d" % i, o, [128, 8, 256], BF16)); o += 4 * KB
            WDN = mem.view("WDN", o, [128, 22, 1024], BF16); o += 44 * KB
            XIN = []
            for i in range(2):
                XIN.append(mem.view("xin%d" % i, o, [128, 1024], F32)); o += 4 * KB
            JUNK = mem.view("junk", o, [128, 1024], F32); o += 4 * KB
            XS = mem.view("xs", o, [128, 1024], F32); o += 4 * KB
            TMPY = mem.view("tmpy", o, [128, 1024], F32); o += 4 * KB
            SIL = []
            for i in range(2):
                SIL.append(mem.view("sil%d" % i, o, [128, 512], F32)); o += 2 * KB
            assert o <= TOT, o
            ST = Buf(SMALL[:, 44:52], "ST")
            if not cv_waited[0]:
                for sem_cv in cvsems:
                    nc.sync.wait_ge(sem_cv, 16)
                cv_waited[0] = True
            cx.dma("sp", WDN[:], wdnbf_d.rearrange("(c p) n -> p c n", p=128), writes=[WDN])
            wout_v = woutbf_d.rearrange("(kc p) n -> p kc n", p=128)

            cx.dma("sp", WOUT[:], wout_v, writes=[WOUT])
            def tail_y(gi, j):
                T = 4 * gi + j
                xi = XIN[j % 2]
                cx.dma("sp", xi[:], x_d[seq, 128 * T:128 * T + 128, :], writes=[xi])
                yb = [PS[2 * (j % 2)], PS[2 * (j % 2) + 1]]
                for n in range(2):
                    for kc in range(8):
                        cx.op("pe", "matmul", yb[n][:], OT[:, kc, 128 * T:128 * T + 128], WOUT[:, kc, 512 * n:512 * n + 512],
                              start=(kc == 0), stop=(kc == 7), reads=[OT, WOUT], writes=[yb[n]])

            for gi in range(4):
                tail_y(gi, 0)
                for j in range(4):
                    T = 4 * gi + j
                    xi = XIN[j % 2]
                    yb = [PS[2 * (j % 2)], PS[2 * (j % 2) + 1]]
                    for n in range(2):
                        cx.op("act", "activation", out=JUNK[:, 512 * n:512 * n + 512], in_=yb[n][:], func=AF.Square,
                              accum_out=ST[:, n:n + 1], reads=[yb[n]], writes=[JUNK, ST])
                    cx.op("dve", "tensor_tensor", out=ST[:, 2:3], in0=ST[:, 0:1], in1=ST[:, 1:2], op=ALU.add, reads=[ST], writes=[ST])
                    cx.op("act", "activation", out=ST[:, 3:4], in_=ST[:, 2:3], func=AF.Sqrt, scale=1.0 / D, bias=1e-6, reads=[ST], writes=[ST])
                    cx.op("dve", "reciprocal", out=ST[:, 4:5], in_=ST[:, 3:4], reads=[ST], writes=[ST])
                    for n in range(2):
                        cx.op("dve", "scalar_tensor_tensor", out=TMPY[:, 512 * n:512 * n + 512], in0=yb[n][:], scalar=ST[:, 4:5],
                              in1=G1[:, 512 * n:512 * n + 512], op0=ALU.mult, op1=ALU.mult, reads=[yb[n], ST, G1], writes=[TMPY])
                    cx.op("dve", "tensor_tensor", out=X1[:, j, :], in0=TMPY[:], in1=xi[:], op=ALU.add, reads=[TMPY, xi], writes=[X1])
                    cx.op("act", "activation", out=JUNK[:], in_=X1[:, j, :], func=AF.Square, accum_out=ST[:, 0:1],
                          reads=[X1], writes=[JUNK, ST])
                    cx.op("act", "activation", out=ST[:, 3:4], in_=ST[:, 0:1], func=AF.Sqrt, scale=1.0 / D, bias=1e-6, reads=[ST], writes=[ST])
                    cx.op("dve", "reciprocal", out=ST[:, 4:5], in_=ST[:, 3:4], reads=[ST], writes=[ST])
                    cx.op("dve", "tensor_scalar", out=XS[:], in0=X1[:, j, :], scalar1=ST[:, 4:5], scalar2=None, op0=ALU.mult,
                          reads=[X1, ST], writes=[XS])
                    if j + 1 < 4:
                        tail_y(gi, j + 1)
                    for fc in range(8):
                        pb = PS[4 + fc // 4]
                        cx.op("pe", "transpose", pb[:, 128 * (fc % 4):128 * (fc % 4) + 128], XS[:, 128 * fc:128 * fc + 128],
                              ident_f, reads=[XS, CFB], writes=[pb])
                    for fc in range(8):
                        pb = PS[4 + fc // 4]
                        if fc < 4:
                            cx.op("act", "activation", out=H2T[:, fc, 128 * j:128 * j + 128],
                                  in_=pb[:, 128 * (fc % 4):128 * (fc % 4) + 128], func=AF.Identity,
                                  scale=DERV[:, 2, fc, seq:seq + 1], bias=DERV[:, 3, fc, seq:seq + 1],
                                  reads=[pb, DERV], writes=[H2T])
                        else:
                            cx.op("dve", "tensor_scalar", out=H2T[:, fc, 128 * j:128 * j + 128],
                                  in0=pb[:, 128 * (fc % 4):128 * (fc % 4) + 128],
                                  scalar1=DERV[:, 2, fc, seq:seq + 1], scalar2=DERV[:, 3, fc, seq:seq + 1],
                                  op0=ALU.mult, op1=ALU.add, reads=[pb, DERV], writes=[H2T])
                for ch in range(22):
                    wg = WG[ch % 3]
                    if not cv_waited[0]:
                        for sem_cv in cvsems:
                            nc.sync.wait_ge(sem_cv, 16)
                        cv_waited[0] = True
                    cx.dma("sp", wg[:].rearrange("p a b -> p (a b)"), wgubf_d[ch], writes=[wg])
                    pg, pu = PS[2 * (ch % 2)], PS[2 * (ch % 2) + 1]
                    for kc in range(8):
                        cx.op("pe", "matmul", pg[:], wg[:, kc, 0:128], H2T[:, kc, :], start=(kc == 0), stop=(kc == 7),
                              reads=[wg, H2T], writes=[pg])
                    for kc in range(8):
                        cx.op("pe", "matmul", pu[:], wg[:, kc, 128:256], H2T[:, kc, :], start=(kc == 0), stop=(kc == 7),
                              reads=[wg, H2T], writes=[pu])
                    sl_ = SIL[ch % 2]
                    cx.op("act", "activation", out=sl_[:], in_=pg[:], func=AF.Silu, reads=[pg], writes=[sl_])
                    cx.op("dve", "tensor_tensor", out=ACTT[:, ch, :], in0=sl_[:], in1=pu[:], op=ALU.mult,
                          reads=[sl_, pu], writes=[ACTT])
                for j in range(4):
                    T = 4 * gi + j
                    zb = [PS[4 + 2 * (j % 2)], PS[5 + 2 * (j % 2)]]
                    for n in range(2):
                        for ch in range(22):
                            cx.op("pe", "matmul", zb[n][:], ACTT[:, ch, 128 * j:128 * j + 128], WDN[:, ch, 512 * n:512 * n + 512],
                                  start=(ch == 0), stop=(ch == 21), reads=[ACTT, WDN], writes=[zb[n]])
                    for n in range(2):
                        cx.op("act", "activation", out=JUNK[:, 512 * n:512 * n + 512], in_=zb[n][:], func=AF.Square,
                              accum_out=ST[:, n:n + 1], reads=[zb[n]], writes=[JUNK, ST])
                    cx.op("dve", "tensor_tensor", out=ST[:, 2:3], in0=ST[:, 0:1], in1=ST[:, 1:2], op=ALU.add, reads=[ST], writes=[ST])
                    cx.op("act", "activation", out=ST[:, 3:4], in_=ST[:, 2:3], func=AF.Sqrt, scale=1.0 / D, bias=1e-6, reads=[ST], writes=[ST])
                    cx.op("dve", "reciprocal", out=ST[:, 4:5], in_=ST[:, 3:4], reads=[ST], writes=[ST])
                    for n in range(2):
                        cx.op("dve", "scalar_tensor_tensor", out=TMPY[:, 512 * n:512 * n + 512], in0=zb[n][:], scalar=ST[:, 4:5],
                              in1=G2[:, 512 * n:512 * n + 512], op0=ALU.mult, op1=ALU.mult, reads=[zb[n], ST, G2], writes=[TMPY])
                    cx.op("dve", "tensor_tensor", out=XS[:], in0=TMPY[:], in1=X1[:, j, :], op=ALU.add, reads=[TMPY, X1], writes=[XS])
                    cx.dma("sp", out_d[seq, 128 * T:128 * T + 128, :], XS[:], reads=[XS])
            cx.barrier()

        cx.barrier()
        cx.finish()
    return nc


def _prep(inputs):
    inp = {k: np.asarray(v) for k, v in inputs.items()}
    cf, cb, imt, wsm = _host_consts(inp)
    A, B = _col_index()
    shared = {
        "w_ada": np.ascontiguousarray(inp['w_ada'][0]),
        "cf": cf, "cb": cb, "imt": imt, "wsm": wsm,
        "w_inA": np.ascontiguousarray(inp['w_in'][0][:, A]),
        "w_inB": np.ascontiguousarray(inp['w_in'][0][:, B]),
        "cmp_w1": np.ascontiguousarray(inp['cmp_w1'][0]),
        "b2v": np.ascontiguousarray(inp['cmp_b2'][0, 1]),
        "w_out": np.ascontiguousarray(inp['w_out'][0]),
        "w_gate_up": np.ascontiguousarray(
            inp['w_gate_up'][0].reshape(8, 128, 2, 22, 128).transpose(3, 1, 0, 2, 4).reshape(22, 128, 2048)),
        "w_down": np.ascontiguousarray(inp['w_down'][0]),
    }
    maps = []
    for c in range(8):
        m = dict(shared)
        m["x"] = np.ascontiguousarray(inp['x'][2 * c:2 * c + 2])
        m["cT"] = np.ascontiguousarray(inp['c'][2 * c:2 * c + 2].T.reshape(8, 128, 2).transpose(1, 0, 2))
        maps.append(m)
    return maps


def kernel(**inputs):
    maps = _prep(inputs)
    nc = build()
    res = run_bass_kernel_spmd(nc, maps, core_ids=list(range(8)))
    return np.concatenate([r["out"] for r in res.results], axis=0).astype(np.float32)
```
